# Optimizing a Trainium2 kernel written in Bass

```python
import jax, jax.numpy as jnp
from jax import lax
import numpy as np

D_MODEL = 1024
BATCH = 4
SEQ = 8192
DEPTH = 4

D_MIX = D_MODEL
SSD_WIDTH = D_MIX // 2
SSD_HEAD_DIM = 64
SSD_HEADS = SSD_WIDTH // SSD_HEAD_DIM
SSD_GROUPS = 2
SSD_STATE = 128
SSD_CONV = 4
SSD_CHUNK = 128
ATTN_WIDTH = D_MIX // 4
ATTN_HEAD_DIM = 64
ATTN_HEADS = ATTN_WIDTH // ATTN_HEAD_DIM
ATTN_BLOCK = 128
CONV_WIDTH = D_MIX - SSD_WIDTH - ATTN_WIDTH
CONV_KERNEL = 31
XBC_WIDTH = SSD_WIDTH + 2 * SSD_GROUPS * SSD_STATE
D_IN = SSD_WIDTH + XBC_WIDTH + SSD_HEADS + 3 * ATTN_WIDTH + ATTN_HEADS + 2 * CONV_WIDTH
N_EXPERT_GROUPS = 4
EXPERTS_PER_GROUP = 8
N_EXPERTS = N_EXPERT_GROUPS * EXPERTS_PER_GROUP
TOP_K = 2
D_EXPERT = 512
MOE_BLOCK = 256
NORM_EPS = 1e-6

kernel_name = 'hymba_ssd_fox_conformer_hmoe_adaln'


def _rms(x):
    xf = x.astype(jnp.float32)
    return xf * lax.rsqrt(jnp.mean(xf * xf, axis=-1, keepdims=True) + NORM_EPS)


def rms_norm(x, g):
    return (_rms(x) * g.astype(jnp.float32)).astype(x.dtype)


def layer_norm(x, g, b):
    xf = x.astype(jnp.float32)
    mu = jnp.mean(xf, axis=-1, keepdims=True)
    var = jnp.mean(jnp.square(xf - mu), axis=-1, keepdims=True)
    y = (xf - mu) * lax.rsqrt(var + NORM_EPS) * g.astype(jnp.float32) + b.astype(jnp.float32)
    return y.astype(x.dtype)


def causal_depthwise_conv(x, w, b):
    k = w.shape[0]
    y = lax.conv_general_dilated(
        x, w[:, None, :].astype(x.dtype), window_strides=(1,), padding=[(k - 1, 0)],
        dimension_numbers=('NWC', 'WIO', 'NWC'), feature_group_count=x.shape[-1])
    return y + b.astype(x.dtype)


def ssd_mixer(z, xbc, dt_raw, conv_w, conv_b, dt_bias, a_log, d_skip, norm_g):
    f32 = jnp.float32
    bsz, seq, _ = z.shape
    nc = seq // SSD_CHUNK
    r = SSD_HEADS // SSD_GROUPS
    gn = SSD_GROUPS * SSD_STATE
    xbc = jax.nn.silu(causal_depthwise_conv(xbc, conv_w, conv_b))
    xs, bmat, cmat = jnp.split(xbc, [SSD_WIDTH, SSD_WIDTH + gn], axis=-1)
    dt = jax.nn.softplus(dt_raw.astype(f32) + dt_bias.astype(f32))
    a = -jnp.exp(a_log.astype(f32))
    x_h = xs.reshape(bsz, nc, SSD_CHUNK, SSD_GROUPS, r, SSD_HEAD_DIM).astype(f32)
    dt_c = dt.reshape(bsz, nc, SSD_CHUNK, SSD_GROUPS, r)
    xdt = x_h * dt_c[..., None]
    bm = bmat.reshape(bsz, nc, SSD_CHUNK, SSD_GROUPS, SSD_STATE).astype(f32)
    cm = cmat.reshape(bsz, nc, SSD_CHUNK, SSD_GROUPS, SSD_STATE).astype(f32)
    acs = jnp.cumsum(dt_c * a.reshape(SSD_GROUPS, r), axis=2)
    acs = acs.transpose(0, 1, 3, 4, 2)
    causal = jnp.tril(jnp.ones((SSD_CHUNK, SSD_CHUNK), dtype=bool))
    decay_in = jnp.exp(jnp.where(causal, acs[..., :, None] - acs[..., None, :], -jnp.inf))
    cb = jnp.einsum('bclgn,bcsgn->bcgls', cm, bm)
    y_diag = jnp.einsum('bcgls,bcgrls,bcsgrp->bclgrp', cb, decay_in, xdt)
    decay_to_end = jnp.exp(acs[..., -1:] - acs)
    states = jnp.einsum('bclgn,bcgrl,bclgrp->bcgrpn', bm, decay_to_end, xdt)
    chunk_decay = jnp.exp(acs[..., -1])

    def step(h, inp):
        st, dec = inp
        return h * dec[..., None, None] + st, h

    h0 = jnp.zeros_like(states[:, 0])
    _, prev = lax.scan(step, h0, (states.swapaxes(0, 1), chunk_decay.swapaxes(0, 1)))
    prev = prev.swapaxes(0, 1)
    y_off = jnp.einsum('bclgn,bcgrpn,bcgrl->bclgrp', cm, prev, jnp.exp(acs))
    y = y_diag + y_off + x_h * d_skip.astype(f32).reshape(SSD_GROUPS, r, 1)
    y = y.reshape(bsz, seq, SSD_GROUPS, r * SSD_HEAD_DIM)
    y = y * jax.nn.silu(z.astype(f32)).reshape(bsz, seq, SSD_GROUPS, r * SSD_HEAD_DIM)
    y = _rms(y).reshape(bsz, seq, SSD_WIDTH) * norm_g.astype(f32)
    return y.astype(z.dtype)


def forgetting_attention(q, k, v, f_logit, f_bias, norm_g):
    f32 = jnp.float32
    bsz, seq, _ = q.shape
    nb = seq // ATTN_BLOCK
    scale = ATTN_HEAD_DIM ** -0.5
    qh = q.reshape(bsz, seq, ATTN_HEADS, ATTN_HEAD_DIM).transpose(0, 2, 1, 3)
    kh = k.reshape(bsz, seq, ATTN_HEADS, ATTN_HEAD_DIM).transpose(0, 2, 1, 3)
    vh = v.reshape(bsz, seq, ATTN_HEADS, ATTN_HEAD_DIM).transpose(0, 2, 1, 3)
    log_f = jax.nn.log_sigmoid(f_logit.astype(f32) + f_bias.astype(f32))
    cum = jnp.cumsum(log_f, axis=1).transpose(0, 2, 1)
    qb = qh.reshape(bsz, ATTN_HEADS, nb, ATTN_BLOCK, ATTN_HEAD_DIM).transpose(2, 0, 1, 3, 4)
    cb = cum.reshape(bsz, ATTN_HEADS, nb, ATTN_BLOCK).transpose(2, 0, 1, 3)
    k_pos = jnp.arange(seq)

    def block(args):
        qi, ci, i = args
        s = jnp.einsum('bhqd,bhkd->bhqk', qi, kh).astype(f32) * scale
        s = s + ci[..., :, None] - cum[:, :, None, :]
        q_pos = i * ATTN_BLOCK + jnp.arange(ATTN_BLOCK)
        s = jnp.where(k_pos[None, :] <= q_pos[:, None], s, -jnp.inf)
        p = jax.nn.softmax(s, axis=-1)
        return jnp.einsum('bhqk,bhkd->bhqd', p.astype(vh.dtype), vh)

    o = lax.map(block, (qb, cb, jnp.arange(nb)))
    o = o.transpose(1, 0, 3, 2, 4).reshape(bsz, seq, ATTN_WIDTH)
    return rms_norm(o, norm_g)


def conformer_conv(glu_a, glu_b, conv_w, conv_b, ln_g, ln_b):
    u = glu_a * jax.nn.sigmoid(glu_b)
    u = causal_depthwise_conv(u, conv_w, conv_b)
    u = layer_norm(u, ln_g, ln_b)
    return jax.nn.silu(u)


def hybrid_mixer(h, w_in, ssd_conv_w, ssd_conv_b, ssd_dt_bias, ssd_a_log, ssd_d, ssd_norm_g,
                 fox_f_bias, fox_norm_g, cm_conv_w, cm_conv_b, cm_ln_g, cm_ln_b, w_out):
    sizes = (SSD_WIDTH, XBC_WIDTH, SSD_HEADS, ATTN_WIDTH, ATTN_WIDTH, ATTN_WIDTH,
             ATTN_HEADS, CONV_WIDTH, CONV_WIDTH)
    splits = np.cumsum(sizes)[:-1].tolist()
    proj = h @ w_in
    z, xbc, dt_raw, q, k, v, f_logit, glu_a, glu_b = jnp.split(proj, splits, axis=-1)
    y_ssd = ssd_mixer(z, xbc, dt_raw, ssd_conv_w, ssd_conv_b, ssd_dt_bias, ssd_a_log, ssd_d, ssd_norm_g)
    y_att = forgetting_attention(q, k, v, f_logit, fox_f_bias, fox_norm_g)
    y_cnv = conformer_conv(glu_a, glu_b, cm_conv_w, cm_conv_b, cm_ln_g, cm_ln_b)
    y = jnp.concatenate([y_ssd.astype(h.dtype), y_att.astype(h.dtype), y_cnv.astype(h.dtype)], axis=-1)
    return y @ w_out


def hierarchical_moe(h, w_rg, b_rg, w_re, b_re, w_gate, w_up, w_down):
    f32 = jnp.float32
    bsz, seq, d = h.shape
    m = bsz * seq
    hf = h.reshape(m, d)
    p_g = jax.nn.softmax((hf @ w_rg + b_rg).astype(f32), axis=-1)
    g_sel = jnp.argmax(p_g, axis=-1).astype(jnp.int32)
    p_gsel = jnp.take_along_axis(p_g, g_sel[:, None], axis=-1)
    e_logits = (hf @ w_re + b_re).astype(f32).reshape(m, N_EXPERT_GROUPS, EXPERTS_PER_GROUP)
    e_logits = jnp.take_along_axis(e_logits, g_sel[:, None, None], axis=1)[:, 0]
    top_p, top_i = lax.top_k(jax.nn.softmax(e_logits, axis=-1), TOP_K)
    weights = top_p / jnp.sum(top_p, axis=-1, keepdims=True) * p_gsel
    expert = g_sel[:, None] * EXPERTS_PER_GROUP + top_i.astype(jnp.int32)
    n_assign = m * TOP_K
    flat_e = expert.reshape(-1)
    flat_w = weights.reshape(-1)
    flat_tok = jnp.arange(n_assign, dtype=jnp.int32) // TOP_K
    order = jnp.argsort(flat_e)
    se, stok, sw = flat_e[order], flat_tok[order], flat_w[order]
    counts = jnp.zeros((N_EXPERTS,), jnp.int32).at[flat_e].add(1)
    padded = (counts + MOE_BLOCK - 1) // MOE_BLOCK * MOE_BLOCK
    start = jnp.cumsum(counts) - counts
    pad_end = jnp.cumsum(padded)
    pad_start = pad_end - padded
    dest = pad_start[se] + jnp.arange(n_assign, dtype=jnp.int32) - start[se]
    n_blocks = (n_assign + MOE_BLOCK - 1) // MOE_BLOCK + N_EXPERTS
    rows = n_blocks * MOE_BLOCK
    xs = jnp.zeros((rows, d), h.dtype).at[dest].set(hf[stok])
    block_e = jnp.minimum(
        jnp.searchsorted(pad_end, jnp.arange(n_blocks, dtype=jnp.int32) * MOE_BLOCK, side='right'),
        N_EXPERTS - 1)

    def expert_block(args):
        xb, e = args
        hid = jax.nn.silu(xb @ w_gate[e]) * (xb @ w_up[e])
        return hid @ w_down[e]

    ys = lax.map(expert_block, (xs.reshape(n_blocks, MOE_BLOCK, d), block_e)).reshape(rows, d)
    contrib = (ys[dest] * sw[:, None]).astype(h.dtype)
    out = jnp.zeros((m, d), h.dtype).at[stok].add(contrib)
    return out.reshape(bsz, seq, d)


def setup_inputs(seed: int = 0) -> dict:
    key = jax.random.key(seed)
    ks = jax.random.split(key, 32)
    nrm = jax.random.normal
    uni = jax.random.uniform
    f32 = jnp.float32
    dt0 = jnp.exp(uni(ks[8], (DEPTH, SSD_HEADS), f32, np.log(1e-3), np.log(1e-1)))
    return {
        'x': nrm(ks[0], (BATCH, SEQ, D_MODEL), f32),
        'c': nrm(ks[1], (BATCH, D_MODEL), f32),
        'ada_w': nrm(ks[2], (DEPTH, D_MODEL, 6 * D_MODEL), f32) * (0.5 * D_MODEL ** -0.5),
        'ada_b': 0.02 * nrm(ks[3], (DEPTH, 6 * D_MODEL), f32),
        'norm_mix_g': 1.0 + 0.02 * nrm(ks[4], (DEPTH, D_MODEL), f32),
        'w_in': nrm(ks[5], (DEPTH, D_MODEL, D_IN), f32) * D_MODEL ** -0.5,
        'ssd_conv_w': nrm(ks[6], (DEPTH, SSD_CONV, XBC_WIDTH), f32) * SSD_CONV ** -0.5,
        'ssd_conv_b': 0.02 * nrm(ks[7], (DEPTH, XBC_WIDTH), f32),
        'ssd_dt_bias': dt0 + jnp.log(-jnp.expm1(-dt0)),
        'ssd_a_log': jnp.log(uni(ks[9], (DEPTH, SSD_HEADS), f32, 1.0, 16.0)),
        'ssd_d': 1.0 + 0.02 * nrm(ks[10], (DEPTH, SSD_HEADS), f32),
        'ssd_norm_g': 1.0 + 0.02 * nrm(ks[11], (DEPTH, SSD_WIDTH), f32),
        'fox_f_bias': uni(ks[12], (DEPTH, ATTN_HEADS), f32, 1.0, 4.0),
        'fox_norm_g': 1.0 + 0.02 * nrm(ks[13], (DEPTH, ATTN_WIDTH), f32),
        'cm_conv_w': nrm(ks[14], (DEPTH, CONV_KERNEL, CONV_WIDTH), f32) * CONV_KERNEL ** -0.5,
        'cm_conv_b': 0.02 * nrm(ks[15], (DEPTH, CONV_WIDTH), f32),
        'cm_ln_g': 1.0 + 0.02 * nrm(ks[16], (DEPTH, CONV_WIDTH), f32),
        'cm_ln_b': 0.02 * nrm(ks[17], (DEPTH, CONV_WIDTH), f32),
        'w_out': nrm(ks[18], (DEPTH, D_MIX, D_MODEL), f32) * D_MIX ** -0.5,
        'norm_ffn_g': 1.0 + 0.02 * nrm(ks[19], (DEPTH, D_MODEL), f32),
        'w_router_group': nrm(ks[20], (DEPTH, D_MODEL, N_EXPERT_GROUPS), f32) * D_MODEL ** -0.5,
        'b_router_group': 0.01 * nrm(ks[21], (DEPTH, N_EXPERT_GROUPS), f32),
        'w_router_expert': nrm(ks[22], (DEPTH, D_MODEL, N_EXPERTS), f32) * D_MODEL ** -0.5,
        'b_router_expert': 0.01 * nrm(ks[23], (DEPTH, N_EXPERTS), f32),
        'w_gate': nrm(ks[24], (DEPTH, N_EXPERTS, D_MODEL, D_EXPERT), f32) * D_MODEL ** -0.5,
        'w_up': nrm(ks[25], (DEPTH, N_EXPERTS, D_MODEL, D_EXPERT), f32) * D_MODEL ** -0.5,
        'w_down': nrm(ks[26], (DEPTH, N_EXPERTS, D_EXPERT, D_MODEL), f32) * D_EXPERT ** -0.5,
        'final_norm_g': 1.0 + 0.02 * nrm(ks[27], (D_MODEL,), f32),
    }


def reference(x, c, ada_w, ada_b, norm_mix_g, w_in, ssd_conv_w, ssd_conv_b, ssd_dt_bias,
              ssd_a_log, ssd_d, ssd_norm_g, fox_f_bias, fox_norm_g, cm_conv_w, cm_conv_b,
              cm_ln_g, cm_ln_b, w_out, norm_ffn_g, w_router_group, b_router_group,
              w_router_expert, b_router_expert, w_gate, w_up, w_down, final_norm_g):
    cond = jax.nn.silu(c)
    for l in range(DEPTH):
        mod = (cond @ ada_w[l] + ada_b[l])[:, None, :]
        sh_m, sc_m, g_m, sh_f, sc_f, g_f = jnp.split(mod, 6, axis=-1)
        h = rms_norm(x, norm_mix_g[l]) * (1.0 + sc_m) + sh_m
        y = hybrid_mixer(h, w_in[l], ssd_conv_w[l], ssd_conv_b[l], ssd_dt_bias[l], ssd_a_log[l],
                         ssd_d[l], ssd_norm_g[l], fox_f_bias[l], fox_norm_g[l], cm_conv_w[l],
                         cm_conv_b[l], cm_ln_g[l], cm_ln_b[l], w_out[l])
        x = x + g_m * y
        h = rms_norm(x, norm_ffn_g[l]) * (1.0 + sc_f) + sh_f
        y = hierarchical_moe(h, w_router_group[l], b_router_group[l], w_router_expert[l],
                             b_router_expert[l], w_gate[l], w_up[l], w_down[l])
        x = x + g_f * y
    return rms_norm(x, final_norm_g)
```

```python
import contextlib
import numpy as np
import concourse.bass as bass
import concourse.mybir as mybir
from concourse.bass_utils import run_bass_kernel_spmd

F32 = mybir.dt.float32
BF16 = mybir.dt.bfloat16
I32 = mybir.dt.int32
AF = mybir.ActivationFunctionType
ALU = mybir.AluOpType
AX = mybir.AxisListType

ENGS = ('tensor', 'vector', 'scalar', 'gpsimd', 'sync')
CENGS = ('tensor', 'vector', 'scalar', 'gpsimd')
NDS = 10

D = 1024
D_IN = 2828
EPS = 1e-6
NE = 32
DE = 512


class Prog:
    def __init__(self, nc, same_engine_sync=True):
        self.nc = nc
        self.same = same_engine_sync
        self.esem = {e: nc.alloc_semaphore(name=f"es_{e}") for e in CENGS}
        self.dsem = {e: [nc.alloc_semaphore(name=f"ds_{e}_{i}") for i in range(NDS)]
                     for e in ('sync', 'scalar', 'gpsimd')}
        self._reset()
        self.q = None

    def _reset(self):
        keep = getattr(self, 'dcnt', {}).get('gpsimd', [0] * NDS)
        self.ecnt = {e: 0 for e in CENGS}
        self.dcnt = {e: [0] * NDS for e in self.dsem}
        self.dcnt['gpsimd'] = list(keep)
        self.dnext = {e: 0 for e in self.dsem}
        self.known = {e: {('d', 'gpsimd', i): keep[i] for i in range(NDS)} for e in ENGS}
        self.res_w = {}
        self.res_r = {}

    def semof(self, key):
        if key[0] == 'e':
            return self.esem[key[1]]
        return self.dsem[key[1]][key[2]]

    def begin(self):
        self.q = {e: [] for e in ENGS}

    def _deps(self, reads, writes):
        deps = []
        for r in reads:
            t = self.res_w.get(r)
            if t is not None:
                deps.append(t)
        for w in writes:
            t = self.res_w.get(w)
            if t is not None:
                deps.append(t)
            deps.extend(self.res_r.get(w, {}).items())
        return deps

    def _record(self, tok, reads, writes):
        for r in reads:
            d = self.res_r.setdefault(r, {})
            if d.get(tok[0], 0) < tok[1]:
                d[tok[0]] = tok[1]
        for w in writes:
            self.res_w[w] = tok
            self.res_r[w] = {}

    def _waits(self, eng, deps):
        waits = []
        best = {}
        for (key, c) in deps:
            if best.get(key, 0) < c:
                best[key] = c
        for key, c in best.items():
            if key == ('e', 'tensor') and eng == 'tensor':
                continue
            if (not self.same) and key == ('e', eng):
                continue
            if self.known[eng].get(key, 0) >= c:
                continue
            self.known[eng][key] = c
            waits.append((self.semof(key), c))
        return waits

    def op(self, eng, name, reads=(), writes=(), **kw):
        writes = list(writes) + [r for r in reads if r.startswith('ps') and r not in writes]
        waits = self._waits(eng, self._deps(reads, writes))
        self.ecnt[eng] += 1
        tok = (('e', eng), self.ecnt[eng])
        sem = self.esem[eng]

        def emit(e, name=name, kw=kw, waits=waits, sem=sem):
            for (s, c) in waits:
                e.wait_ge(s, c)
            getattr(e, name)(**kw).then_inc(sem, 1)
        self.q[eng].append(emit)
        self._record(tok, reads, writes)
        return tok

    def raw(self, eng, name, *args, **kw):
        self.q[eng].append(lambda e: getattr(e, name)(*args, **kw))

    def dma(self, eng, out, in_, reads=(), writes=(), fn=None, **kw):
        deps = self._deps(reads, writes)
        idx = self.dnext[eng]
        self.dnext[eng] = (idx + 1) % NDS
        key = ('d', eng, idx)
        prev = self.dcnt[eng][idx]
        if prev:
            deps.append((key, prev))
        waits = self._waits(eng, deps)
        self.dcnt[eng][idx] += 16
        tok = (key, self.dcnt[eng][idx])
        sem = self.dsem[eng][idx]

        def emit(e, waits=waits, sem=sem):
            for (s, c) in waits:
                e.wait_ge(s, c)
            if fn is not None:
                try:
                    ins = getattr(e, fn[0])(**fn[1])
                except Exception:
                    print("DMA builder failed:", fn[0], {k: (v.shape if hasattr(v, 'shape') else v) for k, v in fn[1].items()})
                    raise
                ins.then_inc(sem, 16)
            else:
                e.dma_start(out=out, in_=in_, **kw).then_inc(sem, 16)
        self.q[eng].append(emit)
        self._record(tok, reads, writes)
        return tok

    def end(self):
        nc = self.nc
        fin = []
        for e in self.dsem:
            for i in range(NDS):
                c = self.dcnt[e][i]
                if c and self.known['sync'].get(('d', e, i), 0) < c:
                    fin.append((self.dsem[e][i], c))

        def drain(e, fin=fin):
            for (s, c) in fin:
                e.wait_ge(s, c)
        self.q['sync'].append(drain)
        with nc.Block() as block:
            for en in ENGS:
                ops = self.q[en]
                if not ops:
                    continue

                def body(e, ops=ops):
                    for o in ops:
                        o(e)
                getattr(block, en)(body)
        allsems = list(self.esem.values()) + [s for e in self.dsem if e != 'gpsimd' for s in self.dsem[e]]
        with nc.Block() as block:
            def clr(e):
                for s in allsems:
                    e.sem_clear(s)
            block.sync(clr)
        self._reset()
        self.q = None


class Builder:
    def __init__(self, T, L, dbg=(), moe=True):
        self.T, self.L = T, L
        self.moe = moe
        self.NT = T // 128
        self.NQ = T // 512
        self.dbg = set(dbg)
        nc = self.nc = bass.Bass("TRN2", target_bir_lowering=False)
        self.P = Prog(nc)
        self.ins = {}
        self.outs = {}
        self._uid = 0
        self.declare_io()
        self.consts()

    def din(self, name, shape, dt=F32):
        t = self.nc.dram_tensor(name, list(shape), dt, kind="ExternalInput").ap()
        self.ins[name] = t
        return t

    def dscr(self, name, shape, dt=F32):
        kind = "ExternalOutput" if name in self.dbg else "Internal"
        t = self.nc.dram_tensor(name, list(shape), dt, kind=kind).ap()
        if kind == "ExternalOutput":
            self.outs[name] = t
        return t

    def sb(self, st, name, shape, dt=F32):
        self._uid += 1
        return st.enter_context(self.nc.sbuf_tensor(f"{name}_{self._uid}", list(shape), dt)).ap()

    def declare_io(self):
        T, L = self.T, self.L
        d = self.din
        self.x_in = d("x", [T, D])
        self.c_in = d("c", [128, 8])
        self.ada_w = d("ada_w", [L, D, 6 * D])
        self.ada_b = d("ada_b", [L, 6 * D])
        self.norm_mix_g = d("norm_mix_g", [L, D])
        self.w_in = d("w_in", [L, D, D_IN])
        self.convw = d("convw_fm", [L, 128, 8, 4])
        self.convb = d("convb_fm", [L, 128, 8])
        self.dt_bias = d("ssd_dt_bias", [L, 8])
        self.a_log = d("ssd_a_log", [L, 8])
        self.ssd_d = d("ssd_d", [L, 8])
        self.ssd_norm_g = d("ssd_norm_g", [L, 512])
        self.f_bias = d("fox_f_bias_fm", [L, 4, 1])
        self.fox_g = d("fox_g_fm", [L, 128, 2])
        self.cmw = d("cmw_fm", [L, 128, 2, 31])
        self.cmb = d("cmb_fm", [L, 128, 2])
        self.cmg = d("cmg_fm", [L, 128, 2])
        self.cmbeta = d("cmbeta_fm", [L, 128, 2])
        self.w_out = d("w_out", [L, D, D])
        self.norm_ffn_g = d("norm_ffn_g", [L, D])
        self.w_rg = d("w_router_group", [L, D, 4])
        self.b_rg = d("b_router_group", [L, 4])
        self.w_re = d("w_router_expert", [L, D, NE])
        self.b_re = d("b_router_expert", [L, NE])
        if self.moe:
            self.w_gate = d("w_gate", [L, NE, D, DE])
            self.w_up = d("w_up", [L, NE, D, DE])
            self.w_down = d("w_down", [L, NE, DE, D])
        self.final_g = d("final_norm_g", [D])
        self.out = self.nc.dram_tensor("out", [T, D], F32, kind="ExternalOutput").ap()
        self.outs["out"] = self.out
        s = self.dscr
        self.xres = s("xres", [T, D])
        self.zs = s("zs", [T, 512])
        self.xbcT = s("xbcT", [1024, T])
        self.dtr = s("dtr", [T, 8])
        self.flT = s("flT", [4, T])
        self.qT = s("qT", [256, T], BF16)
        self.kT = s("kT", [256, T], BF16)
        self.vtok = s("vtok", [T, 256], BF16)
        self.gaT = s("gaT", [256, T])
        self.gbT = s("gbT", [256, T])
        self.yT = s("yT", [1024, T], BF16)
        self.attT = s("attT", [256, T])
        self.cs = s("cs", [4, 6, T], BF16)
        self.hfp = s("hfp", [T, D], BF16)
        self.BLK = 512
        self.NB = (2 * T) // self.BLK + NE
        self.xs = s("xs", [self.NB * self.BLK, D], BF16)
        self.ys = s("ys", [self.NB * self.BLK, D])
        self.rdbg = s("rdbg", [T, 68])

    def consts(self):
        nc, P = self.nc, self.P
        a = lambda n, s, dt=F32: nc.alloc_sbuf_tensor(n, list(s), dt).ap()
        self.ident = a("ident", [128, 128])
        self.identb = a("identb", [128, 128], BF16)
        self.tri = a("tri", [128, 128])
        self.ustr = a("ustr", [128, 128])
        self.ones = a("ones", [128, 128])
        self.onesb = a("onesb", [128, 128], BF16)
        self.epsb = a("epsb", [128, 1])
        self.sutb = a("sutb", [128, 128], BF16)
        self.ps = [nc.alloc_psum_tensor(f"ps{i}", [128, 512], F32).ap() for i in range(8)]
        self.reg_rows = nc.gpsimd.alloc_register("bc_rows")
        self.reg_w = nc.gpsimd.alloc_register("bc_w")
        P.begin()
        g = 'gpsimd'
        P.op(g, 'memset', writes=['epsb'], ap=self.epsb, constant=EPS)
        P.op(g, 'memset', writes=['ones'], ap=self.ones, constant=1.0)
        P.op(g, 'memset', writes=['onesb'], ap=self.onesb, constant=1.0)
        P.op(g, 'memset', writes=['ident'], ap=self.ident, constant=1.0)
        P.op(g, 'affine_select', reads=['ident'], writes=['ident'], out=self.ident, in_=self.ident,
             pattern=[[-1, 128]], compare_op=ALU.is_equal, fill=0.0, base=0, channel_multiplier=1)
        P.op(g, 'tensor_copy', reads=['ident'], writes=['identb'], out=self.identb, in_=self.ident)
        P.op(g, 'memset', writes=['tri'], ap=self.tri, constant=1.0)
        P.op(g, 'affine_select', reads=['tri'], writes=['tri'], out=self.tri, in_=self.tri,
             pattern=[[1, 128]], compare_op=ALU.is_ge, fill=0.0, base=0, channel_multiplier=-1)
        P.op(g, 'tensor_tensor', reads=['tri', 'ident'], writes=['sutb'], out=self.sutb, in0=self.tri, in1=self.ident,
             op=ALU.subtract)
        P.op(g, 'memset', writes=['ustr'], ap=self.ustr, constant=1.0)
        P.op(g, 'affine_select', reads=['ustr'], writes=['ustr'], out=self.ustr, in_=self.ustr,
             pattern=[[-1, 128]], compare_op=ALU.is_gt, fill=0.0, base=0, channel_multiplier=1)
        P.end()

    def phase_ada(self, l, st):
        nc, P = self.nc, self.P
        self.modb = self.sb(st, "modb", [128, 6 * D])
        with contextlib.ExitStack() as s2:
            cs = self.sb(s2, "c_s", [128, 8])
            cb = self.sb(s2, "c_b", [128, 8, 128])
            adab = self.sb(s2, "adab", [128, 6 * D])
            wch = [self.sb(s2, f"adaw{i}", [128, 8, 512]) for i in range(2)]
            P.begin()
            P.dma('sync', cs, self.c_in, writes=['c_s'])
            P.dma('sync', adab, self.ada_b[l].partition_broadcast(128), writes=['adab'])
            P.op('scalar', 'activation', reads=['c_s'], writes=['c_s'], out=cs, in_=cs, func=AF.Silu)
            P.op('vector', 'tensor_copy', reads=['c_s'], writes=['c_b'], out=cb,
                 in_=cs.unsqueeze(2).broadcast_to([128, 8, 128]))
            for j in range(12):
                w = wch[j % 2]
                wr = f'adaw{j % 2}'
                P.dma('sync', w,
                      self.ada_w[l][:, j * 512:(j + 1) * 512].rearrange("(kc p) n -> p kc n", p=128),
                      writes=[wr])
                pt = self.ps[j % 2]
                for kc in range(8):
                    P.op('tensor', 'matmul', reads=['c_b', wr], writes=[f'ps{j % 2}'], out=pt,
                         lhsT=cb[:, kc, :], rhs=w[:, kc, :], start=(kc == 0), stop=(kc == 7))
                P.op('vector', 'tensor_tensor', reads=[f'ps{j % 2}', 'adab'], writes=['modb'],
                     out=self.modb[:, j * 512:(j + 1) * 512], in0=pt, in1=adab[:, j * 512:(j + 1) * 512], op=ALU.add)
            P.end()

    def mod(self, i):
        return self.modb[:, i * D:(i + 1) * D]

    def load_cast(self, st, dst, src, res, width):
        P = self.P
        if getattr(self, '_stg_owner', None) is not st:
            self._stg = [self.sb(st, f"stg{i}", [128, 2048]) for i in range(2)]
            self._stg_owner = st
            self._stg_n = 0
        for c0 in range(0, width, 2048):
            n = min(2048, width - c0)
            k = self._stg_n
            self._stg_n += 1
            S = self._stg[k % 2]
            rs = f'stg{k % 2}'
            P.dma('sync', S[:, 0:n], src[:, c0:c0 + n], writes=[rs])
            P.op('gpsimd' if k % 2 else 'vector', 'tensor_copy', reads=[rs], writes=[res], out=dst[:, c0:c0 + n],
                 in_=S[:, 0:n])

    def rstd_ops(self, ss, rstd, n, rd, wr):
        P = self.P
        P.op('scalar', 'activation', reads=rd, writes=wr, out=rstd, in_=ss, func=AF.Ln,
             bias=self.epsb[:ss.shape[0], :], scale=1.0 / n)
        P.op('scalar', 'activation', reads=wr, writes=wr, out=rstd, in_=rstd, func=AF.Exp, scale=-0.5)

    def phase_inproj(self, l, st0):
        nc, P, T = self.nc, self.P, self.T
        src = self.x_in if l == 0 else self.xres
        self._ip_bufs = None
        with contextlib.ExitStack() as st:
            sb = lambda n, s, dt=F32: self.sb(st, n, s, dt)
            wz = sb("wz", [128, 8, 512], BF16)
            wx = sb("wx", [128, 8, 1024], BF16)
            wqk = sb("wqk", [128, 8, 512], BF16)
            wv = sb("wv", [128, 8, 256], BF16)
            wg = sb("wg", [128, 8, 512], BF16)
            wdt = sb("wdt", [128, 8, 8])
            wf = sb("wf", [128, 8, 4])
            gsc = sb("gsc", [128, D])
            W = self.w_in[l].rearrange("(kc p) n -> p kc n", p=128)
            P.begin()
            for kc in range(8):
                self.load_cast(st, wz[:, kc, :], W[:, kc, 0:512], 'wz', 512)
                self.load_cast(st, wx[:, kc, :], W[:, kc, 512:1536], 'wx', 1024)
                self.load_cast(st, wqk[:, kc, :], W[:, kc, 1544:2056], 'wqk', 512)
                self.load_cast(st, wv[:, kc, :], W[:, kc, 2056:2312], 'wv', 256)
                self.load_cast(st, wg[:, kc, :], W[:, kc, 2316:2828], 'wg', 512)
            P.dma('sync', wdt, W[:, :, 1536:1544], writes=['wdt'])
            P.dma('sync', wf, W[:, :, 2312:2316], writes=['wf'])
            P.dma('sync', gsc, self.norm_mix_g[l].partition_broadcast(128), writes=['gsc'])
            P.op('vector', 'scalar_tensor_tensor', reads=['gsc', 'modb'], writes=['gsc'], out=gsc,
                 in0=self.mod(1), scalar=1.0, in1=gsc, op0=ALU.add, op1=ALU.mult)
            self.norm_and_transpose_loop(st, src, gsc, self.mod(0), consumer=lambda q, hT, hT32: self.inproj_chunk(
                q, hT, hT32, wz, wx, wqk, wv, wg, wdt, wf, st))
            P.end()

    def norm_and_transpose_loop(self, st, src, gsc, shift, consumer, pre=None, after_h=None):
        P = self.P
        sb = lambda n, s, dt=F32: self.sb(st, n, s, dt)
        xt = [sb(f"xt{i}", [128, D]) for i in range(2)]
        ht = [sb(f"ht{i}", [128, D]) for i in range(2)]
        sq = sb("sq", [128, D])
        ss = [sb(f"ss{i}", [128, 1]) for i in range(2)]
        rs = [sb(f"rs{i}", [128, 1]) for i in range(2)]
        hT32 = [sb(f"hT32_{i}", [128, 8, 512]) for i in range(2)]
        hT = [sb(f"hT_{i}", [128, 8, 512], BF16) for i in range(2)]
        for q in range(self.NQ):
            b = q % 2
            for j in range(4):
                i = q * 4 + j
                a = i % 2
                X, H = xt[a], ht[a]
                rx, rh = f'xt{a}', f'ht{a}'
                if pre is not None:
                    pre(i, X, rx)
                else:
                    P.dma('sync', X, src[i * 128:(i + 1) * 128, :], writes=[rx])
                P.op('scalar', 'activation', reads=[rx], writes=['sq', f'ss{a}'], out=sq, in_=X, func=AF.Square,
                     accum_out=ss[a])
                self.rstd_ops(ss[a], rs[a], D, [f'ss{a}'], [f'rs{a}'])
                P.op('vector', 'scalar_tensor_tensor', reads=[rx, f'rs{a}', 'gsc'], writes=[rh], out=H, in0=X,
                     scalar=rs[a], in1=gsc, op0=ALU.mult, op1=ALU.mult)
                P.op('gpsimd', 'tensor_tensor', reads=[rh, 'modb'], writes=[rh], out=H, in0=H, in1=shift, op=ALU.add)
                if after_h is not None:
                    after_h(i, H, rh, X, rx)
                for half in range(2):
                    pt = self.ps[half]
                    for k4 in range(4):
                        kc = half * 4 + k4
                        P.op('tensor', 'transpose', reads=[rh, 'ident'], writes=[f'ps{half}'],
                             out=pt[:, k4 * 128:(k4 + 1) * 128], in_=H[:, kc * 128:(kc + 1) * 128], identity=self.ident)
                    dst = hT32[b][:, half * 4:(half + 1) * 4, j * 128:(j + 1) * 128]
                    P.op('scalar', 'activation', reads=[f'ps{half}'], writes=[f'hT32_{b}'], out=dst,
                         in_=pt.rearrange("p (k t) -> p k t", k=4), func=AF.Copy)
                P.op('gpsimd', 'tensor_copy', reads=[f'hT32_{b}'], writes=[f'hT_{b}'],
                     out=hT[b][:, :, j * 128:(j + 1) * 128], in_=hT32[b][:, :, j * 128:(j + 1) * 128])
            consumer(q, (hT[b], f'hT_{b}'), (hT32[b], f'hT32_{b}'))

    def evac(self, k, out, in_, reads, writes, scale=None):
        P = self.P
        if k % 2 == 0:
            if scale is None:
                P.op('scalar', 'activation', reads=reads, writes=writes, out=out, in_=in_, func=AF.Copy)
            else:
                P.op('scalar', 'activation', reads=reads, writes=writes, out=out, in_=in_, func=AF.Copy, scale=scale)
        else:
            if scale is None:
                P.op('vector', 'tensor_copy', reads=reads, writes=writes, out=out, in_=in_)
            else:
                P.op('vector', 'tensor_scalar', reads=reads, writes=writes, out=out, in0=in_, scalar1=scale,
                     scalar2=None, op0=ALU.mult)

    def inproj_chunk(self, q, hTb, hT32b, wz, wx, wqk, wv, wg, wdt, wf, st):
        P = self.P
        hT, rhT = hTb
        hT32, rhT32 = hT32b
        if self._ip_bufs is None:
            sb = lambda n, s, dt=F32: self.sb(st, n, s, dt)
            self._ip_bufs = dict(
                o32=[sb(f"o32_{i}", [128, 512]) for i in range(3)],
                o16=[sb(f"o16_{i}", [128, 512], BF16) for i in range(3)],
                osm=[sb(f"osm_{i}", [128, 8]) for i in range(2)],
                ofl=[sb(f"ofl_{i}", [4, 512]) for i in range(2)],
                n=[0],
            )
        B = self._ip_bufs
        tok = slice(q * 512, (q + 1) * 512)

        def nxt():
            B['n'][0] += 1
            return B['n'][0]
        PB = [2, 3, 4, 5]

        def fm(w, wres, c0, dst, dt16=False, scale=None):
            k = nxt()
            pb = PB[k % 4]
            pt = self.ps[pb]
            for kc in range(8):
                P.op('tensor', 'matmul', reads=[wres, rhT], writes=[f'ps{pb}'], out=pt, lhsT=w[:, kc, c0:c0 + 128],
                     rhs=hT[:, kc, :], start=(kc == 0), stop=(kc == 7))
            o = (B['o16'] if dt16 else B['o32'])[k % 3]
            ores = ('o16_' if dt16 else 'o32_') + str(k % 3)
            self.evac(k, o, pt, [f'ps{pb}'], [ores], scale=scale)
            P.dma('sync', dst, o, reads=[ores], writes=[])
        for ct in range(8):
            fm(wx, 'wx', ct * 128, self.xbcT[ct * 128:(ct + 1) * 128, tok])
        for ct in range(2):
            fm(wqk, 'wqk', ct * 128, self.qT[ct * 128:(ct + 1) * 128, tok], dt16=True, scale=0.125)
        for ct in range(2):
            fm(wqk, 'wqk', 256 + ct * 128, self.kT[ct * 128:(ct + 1) * 128, tok], dt16=True)
        for ct in range(2):
            fm(wg, 'wg', ct * 128, self.gaT[ct * 128:(ct + 1) * 128, tok])
        for ct in range(2):
            fm(wg, 'wg', 256 + ct * 128, self.gbT[ct * 128:(ct + 1) * 128, tok])
        k = nxt()
        pb = PB[k % 4]
        pt = self.ps[pb]
        for kc in range(8):
            P.op('tensor', 'matmul', reads=['wf', rhT32], writes=[f'ps{pb}'], out=pt[0:4, :], lhsT=wf[:, kc, :],
                 rhs=hT32[:, kc, :], start=(kc == 0), stop=(kc == 7))
        o = B['ofl'][q % 2]
        P.op('vector', 'tensor_copy', reads=[f'ps{pb}'], writes=[f'ofl_{q % 2}'], out=o, in_=pt[0:4, :])
        P.dma('sync', self.flT[:, tok], o, reads=[f'ofl_{q % 2}'])
        for j in range(4):
            tt = slice(q * 512 + j * 128, q * 512 + (j + 1) * 128)
            k = nxt()
            pb = PB[k % 4]
            pt = self.ps[pb]
            for kc in range(8):
                P.op('tensor', 'matmul', reads=['wz', rhT], writes=[f'ps{pb}'], out=pt,
                     lhsT=hT[:, kc, j * 128:(j + 1) * 128], rhs=wz[:, kc, :], start=(kc == 0), stop=(kc == 7))
            o = B['o32'][k % 3]
            P.op('scalar', 'activation', reads=[f'ps{pb}'], writes=[f'o32_{k % 3}'], out=o, in_=pt, func=AF.Silu)
            P.dma('sync', self.zs[tt, :], o, reads=[f'o32_{k % 3}'])
            k = nxt()
            pb = PB[k % 4]
            pt = self.ps[pb]
            for kc in range(8):
                P.op('tensor', 'matmul', reads=['wv', rhT], writes=[f'ps{pb}'], out=pt[:, 0:256],
                     lhsT=hT[:, kc, j * 128:(j + 1) * 128], rhs=wv[:, kc, :], start=(kc == 0), stop=(kc == 7))
            for kc in range(8):
                P.op('tensor', 'matmul', reads=['wdt', rhT32], writes=[f'ps{pb}'], out=pt[:, 256:264],
                     lhsT=hT32[:, kc, j * 128:(j + 1) * 128], rhs=wdt[:, kc, :], start=(kc == 0), stop=(kc == 7))
            o = B['o16'][k % 3]
            self.evac(k, o[:, 0:256], pt[:, 0:256], [f'ps{pb}'], [f'o16_{k % 3}'])
            P.dma('sync', self.vtok[tt, :], o[:, 0:256], reads=[f'o16_{k % 3}'])
            o2 = B['osm'][j % 2]
            P.op('vector', 'tensor_copy', reads=[f'ps{pb}'], writes=[f'osm_{j % 2}'], out=o2, in_=pt[:, 256:264])
            P.dma('sync', self.dtr[tt, :], o2, reads=[f'osm_{j % 2}'])

    def phase_conv(self, l):
        P, T = self.P, self.T
        TC = min(T, 2048)
        HALO = 30
        with contextlib.ExitStack() as st:
            sb = lambda n, s, dt=F32: self.sb(st, n, s, dt)
            cw = sb("cw", [128, 2, 31])
            cbias = sb("cbias", [128, 2])
            cg = sb("cg", [128, 2])
            cbeta = sb("cbeta", [128, 2])
            ua = [sb(f"ua{i}", [128, TC + HALO]) for i in range(2)]
            ub = [sb(f"ub{i}", [128, TC + HALO]) for i in range(2)]
            co = [sb(f"co{i}", [128, TC]) for i in range(2)]
            sqt = sb("csq", [128, 512])
            mean = sb("cmean", [128, 512])
            rstd = sb("crstd", [128, 512])
            tmp = [sb(f"ctmp{i}", [128, 512]) for i in range(2)]
            yo = [sb(f"cyo{i}", [128, 512], BF16) for i in range(2)]
            P.begin()
            P.dma('sync', cw, self.cmw[l], writes=['cw'])
            P.dma('sync', cbias, self.cmb[l], writes=['cbias'])
            P.dma('sync', cg, self.cmg[l], writes=['cg'])
            P.dma('sync', cbeta, self.cmbeta[l], writes=['cbeta'])
            n = 0
            for c0 in range(0, T, TC):
                for ct in range(2):
                    A, Bt = ua[ct], ub[ct]
                    ra, rb, rc = f'ua{ct}', f'ub{ct}', f'co{ct}'
                    rows = slice(ct * 128, (ct + 1) * 128)
                    if c0 == 0:
                        P.dma('sync', A[:, HALO:], self.gaT[rows, 0:TC], writes=[ra])
                        P.dma('sync', Bt[:, HALO:], self.gbT[rows, 0:TC], writes=[rb])
                        P.op('gpsimd', 'memset', writes=[ra], ap=A[:, 0:HALO], constant=0.0)
                        P.op('gpsimd', 'memset', writes=[rb], ap=Bt[:, 0:HALO], constant=0.0)
                    else:
                        P.dma('sync', A, self.gaT[rows, c0 - HALO:c0 + TC], writes=[ra])
                        P.dma('sync', Bt, self.gbT[rows, c0 - HALO:c0 + TC], writes=[rb])
                    P.op('scalar', 'activation', reads=[rb], writes=[rb], out=Bt, in_=Bt, func=AF.Sigmoid)
                    P.op('gpsimd', 'tensor_tensor', reads=[ra, rb], writes=[ra], out=A, in0=A, in1=Bt, op=ALU.mult)
                    C = co[ct]
                    P.op('vector', 'tensor_scalar', reads=[ra, 'cw', 'cbias'], writes=[rc], out=C, in0=A[:, 0:TC],
                         scalar1=cw[:, ct, 0:1], scalar2=cbias[:, ct:ct + 1], op0=ALU.mult, op1=ALU.add)
                    for k in range(1, 31):
                        P.op('vector', 'scalar_tensor_tensor', reads=[ra, 'cw', rc], writes=[rc], out=C,
                             in0=A[:, k:k + TC], scalar=cw[:, ct, k:k + 1], in1=C, op0=ALU.mult, op1=ALU.add)
                for s0 in range(0, TC, 512):
                    cs_ = slice(s0, s0 + 512)
                    p1, p2 = self.ps[0], self.ps[1]
                    for ct in range(2):
                        P.op('tensor', 'matmul', reads=['ones', f'co{ct}'], writes=['ps0'], out=p1, lhsT=self.ones,
                             rhs=co[ct][:, cs_], start=(ct == 0), stop=(ct == 1))
                    for ct in range(2):
                        P.op('scalar', 'activation', reads=[f'co{ct}'], writes=['csq'], out=sqt, in_=co[ct][:, cs_],
                             func=AF.Square)
                        P.op('tensor', 'matmul', reads=['ones', 'csq'], writes=['ps1'], out=p2, lhsT=self.ones,
                             rhs=sqt, start=(ct == 0), stop=(ct == 1))
                    P.op('vector', 'tensor_scalar', reads=['ps0'], writes=['cmean'], out=mean, in0=p1,
                         scalar1=1.0 / 256, scalar2=None, op0=ALU.mult)
                    P.op('vector', 'tensor_tensor', reads=['cmean'], writes=['crstd'], out=rstd, in0=mean, in1=mean,
                         op=ALU.mult)
                    P.op('vector', 'scalar_tensor_tensor', reads=['ps1', 'crstd'], writes=['crstd'], out=rstd, in0=p2,
                         scalar=1.0 / 256, in1=rstd, op0=ALU.mult, op1=ALU.subtract)
                    self.rstd_ops(rstd, rstd, 1.0, ['crstd'], ['crstd'])
                    for ct in range(2):
                        n += 1
                        t = tmp[n % 2]
                        rt = f'ctmp{n % 2}'
                        P.op('vector', 'tensor_tensor', reads=[f'co{ct}', 'cmean'], writes=[rt], out=t,
                             in0=co[ct][:, cs_], in1=mean, op=ALU.subtract)
                        P.op('gpsimd', 'tensor_tensor', reads=[rt, 'crstd'], writes=[rt], out=t, in0=t, in1=rstd,
                             op=ALU.mult)
                        y = yo[n % 2]
                        ry = f'cyo{n % 2}'
                        P.op('scalar', 'activation', reads=[rt, 'cg', 'cbeta'], writes=[ry], out=y, in_=t, func=AF.Silu,
                             scale=cg[:, ct:ct + 1], bias=cbeta[:, ct:ct + 1])
                        P.dma('sync', self.yT[768 + ct * 128:768 + (ct + 1) * 128, c0 + s0:c0 + s0 + 512], y,
                              reads=[ry])
            P.end()

    def phase_attn(self, l):
        P, T, NT, NQ = self.P, self.T, self.NT, self.NQ
        CW = min(T, 2048)
        with contextlib.ExitStack() as st:
            sb = lambda n, s, dt=F32: self.sb(st, n, s, dt)
            fb = sb("fb", [4, 1])
            xx = sb("fx", [4, CW])
            ax = sb("fax", [4, CW])
            mn = sb("fmn", [4, CW])
            cum = [sb(f"fcum{i}", [4, CW]) for i in range(2)]
            r1 = sb("fr1", [4, CW])
            sp = sb("fsp", [4, 6, CW], BF16)
            P.begin()
            P.dma('sync', fb, self.f_bias[l], writes=['fb'])
            for ci, c0 in enumerate(range(0, T, CW)):
                cc = cum[ci % 2]
                rcum = f'fcum{ci % 2}'
                P.dma('sync', xx, self.flT[:, c0:c0 + CW], writes=['fx'])
                P.op('scalar', 'activation', reads=['fx', 'fb'], writes=['fx'], out=xx, in_=xx, func=AF.Identity,
                     bias=fb[:, 0:1], scale=1.0)
                P.op('vector', 'tensor_scalar', reads=['fx'], writes=['fmn'], out=mn, in0=xx, scalar1=-1.0, scalar2=0.0,
                     op0=ALU.mult, op1=ALU.max)
                P.op('vector', 'scalar_tensor_tensor', reads=['fmn', 'fx'], writes=['fax'], out=ax, in0=mn, scalar=-2.0,
                     in1=xx, op0=ALU.mult, op1=ALU.subtract)
                P.op('scalar', 'activation', reads=['fax'], writes=['fax'], out=ax, in_=ax, func=AF.Exp)
                P.op('scalar', 'activation', reads=['fax'], writes=['fax'], out=ax, in_=ax, func=AF.Ln, bias=1.0,
                     scale=1.0)
                P.op('vector', 'scalar_tensor_tensor', reads=['fmn', 'fax'], writes=['fmn'], out=mn, in0=mn, scalar=-1.0,
                     in1=ax, op0=ALU.mult, op1=ALU.subtract)
                init = 0.0 if ci == 0 else cum[(ci - 1) % 2][:, CW - 1:CW]
                P.op('vector', 'tensor_tensor_scan', reads=['fmn', 'ones', f'fcum{(ci - 1) % 2}'], writes=[rcum], out=cc,
                     data0=self.ones[0:4, 0:1].broadcast_to([4, CW]), data1=mn, initial=init, op0=ALU.mult, op1=ALU.add)
                P.op('vector', 'tensor_copy', reads=[rcum], writes=['fsp'], out=sp[:, 0, :], in_=cc)
                P.op('vector', 'tensor_tensor', reads=[rcum, 'fsp'], writes=['fr1'], out=r1, in0=cc, in1=sp[:, 0, :],
                     op=ALU.subtract)
                P.op('vector', 'tensor_copy', reads=['fr1'], writes=['fsp'], out=sp[:, 1, :], in_=r1)
                P.op('vector', 'tensor_tensor', reads=['fr1', 'fsp'], writes=['fr1'], out=r1, in0=r1, in1=sp[:, 1, :],
                     op=ALU.subtract)
                P.op('vector', 'tensor_copy', reads=['fr1'], writes=['fsp'], out=sp[:, 2, :], in_=r1)
                P.op('vector', 'tensor_scalar', reads=['fsp'], writes=['fsp'], out=sp[:, 3:6, :], in0=sp[:, 0:3, :],
                     scalar1=-1.0, scalar2=None, op0=ALU.mult)
                P.dma('sync', self.cs[:, :, c0:c0 + CW], sp, reads=['fsp'])
            P.end()
        with contextlib.ExitStack() as st:
            sb = lambda n, s, dt=F32: self.sb(st, n, s, dt)
            qp = [sb(f"qp{i}", [70, T], BF16) for i in range(2)]
            kp = [sb(f"kp{i}", [70, T], BF16) for i in range(2)]
            vp = [sb(f"vp{i}", [128, NT, 65], BF16) for i in range(2)]
            nm = sb("negmask", [128, 4, 512], BF16)
            pt_ = [sb(f"pT{i}", [128, 512], BF16) for i in range(3)]
            rec = sb("rec", [65, 512])
            bcs = sb("bcs", [64, 512])
            on = [sb(f"on{i}", [64, 512]) for i in range(2)]
            P.begin()
            P.op('gpsimd', 'memset', writes=['negmask'], ap=nm, constant=0.0)
            for d in range(4):
                P.op('gpsimd', 'affine_select', reads=['negmask'], writes=['negmask'], out=nm[:, d, :], in_=nm[:, d, :],
                     pattern=[[1, 512]], compare_op=ALU.is_ge, fill=-30000.0, base=-128 * d, channel_multiplier=-1)
            step = 0
            for h in range(4):
                hb = h % 2
                Q, Kp, V = qp[hb], kp[hb], vp[hb]
                rq, rk, rv = f'qp{hb}', f'kp{hb}', f'vp{hb}'
                hr = slice(h * 64, (h + 1) * 64)
                P.op('gpsimd', 'memset', writes=[rq], ap=Q[64:70, :], constant=1.0)
                P.op('gpsimd', 'memset', writes=[rk], ap=Kp[64:70, :], constant=1.0)
                P.op('gpsimd', 'memset', writes=[rv], ap=V[:, :, 64:65], constant=1.0)
                P.dma('sync', Q[0:64, :], self.qT[hr, :], writes=[rq])
                P.dma('sync', Kp[0:64, :], self.kT[hr, :], writes=[rk])
                P.dma('sync', Q[67:70, :], self.cs[h, 0:3, :], writes=[rq])
                P.dma('sync', Kp[64:67, :], self.cs[h, 3:6, :], writes=[rk])
                for i0 in range(0, NT, 4):
                    P.dma('sync', V[:, i0:i0 + 4, 0:64],
                          self.vtok[i0 * 128:(i0 + 4) * 128, hr].rearrange("(i p) d -> p i d", p=128), writes=[rv])
                for qc in range(NQ):
                    nk = 4 * qc + 4
                    ob = 3 + qc % 2
                    O = self.ps[ob]
                    qs = slice(qc * 512, (qc + 1) * 512)
                    for s_ in range(nk + 2):
                        if s_ < nk:
                            kt = s_
                            sbk = (step + s_) % 3
                            S = self.ps[sbk]
                            diag = kt >= 4 * qc
                            P.op('tensor', 'matmul', reads=[rq, rk], writes=[f'ps{sbk}'], out=S,
                                 lhsT=Kp[:, kt * 128:(kt + 1) * 128], rhs=Q[:, qs], start=True, stop=not diag)
                            if diag:
                                P.op('tensor', 'matmul', reads=['identb', 'negmask'], writes=[f'ps{sbk}'], out=S,
                                     lhsT=self.identb, rhs=nm[:, kt - 4 * qc, :], start=False, stop=True)
                        if 1 <= s_ <= nk:
                            kt = s_ - 1
                            sbk = (step + kt) % 3
                            P.op('scalar', 'activation', reads=[f'ps{sbk}'], writes=[f'pT{sbk}'], out=pt_[sbk],
                                 in_=self.ps[sbk], func=AF.Exp)
                        if s_ >= 2:
                            kt = s_ - 2
                            sbk = (step + kt) % 3
                            P.op('tensor', 'matmul', reads=[f'pT{sbk}', rv], writes=[f'ps{ob}'], out=O[0:65, :],
                                 lhsT=V[:, kt, :], rhs=pt_[sbk], start=(kt == 0), stop=(kt == nk - 1))
                    step += nk
                    P.op('vector', 'reciprocal', reads=[f'ps{ob}'], writes=['rec'], out=rec[64:65, :], in_=O[64:65, :])
                    P.op('tensor', 'matmul', reads=['ones', 'rec'], writes=['ps5'], out=self.ps[5][0:64, :],
                         lhsT=self.ones[64:65, 0:64], rhs=rec[64:65, :], start=True, stop=True)
                    P.op('scalar', 'activation', reads=['ps5'], writes=['bcs'], out=bcs, in_=self.ps[5][0:64, :],
                         func=AF.Copy)
                    o_ = on[qc % 2]
                    P.op('vector', 'tensor_tensor', reads=[f'ps{ob}', 'bcs'], writes=[f'on{qc % 2}'], out=o_,
                         in0=O[0:64, :], in1=bcs, op=ALU.mult)
                    P.dma('sync', self.attT[hr, qs], o_, reads=[f'on{qc % 2}'])
                if h < 3:
                    P.end()
                    P.begin()
            P.end()
        with contextlib.ExitStack() as st:
            sb = lambda n, s, dt=F32: self.sb(st, n, s, dt)
            fg = sb("foxg", [128, 2])
            at = [[sb(f"at{i}{c}", [128, 512]) for c in range(2)] for i in range(2)]
            sq = sb("asq", [128, 512])
            rs = sb("ars", [128, 512])
            yo = [sb(f"ayo{i}", [128, 512], BF16) for i in range(2)]
            P.begin()
            P.dma('sync', fg, self.fox_g[l], writes=['foxg'])
            n = 0
            for qc in range(NQ):
                qs = slice(qc * 512, (qc + 1) * 512)
                b = qc % 2
                for ct in range(2):
                    P.dma('sync', at[b][ct], self.attT[ct * 128:(ct + 1) * 128, qs], writes=[f'at{b}{ct}'])
                    P.op('scalar', 'activation', reads=[f'at{b}{ct}'], writes=['asq'], out=sq, in_=at[b][ct],
                         func=AF.Square)
                    P.op('tensor', 'matmul', reads=['ones', 'asq'], writes=['ps0'], out=self.ps[0], lhsT=self.ones,
                         rhs=sq, start=(ct == 0), stop=(ct == 1))
                self.rstd_ops(self.ps[0], rs, 256.0, ['ps0'], ['ars'])
                for ct in range(2):
                    n += 1
                    P.op('vector', 'tensor_tensor', reads=[f'at{b}{ct}', 'ars'], writes=[f'at{b}{ct}'], out=at[b][ct],
                         in0=at[b][ct], in1=rs, op=ALU.mult)
                    y = yo[n % 2]
                    P.op('scalar', 'activation', reads=[f'at{b}{ct}', 'foxg'], writes=[f'ayo{n % 2}'], out=y,
                         in_=at[b][ct], func=AF.Copy, scale=fg[:, ct:ct + 1])
                    P.dma('sync', self.yT[512 + ct * 128:512 + (ct + 1) * 128, qs], y, reads=[f'ayo{n % 2}'])
            P.end()

    def phase_ssd(self, l):
        P, T, NT = self.P, self.T, self.NT
        SC = 512
        assert NT * 8 <= 512
        with contextlib.ExitStack() as st:
            sb = lambda n, s, dt=F32: self.sb(st, n, s, dt)
            cw4 = sb("cw4", [128, 8, 4])
            cb4 = sb("cb4", [128, 8])
            dtb = sb("dtb", [128, 8])
            aneg = sb("aneg", [128, 8])
            dsk = sb("dsk", [128, 8])
            ng = sb("ssdng", [128, 512])
            dt = sb("dt_all", [128, NT, 8])
            dmn = sb("dt_mn", [128, NT, 8])
            dtA = sb("dtA", [128, NT, 8])
            El = sb("El", [128, NT, 8])
            Wl = sb("Wl", [128, NT, 8])
            cd = sb("cd", [128, NT, 8])
            xin = [sb(f"xin{i}", [128, SC + 3]) for i in range(2)]
            cacc = [sb(f"cacc{i}", [128, SC]) for i in range(2)]
            xsT = [sb(f"xsT{i}", [128, SC]) for i in range(4)]
            BT = [sb(f"BT{i}", [128, SC], BF16) for i in range(2)]
            CT = [sb(f"CT{i}", [128, SC], BF16) for i in range(2)]
            x32 = sb("x32", [128, 8, 64])
            Btok = sb("Btok", [128, 256], BF16)
            R = sb("Rall", [128, 8, 128])
            E = sb("Eall", [128, 8, 128])
            CBm = sb("CBm", [128, 2, 128])
            M = sb("Mall", [128, 8, 128], BF16)
            xdt = sb("xdt", [128, 8, 64], BF16)
            xw = sb("xw", [128, 8, 64], BF16)
            H = sb("Hst", [128, 8, 64])
            Hb = sb("Hb", [128, 8, 64], BF16)
            t1 = sb("sst1", [128, 8, 64])
            t2 = sb("sst2", [128, 8, 64])
            zt = sb("zt", [128, 512])
            ssq = sb("ssq", [128, 512])
            gss = sb("gss", [128, 2])
            grs = sb("grs", [128, 2])
            yTs = sb("yTs", [128, 4, 128], BF16)
            ps = self.ps
            psb1 = ps[1].bitcast(BF16)
            P.begin()
            P.dma('sync', cw4, self.convw[l], writes=['cw4'])
            P.dma('sync', cb4, self.convb[l], writes=['cb4'])
            P.dma('sync', dtb, self.dt_bias[l].partition_broadcast(128), writes=['dtb'])
            P.dma('sync', aneg, self.a_log[l].partition_broadcast(128), writes=['aneg'])
            P.dma('sync', dsk, self.ssd_d[l].partition_broadcast(128), writes=['dsk'])
            P.dma('sync', ng, self.ssd_norm_g[l].partition_broadcast(128), writes=['ssdng'])
            for i0 in range(0, NT, 4):
                n_ = min(4, NT - i0)
                P.dma('sync', dt[:, i0:i0 + n_, :],
                      self.dtr[i0 * 128:(i0 + n_) * 128, :].rearrange("(i p) h -> p i h", p=128), writes=['dt_all'])
            P.op('scalar', 'activation', reads=['aneg'], writes=['aneg'], out=aneg, in_=aneg, func=AF.Exp)
            P.op('vector', 'tensor_scalar', reads=['aneg'], writes=['aneg'], out=aneg, in0=aneg, scalar1=-1.0,
                 scalar2=None, op0=ALU.mult)
            bc3 = lambda t: t.unsqueeze(1).broadcast_to([128, NT, 8])
            P.op('vector', 'tensor_tensor', reads=['dt_all', 'dtb'], writes=['dt_all'], out=dt, in0=dt, in1=bc3(dtb),
                 op=ALU.add)
            P.op('vector', 'tensor_scalar', reads=['dt_all'], writes=['dt_mn'], out=dmn, in0=dt, scalar1=0.0,
                 scalar2=None, op0=ALU.max)
            P.op('vector', 'scalar_tensor_tensor', reads=['dt_mn', 'dt_all'], writes=['dt_all'],
                 out=dt.rearrange("p i h -> p (i h)"), in0=dmn.rearrange("p i h -> p (i h)"), scalar=-2.0,
                 in1=dt.rearrange("p i h -> p (i h)"), op0=ALU.mult, op1=ALU.add)
            P.op('scalar', 'activation', reads=['dt_all'], writes=['dt_all'], out=dt, in_=dt, func=AF.Exp)
            P.op('scalar', 'activation', reads=['dt_all'], writes=['dt_all'], out=dt, in_=dt, func=AF.Ln, bias=1.0,
                 scale=1.0)
            P.op('vector', 'tensor_tensor', reads=['dt_all', 'dt_mn'], writes=['dt_all'], out=dt, in0=dt, in1=dmn,
                 op=ALU.add)
            P.op('vector', 'tensor_tensor', reads=['dt_all', 'aneg'], writes=['dtA'], out=dtA, in0=dt, in1=bc3(aneg),
                 op=ALU.mult)
            dtA2 = dtA.rearrange("p i h -> p (i h)")
            P.op('tensor', 'matmul', reads=['tri', 'dtA'], writes=['ps2'], out=ps[2][:, 0:NT * 8], lhsT=self.tri,
                 rhs=dtA2, start=True, stop=True)
            P.op('tensor', 'matmul', reads=['ones', 'dtA'], writes=['ps3'], out=ps[3][:, 0:NT * 8], lhsT=self.ones,
                 rhs=dtA2, start=True, stop=True)
            f2 = lambda t: t.rearrange("p i h -> p (i h)")
            P.op('scalar', 'activation', reads=['ps2'], writes=['El'], out=f2(El), in_=ps[2][:, 0:NT * 8], func=AF.Exp)
            P.op('scalar', 'activation', reads=['ps3'], writes=['cd'], out=f2(cd), in_=ps[3][:, 0:NT * 8], func=AF.Exp)
            P.op('vector', 'tensor_copy', reads=['ps3'], writes=['Wl'], out=f2(Wl), in_=ps[3][:, 0:NT * 8])
            P.op('vector', 'tensor_tensor', reads=['Wl', 'ps2'], writes=['Wl'], out=f2(Wl), in0=f2(Wl),
                 in1=ps[2][:, 0:NT * 8], op=ALU.subtract)
            P.op('scalar', 'activation', reads=['Wl'], writes=['Wl'], out=Wl, in_=Wl, func=AF.Exp)
            P.op('vector', 'tensor_tensor', reads=['Wl', 'dt_all'], writes=['Wl'], out=Wl, in0=Wl, in1=dt, op=ALU.mult)
            P.op('gpsimd', 'memset', writes=['Hst'], ap=H, constant=0.0)
            P.op('gpsimd', 'memset', writes=['Hb'], ap=Hb, constant=0.0)
            for c0 in range(0, T, SC):
                for ct in range(8):
                    X = xin[ct % 2]
                    rx = f'xin{ct % 2}'
                    A = cacc[ct % 2]
                    ra = f'cacc{ct % 2}'
                    rows = slice(ct * 128, (ct + 1) * 128)
                    if c0 == 0:
                        P.op('gpsimd', 'memset', writes=[rx], ap=X[:, 0:3], constant=0.0)
                        P.dma('sync', X[:, 3:], self.xbcT[rows, 0:SC], writes=[rx])
                    else:
                        P.dma('sync', X, self.xbcT[rows, c0 - 3:c0 + SC], writes=[rx])
                    P.op('vector', 'tensor_scalar', reads=[rx, 'cw4', 'cb4'], writes=[ra], out=A, in0=X[:, 0:SC],
                         scalar1=cw4[:, ct, 0:1], scalar2=cb4[:, ct:ct + 1], op0=ALU.mult, op1=ALU.add)
                    for k in range(1, 4):
                        P.op('vector', 'scalar_tensor_tensor', reads=[rx, 'cw4', ra], writes=[ra], out=A,
                             in0=X[:, k:k + SC], scalar=cw4[:, ct, k:k + 1], in1=A, op0=ALU.mult, op1=ALU.add)
                    if ct < 4:
                        dst, rd = xsT[ct], f'xsT{ct}'
                    elif ct < 6:
                        dst, rd = BT[ct - 4], f'BT{ct - 4}'
                    else:
                        dst, rd = CT[ct - 6], f'CT{ct - 6}'
                    P.op('scalar', 'activation', reads=[ra], writes=[rd], out=dst, in_=A, func=AF.Silu)
                for cc in range(SC // 128):
                    c = c0 // 128 + cc
                    cs_ = slice(cc * 128, (cc + 1) * 128)
                    tok = slice(c * 128, (c + 1) * 128)
                    for ct in range(4):
                        P.op('tensor', 'transpose', reads=[f'xsT{ct}', 'ident'], writes=['ps0'],
                             out=ps[0][:, ct * 128:(ct + 1) * 128], in_=xsT[ct][:, cs_], identity=self.ident)
                    P.op('scalar', 'activation', reads=['ps0'], writes=['x32'], out=x32.rearrange("p h d -> p (h d)"),
                         in_=ps[0], func=AF.Copy)
                    for g in range(2):
                        P.op('tensor', 'transpose', reads=[f'BT{g}', 'identb'], writes=['ps1'],
                             out=psb1[:, g * 128:(g + 1) * 128], in_=BT[g][:, cs_], identity=self.identb)
                    P.op('vector', 'tensor_copy', reads=['ps1'], writes=['Btok'], out=Btok, in_=psb1[:, 0:256])
                    P.dma('sync', zt, self.zs[tok, :], writes=['zt'])
                    P.op('vector', 'tensor_tensor', reads=['tri', 'dtA'], writes=['Rall'], out=R,
                         in0=self.tri.unsqueeze(1).broadcast_to([128, 8, 128]),
                         in1=dtA[:, c, :].unsqueeze(2).broadcast_to([128, 8, 128]), op=ALU.mult)
                    for hh in range(2):
                        P.op('tensor', 'matmul', reads=['ustr', 'Rall'], writes=[f'ps{2 + hh}'], out=ps[2 + hh],
                             lhsT=self.ustr, rhs=R[:, hh * 4:(hh + 1) * 4, :].rearrange("p h l -> p (h l)"),
                             start=True, stop=True)
                        P.op('scalar', 'activation', reads=[f'ps{2 + hh}'], writes=['Eall'],
                             out=E[:, hh * 4:(hh + 1) * 4, :].rearrange("p h l -> p (h l)"), in_=ps[2 + hh], func=AF.Exp)
                    for g in range(2):
                        P.op('tensor', 'matmul', reads=[f'BT{g}', f'CT{g}'], writes=['ps4'],
                             out=ps[4][:, g * 128:(g + 1) * 128], lhsT=BT[g][:, cs_], rhs=CT[g][:, cs_],
                             start=True, stop=True)
                    P.op('vector', 'tensor_tensor', reads=['ps4', 'tri'], writes=['CBm'], out=CBm,
                         in0=ps[4][:, 0:256].rearrange("p (g l) -> p g l", g=2),
                         in1=self.tri.unsqueeze(1).broadcast_to([128, 2, 128]), op=ALU.mult)
                    for g in range(2):
                        P.op('vector', 'tensor_tensor', reads=['Eall', 'CBm'], writes=['Mall'],
                             out=M[:, g * 4:(g + 1) * 4, :], in0=E[:, g * 4:(g + 1) * 4, :],
                             in1=CBm[:, g:g + 1, :].broadcast_to([128, 4, 128]), op=ALU.mult)
                    P.op('gpsimd', 'tensor_tensor', reads=['x32', 'dt_all'], writes=['xdt'], out=xdt, in0=x32,
                         in1=dt[:, c, :].unsqueeze(2).broadcast_to([128, 8, 64]), op=ALU.mult)
                    P.op('gpsimd', 'tensor_tensor', reads=['x32', 'Wl'], writes=['xw'], out=xw, in0=x32,
                         in1=Wl[:, c, :].unsqueeze(2).broadcast_to([128, 8, 64]), op=ALU.mult)
                    for h in range(8):
                        P.op('tensor', 'matmul', reads=['Mall', 'xdt'], writes=['ps5'], out=ps[5][:, h * 64:(h + 1) * 64],
                             lhsT=M[:, h, :], rhs=xdt[:, h, :], start=True, stop=True)
                    for g in range(2):
                        P.op('tensor', 'matmul', reads=[f'CT{g}', 'Hb'], writes=['ps6'],
                             out=ps[6][:, g * 256:(g + 1) * 256], lhsT=CT[g][:, cs_],
                             rhs=Hb[:, g * 4:(g + 1) * 4, :].rearrange("p h d -> p (h d)"), start=True, stop=True)
                    for g in range(2):
                        P.op('tensor', 'matmul', reads=['Btok', 'xw'], writes=['ps7'],
                             out=ps[7][:, g * 256:(g + 1) * 256], lhsT=Btok[:, g * 128:(g + 1) * 128],
                             rhs=xw[:, g * 4:(g + 1) * 4, :].rearrange("p h d -> p (h d)"), start=True, stop=True)
                    v3 = lambda t: t.rearrange("p (h d) -> p h d", h=8)
                    b3 = lambda t: t.unsqueeze(2).broadcast_to([128, 8, 64])
                    P.op('vector', 'tensor_tensor', reads=['ps6', 'El'], writes=['sst1'], out=t1, in0=v3(ps[6]),
                         in1=b3(El[:, c, :]), op=ALU.mult)
                    P.op('vector', 'tensor_tensor', reads=['sst1', 'ps5'], writes=['sst1'], out=t1, in0=t1, in1=v3(ps[5]),
                         op=ALU.add)
                    P.op('gpsimd', 'tensor_tensor', reads=['x32', 'dsk'], writes=['sst2'], out=t2, in0=x32, in1=b3(dsk),
                         op=ALU.mult)
                    P.op('gpsimd', 'tensor_tensor', reads=['sst1', 'sst2'], writes=['sst1'], out=t1, in0=t1, in1=t2,
                         op=ALU.add)
                    P.op('vector', 'tensor_tensor', reads=['Hst', 'cd'], writes=['Hst'], out=H, in0=H, in1=b3(cd[:, c, :]),
                         op=ALU.mult)
                    P.op('vector', 'tensor_tensor', reads=['Hst', 'ps7'], writes=['Hst'], out=H, in0=H, in1=v3(ps[7]),
                         op=ALU.add)
                    P.op('gpsimd', 'tensor_copy', reads=['Hst'], writes=['Hb'], out=Hb, in_=H)
                    y2 = t1.rearrange("p h d -> p (h d)")
                    P.op('gpsimd', 'tensor_tensor', reads=['sst1', 'zt'], writes=['sst1'], out=y2, in0=y2, in1=zt,
                         op=ALU.mult)
                    for g in range(2):
                        P.op('scalar', 'activation', reads=['sst1'], writes=['ssq', 'gss'],
                             out=ssq[:, g * 256:(g + 1) * 256], in_=y2[:, g * 256:(g + 1) * 256], func=AF.Square,
                             accum_out=gss[:, g:g + 1])
                    self.rstd_ops(gss, grs, 256.0, ['gss'], ['grs'])
                    for g in range(2):
                        P.op('vector', 'scalar_tensor_tensor', reads=['sst1', 'grs', 'ssdng'], writes=['sst2'],
                             out=t2.rearrange("p h d -> p (h d)")[:, g * 256:(g + 1) * 256],
                             in0=y2[:, g * 256:(g + 1) * 256], scalar=grs[:, g:g + 1],
                             in1=ng[:, g * 256:(g + 1) * 256], op0=ALU.mult, op1=ALU.mult)
                    yn = t2.rearrange("p h d -> p (h d)")
                    for ct in range(4):
                        P.op('tensor', 'transpose', reads=['sst2', 'ident'], writes=['ps0'],
                             out=ps[0][:, ct * 128:(ct + 1) * 128], in_=yn[:, ct * 128:(ct + 1) * 128],
                             identity=self.ident)
                    P.op('scalar', 'activation', reads=['ps0'], writes=['yTs'], out=yTs.rearrange("p c t -> p (c t)"),
                         in_=ps[0], func=AF.Copy)
                    P.dma('sync', self.yT[0:512, tok].rearrange("(ct p) t -> p ct t", p=128), yTs, reads=['yTs'])
            P.end()

    def phase_wout_router(self, l, st0):
        P, T, NT = self.P, self.T, self.NT
        src = self.x_in if l == 0 else self.xres
        ps = self.ps
        self.ohb = self.sb(st0, "ohb", [128, NT, 64], BF16)
        self.rw = self.sb(st0, "rw", [128, NT, 2])
        with contextlib.ExitStack() as st:
            sb = lambda n, s, dt=F32: self.sb(st, n, s, dt)
            wo = sb("wo", [128, 8, D], BF16)
            gsc = sb("gscf", [128, D])
            wr = sb("wr", [128, 8, 36])
            rb = sb("rbias", [128, 36])
            yTc = [sb(f"yTc{i}", [128, 8, 512], BF16) for i in range(2)]
            xl = [sb(f"xl{i}", [128, D]) for i in range(2)]
            tt = sb("wtmp", [128, D])
            hb = [sb(f"hperm{i}", [128, D], BF16) for i in range(2)]
            lg = sb("lg", [128, 36])
            sm = sb("rsm", [128, 64])
            gexp = sb("gexp", [128, 4])
            ohg = sb("ohg", [128, 4])
            em = sb("em", [128, 4, 8])
            es = sb("esel", [128, 8])
            t8 = sb("top8", [128, 8])
            s1 = sb("sel1", [128, 8])
            s2 = sb("sel2", [128, 8])
            W = self.w_out[l].rearrange("(kc p) n -> p kc n", p=128)
            P.begin()
            for kc in range(8):
                self.load_cast(st, wo[:, kc, :], W[:, kc, :], 'wo', D)
            P.dma('sync', wr[:, :, 0:4], self.w_rg[l].rearrange("(kc p) n -> p kc n", p=128), writes=['wr'])
            P.dma('sync', wr[:, :, 4:36], self.w_re[l].rearrange("(kc p) n -> p kc n", p=128), writes=['wr'])
            P.dma('sync', rb[:, 0:4], self.b_rg[l].partition_broadcast(128), writes=['rbias'])
            P.dma('sync', rb[:, 4:36], self.b_re[l].partition_broadcast(128), writes=['rbias'])
            P.dma('sync', gsc, self.norm_ffn_g[l].partition_broadcast(128), writes=['gscf'])
            P.op('vector', 'scalar_tensor_tensor', reads=['gscf', 'modb'], writes=['gscf'], out=gsc, in0=self.mod(4),
                 scalar=1.0, in1=gsc, op0=ALU.add, op1=ALU.mult)

            def pre(i, X, rx):
                q, j = divmod(i, 4)
                Y = yTc[q % 2]
                ry = f'yTc{q % 2}'
                if j == 0:
                    P.dma('sync', Y, self.yT[:, q * 512:(q + 1) * 512].rearrange("(kc p) t -> p kc t", p=128),
                          writes=[ry])
                XL = xl[i % 2]
                rl = f'xl{i % 2}'
                P.dma('sync', XL, src[i * 128:(i + 1) * 128, :], writes=[rl])
                for half in range(2):
                    pb = 2 + half
                    for kc in range(8):
                        P.op('tensor', 'matmul', reads=[ry, 'wo'], writes=[f'ps{pb}'], out=ps[pb],
                             lhsT=Y[:, kc, j * 128:(j + 1) * 128], rhs=wo[:, kc, half * 512:(half + 1) * 512],
                             start=(kc == 0), stop=(kc == 7))
                    hs = slice(half * 512, (half + 1) * 512)
                    P.op('vector', 'tensor_tensor', reads=[f'ps{pb}', 'modb'], writes=['wtmp'], out=tt[:, hs], in0=ps[pb],
                         in1=self.mod(2)[:, hs], op=ALU.mult)
                P.op('gpsimd', 'tensor_tensor', reads=['wtmp', rl], writes=[rx], out=X, in0=tt, in1=XL, op=ALU.add)
                P.dma('sync', self.xres[i * 128:(i + 1) * 128, :], X, reads=[rx])

            def after_h(i, Hh, rh, X, rx):
                Hp = hb[i % 2]
                rp = f'hperm{i % 2}'
                P.op('gpsimd', 'tensor_copy', reads=[rh], writes=[rp], out=Hp.rearrange("t (kc p) -> t kc p", kc=8),
                     in_=Hh.rearrange("t (p kc) -> t kc p", kc=8))
                P.dma('sync', self.hfp[i * 128:(i + 1) * 128, :], Hp, reads=[rp])

            def consumer(q, hTb, hT32b):
                hT32, r32 = hT32b
                for j in range(4):
                    i = q * 4 + j
                    for kc in range(8):
                        P.op('tensor', 'matmul', reads=[r32, 'wr'], writes=['ps4'], out=ps[4][:, 0:36],
                             lhsT=hT32[:, kc, j * 128:(j + 1) * 128], rhs=wr[:, kc, :], start=(kc == 0), stop=(kc == 7))
                    P.op('vector', 'tensor_tensor', reads=['ps4', 'rbias'], writes=['lg'], out=lg, in0=ps[4][:, 0:36],
                         in1=rb, op=ALU.add)
                    V = lambda name, **kw: P.op('vector', name, **kw)
                    gl = lg[:, 0:4]
                    el = lg[:, 4:36].rearrange("p (g e) -> p g e", g=4)
                    gmax, ngmax, gsum, pg = sm[:, 0:1], sm[:, 1:2], sm[:, 2:3], sm[:, 3:4]
                    ne1, r_, den, w1 = sm[:, 4:5], sm[:, 5:6], sm[:, 6:7], sm[:, 7:8]
                    V('reduce_max', reads=['lg'], writes=['rsm'], out=gmax, in_=gl, axis=AX.X)
                    V('tensor_scalar', reads=['rsm'], writes=['rsm'], out=ngmax, in0=gmax, scalar1=-1.0, scalar2=None,
                      op0=ALU.mult)
                    V('tensor_scalar', reads=['lg', 'rsm'], writes=['ohg'], out=ohg, in0=gl, scalar1=gmax, scalar2=None,
                      op0=ALU.is_ge)
                    P.op('scalar', 'activation', reads=['lg', 'rsm'], writes=['gexp', 'rsm'], out=gexp, in_=gl,
                         func=AF.Exp, bias=ngmax, scale=1.0, accum_out=gsum)
                    V('reciprocal', reads=['rsm'], writes=['rsm'], out=pg, in_=gsum)
                    V('tensor_tensor', reads=['lg', 'ohg'], writes=['em'], out=em, in0=el,
                      in1=ohg.unsqueeze(2).broadcast_to([128, 4, 8]), op=ALU.mult)
                    V('tensor_reduce', reads=['em'], writes=['esel'], out=es, in_=em.rearrange("p g e -> p e g"),
                      axis=AX.X, op=ALU.add)
                    V('max', reads=['esel'], writes=['top8'], out=t8, in_=es)
                    V('tensor_scalar', reads=['esel', 'top8'], writes=['sel1'], out=s1, in0=es, scalar1=t8[:, 0:1],
                      scalar2=None, op0=ALU.is_ge)
                    V('tensor_scalar', reads=['esel', 'top8'], writes=['sel2'], out=s2, in0=es, scalar1=t8[:, 1:2],
                      scalar2=None, op0=ALU.is_ge)
                    V('tensor_tensor', reads=['sel2', 'sel1'], writes=['sel2'], out=s2, in0=s2, in1=s1, op=ALU.subtract)
                    V('tensor_scalar', reads=['top8'], writes=['rsm'], out=ne1, in0=t8[:, 0:1], scalar1=-1.0,
                      scalar2=None, op0=ALU.mult)
                    P.op('scalar', 'activation', reads=['top8', 'rsm'], writes=['rsm'], out=r_, in_=t8[:, 1:2],
                         func=AF.Exp, bias=ne1, scale=1.0)
                    V('tensor_scalar', reads=['rsm'], writes=['rsm'], out=den, in0=r_, scalar1=1.0, scalar2=None,
                      op0=ALU.add)
                    V('reciprocal', reads=['rsm'], writes=['rsm'], out=den, in_=den)
                    V('tensor_tensor', reads=['rsm'], writes=['rw'], out=self.rw[:, i, 0:1], in0=den, in1=pg, op=ALU.mult)
                    V('tensor_tensor', reads=['rw', 'rsm'], writes=['rw'], out=self.rw[:, i, 1:2], in0=self.rw[:, i, 0:1],
                      in1=r_, op=ALU.mult)
                    for k, sel in enumerate((s1, s2)):
                        V('tensor_tensor', reads=['ohg', f'sel{k + 1}'], writes=['ohb'],
                          out=self.ohb[:, i, k * 32:(k + 1) * 32].rearrange("p (g e) -> p g e", g=4),
                          in0=ohg.unsqueeze(2).broadcast_to([128, 4, 8]),
                          in1=sel.unsqueeze(1).broadcast_to([128, 4, 8]), op=ALU.mult)
            self.norm_and_transpose_loop(st, None, gsc, self.mod(3), consumer, pre=pre, after_h=after_h)
            P.end()

    def phase_moe(self, l, st0):
        P, T, NT, NB, BLK = self.P, self.T, self.NT, self.NB, self.BLK
        ps = self.ps
        last = (l == self.L - 1)
        NR = NB * BLK
        wgv = self.w_gate.rearrange("l e (p two k4) f -> (l e p two) (k4 f)", two=2, k4=4)
        wuv = self.w_up.rearrange("l e (p two k4) f -> (l e p two) (k4 f)", two=2, k4=4)
        wdv = self.w_down.rearrange("l e (p two f2) d -> (l e p two) (f2 d)", two=2, f2=2)
        with contextlib.ExitStack() as st:
            sb = lambda n, s, dt=F32: self.sb(st, n, s, dt)
            dest_i = sb("dest_i", [128, NT, 2], I32)
            widx = sb("widx", [128, NB, 2], I32)
            with contextlib.ExitStack() as s1:
                sb1 = lambda n, s, dt=F32: self.sb(s1, n, s, dt)
                pre = sb1("pre_all", [128, NT, 64])
                tot = sb1("tot_all", [128, NT, 64])
                base = sb1("base_all", [128, NT, 64])
                cnt = sb1("cnt", [128, 64])
                tl = sb1("mtotal", [128, 32])
                md = sb1("mmod", [128, 32])
                pend = sb1("pend", [128, 32])
                off = sb1("moff", [128, 64])
                dest_f = sb1("dest_f", [128, NT, 2])
                blk0 = sb1("blk0", [128, NB])
                cmp_ = sb1("mcmp", [128, NB, 32])
                be = sb1("mbe", [128, NB])
                pidx = sb1("pidx", [128, 2])
                wf = sb1("widx_f", [128, NB, 2])
                dbg_t = sb1("rdbg_t", [128, 68])
                P.begin()
                ohb2 = self.ohb.rearrange("p i c -> p (i c)")
                f2 = lambda t: t.rearrange("p i c -> p (i c)")
                for k, c0 in enumerate(range(0, NT * 64, 512)):
                    n = min(512, NT * 64 - c0)
                    P.op('tensor', 'matmul', reads=['sutb', 'ohb'], writes=['ps0'], out=ps[0][:, 0:n], lhsT=self.sutb,
                         rhs=ohb2[:, c0:c0 + n], start=True, stop=True)
                    P.op('scalar', 'activation', reads=['ps0'], writes=['pre_all'], out=f2(pre)[:, c0:c0 + n],
                         in_=ps[0][:, 0:n], func=AF.Copy)
                    P.op('tensor', 'matmul', reads=['onesb', 'ohb'], writes=['ps1'], out=ps[1][:, 0:n], lhsT=self.onesb,
                         rhs=ohb2[:, c0:c0 + n], start=True, stop=True)
                    P.op('vector', 'tensor_copy', reads=['ps1'], writes=['tot_all'], out=f2(tot)[:, c0:c0 + n],
                         in_=ps[1][:, 0:n])
                V = lambda name, **kw: P.op('vector', name, **kw)
                P.op('gpsimd', 'memset', writes=['base_all'], ap=base[:, 0, :], constant=0.0)
                for i in range(1, NT):
                    V('tensor_tensor', reads=['base_all', 'tot_all'], writes=['base_all'], out=base[:, i, :],
                      in0=base[:, i - 1, :], in1=tot[:, i - 1, :], op=ALU.add)
                V('tensor_tensor', reads=['base_all', 'tot_all'], writes=['cnt'], out=cnt, in0=base[:, NT - 1, :],
                  in1=tot[:, NT - 1, :], op=ALU.add)
                V('tensor_tensor', reads=['cnt'], writes=['mtotal'], out=tl, in0=cnt[:, 0:32], in1=cnt[:, 32:64],
                  op=ALU.add)
                P.op('gpsimd', 'iota', writes=['blk0'], out=blk0, pattern=[[BLK, NB]], base=0, channel_multiplier=0,
                     allow_small_or_imprecise_dtypes=True)
                V('tensor_tensor', reads=['mtotal', 'blk0'], writes=['mcmp'], out=cmp_.rearrange("p b e -> p (b e)").rearrange("p (e b) -> p e b", e=32),
                  in0=blk0.unsqueeze(1).broadcast_to([128, 32, NB]), in1=tl.unsqueeze(2).broadcast_to([128, 32, NB]),
                  op=ALU.is_lt)
                V('tensor_reduce', reads=['mcmp'], writes=['mtotal'], out=tl,
                  in_=cmp_.rearrange("p b e -> p (b e)").rearrange("p (e b) -> p e b", e=32), axis=AX.X, op=ALU.add)
                V('tensor_scalar', reads=['mtotal'], writes=['mtotal'], out=tl, in0=tl, scalar1=float(BLK), scalar2=None,
                  op0=ALU.mult)
                V('tensor_tensor_scan', reads=['mtotal', 'ones'], writes=['pend'], out=pend,
                  data0=self.ones[:, 0:32], data1=tl, initial=0.0, op0=ALU.mult, op1=ALU.add)
                V('tensor_tensor', reads=['pend', 'mtotal'], writes=['moff'], out=off[:, 0:32], in0=pend, in1=tl,
                  op=ALU.subtract)
                V('tensor_tensor', reads=['moff', 'cnt'], writes=['moff'], out=off[:, 32:64], in0=off[:, 0:32],
                  in1=cnt[:, 0:32], op=ALU.add)
                V('tensor_tensor', reads=['pre_all', 'base_all'], writes=['pre_all'], out=pre, in0=pre, in1=base,
                  op=ALU.add)
                V('tensor_tensor', reads=['pre_all', 'moff'], writes=['pre_all'], out=pre, in0=pre,
                  in1=off.unsqueeze(1).broadcast_to([128, NT, 64]), op=ALU.add)
                V('tensor_tensor', reads=['pre_all', 'ohb'], writes=['pre_all'], out=pre, in0=pre, in1=self.ohb,
                  op=ALU.mult)
                V('tensor_reduce', reads=['pre_all'], writes=['dest_f'], out=dest_f,
                  in_=pre.rearrange("p i (k e) -> p i k e", k=2), axis=AX.X, op=ALU.add)
                V('tensor_copy', reads=['dest_f'], writes=['dest_i'], out=dest_i, in_=dest_f)
                V('tensor_tensor', reads=['pend', 'blk0'], writes=['mcmp'], out=cmp_,
                  in0=pend.unsqueeze(1).broadcast_to([128, NB, 32]), in1=blk0.unsqueeze(2).broadcast_to([128, NB, 32]),
                  op=ALU.is_le)
                V('tensor_reduce', reads=['mcmp'], writes=['mbe'], out=be, in_=cmp_, axis=AX.X, op=ALU.add)
                V('tensor_scalar', reads=['mbe'], writes=['mbe'], out=be, in0=be, scalar1=float(NE - 1), scalar2=256.0,
                  op0=ALU.min, op1=ALU.mult)
                P.op('gpsimd', 'iota', writes=['pidx'], out=pidx, pattern=[[1, 2]], base=l * NE * 256, channel_multiplier=2,
                     allow_small_or_imprecise_dtypes=True)
                V('tensor_tensor', reads=['mbe', 'pidx'], writes=['widx_f'], out=wf,
                  in0=be.unsqueeze(2).broadcast_to([128, NB, 2]), in1=pidx.unsqueeze(1).broadcast_to([128, NB, 2]),
                  op=ALU.add)
                V('tensor_copy', reads=['widx_f'], writes=['widx'], out=widx, in_=wf)
                if 'rdbg' in self.dbg:
                    for i in range(NT):
                        V('tensor_copy', reads=['ohb'], writes=['rdbg_t'], out=dbg_t[:, 0:64], in_=self.ohb[:, i, :])
                        V('tensor_copy', reads=['rw'], writes=['rdbg_t'], out=dbg_t[:, 64:66], in_=self.rw[:, i, :])
                        V('tensor_copy', reads=['dest_f'], writes=['rdbg_t'], out=dbg_t[:, 66:68], in_=dest_f[:, i, :])
                        P.dma('sync', self.rdbg[i * 128:(i + 1) * 128, :], dbg_t, reads=['rdbg_t'])
                P.end()
            with contextlib.ExitStack() as s2:
                hrow = [self.sb(s2, f"hrow{i}", [128, D], BF16) for i in range(3)]
                P.begin()
                P.raw('gpsimd', 'reg_mov', self.reg_rows, NR - 1)
                for i in range(NT):
                    Hr = hrow[i % 3]
                    rr = f'hrow{i % 3}'
                    P.dma('sync', Hr, self.hfp[i * 128:(i + 1) * 128, :], writes=[rr])
                    for k in range(2):
                        P.dma('gpsimd', None, None, reads=[rr, 'dest_i'], writes=['xs'],
                              fn=('indirect_dma_start', dict(
                                  out=self.xs, out_offset=bass.IndirectOffsetOnAxis(ap=dest_i[:, i, k:k + 1], axis=0),
                                  in_=Hr, in_offset=None, bounds_check=self.reg_rows, oob_is_err=False)))
                P.end()
            with contextlib.ExitStack() as s3:
                sb3 = lambda n, s, dt=F32: self.sb(s3, n, s, dt)
                Wg = [sb3(f"Wg{i}", [128, 8, 512], BF16) for i in range(2)]
                Wu = [sb3(f"Wu{i}", [128, 8, 512], BF16) for i in range(2)]
                Wd = [sb3(f"Wd{i}", [128, 4, 1024], BF16) for i in range(2)]
                xsT = [sb3(f"xsT{i}", [128, 8, 512], BF16) for i in range(2)]
                hid = [sb3(f"hid{i}", [128, 4, 512], BF16) for i in range(2)]
                sg = [sb3(f"sg{i}", [128, 512]) for i in range(2)]
                yb = [sb3(f"yb{i}", [128, D]) for i in range(2)]
                P.begin()
                P.raw('gpsimd', 'reg_mov', self.reg_w, self.L * NE * 256 - 1)
                n_y = 0
                for b in range(NB):
                    a = b % 2
                    for (Wt, view, nm) in ((Wg[a], wgv, f'Wg{a}'), (Wu[a], wuv, f'Wu{a}'), (Wd[a], wdv, f'Wd{a}')):
                        flat = Wt.rearrange("p a f -> p (a f)")
                        for hf in range(2):
                            P.dma('gpsimd', None, None, reads=['widx'], writes=[nm],
                                  fn=('indirect_dma_start', dict(
                                      out=flat[:, hf * 2048:(hf + 1) * 2048], out_offset=None, in_=view,
                                      in_offset=bass.IndirectOffsetOnAxis(ap=widx[:, b, hf:hf + 1], axis=0),
                                      bounds_check=self.reg_w, oob_is_err=False)))
                    X = xsT[a]
                    rxs = f'xsT{a}'
                    for kc in range(8):
                        P.dma('sync', None, None, reads=['xs'], writes=[rxs],
                              fn=('dma_start_transpose', dict(
                                  out=X[:, kc, :], in_=self.xs[b * BLK:(b + 1) * BLK, kc * 128:(kc + 1) * 128])))
                    Hd = hid[a]
                    rh = f'hid{a}'
                    wg4 = Wg[a].rearrange("p kc (m four) -> p kc four m", four=4)
                    wu4 = Wu[a].rearrange("p kc (m four) -> p kc four m", four=4)
                    for fc in range(4):
                        pg_, pu_ = 2 + (fc % 2) * 2, 3 + (fc % 2) * 2
                        for kc in range(8):
                            P.op('tensor', 'matmul', reads=[f'Wg{a}', rxs], writes=[f'ps{pg_}'], out=ps[pg_],
                                 lhsT=wg4[:, kc, fc, :], rhs=X[:, kc, :], start=(kc == 0), stop=(kc == 7))
                        for kc in range(8):
                            P.op('tensor', 'matmul', reads=[f'Wu{a}', rxs], writes=[f'ps{pu_}'], out=ps[pu_],
                                 lhsT=wu4[:, kc, fc, :], rhs=X[:, kc, :], start=(kc == 0), stop=(kc == 7))
                        S = sg[fc % 2]
                        P.op('scalar', 'activation', reads=[f'ps{pg_}'], writes=[f'sg{fc % 2}'], out=S, in_=ps[pg_],
                             func=AF.Silu)
                        P.op('vector', 'tensor_tensor', reads=[f'sg{fc % 2}', f'ps{pu_}'], writes=[rh], out=Hd[:, fc, :],
                             in0=S, in1=ps[pu_], op=ALU.mult)
                    for rt in range(4):
                        n_y += 1
                        Y = yb[n_y % 2]
                        ry = f'yb{n_y % 2}'
                        for half in range(2):
                            pb = 6 + half
                            for fc in range(4):
                                P.op('tensor', 'matmul', reads=[rh, f'Wd{a}'], writes=[f'ps{pb}'], out=ps[pb],
                                     lhsT=Hd[:, fc, rt * 128:(rt + 1) * 128], rhs=Wd[a][:, fc, half * 512:(half + 1) * 512],
                                     start=(fc == 0), stop=(fc == 3))
                            self.evac(half, Y[:, half * 512:(half + 1) * 512], ps[pb], [f'ps{pb}'], [ry])
                        r0 = b * BLK + rt * 128
                        P.dma('sync', self.ys[r0:r0 + 128, :], Y, reads=[ry], writes=['ys'])
                P.end()
            with contextlib.ExitStack() as s4:
                sb4 = lambda n, s, dt=F32: self.sb(s4, n, s, dt)
                y1 = [sb4(f"y1_{i}", [128, D]) for i in range(2)]
                y2 = [sb4(f"y2_{i}", [128, D]) for i in range(2)]
                xr = [sb4(f"xr{i}", [128, D]) for i in range(2)]
                fgb = sb4("fgb", [128, D])
                fsq = sb4("fsq", [128, D])
                fss = sb4("fss", [128, 1])
                frs = sb4("frs", [128, 1])
                P.begin()
                P.raw('gpsimd', 'reg_mov', self.reg_rows, NR - 1)
                if last:
                    P.dma('sync', fgb, self.final_g.partition_broadcast(128), writes=['fgb'])
                for i in range(NT):
                    a = i % 2
                    Y1, Y2, X = y1[a], y2[a], xr[a]
                    r1, r2, rx = f'y1_{a}', f'y2_{a}', f'xr{a}'
                    rows = slice(i * 128, (i + 1) * 128)
                    P.dma('sync', X, self.xres[rows, :], writes=[rx])
                    for (Yk, rk, k) in ((Y1, r1, 0), (Y2, r2, 1)):
                        P.dma('gpsimd', None, None, reads=['dest_i'], writes=[rk],
                              fn=('indirect_dma_start', dict(
                                  out=Yk, out_offset=None, in_=self.ys,
                                  in_offset=bass.IndirectOffsetOnAxis(ap=dest_i[:, i, k:k + 1], axis=0),
                                  bounds_check=self.reg_rows, oob_is_err=False)))
                    P.op('vector', 'tensor_scalar', reads=[r1, 'rw'], writes=[r1], out=Y1, in0=Y1,
                         scalar1=self.rw[:, i, 0:1], scalar2=None, op0=ALU.mult)
                    P.op('vector', 'scalar_tensor_tensor', reads=[r1, r2, 'rw'], writes=[r1], out=Y1, in0=Y2,
                         scalar=self.rw[:, i, 1:2], in1=Y1, op0=ALU.mult, op1=ALU.add)
                    P.op('gpsimd', 'tensor_tensor', reads=[r1, 'modb'], writes=[r1], out=Y1, in0=Y1, in1=self.mod(5),
                         op=ALU.mult)
                    P.op('gpsimd', 'tensor_tensor', reads=[r1, rx], writes=[rx], out=X, in0=X, in1=Y1, op=ALU.add)
                    if not last:
                        P.dma('sync', self.xres[rows, :], X, reads=[rx])
                    else:
                        P.op('scalar', 'activation', reads=[rx], writes=['fsq', 'fss'], out=fsq, in_=X, func=AF.Square,
                             accum_out=fss)
                        self.rstd_ops(fss, frs, D, ['fss'], ['frs'])
                        P.op('vector', 'scalar_tensor_tensor', reads=[rx, 'frs', 'fgb'], writes=[r2], out=Y2, in0=X,
                             scalar=frs, in1=fgb, op0=ALU.mult, op1=ALU.mult)
                        P.dma('sync', self.out[rows, :], Y2, reads=[r2])
                P.end()

    def build_layer(self, l):
        with contextlib.ExitStack() as st:
            self.phase_ada(l, st)
            self.phase_inproj(l, st)
            self.phase_conv(l)
            self.phase_attn(l)
            self.phase_ssd(l)
            self.phase_wout_router(l, st)
            if self.moe:
                self.phase_moe(l, st)

    def zero_xs(self):
        P = self.P
        NR = self.NB * self.BLK
        with contextlib.ExitStack() as st:
            z = self.sb(st, "zeros", [128, 8192], BF16)
            P.begin()
            P.op('gpsimd', 'memset', writes=['zeros'], ap=z, constant=0.0)
            xv = self.xs.rearrange("(a p r) d -> a p (r d)", p=128, r=8)
            for a in range(NR // 1024):
                P.dma('sync', xv[a], z, reads=['zeros'])
            P.end()

    def build(self):
        if self.moe:
            self.zero_xs()
        for l in range(self.L):
            self.build_layer(l)
        return self.nc


def prep_core_inputs(inp, b, T, L):
    f = lambda a: np.ascontiguousarray(a, dtype=np.float32)
    m = {}
    m["x"] = f(inp["x"][b, :T])
    m["c"] = f(inp["c"][b].reshape(8, 128).T)
    for k in ("ada_w", "ada_b", "norm_mix_g", "w_in", "ssd_dt_bias", "ssd_a_log", "ssd_d", "ssd_norm_g",
              "w_out", "norm_ffn_g", "w_router_group", "b_router_group", "w_router_expert", "b_router_expert",
              "w_gate", "w_up", "w_down"):
        m[k] = f(inp[k][:L])
    m["final_norm_g"] = f(inp["final_norm_g"])
    m["convw_fm"] = f(inp["ssd_conv_w"][:L].reshape(L, 4, 8, 128).transpose(0, 3, 2, 1))
    m["convb_fm"] = f(inp["ssd_conv_b"][:L].reshape(L, 8, 128).transpose(0, 2, 1))
    m["fox_f_bias_fm"] = f(inp["fox_f_bias"][:L].reshape(L, 4, 1))
    m["fox_g_fm"] = f(inp["fox_norm_g"][:L].reshape(L, 2, 128).transpose(0, 2, 1))
    m["cmw_fm"] = f(inp["cm_conv_w"][:L].reshape(L, 31, 2, 128).transpose(0, 3, 2, 1))
    m["cmb_fm"] = f(inp["cm_conv_b"][:L].reshape(L, 2, 128).transpose(0, 2, 1))
    m["cmg_fm"] = f(inp["cm_ln_g"][:L].reshape(L, 2, 128).transpose(0, 2, 1))
    m["cmbeta_fm"] = f(inp["cm_ln_b"][:L].reshape(L, 2, 128).transpose(0, 2, 1))
    return m


_CACHE = {}
T_FULL, L_FULL, B_FULL = 8192, 4, 4


def kernel(**inputs):
    if 'nc' not in _CACHE:
        bd = Builder(T_FULL, L_FULL)
        _CACHE['nc'] = bd.build()
        _CACHE['names'] = set(bd.ins)
    nc = _CACHE['nc']
    inp = {k: np.asarray(v) for k, v in inputs.items()}
    maps = []
    for b in range(B_FULL):
        m = prep_core_inputs(inp, b, T_FULL, L_FULL)
        maps.append({k: v for k, v in m.items() if k in _CACHE['names']})
    res = run_bass_kernel_spmd(nc, maps, core_ids=list(range(B_FULL)))
    out = np.stack([np.asarray(r["out"], dtype=np.float32) for r in res.results], axis=0)
    return out
```

```python
import contextlib
import numpy as np
import concourse.bass as bass
import concourse.mybir as mybir
from concourse.bass_utils import run_bass_kernel_spmd

F32 = mybir.dt.float32
BF16 = mybir.dt.bfloat16
I32 = mybir.dt.int32
AF = mybir.ActivationFunctionType
ALU = mybir.AluOpType
AX = mybir.AxisListType

ENGS = ('tensor', 'vector', 'scalar', 'gpsimd', 'sync')
CENGS = ('tensor', 'vector', 'scalar', 'gpsimd')
NDS = 10

D = 1024
D_IN = 2828
EPS = 1e-6
NE = 32
DE = 512


class Prog:
    def __init__(self, nc, same_engine_sync=True):
        self.nc = nc
        self.same = same_engine_sync
        self.esem = {e: nc.alloc_semaphore(name=f"es_{e}") for e in CENGS}
        self.dsem = {e: [nc.alloc_semaphore(name=f"ds_{e}_{i}") for i in range(NDS)]
                     for e in ('sync', 'scalar', 'gpsimd')}
        self._reset()
        self.q = None

    def _reset(self):
        keep = getattr(self, 'dcnt', {}).get('gpsimd', [0] * NDS)
        self.ecnt = {e: 0 for e in CENGS}
        self.dcnt = {e: [0] * NDS for e in self.dsem}
        self.dcnt['gpsimd'] = list(keep)
        self.dnext = {e: 0 for e in self.dsem}
        self.known = {e: {('d', 'gpsimd', i): keep[i] for i in range(NDS)} for e in ENGS}
        self.res_w = {}
        self.res_r = {}

    def semof(self, key):
        if key[0] == 'e':
            return self.esem[key[1]]
        return self.dsem[key[1]][key[2]]

    def begin(self):
        self.q = {e: [] for e in ENGS}

    def _deps(self, reads, writes):
        deps = []
        for r in reads:
            t = self.res_w.get(r)
            if t is not None:
                deps.append(t)
        for w in writes:
            t = self.res_w.get(w)
            if t is not None:
                deps.append(t)
            deps.extend(self.res_r.get(w, {}).items())
        return deps

    def _record(self, tok, reads, writes):
        for r in reads:
            d = self.res_r.setdefault(r, {})
            if d.get(tok[0], 0) < tok[1]:
                d[tok[0]] = tok[1]
        for w in writes:
            self.res_w[w] = tok
            self.res_r[w] = {}

    def _waits(self, eng, deps):
        waits = []
        best = {}
        for (key, c) in deps:
            if best.get(key, 0) < c:
                best[key] = c
        for key, c in best.items():
            if key == ('e', 'tensor') and eng == 'tensor':
                continue
            if (not self.same) and key == ('e', eng):
                continue
            if self.known[eng].get(key, 0) >= c:
                continue
            self.known[eng][key] = c
            waits.append((self.semof(key), c))
        return waits

    def op(self, eng, name, reads=(), writes=(), **kw):
        writes = list(writes) + [r for r in reads if r.startswith('ps') and r not in writes]
        waits = self._waits(eng, self._deps(reads, writes))
        self.ecnt[eng] += 1
        tok = (('e', eng), self.ecnt[eng])
        sem = self.esem[eng]

        def emit(e, name=name, kw=kw, waits=waits, sem=sem):
            for (s, c) in waits:
                e.wait_ge(s, c)
            getattr(e, name)(**kw).then_inc(sem, 1)
        self.q[eng].append(emit)
        self._record(tok, reads, writes)
        return tok

    def raw(self, eng, name, *args, **kw):
        self.q[eng].append(lambda e: getattr(e, name)(*args, **kw))

    def dma(self, eng, out, in_, reads=(), writes=(), fn=None, **kw):
        deps = self._deps(reads, writes)
        idx = self.dnext[eng]
        self.dnext[eng] = (idx + 1) % NDS
        key = ('d', eng, idx)
        prev = self.dcnt[eng][idx]
        if prev:
            deps.append((key, prev))
        waits = self._waits(eng, deps)
        self.dcnt[eng][idx] += 16
        tok = (key, self.dcnt[eng][idx])
        sem = self.dsem[eng][idx]

        def emit(e, waits=waits, sem=sem):
            for (s, c) in waits:
                e.wait_ge(s, c)
            if fn is not None:
                try:
                    ins = getattr(e, fn[0])(**fn[1])
                except Exception:
                    print("DMA builder failed:", fn[0], {k: (v.shape if hasattr(v, 'shape') else v) for k, v in fn[1].items()})
                    raise
                ins.then_inc(sem, 16)
            else:
                e.dma_start(out=out, in_=in_, **kw).then_inc(sem, 16)
        self.q[eng].append(emit)
        self._record(tok, reads, writes)
        return tok

    def end(self):
        nc = self.nc
        fin = []
        for e in self.dsem:
            for i in range(NDS):
                c = self.dcnt[e][i]
                if c and self.known['sync'].get(('d', e, i), 0) < c:
                    fin.append((self.dsem[e][i], c))

        def drain(e, fin=fin):
            for (s, c) in fin:
                e.wait_ge(s, c)
        self.q['sync'].append(drain)
        with nc.Block() as block:
            for en in ENGS:
                ops = self.q[en]
                if not ops:
                    continue

                def body(e, ops=ops):
                    for o in ops:
                        o(e)
                getattr(block, en)(body)
        allsems = list(self.esem.values()) + [s for e in self.dsem if e != 'gpsimd' for s in self.dsem[e]]
        with nc.Block() as block:
            def clr(e):
                for s in allsems:
                    e.sem_clear(s)
            block.sync(clr)
        self._reset()
        self.q = None


class Builder:
    def __init__(self, T, L, dbg=(), moe=True, phases='aicfsom', opt=''):
        self.opt = opt
        self.T, self.L = T, L
        self.moe = moe
        self.phases = phases
        self.NT = T // 128
        self.NQ = T // 512
        self.dbg = set(dbg)
        nc = self.nc = bass.Bass("TRN2", target_bir_lowering=False)
        self.P = Prog(nc, same_engine_sync=('S' not in self.opt))
        self.ins = {}
        self.outs = {}
        self._uid = 0
        self.declare_io()
        self.consts()

    def din(self, name, shape, dt=F32):
        t = self.nc.dram_tensor(name, list(shape), dt, kind="ExternalInput").ap()
        self.ins[name] = t
        return t

    def dscr(self, name, shape, dt=F32):
        kind = "ExternalOutput" if name in self.dbg else "Internal"
        t = self.nc.dram_tensor(name, list(shape), dt, kind=kind).ap()
        if kind == "ExternalOutput":
            self.outs[name] = t
        return t

    def sb(self, st, name, shape, dt=F32):
        self._uid += 1
        return st.enter_context(self.nc.sbuf_tensor(f"{name}_{self._uid}", list(shape), dt)).ap()

    def declare_io(self):
        T, L = self.T, self.L
        d = self.din
        self.x_in = d("x", [T, D])
        self.c_in = d("c", [128, 8])
        self.ada_w = d("ada_w", [L, D, 6 * D])
        self.ada_b = d("ada_b", [L, 6 * D])
        self.norm_mix_g = d("norm_mix_g", [L, D])
        self.w_in = d("w_in", [L, D, D_IN])
        self.convw = d("convw_fm", [L, 128, 8, 4])
        self.convb = d("convb_fm", [L, 128, 8])
        self.dt_bias = d("ssd_dt_bias", [L, 8])
        self.a_log = d("ssd_a_log", [L, 8])
        self.ssd_d = d("ssd_d", [L, 8])
        self.ssd_norm_g = d("ssd_norm_g", [L, 512])
        self.f_bias = d("fox_f_bias_fm", [L, 4, 1])
        self.fox_g = d("fox_g_fm", [L, 128, 2])
        self.cmw = d("cmw_fm", [L, 128, 2, 31])
        self.cmb = d("cmb_fm", [L, 128, 2])
        self.cmg = d("cmg_fm", [L, 128, 2])
        self.cmbeta = d("cmbeta_fm", [L, 128, 2])
        self.w_out = d("w_out", [L, D, D])
        self.norm_ffn_g = d("norm_ffn_g", [L, D])
        self.w_rg = d("w_router_group", [L, D, 4])
        self.b_rg = d("b_router_group", [L, 4])
        self.w_re = d("w_router_expert", [L, D, NE])
        self.b_re = d("b_router_expert", [L, NE])
        if self.moe:
            self.w_gate = d("w_gate", [L, NE, D, DE])
            self.w_up = d("w_up", [L, NE, D, DE])
            self.w_down = d("w_down", [L, NE, DE, D])
        self.final_g = d("final_norm_g", [D])
        self.out = self.nc.dram_tensor("out", [T, D], F32, kind="ExternalOutput").ap()
        self.outs["out"] = self.out
        s = self.dscr
        self.xres = s("xres", [T, D])
        self.zs = s("zs", [T, 512])
        self.xbcT = s("xbcT", [1024, T])
        self.dtr = s("dtr", [T, 8])
        self.flT = s("flT", [4, T])
        self.qT = s("qT", [256, T], BF16)
        self.kT = s("kT", [256, T], BF16)
        self.vtok = s("vtok", [T, 256], BF16)
        self.gaT = s("gaT", [256, T])
        self.gbT = s("gbT", [256, T])
        self.yT = s("yT", [1024, T], BF16)
        self.attT = s("attT", [256, T])
        self.cs = s("cs", [4, 6, T], BF16)
        self.hfp = s("hfp", [T, D], BF16)
        self.BLK = 512
        self.NB = (2 * T) // self.BLK + NE
        self.xs = s("xs", [self.NB * self.BLK, D], BF16)
        self.ys = s("ys", [self.NB * self.BLK, D])
        self.rdbg = s("rdbg", [T, 68])

    def consts(self):
        nc, P = self.nc, self.P
        a = lambda n, s, dt=F32: nc.alloc_sbuf_tensor(n, list(s), dt).ap()
        self.ident = a("ident", [128, 128])
        self.identb = a("identb", [128, 128], BF16)
        self.tri = a("tri", [128, 128])
        self.ustr = a("ustr", [128, 128])
        self.ones = a("ones", [128, 128])
        self.onesb = a("onesb", [128, 128], BF16)
        self.epsb = a("epsb", [128, 1])
        self.sutb = a("sutb", [128, 128], BF16)
        self.ps = [nc.alloc_psum_tensor(f"ps{i}", [128, 512], F32).ap() for i in range(8)]
        self.reg_rows = nc.gpsimd.alloc_register("bc_rows")
        self.reg_w = nc.gpsimd.alloc_register("bc_w")
        P.begin()
        g = 'gpsimd'
        P.op(g, 'memset', writes=['epsb'], ap=self.epsb, constant=EPS)
        P.op(g, 'memset', writes=['ones'], ap=self.ones, constant=1.0)
        P.op(g, 'memset', writes=['onesb'], ap=self.onesb, constant=1.0)
        P.op(g, 'memset', writes=['ident'], ap=self.ident, constant=1.0)
        P.op(g, 'affine_select', reads=['ident'], writes=['ident'], out=self.ident, in_=self.ident,
             pattern=[[-1, 128]], compare_op=ALU.is_equal, fill=0.0, base=0, channel_multiplier=1)
        P.op(g, 'tensor_copy', reads=['ident'], writes=['identb'], out=self.identb, in_=self.ident)
        P.op(g, 'memset', writes=['tri'], ap=self.tri, constant=1.0)
        P.op(g, 'affine_select', reads=['tri'], writes=['tri'], out=self.tri, in_=self.tri,
             pattern=[[1, 128]], compare_op=ALU.is_ge, fill=0.0, base=0, channel_multiplier=-1)
        P.op(g, 'tensor_tensor', reads=['tri', 'ident'], writes=['sutb'], out=self.sutb, in0=self.tri, in1=self.ident,
             op=ALU.subtract)
        P.op(g, 'memset', writes=['ustr'], ap=self.ustr, constant=1.0)
        P.op(g, 'affine_select', reads=['ustr'], writes=['ustr'], out=self.ustr, in_=self.ustr,
             pattern=[[-1, 128]], compare_op=ALU.is_gt, fill=0.0, base=0, channel_multiplier=1)
        P.end()

    def phase_ada(self, l, st):
        nc, P = self.nc, self.P
        self.modb = self.sb(st, "modb", [128, 6 * D])
        with contextlib.ExitStack() as s2:
            cs = self.sb(s2, "c_s", [128, 8])
            cb = self.sb(s2, "c_b", [128, 8, 128])
            adab = self.sb(s2, "adab", [128, 6 * D])
            wch = [self.sb(s2, f"adaw{i}", [128, 8, 512]) for i in range(2)]
            P.begin()
            P.dma('sync', cs, self.c_in, writes=['c_s'])
            P.dma('sync', adab, self.ada_b[l].partition_broadcast(128), writes=['adab'])
            P.op('scalar', 'activation', reads=['c_s'], writes=['c_s'], out=cs, in_=cs, func=AF.Silu)
            P.op('vector', 'tensor_copy', reads=['c_s'], writes=['c_b'], out=cb,
                 in_=cs.unsqueeze(2).broadcast_to([128, 8, 128]))
            for j in range(12):
                w = wch[j % 2]
                wr = f'adaw{j % 2}'
                P.dma('sync', w,
                      self.ada_w[l][:, j * 512:(j + 1) * 512].rearrange("(kc p) n -> p kc n", p=128),
                      writes=[wr])
                pt = self.ps[j % 2]
                for kc in range(8):
                    P.op('tensor', 'matmul', reads=['c_b', wr], writes=[f'ps{j % 2}'], out=pt,
                         lhsT=cb[:, kc, :], rhs=w[:, kc, :], start=(kc == 0), stop=(kc == 7))
                P.op('vector', 'tensor_tensor', reads=[f'ps{j % 2}', 'adab'], writes=['modb'],
                     out=self.modb[:, j * 512:(j + 1) * 512], in0=pt, in1=adab[:, j * 512:(j + 1) * 512], op=ALU.add)
            P.end()

    def mod(self, i):
        return self.modb[:, i * D:(i + 1) * D]

    def load_cast(self, st, dst, src, res, width):
        P = self.P
        if getattr(self, '_stg_owner', None) is not st:
            self._stg = [self.sb(st, f"stg{i}", [128, 2048]) for i in range(2)]
            self._stg_owner = st
            self._stg_n = 0
        for c0 in range(0, width, 2048):
            n = min(2048, width - c0)
            k = self._stg_n
            self._stg_n += 1
            S = self._stg[k % 2]
            rs = f'stg{k % 2}'
            P.dma('sync', S[:, 0:n], src[:, c0:c0 + n], writes=[rs])
            P.op('gpsimd' if k % 2 else 'vector', 'tensor_copy', reads=[rs], writes=[res], out=dst[:, c0:c0 + n],
                 in_=S[:, 0:n])

    def rstd_ops(self, ss, rstd, n, rd, wr):
        P = self.P
        P.op('scalar', 'activation', reads=rd, writes=wr, out=rstd, in_=ss, func=AF.Ln,
             bias=self.epsb[:ss.shape[0], :], scale=1.0 / n)
        P.op('scalar', 'activation', reads=wr, writes=wr, out=rstd, in_=rstd, func=AF.Exp, scale=-0.5)

    def phase_inproj(self, l, st0):
        nc, P, T = self.nc, self.P, self.T
        src = self.x_in if l == 0 else self.xres
        self._ip_bufs = None
        with contextlib.ExitStack() as st:
            sb = lambda n, s, dt=F32: self.sb(st, n, s, dt)
            wz = sb("wz", [128, 8, 512], BF16)
            wx = sb("wx", [128, 8, 1024], BF16)
            wqk = sb("wqk", [128, 8, 512], BF16)
            wv = sb("wv", [128, 8, 256], BF16)
            wg = sb("wg", [128, 8, 512], BF16)
            wdt = sb("wdt", [128, 8, 8])
            wf = sb("wf", [128, 8, 4])
            gsc = sb("gsc", [128, D])
            W = self.w_in[l].rearrange("(kc p) n -> p kc n", p=128)
            P.begin()
            for kc in range(8):
                self.load_cast(st, wz[:, kc, :], W[:, kc, 0:512], 'wz', 512)
                self.load_cast(st, wx[:, kc, :], W[:, kc, 512:1536], 'wx', 1024)
                self.load_cast(st, wqk[:, kc, :], W[:, kc, 1544:2056], 'wqk', 512)
                self.load_cast(st, wv[:, kc, :], W[:, kc, 2056:2312], 'wv', 256)
                self.load_cast(st, wg[:, kc, :], W[:, kc, 2316:2828], 'wg', 512)
            P.dma('sync', wdt, W[:, :, 1536:1544], writes=['wdt'])
            P.dma('sync', wf, W[:, :, 2312:2316], writes=['wf'])
            P.dma('sync', gsc, self.norm_mix_g[l].partition_broadcast(128), writes=['gsc'])
            P.op('vector', 'scalar_tensor_tensor', reads=['gsc', 'modb'], writes=['gsc'], out=gsc,
                 in0=self.mod(1), scalar=1.0, in1=gsc, op0=ALU.add, op1=ALU.mult)
            self.norm_and_transpose_loop(st, src, gsc, self.mod(0), consumer=lambda q, hT, hT32: self.inproj_chunk(
                q, hT, hT32, wz, wx, wqk, wv, wg, wdt, wf, st))
            P.end()

    def norm_and_transpose_loop(self, st, src, gsc, shift, consumer, pre=None, after_h=None, pre_load=None):
        P = self.P
        sb = lambda n, s, dt=F32: self.sb(st, n, s, dt)
        NXB = 4
        xt = [sb(f"xt{i}", [128, D]) for i in range(NXB)]
        ht = [sb(f"ht{i}", [128, D]) for i in range(2)]
        sq = sb("sq", [128, D])
        ss = [sb(f"ss{i}", [128, 1]) for i in range(2)]
        rs = [sb(f"rs{i}", [128, 1]) for i in range(2)]
        hT32 = [sb(f"hT32_{i}", [128, 8, 512]) for i in range(2)]
        hT = [sb(f"hT_{i}", [128, 8, 512], BF16) for i in range(2)]
        PF = 2

        def load(i):
            if i >= self.NT:
                return
            if pre_load is not None:
                pre_load(i)
            elif pre is None:
                P.dma('sync', xt[i % NXB], src[i * 128:(i + 1) * 128, :], writes=[f'xt{i % NXB}'])
        for i in range(PF):
            load(i)
        for q in range(self.NQ):
            b = q % 2
            for j in range(4):
                i = q * 4 + j
                a = i % 2
                X, H = xt[i % NXB], ht[a]
                rx, rh = f'xt{i % NXB}', f'ht{a}'
                load(i + PF)
                if pre is not None:
                    pre(i, X, rx)
                P.op('scalar', 'activation', reads=[rx], writes=['sq', f'ss{a}'], out=sq, in_=X, func=AF.Square,
                     accum_out=ss[a])
                self.rstd_ops(ss[a], rs[a], D, [f'ss{a}'], [f'rs{a}'])
                P.op('vector', 'scalar_tensor_tensor', reads=[rx, f'rs{a}', 'gsc'], writes=[rh], out=H, in0=X,
                     scalar=rs[a], in1=gsc, op0=ALU.mult, op1=ALU.mult)
                P.op('gpsimd', 'tensor_tensor', reads=[rh, 'modb'], writes=[rh], out=H, in0=H, in1=shift, op=ALU.add)
                if after_h is not None:
                    after_h(i, H, rh, X, rx)
                for half in range(2):
                    pt = self.ps[half]
                    for k4 in range(4):
                        kc = half * 4 + k4
                        P.op('tensor', 'transpose', reads=[rh, 'ident'], writes=[f'ps{half}'],
                             out=pt[:, k4 * 128:(k4 + 1) * 128], in_=H[:, kc * 128:(kc + 1) * 128], identity=self.ident)
                    dst = hT32[b][:, half * 4:(half + 1) * 4, j * 128:(j + 1) * 128]
                    P.op('scalar', 'activation', reads=[f'ps{half}'], writes=[f'hT32_{b}'], out=dst,
                         in_=pt.rearrange("p (k t) -> p k t", k=4), func=AF.Copy)
                P.op('gpsimd', 'tensor_copy', reads=[f'hT32_{b}'], writes=[f'hT_{b}'],
                     out=hT[b][:, :, j * 128:(j + 1) * 128], in_=hT32[b][:, :, j * 128:(j + 1) * 128])
            consumer(q, (hT[b], f'hT_{b}'), (hT32[b], f'hT32_{b}'))

    def evac(self, k, out, in_, reads, writes, scale=None):
        P = self.P
        if k % 2 == 0:
            if scale is None:
                P.op('scalar', 'activation', reads=reads, writes=writes, out=out, in_=in_, func=AF.Copy)
            else:
                P.op('scalar', 'activation', reads=reads, writes=writes, out=out, in_=in_, func=AF.Copy, scale=scale)
        else:
            if scale is None:
                P.op('vector', 'tensor_copy', reads=reads, writes=writes, out=out, in_=in_)
            else:
                P.op('vector', 'tensor_scalar', reads=reads, writes=writes, out=out, in0=in_, scalar1=scale,
                     scalar2=None, op0=ALU.mult)

    def inproj_chunk(self, q, hTb, hT32b, wz, wx, wqk, wv, wg, wdt, wf, st):
        P = self.P
        hT, rhT = hTb
        hT32, rhT32 = hT32b
        if self._ip_bufs is None:
            sb = lambda n, s, dt=F32: self.sb(st, n, s, dt)
            self._ip_bufs = dict(
                o32=[sb(f"o32_{i}", [128, 512]) for i in range(3)],
                o16=[sb(f"o16_{i}", [128, 512], BF16) for i in range(3)],
                osm=[sb(f"osm_{i}", [128, 8]) for i in range(2)],
                ofl=[sb(f"ofl_{i}", [4, 512]) for i in range(2)],
                n=[0],
            )
        B = self._ip_bufs
        tok = slice(q * 512, (q + 1) * 512)

        def nxt():
            B['n'][0] += 1
            return B['n'][0]
        PB = [2, 3, 4, 5]

        def fm(w, wres, c0, dst, dt16=False, scale=None):
            k = nxt()
            pb = PB[k % 4]
            pt = self.ps[pb]
            for kc in range(8):
                P.op('tensor', 'matmul', reads=[wres, rhT], writes=[f'ps{pb}'], out=pt, lhsT=w[:, kc, c0:c0 + 128],
                     rhs=hT[:, kc, :], start=(kc == 0), stop=(kc == 7))
            o = (B['o16'] if dt16 else B['o32'])[k % 3]
            ores = ('o16_' if dt16 else 'o32_') + str(k % 3)
            self.evac(k, o, pt, [f'ps{pb}'], [ores], scale=scale)
            P.dma('sync', dst, o, reads=[ores], writes=[])
        for ct in range(8):
            fm(wx, 'wx', ct * 128, self.xbcT[ct * 128:(ct + 1) * 128, tok])
        for ct in range(2):
            fm(wqk, 'wqk', ct * 128, self.qT[ct * 128:(ct + 1) * 128, tok], dt16=True, scale=0.125)
        for ct in range(2):
            fm(wqk, 'wqk', 256 + ct * 128, self.kT[ct * 128:(ct + 1) * 128, tok], dt16=True)
        for ct in range(2):
            fm(wg, 'wg', ct * 128, self.gaT[ct * 128:(ct + 1) * 128, tok])
        for ct in range(2):
            fm(wg, 'wg', 256 + ct * 128, self.gbT[ct * 128:(ct + 1) * 128, tok])
        k = nxt()
        pb = PB[k % 4]
        pt = self.ps[pb]
        for kc in range(8):
            P.op('tensor', 'matmul', reads=['wf', rhT32], writes=[f'ps{pb}'], out=pt[0:4, :], lhsT=wf[:, kc, :],
                 rhs=hT32[:, kc, :], start=(kc == 0), stop=(kc == 7))
        o = B['ofl'][q % 2]
        P.op('vector', 'tensor_copy', reads=[f'ps{pb}'], writes=[f'ofl_{q % 2}'], out=o, in_=pt[0:4, :])
        P.dma('sync', self.flT[:, tok], o, reads=[f'ofl_{q % 2}'])
        for j in range(4):
            tt = slice(q * 512 + j * 128, q * 512 + (j + 1) * 128)
            k = nxt()
            pb = PB[k % 4]
            pt = self.ps[pb]
            for kc in range(8):
                P.op('tensor', 'matmul', reads=['wz', rhT], writes=[f'ps{pb}'], out=pt,
                     lhsT=hT[:, kc, j * 128:(j + 1) * 128], rhs=wz[:, kc, :], start=(kc == 0), stop=(kc == 7))
            o = B['o32'][k % 3]
            P.op('scalar', 'activation', reads=[f'ps{pb}'], writes=[f'o32_{k % 3}'], out=o, in_=pt, func=AF.Silu)
            P.dma('sync', self.zs[tt, :], o, reads=[f'o32_{k % 3}'])
            k = nxt()
            pb = PB[k % 4]
            pt = self.ps[pb]
            for kc in range(8):
                P.op('tensor', 'matmul', reads=['wv', rhT], writes=[f'ps{pb}'], out=pt[:, 0:256],
                     lhsT=hT[:, kc, j * 128:(j + 1) * 128], rhs=wv[:, kc, :], start=(kc == 0), stop=(kc == 7))
            for kc in range(8):
                P.op('tensor', 'matmul', reads=['wdt', rhT32], writes=[f'ps{pb}'], out=pt[:, 256:264],
                     lhsT=hT32[:, kc, j * 128:(j + 1) * 128], rhs=wdt[:, kc, :], start=(kc == 0), stop=(kc == 7))
            o = B['o16'][k % 3]
            self.evac(k, o[:, 0:256], pt[:, 0:256], [f'ps{pb}'], [f'o16_{k % 3}'])
            P.dma('sync', self.vtok[tt, :], o[:, 0:256], reads=[f'o16_{k % 3}'])
            o2 = B['osm'][j % 2]
            P.op('vector', 'tensor_copy', reads=[f'ps{pb}'], writes=[f'osm_{j % 2}'], out=o2, in_=pt[:, 256:264])
            P.dma('sync', self.dtr[tt, :], o2, reads=[f'osm_{j % 2}'])

    def phase_conv(self, l):
        P, T = self.P, self.T
        TC = min(T, 2048)
        HALO = 30
        with contextlib.ExitStack() as st:
            sb = lambda n, s, dt=F32: self.sb(st, n, s, dt)
            cw = sb("cw", [128, 2, 31])
            cbias = sb("cbias", [128, 2])
            cg = sb("cg", [128, 2])
            cbeta = sb("cbeta", [128, 2])
            ua = [sb(f"ua{i}", [128, TC + HALO]) for i in range(2)]
            ub = [sb(f"ub{i}", [128, TC + HALO]) for i in range(2)]
            co = [sb(f"co{i}", [128, TC]) for i in range(2)]
            sqt = sb("csq", [128, 512])
            mean = sb("cmean", [128, 512])
            rstd = sb("crstd", [128, 512])
            tmp = [sb(f"ctmp{i}", [128, 512]) for i in range(2)]
            yo = [sb(f"cyo{i}", [128, 512], BF16) for i in range(2)]
            P.begin()
            P.dma('sync', cw, self.cmw[l], writes=['cw'])
            P.dma('sync', cbias, self.cmb[l], writes=['cbias'])
            P.dma('sync', cg, self.cmg[l], writes=['cg'])
            P.dma('sync', cbeta, self.cmbeta[l], writes=['cbeta'])
            n = 0
            for c0 in range(0, T, TC):
                for ct in range(2):
                    A, Bt = ua[ct], ub[ct]
                    ra, rb, rc = f'ua{ct}', f'ub{ct}', f'co{ct}'
                    rows = slice(ct * 128, (ct + 1) * 128)
                    if c0 == 0:
                        P.dma('sync', A[:, HALO:], self.gaT[rows, 0:TC], writes=[ra])
                        P.dma('sync', Bt[:, HALO:], self.gbT[rows, 0:TC], writes=[rb])
                        P.op('gpsimd', 'memset', writes=[ra], ap=A[:, 0:HALO], constant=0.0)
                        P.op('gpsimd', 'memset', writes=[rb], ap=Bt[:, 0:HALO], constant=0.0)
                    else:
                        P.dma('sync', A, self.gaT[rows, c0 - HALO:c0 + TC], writes=[ra])
                        P.dma('sync', Bt, self.gbT[rows, c0 - HALO:c0 + TC], writes=[rb])
                    P.op('scalar', 'activation', reads=[rb], writes=[rb], out=Bt, in_=Bt, func=AF.Sigmoid)
                    P.op('gpsimd', 'tensor_tensor', reads=[ra, rb], writes=[ra], out=A, in0=A, in1=Bt, op=ALU.mult)
                    C = co[ct]
                    P.op('vector', 'tensor_scalar', reads=[ra, 'cw', 'cbias'], writes=[rc], out=C, in0=A[:, 0:TC],
                         scalar1=cw[:, ct, 0:1], scalar2=cbias[:, ct:ct + 1], op0=ALU.mult, op1=ALU.add)
                    for k in range(1, 31):
                        P.op('vector', 'scalar_tensor_tensor', reads=[ra, 'cw', rc], writes=[rc], out=C,
                             in0=A[:, k:k + TC], scalar=cw[:, ct, k:k + 1], in1=C, op0=ALU.mult, op1=ALU.add)
                for s0 in range(0, TC, 512):
                    cs_ = slice(s0, s0 + 512)
                    p1, p2 = self.ps[0], self.ps[1]
                    for ct in range(2):
                        P.op('tensor', 'matmul', reads=['ones', f'co{ct}'], writes=['ps0'], out=p1, lhsT=self.ones,
                             rhs=co[ct][:, cs_], start=(ct == 0), stop=(ct == 1))
                    for ct in range(2):
                        P.op('scalar', 'activation', reads=[f'co{ct}'], writes=['csq'], out=sqt, in_=co[ct][:, cs_],
                             func=AF.Square)
                        P.op('tensor', 'matmul', reads=['ones', 'csq'], writes=['ps1'], out=p2, lhsT=self.ones,
                             rhs=sqt, start=(ct == 0), stop=(ct == 1))
                    P.op('vector', 'tensor_scalar', reads=['ps0'], writes=['cmean'], out=mean, in0=p1,
                         scalar1=1.0 / 256, scalar2=None, op0=ALU.mult)
                    P.op('vector', 'tensor_tensor', reads=['cmean'], writes=['crstd'], out=rstd, in0=mean, in1=mean,
                         op=ALU.mult)
                    P.op('vector', 'scalar_tensor_tensor', reads=['ps1', 'crstd'], writes=['crstd'], out=rstd, in0=p2,
                         scalar=1.0 / 256, in1=rstd, op0=ALU.mult, op1=ALU.subtract)
                    self.rstd_ops(rstd, rstd, 1.0, ['crstd'], ['crstd'])
                    for ct in range(2):
                        n += 1
                        t = tmp[n % 2]
                        rt = f'ctmp{n % 2}'
                        P.op('vector', 'tensor_tensor', reads=[f'co{ct}', 'cmean'], writes=[rt], out=t,
                             in0=co[ct][:, cs_], in1=mean, op=ALU.subtract)
                        P.op('gpsimd', 'tensor_tensor', reads=[rt, 'crstd'], writes=[rt], out=t, in0=t, in1=rstd,
                             op=ALU.mult)
                        y = yo[n % 2]
                        ry = f'cyo{n % 2}'
                        P.op('scalar', 'activation', reads=[rt, 'cg', 'cbeta'], writes=[ry], out=y, in_=t, func=AF.Silu,
                             scale=cg[:, ct:ct + 1], bias=cbeta[:, ct:ct + 1])
                        P.dma('sync', self.yT[768 + ct * 128:768 + (ct + 1) * 128, c0 + s0:c0 + s0 + 512], y,
                              reads=[ry])
            P.end()

    def phase_attn(self, l):
        P, T, NT, NQ = self.P, self.T, self.NT, self.NQ
        CW = min(T, 2048)
        with contextlib.ExitStack() as st:
            sb = lambda n, s, dt=F32: self.sb(st, n, s, dt)
            fb = sb("fb", [4, 1])
            xx = sb("fx", [4, CW])
            ax = sb("fax", [4, CW])
            mn = sb("fmn", [4, CW])
            cum = [sb(f"fcum{i}", [4, CW]) for i in range(2)]
            r1 = sb("fr1", [4, CW])
            sp = sb("fsp", [4, 6, CW], BF16)
            P.begin()
            P.dma('sync', fb, self.f_bias[l], writes=['fb'])
            for ci, c0 in enumerate(range(0, T, CW)):
                cc = cum[ci % 2]
                rcum = f'fcum{ci % 2}'
                P.dma('sync', xx, self.flT[:, c0:c0 + CW], writes=['fx'])
                P.op('scalar', 'activation', reads=['fx', 'fb'], writes=['fx'], out=xx, in_=xx, func=AF.Identity,
                     bias=fb[:, 0:1], scale=1.0)
                P.op('vector', 'tensor_scalar', reads=['fx'], writes=['fmn'], out=mn, in0=xx, scalar1=-1.0, scalar2=0.0,
                     op0=ALU.mult, op1=ALU.max)
                P.op('vector', 'scalar_tensor_tensor', reads=['fmn', 'fx'], writes=['fax'], out=ax, in0=mn, scalar=-2.0,
                     in1=xx, op0=ALU.mult, op1=ALU.subtract)
                P.op('scalar', 'activation', reads=['fax'], writes=['fax'], out=ax, in_=ax, func=AF.Exp)
                P.op('scalar', 'activation', reads=['fax'], writes=['fax'], out=ax, in_=ax, func=AF.Ln, bias=1.0,
                     scale=1.0)
                P.op('vector', 'scalar_tensor_tensor', reads=['fmn', 'fax'], writes=['fmn'], out=mn, in0=mn, scalar=-1.0,
                     in1=ax, op0=ALU.mult, op1=ALU.subtract)
                init = 0.0 if ci == 0 else cum[(ci - 1) % 2][:, CW - 1:CW]
                P.op('vector', 'tensor_tensor_scan', reads=['fmn', 'ones', f'fcum{(ci - 1) % 2}'], writes=[rcum], out=cc,
                     data0=self.ones[0:4, 0:1].broadcast_to([4, CW]), data1=mn, initial=init, op0=ALU.mult, op1=ALU.add)
                P.op('vector', 'tensor_copy', reads=[rcum], writes=['fsp'], out=sp[:, 0, :], in_=cc)
                P.op('vector', 'tensor_tensor', reads=[rcum, 'fsp'], writes=['fr1'], out=r1, in0=cc, in1=sp[:, 0, :],
                     op=ALU.subtract)
                P.op('vector', 'tensor_copy', reads=['fr1'], writes=['fsp'], out=sp[:, 1, :], in_=r1)
                P.op('vector', 'tensor_tensor', reads=['fr1', 'fsp'], writes=['fr1'], out=r1, in0=r1, in1=sp[:, 1, :],
                     op=ALU.subtract)
                P.op('vector', 'tensor_copy', reads=['fr1'], writes=['fsp'], out=sp[:, 2, :], in_=r1)
                P.op('vector', 'tensor_scalar', reads=['fsp'], writes=['fsp'], out=sp[:, 3:6, :], in0=sp[:, 0:3, :],
                     scalar1=-1.0, scalar2=None, op0=ALU.mult)
                P.dma('sync', self.cs[:, :, c0:c0 + CW], sp, reads=['fsp'])
            P.end()
        with contextlib.ExitStack() as st:
            sb = lambda n, s, dt=F32: self.sb(st, n, s, dt)
            qp = [sb(f"qp{i}", [70, T], BF16) for i in range(2)]
            kp = [sb(f"kp{i}", [70, T], BF16) for i in range(2)]
            vp = [sb(f"vp{i}", [128, NT, 65], BF16) for i in range(2)]
            nm = sb("negmask", [128, 4, 512], BF16)
            pt_ = [sb(f"pT{i}", [128, 512], BF16) for i in range(3)]
            rec = sb("rec", [65, 512])
            bcs = sb("bcs", [64, 512])
            on = [sb(f"on{i}", [64, 512]) for i in range(2)]
            P.begin()
            P.op('gpsimd', 'memset', writes=['negmask'], ap=nm, constant=0.0)
            for d in range(4):
                P.op('gpsimd', 'affine_select', reads=['negmask'], writes=['negmask'], out=nm[:, d, :], in_=nm[:, d, :],
                     pattern=[[1, 512]], compare_op=ALU.is_ge, fill=-30000.0, base=-128 * d, channel_multiplier=-1)
            step = 0
            for h in range(4):
                hb = h % 2
                Q, Kp, V = qp[hb], kp[hb], vp[hb]
                rq, rk, rv = f'qp{hb}', f'kp{hb}', f'vp{hb}'
                hr = slice(h * 64, (h + 1) * 64)
                P.op('gpsimd', 'memset', writes=[rq], ap=Q[64:70, :], constant=1.0)
                P.op('gpsimd', 'memset', writes=[rk], ap=Kp[64:70, :], constant=1.0)
                P.op('gpsimd', 'memset', writes=[rv], ap=V[:, :, 64:65], constant=1.0)
                P.dma('sync', Q[0:64, :], self.qT[hr, :], writes=[rq])
                P.dma('sync', Kp[0:64, :], self.kT[hr, :], writes=[rk])
                P.dma('sync', Q[67:70, :], self.cs[h, 0:3, :], writes=[rq])
                P.dma('sync', Kp[64:67, :], self.cs[h, 3:6, :], writes=[rk])
                for i0 in range(0, NT, 4):
                    P.dma('sync', V[:, i0:i0 + 4, 0:64],
                          self.vtok[i0 * 128:(i0 + 4) * 128, hr].rearrange("(i p) d -> p i d", p=128), writes=[rv])
                for qc in range(NQ):
                    nk = 4 * qc + 4
                    ob = 3 + qc % 2
                    O = self.ps[ob]
                    qs = slice(qc * 512, (qc + 1) * 512)
                    for s_ in range(nk + 2):
                        if s_ < nk:
                            kt = s_
                            sbk = (step + s_) % 3
                            S = self.ps[sbk]
                            diag = kt >= 4 * qc
                            P.op('tensor', 'matmul', reads=[rq, rk], writes=[f'ps{sbk}'], out=S,
                                 lhsT=Kp[:, kt * 128:(kt + 1) * 128], rhs=Q[:, qs], start=True, stop=not diag)
                            if diag:
                                P.op('tensor', 'matmul', reads=['identb', 'negmask'], writes=[f'ps{sbk}'], out=S,
                                     lhsT=self.identb, rhs=nm[:, kt - 4 * qc, :], start=False, stop=True)
                        if 1 <= s_ <= nk:
                            kt = s_ - 1
                            sbk = (step + kt) % 3
                            P.op('scalar', 'activation', reads=[f'ps{sbk}'], writes=[f'pT{sbk}'], out=pt_[sbk],
                                 in_=self.ps[sbk], func=AF.Exp)
                        if s_ >= 2:
                            kt = s_ - 2
                            sbk = (step + kt) % 3
                            P.op('tensor', 'matmul', reads=[f'pT{sbk}', rv], writes=[f'ps{ob}'], out=O[0:65, :],
                                 lhsT=V[:, kt, :], rhs=pt_[sbk], start=(kt == 0), stop=(kt == nk - 1))
                    step += nk
                    P.op('vector', 'reciprocal', reads=[f'ps{ob}'], writes=['rec'], out=rec[64:65, :], in_=O[64:65, :])
                    P.op('tensor', 'matmul', reads=['ones', 'rec'], writes=['ps5'], out=self.ps[5][0:64, :],
                         lhsT=self.ones[64:65, 0:64], rhs=rec[64:65, :], start=True, stop=True)
                    P.op('scalar', 'activation', reads=['ps5'], writes=['bcs'], out=bcs, in_=self.ps[5][0:64, :],
                         func=AF.Copy)
                    o_ = on[qc % 2]
                    P.op('vector', 'tensor_tensor', reads=[f'ps{ob}', 'bcs'], writes=[f'on{qc % 2}'], out=o_,
                         in0=O[0:64, :], in1=bcs, op=ALU.mult)
                    P.dma('sync', self.attT[hr, qs], o_, reads=[f'on{qc % 2}'])
                if h < 3:
                    P.end()
                    P.begin()
            P.end()
        with contextlib.ExitStack() as st:
            sb = lambda n, s, dt=F32: self.sb(st, n, s, dt)
            fg = sb("foxg", [128, 2])
            at = [[sb(f"at{i}{c}", [128, 512]) for c in range(2)] for i in range(2)]
            sq = sb("asq", [128, 512])
            rs = sb("ars", [128, 512])
            yo = [sb(f"ayo{i}", [128, 512], BF16) for i in range(2)]
            P.begin()
            P.dma('sync', fg, self.fox_g[l], writes=['foxg'])
            n = 0
            for qc in range(NQ):
                qs = slice(qc * 512, (qc + 1) * 512)
                b = qc % 2
                for ct in range(2):
                    P.dma('sync', at[b][ct], self.attT[ct * 128:(ct + 1) * 128, qs], writes=[f'at{b}{ct}'])
                    P.op('scalar', 'activation', reads=[f'at{b}{ct}'], writes=['asq'], out=sq, in_=at[b][ct],
                         func=AF.Square)
                    P.op('tensor', 'matmul', reads=['ones', 'asq'], writes=['ps0'], out=self.ps[0], lhsT=self.ones,
                         rhs=sq, start=(ct == 0), stop=(ct == 1))
                self.rstd_ops(self.ps[0], rs, 256.0, ['ps0'], ['ars'])
                for ct in range(2):
                    n += 1
                    P.op('vector', 'tensor_tensor', reads=[f'at{b}{ct}', 'ars'], writes=[f'at{b}{ct}'], out=at[b][ct],
                         in0=at[b][ct], in1=rs, op=ALU.mult)
                    y = yo[n % 2]
                    P.op('scalar', 'activation', reads=[f'at{b}{ct}', 'foxg'], writes=[f'ayo{n % 2}'], out=y,
                         in_=at[b][ct], func=AF.Copy, scale=fg[:, ct:ct + 1])
                    P.dma('sync', self.yT[512 + ct * 128:512 + (ct + 1) * 128, qs], y, reads=[f'ayo{n % 2}'])
            P.end()

    def phase_ssd(self, l):
        P, T, NT = self.P, self.T, self.NT
        SC = 512
        assert NT * 8 <= 512
        with contextlib.ExitStack() as st:
            sb = lambda n, s, dt=F32: self.sb(st, n, s, dt)
            cw4 = sb("cw4", [128, 8, 4])
            cb4 = sb("cb4", [128, 8])
            dtb = sb("dtb", [128, 8])
            aneg = sb("aneg", [128, 8])
            dsk = sb("dsk", [128, 8])
            ng = sb("ssdng", [128, 512])
            dt = sb("dt_all", [128, NT, 8])
            dmn = sb("dt_mn", [128, NT, 8])
            dtA = sb("dtA", [128, NT, 8])
            El = sb("El", [128, NT, 8])
            Wl = sb("Wl", [128, NT, 8])
            cd = sb("cd", [128, NT, 8])
            xin = [sb(f"xin{i}", [128, SC + 3]) for i in range(2)]
            cacc = [sb(f"cacc{i}", [128, SC]) for i in range(2)]
            xsT = [sb(f"xsT{i}", [128, SC]) for i in range(4)]
            BT = [sb(f"BT{i}", [128, SC], BF16) for i in range(2)]
            CT = [sb(f"CT{i}", [128, SC], BF16) for i in range(2)]
            x32_2 = [sb(f"x32{i}", [128, 8, 64], F32) for i in range(2)]
            Btok_2 = [sb(f"Btok{i}", [128, 256], BF16) for i in range(2)]
            R_2 = [sb(f"Rall{i}", [128, 8, 128], F32) for i in range(2)]
            E_2 = [sb(f"Eall{i}", [128, 8, 128], F32) for i in range(2)]
            CBm_2 = [sb(f"CBm{i}", [128, 2, 128], F32) for i in range(2)]
            M_2 = [sb(f"Mall{i}", [128, 8, 128], BF16) for i in range(2)]
            xdt_2 = [sb(f"xdt{i}", [128, 8, 64], BF16) for i in range(2)]
            xw_2 = [sb(f"xw{i}", [128, 8, 64], BF16) for i in range(2)]
            H = sb("Hst", [128, 8, 64])
            Hb = sb("Hb", [128, 8, 64], BF16)
            t1_2 = [sb(f"sst1{i}", [128, 8, 64], F32) for i in range(2)]
            t2_2 = [sb(f"sst2{i}", [128, 8, 64], F32) for i in range(2)]
            zt_2 = [sb(f"zt{i}", [128, 512], F32) for i in range(2)]
            ssq_2 = [sb(f"ssq{i}", [128, 512], F32) for i in range(2)]
            gss_2 = [sb(f"gss{i}", [128, 2], F32) for i in range(2)]
            grs_2 = [sb(f"grs{i}", [128, 2], F32) for i in range(2)]
            yTs_2 = [sb(f"yTs{i}", [128, 4, 128], BF16) for i in range(2)]
            ps = self.ps
            psb1 = ps[1].bitcast(BF16)
            P.begin()
            P.dma('sync', cw4, self.convw[l], writes=['cw4'])
            P.dma('sync', cb4, self.convb[l], writes=['cb4'])
            P.dma('sync', dtb, self.dt_bias[l].partition_broadcast(128), writes=['dtb'])
            P.dma('sync', aneg, self.a_log[l].partition_broadcast(128), writes=['aneg'])
            P.dma('sync', dsk, self.ssd_d[l].partition_broadcast(128), writes=['dsk'])
            P.dma('sync', ng, self.ssd_norm_g[l].partition_broadcast(128), writes=['ssdng'])
            for i0 in range(0, NT, 4):
                n_ = min(4, NT - i0)
                P.dma('sync', dt[:, i0:i0 + n_, :],
                      self.dtr[i0 * 128:(i0 + n_) * 128, :].rearrange("(i p) h -> p i h", p=128), writes=['dt_all'])
            P.op('scalar', 'activation', reads=['aneg'], writes=['aneg'], out=aneg, in_=aneg, func=AF.Exp)
            P.op('vector', 'tensor_scalar', reads=['aneg'], writes=['aneg'], out=aneg, in0=aneg, scalar1=-1.0,
                 scalar2=None, op0=ALU.mult)
            bc3 = lambda t: t.unsqueeze(1).broadcast_to([128, NT, 8])
            P.op('vector', 'tensor_tensor', reads=['dt_all', 'dtb'], writes=['dt_all'], out=dt, in0=dt, in1=bc3(dtb),
                 op=ALU.add)
            P.op('vector', 'tensor_scalar', reads=['dt_all'], writes=['dt_mn'], out=dmn, in0=dt, scalar1=0.0,
                 scalar2=None, op0=ALU.max)
            P.op('vector', 'scalar_tensor_tensor', reads=['dt_mn', 'dt_all'], writes=['dt_all'],
                 out=dt.rearrange("p i h -> p (i h)"), in0=dmn.rearrange("p i h -> p (i h)"), scalar=-2.0,
                 in1=dt.rearrange("p i h -> p (i h)"), op0=ALU.mult, op1=ALU.add)
            P.op('scalar', 'activation', reads=['dt_all'], writes=['dt_all'], out=dt, in_=dt, func=AF.Exp)
            P.op('scalar', 'activation', reads=['dt_all'], writes=['dt_all'], out=dt, in_=dt, func=AF.Ln, bias=1.0,
                 scale=1.0)
            P.op('vector', 'tensor_tensor', reads=['dt_all', 'dt_mn'], writes=['dt_all'], out=dt, in0=dt, in1=dmn,
                 op=ALU.add)
            P.op('vector', 'tensor_tensor', reads=['dt_all', 'aneg'], writes=['dtA'], out=dtA, in0=dt, in1=bc3(aneg),
                 op=ALU.mult)
            dtA2 = dtA.rearrange("p i h -> p (i h)")
            P.op('tensor', 'matmul', reads=['tri', 'dtA'], writes=['ps2'], out=ps[2][:, 0:NT * 8], lhsT=self.tri,
                 rhs=dtA2, start=True, stop=True)
            P.op('tensor', 'matmul', reads=['ones', 'dtA'], writes=['ps3'], out=ps[3][:, 0:NT * 8], lhsT=self.ones,
                 rhs=dtA2, start=True, stop=True)
            f2 = lambda t: t.rearrange("p i h -> p (i h)")
            P.op('scalar', 'activation', reads=['ps2'], writes=['El'], out=f2(El), in_=ps[2][:, 0:NT * 8], func=AF.Exp)
            P.op('scalar', 'activation', reads=['ps3'], writes=['cd'], out=f2(cd), in_=ps[3][:, 0:NT * 8], func=AF.Exp)
            P.op('vector', 'tensor_copy', reads=['ps3'], writes=['Wl'], out=f2(Wl), in_=ps[3][:, 0:NT * 8])
            P.op('vector', 'tensor_tensor', reads=['Wl', 'ps2'], writes=['Wl'], out=f2(Wl), in0=f2(Wl),
                 in1=ps[2][:, 0:NT * 8], op=ALU.subtract)
            P.op('scalar', 'activation', reads=['Wl'], writes=['Wl'], out=Wl, in_=Wl, func=AF.Exp)
            P.op('vector', 'tensor_tensor', reads=['Wl', 'dt_all'], writes=['Wl'], out=Wl, in0=Wl, in1=dt, op=ALU.mult)
            P.op('gpsimd', 'memset', writes=['Hst'], ap=H, constant=0.0)
            P.op('gpsimd', 'memset', writes=['Hb'], ap=Hb, constant=0.0)
            for c0 in range(0, T, SC):
                for ct in range(8):
                    X = xin[ct % 2]
                    rx = f'xin{ct % 2}'
                    A = cacc[ct % 2]
                    ra = f'cacc{ct % 2}'
                    rows = slice(ct * 128, (ct + 1) * 128)
                    if c0 == 0:
                        P.op('gpsimd', 'memset', writes=[rx], ap=X[:, 0:3], constant=0.0)
                        P.dma('sync', X[:, 3:], self.xbcT[rows, 0:SC], writes=[rx])
                    else:
                        P.dma('sync', X, self.xbcT[rows, c0 - 3:c0 + SC], writes=[rx])
                    P.op('vector', 'tensor_scalar', reads=[rx, 'cw4', 'cb4'], writes=[ra], out=A, in0=X[:, 0:SC],
                         scalar1=cw4[:, ct, 0:1], scalar2=cb4[:, ct:ct + 1], op0=ALU.mult, op1=ALU.add)
                    for k in range(1, 4):
                        P.op('vector', 'scalar_tensor_tensor', reads=[rx, 'cw4', ra], writes=[ra], out=A,
                             in0=X[:, k:k + SC], scalar=cw4[:, ct, k:k + 1], in1=A, op0=ALU.mult, op1=ALU.add)
                    if ct < 4:
                        dst, rd = xsT[ct], f'xsT{ct}'
                    elif ct < 6:
                        dst, rd = BT[ct - 4], f'BT{ct - 4}'
                    else:
                        dst, rd = CT[ct - 6], f'CT{ct - 6}'
                    P.op('scalar', 'activation', reads=[ra], writes=[rd], out=dst, in_=A, func=AF.Silu)
                for cc in range(SC // 128):
                    c = c0 // 128 + cc
                    cs_ = slice(cc * 128, (cc + 1) * 128)
                    tok = slice(c * 128, (c + 1) * 128)
                    pc = c % 2
                    x32 = x32_2[pc]
                    Btok = Btok_2[pc]
                    R = R_2[pc]
                    E = E_2[pc]
                    CBm = CBm_2[pc]
                    M = M_2[pc]
                    xdt = xdt_2[pc]
                    xw = xw_2[pc]
                    t1 = t1_2[pc]
                    t2 = t2_2[pc]
                    zt = zt_2[pc]
                    ssq = ssq_2[pc]
                    gss = gss_2[pc]
                    grs = grs_2[pc]
                    yTs = yTs_2[pc]
                    n = {k: k + str(pc) for k in ('x32', 'Btok', 'Rall', 'Eall', 'CBm', 'Mall', 'xdt', 'xw', 'sst1', 'sst2', 'zt', 'ssq', 'gss', 'grs', 'yTs')}
                    for ct in range(4):
                        P.op('tensor', 'transpose', reads=[f'xsT{ct}', 'ident'], writes=['ps0'],
                             out=ps[0][:, ct * 128:(ct + 1) * 128], in_=xsT[ct][:, cs_], identity=self.ident)
                    P.op('scalar', 'activation', reads=['ps0'], writes=[n['x32']], out=x32.rearrange("p h d -> p (h d)"),
                         in_=ps[0], func=AF.Copy)
                    for g in range(2):
                        P.op('tensor', 'transpose', reads=[f'BT{g}', 'identb'], writes=['ps1'],
                             out=psb1[:, g * 128:(g + 1) * 128], in_=BT[g][:, cs_], identity=self.identb)
                    P.op('vector', 'tensor_copy', reads=['ps1'], writes=[n['Btok']], out=Btok, in_=psb1[:, 0:256])
                    P.dma('sync', zt, self.zs[tok, :], writes=[n['zt']])
                    P.op('vector', 'tensor_tensor', reads=['tri', 'dtA'], writes=[n['Rall']], out=R,
                         in0=self.tri.unsqueeze(1).broadcast_to([128, 8, 128]),
                         in1=dtA[:, c, :].unsqueeze(2).broadcast_to([128, 8, 128]), op=ALU.mult)
                    for hh in range(2):
                        P.op('tensor', 'matmul', reads=['ustr', n['Rall']], writes=[f'ps{2 + hh}'], out=ps[2 + hh],
                             lhsT=self.ustr, rhs=R[:, hh * 4:(hh + 1) * 4, :].rearrange("p h l -> p (h l)"),
                             start=True, stop=True)
                        P.op('scalar', 'activation', reads=[f'ps{2 + hh}'], writes=[n['Eall']],
                             out=E[:, hh * 4:(hh + 1) * 4, :].rearrange("p h l -> p (h l)"), in_=ps[2 + hh], func=AF.Exp)
                    for g in range(2):
                        P.op('tensor', 'matmul', reads=[f'BT{g}', f'CT{g}'], writes=['ps4'],
                             out=ps[4][:, g * 128:(g + 1) * 128], lhsT=BT[g][:, cs_], rhs=CT[g][:, cs_],
                             start=True, stop=True)
                    P.op('vector', 'tensor_tensor', reads=['ps4', 'tri'], writes=[n['CBm']], out=CBm,
                         in0=ps[4][:, 0:256].rearrange("p (g l) -> p g l", g=2),
                         in1=self.tri.unsqueeze(1).broadcast_to([128, 2, 128]), op=ALU.mult)
                    for g in range(2):
                        P.op('vector', 'tensor_tensor', reads=[n['Eall'], n['CBm']], writes=[n['Mall']],
                             out=M[:, g * 4:(g + 1) * 4, :], in0=E[:, g * 4:(g + 1) * 4, :],
                             in1=CBm[:, g:g + 1, :].broadcast_to([128, 4, 128]), op=ALU.mult)
                    P.op('gpsimd', 'tensor_tensor', reads=[n['x32'], 'dt_all'], writes=[n['xdt']], out=xdt, in0=x32,
                         in1=dt[:, c, :].unsqueeze(2).broadcast_to([128, 8, 64]), op=ALU.mult)
                    P.op('gpsimd', 'tensor_tensor', reads=[n['x32'], 'Wl'], writes=[n['xw']], out=xw, in0=x32,
                         in1=Wl[:, c, :].unsqueeze(2).broadcast_to([128, 8, 64]), op=ALU.mult)
                    for h in range(8):
                        P.op('tensor', 'matmul', reads=[n['Mall'], n['xdt']], writes=['ps5'], out=ps[5][:, h * 64:(h + 1) * 64],
                             lhsT=M[:, h, :], rhs=xdt[:, h, :], start=True, stop=True)
                    for g in range(2):
                        P.op('tensor', 'matmul', reads=[f'CT{g}', 'Hb'], writes=['ps6'],
                             out=ps[6][:, g * 256:(g + 1) * 256], lhsT=CT[g][:, cs_],
                             rhs=Hb[:, g * 4:(g + 1) * 4, :].rearrange("p h d -> p (h d)"), start=True, stop=True)
                    for g in range(2):
                        P.op('tensor', 'matmul', reads=[n['Btok'], n['xw']], writes=['ps7'],
                             out=ps[7][:, g * 256:(g + 1) * 256], lhsT=Btok[:, g * 128:(g + 1) * 128],
                             rhs=xw[:, g * 4:(g + 1) * 4, :].rearrange("p h d -> p (h d)"), start=True, stop=True)
                    v3 = lambda t: t.rearrange("p (h d) -> p h d", h=8)
                    b3 = lambda t: t.unsqueeze(2).broadcast_to([128, 8, 64])
                    P.op('vector', 'tensor_tensor', reads=['ps6', 'El'], writes=[n['sst1']], out=t1, in0=v3(ps[6]),
                         in1=b3(El[:, c, :]), op=ALU.mult)
                    P.op('vector', 'tensor_tensor', reads=[n['sst1'], 'ps5'], writes=[n['sst1']], out=t1, in0=t1, in1=v3(ps[5]),
                         op=ALU.add)
                    P.op('gpsimd', 'tensor_tensor', reads=[n['x32'], 'dsk'], writes=[n['sst2']], out=t2, in0=x32, in1=b3(dsk),
                         op=ALU.mult)
                    P.op('gpsimd', 'tensor_tensor', reads=[n['sst1'], n['sst2']], writes=[n['sst1']], out=t1, in0=t1, in1=t2,
                         op=ALU.add)
                    P.op('vector', 'tensor_tensor', reads=['Hst', 'cd'], writes=['Hst'], out=H, in0=H, in1=b3(cd[:, c, :]),
                         op=ALU.mult)
                    P.op('vector', 'tensor_tensor', reads=['Hst', 'ps7'], writes=['Hst'], out=H, in0=H, in1=v3(ps[7]),
                         op=ALU.add)
                    P.op('gpsimd', 'tensor_copy', reads=['Hst'], writes=['Hb'], out=Hb, in_=H)
                    y2 = t1.rearrange("p h d -> p (h d)")
                    P.op('gpsimd', 'tensor_tensor', reads=[n['sst1'], n['zt']], writes=[n['sst1']], out=y2, in0=y2, in1=zt,
                         op=ALU.mult)
                    for g in range(2):
                        P.op('scalar', 'activation', reads=[n['sst1']], writes=[n['ssq'], n['gss']],
                             out=ssq[:, g * 256:(g + 1) * 256], in_=y2[:, g * 256:(g + 1) * 256], func=AF.Square,
                             accum_out=gss[:, g:g + 1])
                    self.rstd_ops(gss, grs, 256.0, [n['gss']], [n['grs']])
                    for g in range(2):
                        P.op('vector', 'scalar_tensor_tensor', reads=[n['sst1'], n['grs'], 'ssdng'], writes=[n['sst2']],
                             out=t2.rearrange("p h d -> p (h d)")[:, g * 256:(g + 1) * 256],
                             in0=y2[:, g * 256:(g + 1) * 256], scalar=grs[:, g:g + 1],
                             in1=ng[:, g * 256:(g + 1) * 256], op0=ALU.mult, op1=ALU.mult)
                    yn = t2.rearrange("p h d -> p (h d)")
                    for ct in range(4):
                        P.op('tensor', 'transpose', reads=[n['sst2'], 'ident'], writes=['ps0'],
                             out=ps[0][:, ct * 128:(ct + 1) * 128], in_=yn[:, ct * 128:(ct + 1) * 128],
                             identity=self.ident)
                    P.op('scalar', 'activation', reads=['ps0'], writes=[n['yTs']], out=yTs.rearrange("p c t -> p (c t)"),
                         in_=ps[0], func=AF.Copy)
                    P.dma('sync', self.yT[0:512, tok].rearrange("(ct p) t -> p ct t", p=128), yTs, reads=[n['yTs']])
            P.end()

    def phase_wout_router(self, l, st0):
        P, T, NT = self.P, self.T, self.NT
        src = self.x_in if l == 0 else self.xres
        ps = self.ps
        self.ohb = self.sb(st0, "ohb", [128, NT, 64], BF16)
        self.rw = self.sb(st0, "rw", [128, NT, 2])
        with contextlib.ExitStack() as st:
            sb = lambda n, s, dt=F32: self.sb(st, n, s, dt)
            wo = sb("wo", [128, 8, D], BF16)
            gsc = sb("gscf", [128, D])
            wr = sb("wr", [128, 8, 36])
            rb = sb("rbias", [128, 36])
            yTc = [sb(f"yTc{i}", [128, 8, 512], BF16) for i in range(2)]
            xl = [sb(f"xl{i}", [128, D]) for i in range(4)]
            tt = sb("wtmp", [128, D])
            hb = [sb(f"hperm{i}", [128, D], BF16) for i in range(2)]
            lg = sb("lg", [128, 36])
            sm = sb("rsm", [128, 64])
            gexp = sb("gexp", [128, 4])
            ohg = sb("ohg", [128, 4])
            em = sb("em", [128, 4, 8])
            es = sb("esel", [128, 8])
            t8 = sb("top8", [128, 8])
            s1 = sb("sel1", [128, 8])
            s2 = sb("sel2", [128, 8])
            W = self.w_out[l].rearrange("(kc p) n -> p kc n", p=128)
            P.begin()
            for kc in range(8):
                self.load_cast(st, wo[:, kc, :], W[:, kc, :], 'wo', D)
            P.dma('sync', wr[:, :, 0:4], self.w_rg[l].rearrange("(kc p) n -> p kc n", p=128), writes=['wr'])
            P.dma('sync', wr[:, :, 4:36], self.w_re[l].rearrange("(kc p) n -> p kc n", p=128), writes=['wr'])
            P.dma('sync', rb[:, 0:4], self.b_rg[l].partition_broadcast(128), writes=['rbias'])
            P.dma('sync', rb[:, 4:36], self.b_re[l].partition_broadcast(128), writes=['rbias'])
            P.dma('sync', gsc, self.norm_ffn_g[l].partition_broadcast(128), writes=['gscf'])
            P.op('vector', 'scalar_tensor_tensor', reads=['gscf', 'modb'], writes=['gscf'], out=gsc, in0=self.mod(4),
                 scalar=1.0, in1=gsc, op0=ALU.add, op1=ALU.mult)

            def pre_load(i):
                q, j = divmod(i, 4)
                if j == 0:
                    P.dma('sync', yTc[q % 2], self.yT[:, q * 512:(q + 1) * 512].rearrange("(kc p) t -> p kc t", p=128),
                          writes=[f'yTc{q % 2}'])
                P.dma('sync', xl[i % 4], src[i * 128:(i + 1) * 128, :], writes=[f'xl{i % 4}'])

            def pre(i, X, rx):
                q, j = divmod(i, 4)
                Y = yTc[q % 2]
                ry = f'yTc{q % 2}'
                XL = xl[i % 4]
                rl = f'xl{i % 4}'
                for half in range(2):
                    pb = 2 + half
                    for kc in range(8):
                        P.op('tensor', 'matmul', reads=[ry, 'wo'], writes=[f'ps{pb}'], out=ps[pb],
                             lhsT=Y[:, kc, j * 128:(j + 1) * 128], rhs=wo[:, kc, half * 512:(half + 1) * 512],
                             start=(kc == 0), stop=(kc == 7))
                    hs = slice(half * 512, (half + 1) * 512)
                    P.op('vector', 'tensor_tensor', reads=[f'ps{pb}', 'modb'], writes=['wtmp'], out=tt[:, hs], in0=ps[pb],
                         in1=self.mod(2)[:, hs], op=ALU.mult)
                P.op('gpsimd', 'tensor_tensor', reads=['wtmp', rl], writes=[rx], out=X, in0=tt, in1=XL, op=ALU.add)
                P.dma('sync', self.xres[i * 128:(i + 1) * 128, :], X, reads=[rx])

            def after_h(i, Hh, rh, X, rx):
                Hp = hb[i % 2]
                rp = f'hperm{i % 2}'
                P.op('gpsimd', 'tensor_copy', reads=[rh], writes=[rp], out=Hp.rearrange("t (kc p) -> t kc p", kc=8),
                     in_=Hh.rearrange("t (p kc) -> t kc p", kc=8))
                P.dma('sync', self.hfp[i * 128:(i + 1) * 128, :], Hp, reads=[rp])

            def consumer(q, hTb, hT32b):
                hT32, r32 = hT32b
                for j in range(4):
                    i = q * 4 + j
                    for kc in range(8):
                        P.op('tensor', 'matmul', reads=[r32, 'wr'], writes=['ps4'], out=ps[4][:, 0:36],
                             lhsT=hT32[:, kc, j * 128:(j + 1) * 128], rhs=wr[:, kc, :], start=(kc == 0), stop=(kc == 7))
                    P.op('vector', 'tensor_tensor', reads=['ps4', 'rbias'], writes=['lg'], out=lg, in0=ps[4][:, 0:36],
                         in1=rb, op=ALU.add)
                    V = lambda name, **kw: P.op('vector', name, **kw)
                    gl = lg[:, 0:4]
                    el = lg[:, 4:36].rearrange("p (g e) -> p g e", g=4)
                    gmax, ngmax, gsum, pg = sm[:, 0:1], sm[:, 1:2], sm[:, 2:3], sm[:, 3:4]
                    ne1, r_, den, w1 = sm[:, 4:5], sm[:, 5:6], sm[:, 6:7], sm[:, 7:8]
                    V('reduce_max', reads=['lg'], writes=['rsm'], out=gmax, in_=gl, axis=AX.X)
                    V('tensor_scalar', reads=['rsm'], writes=['rsm'], out=ngmax, in0=gmax, scalar1=-1.0, scalar2=None,
                      op0=ALU.mult)
                    V('tensor_scalar', reads=['lg', 'rsm'], writes=['ohg'], out=ohg, in0=gl, scalar1=gmax, scalar2=None,
                      op0=ALU.is_ge)
                    P.op('scalar', 'activation', reads=['lg', 'rsm'], writes=['gexp', 'rsm'], out=gexp, in_=gl,
                         func=AF.Exp, bias=ngmax, scale=1.0, accum_out=gsum)
                    V('reciprocal', reads=['rsm'], writes=['rsm'], out=pg, in_=gsum)
                    V('tensor_tensor', reads=['lg', 'ohg'], writes=['em'], out=em, in0=el,
                      in1=ohg.unsqueeze(2).broadcast_to([128, 4, 8]), op=ALU.mult)
                    V('tensor_reduce', reads=['em'], writes=['esel'], out=es, in_=em.rearrange("p g e -> p e g"),
                      axis=AX.X, op=ALU.add)
                    V('max', reads=['esel'], writes=['top8'], out=t8, in_=es)
                    V('tensor_scalar', reads=['esel', 'top8'], writes=['sel1'], out=s1, in0=es, scalar1=t8[:, 0:1],
                      scalar2=None, op0=ALU.is_ge)
                    V('tensor_scalar', reads=['esel', 'top8'], writes=['sel2'], out=s2, in0=es, scalar1=t8[:, 1:2],
                      scalar2=None, op0=ALU.is_ge)
                    V('tensor_tensor', reads=['sel2', 'sel1'], writes=['sel2'], out=s2, in0=s2, in1=s1, op=ALU.subtract)
                    V('tensor_scalar', reads=['top8'], writes=['rsm'], out=ne1, in0=t8[:, 0:1], scalar1=-1.0,
                      scalar2=None, op0=ALU.mult)
                    P.op('scalar', 'activation', reads=['top8', 'rsm'], writes=['rsm'], out=r_, in_=t8[:, 1:2],
                         func=AF.Exp, bias=ne1, scale=1.0)
                    V('tensor_scalar', reads=['rsm'], writes=['rsm'], out=den, in0=r_, scalar1=1.0, scalar2=None,
                      op0=ALU.add)
                    V('reciprocal', reads=['rsm'], writes=['rsm'], out=den, in_=den)
                    V('tensor_tensor', reads=['rsm'], writes=['rw'], out=self.rw[:, i, 0:1], in0=den, in1=pg, op=ALU.mult)
                    V('tensor_tensor', reads=['rw', 'rsm'], writes=['rw'], out=self.rw[:, i, 1:2], in0=self.rw[:, i, 0:1],
                      in1=r_, op=ALU.mult)
                    for k, sel in enumerate((s1, s2)):
                        V('tensor_tensor', reads=['ohg', f'sel{k + 1}'], writes=['ohb'],
                          out=self.ohb[:, i, k * 32:(k + 1) * 32].rearrange("p (g e) -> p g e", g=4),
                          in0=ohg.unsqueeze(2).broadcast_to([128, 4, 8]),
                          in1=sel.unsqueeze(1).broadcast_to([128, 4, 8]), op=ALU.mult)
            self.norm_and_transpose_loop(st, None, gsc, self.mod(3), consumer, pre=pre, after_h=after_h, pre_load=pre_load)
            P.end()

    def phase_moe(self, l, st0):
        P, T, NT, NB, BLK = self.P, self.T, self.NT, self.NB, self.BLK
        ps = self.ps
        last = (l == self.L - 1)
        NR = NB * BLK
        wgv = self.w_gate.rearrange("l e (p two k4) f -> (l e p two) (k4 f)", two=2, k4=4)
        wuv = self.w_up.rearrange("l e (p two k4) f -> (l e p two) (k4 f)", two=2, k4=4)
        wdv = self.w_down.rearrange("l e (p two f2) d -> (l e p two) (f2 d)", two=2, f2=2)
        with contextlib.ExitStack() as st:
            sb = lambda n, s, dt=F32: self.sb(st, n, s, dt)
            dest_i = sb("dest_i", [128, NT, 2], I32)
            widx = sb("widx", [128, NB, 2], I32)
            with contextlib.ExitStack() as s1:
                sb1 = lambda n, s, dt=F32: self.sb(s1, n, s, dt)
                pre = sb1("pre_all", [128, NT, 64])
                tot = sb1("tot_all", [128, NT, 64])
                base = sb1("base_all", [128, NT, 64])
                cnt = sb1("cnt", [128, 64])
                tl = sb1("mtotal", [128, 32])
                md = sb1("mmod", [128, 32])
                pend = sb1("pend", [128, 32])
                off = sb1("moff", [128, 64])
                dest_f = sb1("dest_f", [128, NT, 2])
                blk0 = sb1("blk0", [128, NB])
                cmp_ = sb1("mcmp", [128, NB, 32])
                be = sb1("mbe", [128, NB])
                pidx = sb1("pidx", [128, 2])
                wf = sb1("widx_f", [128, NB, 2])
                dbg_t = sb1("rdbg_t", [128, 68])
                P.begin()
                ohb2 = self.ohb.rearrange("p i c -> p (i c)")
                f2 = lambda t: t.rearrange("p i c -> p (i c)")
                for k, c0 in enumerate(range(0, NT * 64, 512)):
                    n = min(512, NT * 64 - c0)
                    P.op('tensor', 'matmul', reads=['sutb', 'ohb'], writes=['ps0'], out=ps[0][:, 0:n], lhsT=self.sutb,
                         rhs=ohb2[:, c0:c0 + n], start=True, stop=True)
                    P.op('scalar', 'activation', reads=['ps0'], writes=['pre_all'], out=f2(pre)[:, c0:c0 + n],
                         in_=ps[0][:, 0:n], func=AF.Copy)
                    P.op('tensor', 'matmul', reads=['onesb', 'ohb'], writes=['ps1'], out=ps[1][:, 0:n], lhsT=self.onesb,
                         rhs=ohb2[:, c0:c0 + n], start=True, stop=True)
                    P.op('vector', 'tensor_copy', reads=['ps1'], writes=['tot_all'], out=f2(tot)[:, c0:c0 + n],
                         in_=ps[1][:, 0:n])
                V = lambda name, **kw: P.op('vector', name, **kw)
                P.op('gpsimd', 'memset', writes=['base_all'], ap=base[:, 0, :], constant=0.0)
                for i in range(1, NT):
                    V('tensor_tensor', reads=['base_all', 'tot_all'], writes=['base_all'], out=base[:, i, :],
                      in0=base[:, i - 1, :], in1=tot[:, i - 1, :], op=ALU.add)
                V('tensor_tensor', reads=['base_all', 'tot_all'], writes=['cnt'], out=cnt, in0=base[:, NT - 1, :],
                  in1=tot[:, NT - 1, :], op=ALU.add)
                V('tensor_tensor', reads=['cnt'], writes=['mtotal'], out=tl, in0=cnt[:, 0:32], in1=cnt[:, 32:64],
                  op=ALU.add)
                P.op('gpsimd', 'iota', writes=['blk0'], out=blk0, pattern=[[BLK, NB]], base=0, channel_multiplier=0,
                     allow_small_or_imprecise_dtypes=True)
                V('tensor_tensor', reads=['mtotal', 'blk0'], writes=['mcmp'], out=cmp_.rearrange("p b e -> p (b e)").rearrange("p (e b) -> p e b", e=32),
                  in0=blk0.unsqueeze(1).broadcast_to([128, 32, NB]), in1=tl.unsqueeze(2).broadcast_to([128, 32, NB]),
                  op=ALU.is_lt)
                V('tensor_reduce', reads=['mcmp'], writes=['mtotal'], out=tl,
                  in_=cmp_.rearrange("p b e -> p (b e)").rearrange("p (e b) -> p e b", e=32), axis=AX.X, op=ALU.add)
                V('tensor_scalar', reads=['mtotal'], writes=['mtotal'], out=tl, in0=tl, scalar1=float(BLK), scalar2=None,
                  op0=ALU.mult)
                V('tensor_tensor_scan', reads=['mtotal', 'ones'], writes=['pend'], out=pend,
                  data0=self.ones[:, 0:32], data1=tl, initial=0.0, op0=ALU.mult, op1=ALU.add)
                V('tensor_tensor', reads=['pend', 'mtotal'], writes=['moff'], out=off[:, 0:32], in0=pend, in1=tl,
                  op=ALU.subtract)
                V('tensor_tensor', reads=['moff', 'cnt'], writes=['moff'], out=off[:, 32:64], in0=off[:, 0:32],
                  in1=cnt[:, 0:32], op=ALU.add)
                V('tensor_tensor', reads=['pre_all', 'base_all'], writes=['pre_all'], out=pre, in0=pre, in1=base,
                  op=ALU.add)
                V('tensor_tensor', reads=['pre_all', 'moff'], writes=['pre_all'], out=pre, in0=pre,
                  in1=off.unsqueeze(1).broadcast_to([128, NT, 64]), op=ALU.add)
                V('tensor_tensor', reads=['pre_all', 'ohb'], writes=['pre_all'], out=pre, in0=pre, in1=self.ohb,
                  op=ALU.mult)
                V('tensor_reduce', reads=['pre_all'], writes=['dest_f'], out=dest_f,
                  in_=pre.rearrange("p i (k e) -> p i k e", k=2), axis=AX.X, op=ALU.add)
                V('tensor_copy', reads=['dest_f'], writes=['dest_i'], out=dest_i, in_=dest_f)
                V('tensor_tensor', reads=['pend', 'blk0'], writes=['mcmp'], out=cmp_,
                  in0=pend.unsqueeze(1).broadcast_to([128, NB, 32]), in1=blk0.unsqueeze(2).broadcast_to([128, NB, 32]),
                  op=ALU.is_le)
                V('tensor_reduce', reads=['mcmp'], writes=['mbe'], out=be, in_=cmp_, axis=AX.X, op=ALU.add)
                V('tensor_scalar', reads=['mbe'], writes=['mbe'], out=be, in0=be, scalar1=256.0, scalar2=None,
                  op0=ALU.mult)
                P.op('gpsimd', 'iota', writes=['pidx'], out=pidx, pattern=[[1, 2]], base=l * NE * 256, channel_multiplier=2,
                     allow_small_or_imprecise_dtypes=True)
                V('tensor_tensor', reads=['mbe', 'pidx'], writes=['widx_f'], out=wf,
                  in0=be.unsqueeze(2).broadcast_to([128, NB, 2]), in1=pidx.unsqueeze(1).broadcast_to([128, NB, 2]),
                  op=ALU.add)
                V('tensor_copy', reads=['widx_f'], writes=['widx'], out=widx, in_=wf)
                if 'rdbg' in self.dbg:
                    for i in range(NT):
                        V('tensor_copy', reads=['ohb'], writes=['rdbg_t'], out=dbg_t[:, 0:64], in_=self.ohb[:, i, :])
                        V('tensor_copy', reads=['rw'], writes=['rdbg_t'], out=dbg_t[:, 64:66], in_=self.rw[:, i, :])
                        V('tensor_copy', reads=['dest_f'], writes=['rdbg_t'], out=dbg_t[:, 66:68], in_=dest_f[:, i, :])
                        P.dma('sync', self.rdbg[i * 128:(i + 1) * 128, :], dbg_t, reads=['rdbg_t'])
                P.end()
            if '1' in self.opt:
                return
            with contextlib.ExitStack() as s2:
                hrow = [self.sb(s2, f"hrow{i}", [128, D], BF16) for i in range(3)]
                P.begin()
                P.raw('gpsimd', 'reg_mov', self.reg_rows, NR - 1)
                for i in range(NT):
                    Hr = hrow[i % 3]
                    rr = f'hrow{i % 3}'
                    P.dma('sync', Hr, self.hfp[i * 128:(i + 1) * 128, :], writes=[rr])
                    for k in range(2):
                        P.dma('gpsimd', None, None, reads=[rr, 'dest_i'], writes=['xs'],
                              fn=('indirect_dma_start', dict(
                                  out=self.xs, out_offset=bass.IndirectOffsetOnAxis(ap=dest_i[:, i, k:k + 1], axis=0),
                                  in_=Hr, in_offset=None, bounds_check=self.reg_rows, oob_is_err=False)))
                P.end()
            if '2' in self.opt:
                return
            with contextlib.ExitStack() as s3:
                sb3 = lambda n, s, dt=F32: self.sb(s3, n, s, dt)
                Wg = [sb3(f"Wg{i}", [128, 8, 512], BF16) for i in range(2)]
                Wu = [sb3(f"Wu{i}", [128, 8, 512], BF16) for i in range(2)]
                Wd = [sb3(f"Wd{i}", [128, 4, 1024], BF16) for i in range(2)]
                xsT = [sb3(f"xsT{i}", [128, 8, 512], BF16) for i in range(2)]
                hid = [sb3(f"hid{i}", [128, 4, 512], BF16) for i in range(2)]
                sg = [sb3(f"sg{i}", [128, 512]) for i in range(2)]
                yb = [sb3(f"yb{i}", [128, D]) for i in range(2)]
                P.begin()
                P.raw('gpsimd', 'reg_mov', self.reg_w, (l + 1) * NE * 256 - 1)
                n_y = [0]

                def load_blk(b):
                    a = b % 2
                    for (Wt, view, nm) in ((Wg[a], wgv, f'Wg{a}'), (Wu[a], wuv, f'Wu{a}'), (Wd[a], wdv, f'Wd{a}')):
                        flat = Wt.rearrange("p a f -> p (a f)")
                        for hf in range(0 if 'G' in self.opt else 2):
                            P.dma('gpsimd', None, None, reads=['widx'], writes=[nm],
                                  fn=('indirect_dma_start', dict(
                                      out=flat[:, hf * 2048:(hf + 1) * 2048], out_offset=None, in_=view,
                                      in_offset=bass.IndirectOffsetOnAxis(ap=widx[:, b, hf:hf + 1], axis=0),
                                      bounds_check=self.reg_w, oob_is_err=False)))
                    X = xsT[a]
                    for kc in range(8):
                        P.dma('sync', None, None, reads=['xs'], writes=[f'xsT{a}'],
                              fn=('dma_start_transpose', dict(
                                  out=X[:, kc, :], in_=self.xs[b * BLK:(b + 1) * BLK, kc * 128:(kc + 1) * 128])))

                def compute_blk(b):
                    a = b % 2
                    X = xsT[a]
                    rxs = f'xsT{a}'
                    if 'C' in self.opt:
                        return
                    Hd = hid[a]
                    rh = f'hid{a}'
                    wg4 = Wg[a].rearrange("p kc (m four) -> p kc four m", four=4)
                    wu4 = Wu[a].rearrange("p kc (m four) -> p kc four m", four=4)
                    for fc in range(4):
                        pg_, pu_ = 2 + (fc % 2) * 2, 3 + (fc % 2) * 2
                        for kc in range(8):
                            P.op('tensor', 'matmul', reads=[f'Wg{a}', rxs], writes=[f'ps{pg_}'], out=ps[pg_],
                                 lhsT=wg4[:, kc, fc, :], rhs=X[:, kc, :], start=(kc == 0), stop=(kc == 7))
                        for kc in range(8):
                            P.op('tensor', 'matmul', reads=[f'Wu{a}', rxs], writes=[f'ps{pu_}'], out=ps[pu_],
                                 lhsT=wu4[:, kc, fc, :], rhs=X[:, kc, :], start=(kc == 0), stop=(kc == 7))
                        S = sg[fc % 2]
                        P.op('scalar', 'activation', reads=[f'ps{pg_}'], writes=[f'sg{fc % 2}'], out=S, in_=ps[pg_],
                             func=AF.Silu)
                        P.op('vector', 'tensor_tensor', reads=[f'sg{fc % 2}', f'ps{pu_}'], writes=[rh], out=Hd[:, fc, :],
                             in0=S, in1=ps[pu_], op=ALU.mult)
                    for rt in range(4):
                        n_y[0] += 1
                        Y = yb[n_y[0] % 2]
                        ry = f'yb{n_y[0] % 2}'
                        for half in range(2):
                            pb = 6 + half
                            for fc in range(4):
                                P.op('tensor', 'matmul', reads=[rh, f'Wd{a}'], writes=[f'ps{pb}'], out=ps[pb],
                                     lhsT=Hd[:, fc, rt * 128:(rt + 1) * 128], rhs=Wd[a][:, fc, half * 512:(half + 1) * 512],
                                     start=(fc == 0), stop=(fc == 3))
                            self.evac(half, Y[:, half * 512:(half + 1) * 512], ps[pb], [f'ps{pb}'], [ry])
                        r0 = b * BLK + rt * 128
                        P.dma('sync', self.ys[r0:r0 + 128, :], Y, reads=[ry], writes=['ys'])

                load_blk(0)
                for b in range(NB):
                    if b + 1 < NB:
                        load_blk(b + 1)
                    compute_blk(b)
                P.end()
            if '3' in self.opt:
                return
            with contextlib.ExitStack() as s4:
                sb4 = lambda n, s, dt=F32: self.sb(s4, n, s, dt)
                y1 = [sb4(f"y1_{i}", [128, D]) for i in range(2)]
                y2 = [sb4(f"y2_{i}", [128, D]) for i in range(2)]
                xr = [sb4(f"xr{i}", [128, D]) for i in range(2)]
                fgb = sb4("fgb", [128, D])
                fsq = sb4("fsq", [128, D])
                fss = sb4("fss", [128, 1])
                frs = sb4("frs", [128, 1])
                P.begin()
                P.raw('gpsimd', 'reg_mov', self.reg_rows, NR - 1)
                if last:
                    P.dma('sync', fgb, self.final_g.partition_broadcast(128), writes=['fgb'])
                def load_tile(i):
                    a = i % 2
                    rows = slice(i * 128, (i + 1) * 128)
                    P.dma('sync', xr[a], self.xres[rows, :], writes=[f'xr{a}'])
                    for (Yk, rk, k) in ((y1[a], f'y1_{a}', 0), (y2[a], f'y2_{a}', 1)):
                        P.dma('gpsimd', None, None, reads=['dest_i'], writes=[rk],
                              fn=('indirect_dma_start', dict(
                                  out=Yk, out_offset=None, in_=self.ys,
                                  in_offset=bass.IndirectOffsetOnAxis(ap=dest_i[:, i, k:k + 1], axis=0),
                                  bounds_check=self.reg_rows, oob_is_err=False)))
                load_tile(0)
                for i in range(NT):
                    a = i % 2
                    Y1, Y2, X = y1[a], y2[a], xr[a]
                    r1, r2, rx = f'y1_{a}', f'y2_{a}', f'xr{a}'
                    rows = slice(i * 128, (i + 1) * 128)
                    if i + 1 < NT:
                        load_tile(i + 1)
                    P.op('vector', 'tensor_scalar', reads=[r1, 'rw'], writes=[r1], out=Y1, in0=Y1,
                         scalar1=self.rw[:, i, 0:1], scalar2=None, op0=ALU.mult)
                    P.op('vector', 'scalar_tensor_tensor', reads=[r1, r2, 'rw'], writes=[r1], out=Y1, in0=Y2,
                         scalar=self.rw[:, i, 1:2], in1=Y1, op0=ALU.mult, op1=ALU.add)
                    P.op('gpsimd', 'tensor_tensor', reads=[r1, 'modb'], writes=[r1], out=Y1, in0=Y1, in1=self.mod(5),
                         op=ALU.mult)
                    P.op('gpsimd', 'tensor_tensor', reads=[r1, rx], writes=[rx], out=X, in0=X, in1=Y1, op=ALU.add)
                    if not last:
                        P.dma('sync', self.xres[rows, :], X, reads=[rx])
                    else:
                        P.op('scalar', 'activation', reads=[rx], writes=['fsq', 'fss'], out=fsq, in_=X, func=AF.Square,
                             accum_out=fss)
                        self.rstd_ops(fss, frs, D, ['fss'], ['frs'])
                        P.op('vector', 'scalar_tensor_tensor', reads=[rx, 'frs', 'fgb'], writes=[r2], out=Y2, in0=X,
                             scalar=frs, in1=fgb, op0=ALU.mult, op1=ALU.mult)
                        P.dma('sync', self.out[rows, :], Y2, reads=[r2])
                P.end()

    def build_layer(self, l):
        with contextlib.ExitStack() as st:
            ph = self.phases
            self.phase_ada(l, st)
            if 'i' in ph:
                self.phase_inproj(l, st)
            if 'c' in ph:
                self.phase_conv(l)
            if 'f' in ph:
                self.phase_attn(l)
            if 's' in ph:
                self.phase_ssd(l)
            if 'o' in ph:
                self.phase_wout_router(l, st)
            if 'm' in ph and self.moe:
                self.phase_moe(l, st)

    def zero_xs(self):
        P = self.P
        NR = self.NB * self.BLK
        with contextlib.ExitStack() as st:
            z = self.sb(st, "zeros", [128, 8192], BF16)
            P.begin()
            P.op('gpsimd', 'memset', writes=['zeros'], ap=z, constant=0.0)
            xv = self.xs.rearrange("(a p r) d -> a p (r d)", p=128, r=8)
            for a in range(NR // 1024):
                P.dma('sync', xv[a], z, reads=['zeros'])
            P.end()

    def build(self):
        if self.moe:
            self.zero_xs()
        for l in range(self.L):
            self.build_layer(l)
        return self.nc


def prep_core_inputs(inp, b, T, L):
    f = lambda a: np.ascontiguousarray(a, dtype=np.float32)
    m = {}
    m["x"] = f(inp["x"][b, :T])
    m["c"] = f(inp["c"][b].reshape(8, 128).T)
    for k in ("ada_w", "ada_b", "norm_mix_g", "w_in", "ssd_dt_bias", "ssd_a_log", "ssd_d", "ssd_norm_g",
              "w_out", "norm_ffn_g", "w_router_group", "b_router_group", "w_router_expert", "b_router_expert",
              "w_gate", "w_up", "w_down"):
        m[k] = f(inp[k][:L])
    m["final_norm_g"] = f(inp["final_norm_g"])
    m["convw_fm"] = f(inp["ssd_conv_w"][:L].reshape(L, 4, 8, 128).transpose(0, 3, 2, 1))
    m["convb_fm"] = f(inp["ssd_conv_b"][:L].reshape(L, 8, 128).transpose(0, 2, 1))
    m["fox_f_bias_fm"] = f(inp["fox_f_bias"][:L].reshape(L, 4, 1))
    m["fox_g_fm"] = f(inp["fox_norm_g"][:L].reshape(L, 2, 128).transpose(0, 2, 1))
    m["cmw_fm"] = f(inp["cm_conv_w"][:L].reshape(L, 31, 2, 128).transpose(0, 3, 2, 1))
    m["cmb_fm"] = f(inp["cm_conv_b"][:L].reshape(L, 2, 128).transpose(0, 2, 1))
    m["cmg_fm"] = f(inp["cm_ln_g"][:L].reshape(L, 2, 128).transpose(0, 2, 1))
    m["cmbeta_fm"] = f(inp["cm_ln_b"][:L].reshape(L, 2, 128).transpose(0, 2, 1))
    return m


_CACHE = {}
T_FULL, L_FULL, B_FULL = 8192, 4, 4


def kernel(**inputs):
    if 'nc' not in _CACHE:
        bd = Builder(T_FULL, L_FULL)
        _CACHE['nc'] = bd.build()
        _CACHE['names'] = set(bd.ins)
    nc = _CACHE['nc']
    inp = {k: np.asarray(v) for k, v in inputs.items()}
    maps = []
    for b in range(B_FULL):
        m = prep_core_inputs(inp, b, T_FULL, L_FULL)
        maps.append({k: v for k, v in m.items() if k in _CACHE['names']})
    res = run_bass_kernel_spmd(nc, maps, core_ids=list(range(B_FULL)))
    out = np.stack([np.asarray(r["out"], dtype=np.float32) for r in res.results], axis=0)
    return out
```

```python
import contextlib
import numpy as np
import concourse.bass as bass
import concourse.mybir as mybir
from concourse.bass_utils import run_bass_kernel_spmd

F32 = mybir.dt.float32
BF16 = mybir.dt.bfloat16
I32 = mybir.dt.int32
AF = mybir.ActivationFunctionType
ALU = mybir.AluOpType
AX = mybir.AxisListType

ENGS = ('tensor', 'vector', 'scalar', 'gpsimd', 'sync')
CENGS = ('tensor', 'vector', 'scalar', 'gpsimd')
NDS = 10

D = 1024
D_IN = 2828
EPS = 1e-6
NE = 32
DE = 512


class Prog:
    def __init__(self, nc, same_engine_sync=True):
        self.nc = nc
        self.same = same_engine_sync
        self.esem = {e: nc.alloc_semaphore(name=f"es_{e}") for e in CENGS}
        self.dsem = {e: [nc.alloc_semaphore(name=f"ds_{e}_{i}") for i in range(NDS)]
                     for e in ('sync', 'scalar', 'gpsimd')}
        self._reset()
        self.q = None

    def _reset(self):
        keep = getattr(self, 'dcnt', {}).get('gpsimd', [0] * NDS)
        self.ecnt = {e: 0 for e in CENGS}
        self.dcnt = {e: [0] * NDS for e in self.dsem}
        self.dcnt['gpsimd'] = list(keep)
        self.dnext = {e: 0 for e in self.dsem}
        self.known = {e: {('d', 'gpsimd', i): keep[i] for i in range(NDS)} for e in ENGS}
        self.res_w = {}
        self.res_r = {}

    def semof(self, key):
        if key[0] == 'e':
            return self.esem[key[1]]
        return self.dsem[key[1]][key[2]]

    def begin(self):
        self.q = {e: [] for e in ENGS}

    def _deps(self, reads, writes):
        deps = []
        for r in reads:
            t = self.res_w.get(r)
            if t is not None:
                deps.append(t)
        for w in writes:
            t = self.res_w.get(w)
            if t is not None:
                deps.append(t)
            deps.extend(self.res_r.get(w, {}).items())
        return deps

    def _record(self, tok, reads, writes):
        for r in reads:
            d = self.res_r.setdefault(r, {})
            if d.get(tok[0], 0) < tok[1]:
                d[tok[0]] = tok[1]
        for w in writes:
            self.res_w[w] = tok
            self.res_r[w] = {}

    def _waits(self, eng, deps):
        waits = []
        best = {}
        for (key, c) in deps:
            if best.get(key, 0) < c:
                best[key] = c
        for key, c in best.items():
            if key == ('e', 'tensor') and eng == 'tensor':
                continue
            if (not self.same) and key == ('e', eng):
                continue
            if self.known[eng].get(key, 0) >= c:
                continue
            self.known[eng][key] = c
            waits.append((self.semof(key), c))
        return waits

    def op(self, eng, name, reads=(), writes=(), **kw):
        writes = list(writes) + [r for r in reads if r.startswith('ps') and r not in writes]
        waits = self._waits(eng, self._deps(reads, writes))
        self.ecnt[eng] += 1
        tok = (('e', eng), self.ecnt[eng])
        sem = self.esem[eng]

        def emit(e, name=name, kw=kw, waits=waits, sem=sem):
            for (s, c) in waits:
                e.wait_ge(s, c)
            getattr(e, name)(**kw).then_inc(sem, 1)
        self.q[eng].append(emit)
        self._record(tok, reads, writes)
        return tok

    def raw(self, eng, name, *args, **kw):
        self.q[eng].append(lambda e: getattr(e, name)(*args, **kw))

    def dma(self, eng, out, in_, reads=(), writes=(), fn=None, **kw):
        deps = self._deps(reads, writes)
        idx = self.dnext[eng]
        self.dnext[eng] = (idx + 1) % NDS
        key = ('d', eng, idx)
        prev = self.dcnt[eng][idx]
        if prev:
            deps.append((key, prev))
        waits = self._waits(eng, deps)
        self.dcnt[eng][idx] += 16
        tok = (key, self.dcnt[eng][idx])
        sem = self.dsem[eng][idx]

        def emit(e, waits=waits, sem=sem):
            for (s, c) in waits:
                e.wait_ge(s, c)
            if fn is not None:
                try:
                    ins = getattr(e, fn[0])(**fn[1])
                except Exception:
                    print("DMA builder failed:", fn[0], {k: (v.shape if hasattr(v, 'shape') else v) for k, v in fn[1].items()})
                    raise
                ins.then_inc(sem, 16)
            else:
                e.dma_start(out=out, in_=in_, **kw).then_inc(sem, 16)
        self.q[eng].append(emit)
        self._record(tok, reads, writes)
        return tok

    def end(self):
        nc = self.nc
        fin = []
        for e in self.dsem:
            for i in range(NDS):
                c = self.dcnt[e][i]
                if c and self.known['sync'].get(('d', e, i), 0) < c:
                    fin.append((self.dsem[e][i], c))

        def drain(e, fin=fin):
            for (s, c) in fin:
                e.wait_ge(s, c)
        self.q['sync'].append(drain)
        with nc.Block() as block:
            for en in ENGS:
                ops = self.q[en]
                if not ops:
                    continue

                def body(e, ops=ops):
                    for o in ops:
                        o(e)
                getattr(block, en)(body)
        allsems = list(self.esem.values()) + [s for e in self.dsem if e != 'gpsimd' for s in self.dsem[e]]
        with nc.Block() as block:
            def clr(e):
                for s in allsems:
                    e.sem_clear(s)
            block.sync(clr)
        self._reset()
        self.q = None


class Builder:
    def __init__(self, T, L, dbg=(), moe=True, phases='aicfsom', opt=''):
        self.opt = opt
        self.T, self.L = T, L
        self.moe = moe
        self.phases = phases
        self.NT = T // 128
        self.NQ = T // 512
        self.dbg = set(dbg)
        nc = self.nc = bass.Bass("TRN2", target_bir_lowering=False)
        self.P = Prog(nc, same_engine_sync=('S' not in self.opt))
        self.ins = {}
        self.outs = {}
        self._uid = 0
        self.declare_io()
        self.consts()

    def din(self, name, shape, dt=F32):
        t = self.nc.dram_tensor(name, list(shape), dt, kind="ExternalInput").ap()
        self.ins[name] = t
        return t

    def dscr(self, name, shape, dt=F32):
        kind = "ExternalOutput" if name in self.dbg else "Internal"
        t = self.nc.dram_tensor(name, list(shape), dt, kind=kind).ap()
        if kind == "ExternalOutput":
            self.outs[name] = t
        return t

    def sb(self, st, name, shape, dt=F32):
        self._uid += 1
        return st.enter_context(self.nc.sbuf_tensor(f"{name}_{self._uid}", list(shape), dt)).ap()

    def declare_io(self):
        T, L = self.T, self.L
        d = self.din
        self.x_in = d("x", [T, D])
        self.c_in = d("c", [128, 8])
        self.ada_w = d("ada_w", [L, D, 6 * D])
        self.ada_b = d("ada_b", [L, 6 * D])
        self.norm_mix_g = d("norm_mix_g", [L, D])
        self.w_in = d("w_in", [L, D, D_IN])
        self.convw = d("convw_fm", [L, 128, 8, 4])
        self.convb = d("convb_fm", [L, 128, 8])
        self.dt_bias = d("ssd_dt_bias", [L, 8])
        self.a_log = d("ssd_a_log", [L, 8])
        self.ssd_d = d("ssd_d", [L, 8])
        self.ssd_norm_g = d("ssd_norm_g", [L, 512])
        self.f_bias = d("fox_f_bias_fm", [L, 4, 1])
        self.fox_g = d("fox_g_fm", [L, 128, 2])
        self.cmw = d("cmw_fm", [L, 128, 2, 31])
        self.cmb = d("cmb_fm", [L, 128, 2])
        self.cmg = d("cmg_fm", [L, 128, 2])
        self.cmbeta = d("cmbeta_fm", [L, 128, 2])
        self.w_out = d("w_out", [L, D, D])
        self.norm_ffn_g = d("norm_ffn_g", [L, D])
        self.w_rg = d("w_router_group", [L, D, 4])
        self.b_rg = d("b_router_group", [L, 4])
        self.w_re = d("w_router_expert", [L, D, NE])
        self.b_re = d("b_router_expert", [L, NE])
        if self.moe:
            self.w_gate = d("w_gate", [L, NE, D, DE])
            self.w_up = d("w_up", [L, NE, D, DE])
            self.w_down = d("w_down", [L, NE, DE, D])
        self.final_g = d("final_norm_g", [D])
        self.out = self.nc.dram_tensor("out", [T, D], F32, kind="ExternalOutput").ap()
        self.outs["out"] = self.out
        s = self.dscr
        self.xres = s("xres", [T, D])
        self.zs = s("zs", [T, 512])
        self.xbcT = s("xbcT", [1024, T])
        self.dtr = s("dtr", [T, 8])
        self.flT = s("flT", [4, T])
        self.qT = s("qT", [256, T], BF16)
        self.kT = s("kT", [256, T], BF16)
        self.vtok = s("vtok", [T, 256], BF16)
        self.gaT = s("gaT", [256, T])
        self.gbT = s("gbT", [256, T])
        self.yT = s("yT", [1024, T], BF16)
        self.attT = s("attT", [256, T])
        self.cs = s("cs", [4, 6, T], BF16)
        self.hfp = s("hfp", [T, D], BF16)
        self.BLK = 512
        self.NB = (2 * T) // self.BLK + NE
        self.xs = s("xs", [self.NB * self.BLK, D], BF16)
        self.ys = s("ys", [self.NB * self.BLK, D])
        self.rdbg = s("rdbg", [T, 68])

    def consts(self):
        nc, P = self.nc, self.P
        a = lambda n, s, dt=F32: nc.alloc_sbuf_tensor(n, list(s), dt).ap()
        self.ident = a("ident", [128, 128])
        self.identb = a("identb", [128, 128], BF16)
        self.tri = a("tri", [128, 128])
        self.ustr = a("ustr", [128, 128])
        self.ones = a("ones", [128, 128])
        self.onesb = a("onesb", [128, 128], BF16)
        self.epsb = a("epsb", [128, 1])
        self.sutb = a("sutb", [128, 128], BF16)
        self.ps = [nc.alloc_psum_tensor(f"ps{i}", [128, 512], F32).ap() for i in range(8)]
        self.reg_rows = nc.gpsimd.alloc_register("bc_rows")
        self.reg_w = nc.gpsimd.alloc_register("bc_w")
        P.begin()
        g = 'gpsimd'
        P.op(g, 'memset', writes=['epsb'], ap=self.epsb, constant=EPS)
        P.op(g, 'memset', writes=['ones'], ap=self.ones, constant=1.0)
        P.op(g, 'memset', writes=['onesb'], ap=self.onesb, constant=1.0)
        P.op(g, 'memset', writes=['ident'], ap=self.ident, constant=1.0)
        P.op(g, 'affine_select', reads=['ident'], writes=['ident'], out=self.ident, in_=self.ident,
             pattern=[[-1, 128]], compare_op=ALU.is_equal, fill=0.0, base=0, channel_multiplier=1)
        P.op(g, 'tensor_copy', reads=['ident'], writes=['identb'], out=self.identb, in_=self.ident)
        P.op(g, 'memset', writes=['tri'], ap=self.tri, constant=1.0)
        P.op(g, 'affine_select', reads=['tri'], writes=['tri'], out=self.tri, in_=self.tri,
             pattern=[[1, 128]], compare_op=ALU.is_ge, fill=0.0, base=0, channel_multiplier=-1)
        P.op(g, 'tensor_tensor', reads=['tri', 'ident'], writes=['sutb'], out=self.sutb, in0=self.tri, in1=self.ident,
             op=ALU.subtract)
        P.op(g, 'memset', writes=['ustr'], ap=self.ustr, constant=1.0)
        P.op(g, 'affine_select', reads=['ustr'], writes=['ustr'], out=self.ustr, in_=self.ustr,
             pattern=[[-1, 128]], compare_op=ALU.is_gt, fill=0.0, base=0, channel_multiplier=1)
        P.end()

    def phase_ada(self, l, st):
        nc, P = self.nc, self.P
        self.modb = self.sb(st, "modb", [128, 6 * D])
        with contextlib.ExitStack() as s2:
            cs = self.sb(s2, "c_s", [128, 8])
            cb = self.sb(s2, "c_b", [128, 8, 128])
            adab = self.sb(s2, "adab", [128, 6 * D])
            wch = [self.sb(s2, f"adaw{i}", [128, 8, 512]) for i in range(2)]
            P.begin()
            P.dma('sync', cs, self.c_in, writes=['c_s'])
            P.dma('sync', adab, self.ada_b[l].partition_broadcast(128), writes=['adab'])
            P.op('scalar', 'activation', reads=['c_s'], writes=['c_s'], out=cs, in_=cs, func=AF.Silu)
            P.op('vector', 'tensor_copy', reads=['c_s'], writes=['c_b'], out=cb,
                 in_=cs.unsqueeze(2).broadcast_to([128, 8, 128]))
            for j in range(12):
                w = wch[j % 2]
                wr = f'adaw{j % 2}'
                P.dma('sync', w,
                      self.ada_w[l][:, j * 512:(j + 1) * 512].rearrange("(kc p) n -> p kc n", p=128),
                      writes=[wr])
                pt = self.ps[j % 2]
                for kc in range(8):
                    P.op('tensor', 'matmul', reads=['c_b', wr], writes=[f'ps{j % 2}'], out=pt,
                         lhsT=cb[:, kc, :], rhs=w[:, kc, :], start=(kc == 0), stop=(kc == 7))
                P.op('vector', 'tensor_tensor', reads=[f'ps{j % 2}', 'adab'], writes=['modb'],
                     out=self.modb[:, j * 512:(j + 1) * 512], in0=pt, in1=adab[:, j * 512:(j + 1) * 512], op=ALU.add)
            P.end()

    def mod(self, i):
        return self.modb[:, i * D:(i + 1) * D]

    def load_cast(self, st, dst, src, res, width):
        P = self.P
        if getattr(self, '_stg_owner', None) is not st:
            self._stg = [self.sb(st, f"stg{i}", [128, 2048]) for i in range(2)]
            self._stg_owner = st
            self._stg_n = 0
        for c0 in range(0, width, 2048):
            n = min(2048, width - c0)
            k = self._stg_n
            self._stg_n += 1
            S = self._stg[k % 2]
            rs = f'stg{k % 2}'
            P.dma('sync', S[:, 0:n], src[:, c0:c0 + n], writes=[rs])
            P.op('gpsimd' if k % 2 else 'vector', 'tensor_copy', reads=[rs], writes=[res], out=dst[:, c0:c0 + n],
                 in_=S[:, 0:n])

    def rstd_ops(self, ss, rstd, n, rd, wr):
        P = self.P
        P.op('scalar', 'activation', reads=rd, writes=wr, out=rstd, in_=ss, func=AF.Ln,
             bias=self.epsb[:ss.shape[0], :], scale=1.0 / n)
        P.op('scalar', 'activation', reads=wr, writes=wr, out=rstd, in_=rstd, func=AF.Exp, scale=-0.5)

    def phase_inproj(self, l, st0):
        nc, P, T = self.nc, self.P, self.T
        src = self.x_in if l == 0 else self.xres
        self._ip_bufs = None
        with contextlib.ExitStack() as st:
            sb = lambda n, s, dt=F32: self.sb(st, n, s, dt)
            wz = sb("wz", [128, 8, 512], BF16)
            wx = sb("wx", [128, 8, 1024], BF16)
            wqk = sb("wqk", [128, 8, 512], BF16)
            wv = sb("wv", [128, 8, 256], BF16)
            wg = sb("wg", [128, 8, 512], BF16)
            wdt = sb("wdt", [128, 8, 8])
            wf = sb("wf", [128, 8, 4])
            gsc = sb("gsc", [128, D])
            W = self.w_in[l].rearrange("(kc p) n -> p kc n", p=128)
            P.begin()
            for kc in range(8):
                self.load_cast(st, wz[:, kc, :], W[:, kc, 0:512], 'wz', 512)
                self.load_cast(st, wx[:, kc, :], W[:, kc, 512:1536], 'wx', 1024)
                self.load_cast(st, wqk[:, kc, :], W[:, kc, 1544:2056], 'wqk', 512)
                self.load_cast(st, wv[:, kc, :], W[:, kc, 2056:2312], 'wv', 256)
                self.load_cast(st, wg[:, kc, :], W[:, kc, 2316:2828], 'wg', 512)
            P.dma('sync', wdt, W[:, :, 1536:1544], writes=['wdt'])
            P.dma('sync', wf, W[:, :, 2312:2316], writes=['wf'])
            P.dma('sync', gsc, self.norm_mix_g[l].partition_broadcast(128), writes=['gsc'])
            P.op('vector', 'scalar_tensor_tensor', reads=['gsc', 'modb'], writes=['gsc'], out=gsc,
                 in0=self.mod(1), scalar=1.0, in1=gsc, op0=ALU.add, op1=ALU.mult)
            self.norm_and_transpose_loop(st, src, gsc, self.mod(0), consumer=lambda q, hT, hT32: self.inproj_chunk(
                q, hT, hT32, wz, wx, wqk, wv, wg, wdt, wf, st))
            P.end()

    def norm_and_transpose_loop(self, st, src, gsc, shift, consumer, pre=None, after_h=None, pre_load=None):
        P = self.P
        sb = lambda n, s, dt=F32: self.sb(st, n, s, dt)
        NXB = 4
        xt = [sb(f"xt{i}", [128, D]) for i in range(NXB)]
        ht = [sb(f"ht{i}", [128, D]) for i in range(2)]
        sq = sb("sq", [128, D])
        ss = [sb(f"ss{i}", [128, 1]) for i in range(2)]
        rs = [sb(f"rs{i}", [128, 1]) for i in range(2)]
        hT32 = [sb(f"hT32_{i}", [128, 8, 512]) for i in range(2)]
        hT = [sb(f"hT_{i}", [128, 8, 512], BF16) for i in range(2)]
        PF = 2

        def load(i):
            if i >= self.NT:
                return
            if pre_load is not None:
                pre_load(i)
            elif pre is None:
                P.dma('sync', xt[i % NXB], src[i * 128:(i + 1) * 128, :], writes=[f'xt{i % NXB}'])
        for i in range(PF):
            load(i)
        for q in range(self.NQ):
            b = q % 2
            for j in range(4):
                i = q * 4 + j
                a = i % 2
                X, H = xt[i % NXB], ht[a]
                rx, rh = f'xt{i % NXB}', f'ht{a}'
                load(i + PF)
                if pre is not None:
                    pre(i, X, rx)
                P.op('scalar', 'activation', reads=[rx], writes=['sq', f'ss{a}'], out=sq, in_=X, func=AF.Square,
                     accum_out=ss[a])
                self.rstd_ops(ss[a], rs[a], D, [f'ss{a}'], [f'rs{a}'])
                P.op('vector', 'scalar_tensor_tensor', reads=[rx, f'rs{a}', 'gsc'], writes=[rh], out=H, in0=X,
                     scalar=rs[a], in1=gsc, op0=ALU.mult, op1=ALU.mult)
                P.op('gpsimd', 'tensor_tensor', reads=[rh, 'modb'], writes=[rh], out=H, in0=H, in1=shift, op=ALU.add)
                if after_h is not None:
                    after_h(i, H, rh, X, rx)
                for half in range(2):
                    pt = self.ps[half]
                    for k4 in range(4):
                        kc = half * 4 + k4
                        P.op('tensor', 'transpose', reads=[rh, 'ident'], writes=[f'ps{half}'],
                             out=pt[:, k4 * 128:(k4 + 1) * 128], in_=H[:, kc * 128:(kc + 1) * 128], identity=self.ident)
                    dst = hT32[b][:, half * 4:(half + 1) * 4, j * 128:(j + 1) * 128]
                    P.op('scalar', 'activation', reads=[f'ps{half}'], writes=[f'hT32_{b}'], out=dst,
                         in_=pt.rearrange("p (k t) -> p k t", k=4), func=AF.Copy)
                P.op('gpsimd', 'tensor_copy', reads=[f'hT32_{b}'], writes=[f'hT_{b}'],
                     out=hT[b][:, :, j * 128:(j + 1) * 128], in_=hT32[b][:, :, j * 128:(j + 1) * 128])
            consumer(q, (hT[b], f'hT_{b}'), (hT32[b], f'hT32_{b}'))

    def evac(self, k, out, in_, reads, writes, scale=None):
        P = self.P
        if k % 2 == 0:
            if scale is None:
                P.op('scalar', 'activation', reads=reads, writes=writes, out=out, in_=in_, func=AF.Copy)
            else:
                P.op('scalar', 'activation', reads=reads, writes=writes, out=out, in_=in_, func=AF.Copy, scale=scale)
        else:
            if scale is None:
                P.op('vector', 'tensor_copy', reads=reads, writes=writes, out=out, in_=in_)
            else:
                P.op('vector', 'tensor_scalar', reads=reads, writes=writes, out=out, in0=in_, scalar1=scale,
                     scalar2=None, op0=ALU.mult)

    def inproj_chunk(self, q, hTb, hT32b, wz, wx, wqk, wv, wg, wdt, wf, st):
        P = self.P
        hT, rhT = hTb
        hT32, rhT32 = hT32b
        if self._ip_bufs is None:
            sb = lambda n, s, dt=F32: self.sb(st, n, s, dt)
            self._ip_bufs = dict(
                o32=[sb(f"o32_{i}", [128, 512]) for i in range(3)],
                o16=[sb(f"o16_{i}", [128, 512], BF16) for i in range(3)],
                osm=[sb(f"osm_{i}", [128, 8]) for i in range(2)],
                ofl=[sb(f"ofl_{i}", [4, 512]) for i in range(2)],
                n=[0],
            )
        B = self._ip_bufs
        tok = slice(q * 512, (q + 1) * 512)

        def nxt():
            B['n'][0] += 1
            return B['n'][0]
        PB = [2, 3, 4, 5]

        def fm(w, wres, c0, dst, dt16=False, scale=None):
            k = nxt()
            pb = PB[k % 4]
            pt = self.ps[pb]
            for kc in range(8):
                P.op('tensor', 'matmul', reads=[wres, rhT], writes=[f'ps{pb}'], out=pt, lhsT=w[:, kc, c0:c0 + 128],
                     rhs=hT[:, kc, :], start=(kc == 0), stop=(kc == 7))
            o = (B['o16'] if dt16 else B['o32'])[k % 3]
            ores = ('o16_' if dt16 else 'o32_') + str(k % 3)
            self.evac(k, o, pt, [f'ps{pb}'], [ores], scale=scale)
            P.dma('sync', dst, o, reads=[ores], writes=[])
        for ct in range(8):
            fm(wx, 'wx', ct * 128, self.xbcT[ct * 128:(ct + 1) * 128, tok])
        for ct in range(2):
            fm(wqk, 'wqk', ct * 128, self.qT[ct * 128:(ct + 1) * 128, tok], dt16=True, scale=0.125)
        for ct in range(2):
            fm(wqk, 'wqk', 256 + ct * 128, self.kT[ct * 128:(ct + 1) * 128, tok], dt16=True)
        for ct in range(2):
            fm(wg, 'wg', ct * 128, self.gaT[ct * 128:(ct + 1) * 128, tok])
        for ct in range(2):
            fm(wg, 'wg', 256 + ct * 128, self.gbT[ct * 128:(ct + 1) * 128, tok])
        k = nxt()
        pb = PB[k % 4]
        pt = self.ps[pb]
        for kc in range(8):
            P.op('tensor', 'matmul', reads=['wf', rhT32], writes=[f'ps{pb}'], out=pt[0:4, :], lhsT=wf[:, kc, :],
                 rhs=hT32[:, kc, :], start=(kc == 0), stop=(kc == 7))
        o = B['ofl'][q % 2]
        P.op('vector', 'tensor_copy', reads=[f'ps{pb}'], writes=[f'ofl_{q % 2}'], out=o, in_=pt[0:4, :])
        P.dma('sync', self.flT[:, tok], o, reads=[f'ofl_{q % 2}'])
        for j in range(4):
            tt = slice(q * 512 + j * 128, q * 512 + (j + 1) * 128)
            k = nxt()
            pb = PB[k % 4]
            pt = self.ps[pb]
            for kc in range(8):
                P.op('tensor', 'matmul', reads=['wz', rhT], writes=[f'ps{pb}'], out=pt,
                     lhsT=hT[:, kc, j * 128:(j + 1) * 128], rhs=wz[:, kc, :], start=(kc == 0), stop=(kc == 7))
            o = B['o32'][k % 3]
            P.op('scalar', 'activation', reads=[f'ps{pb}'], writes=[f'o32_{k % 3}'], out=o, in_=pt, func=AF.Silu)
            P.dma('sync', self.zs[tt, :], o, reads=[f'o32_{k % 3}'])
            k = nxt()
            pb = PB[k % 4]
            pt = self.ps[pb]
            for kc in range(8):
                P.op('tensor', 'matmul', reads=['wv', rhT], writes=[f'ps{pb}'], out=pt[:, 0:256],
                     lhsT=hT[:, kc, j * 128:(j + 1) * 128], rhs=wv[:, kc, :], start=(kc == 0), stop=(kc == 7))
            for kc in range(8):
                P.op('tensor', 'matmul', reads=['wdt', rhT32], writes=[f'ps{pb}'], out=pt[:, 256:264],
                     lhsT=hT32[:, kc, j * 128:(j + 1) * 128], rhs=wdt[:, kc, :], start=(kc == 0), stop=(kc == 7))
            o = B['o16'][k % 3]
            self.evac(k, o[:, 0:256], pt[:, 0:256], [f'ps{pb}'], [f'o16_{k % 3}'])
            P.dma('sync', self.vtok[tt, :], o[:, 0:256], reads=[f'o16_{k % 3}'])
            o2 = B['osm'][j % 2]
            P.op('vector', 'tensor_copy', reads=[f'ps{pb}'], writes=[f'osm_{j % 2}'], out=o2, in_=pt[:, 256:264])
            P.dma('sync', self.dtr[tt, :], o2, reads=[f'osm_{j % 2}'])

    def conv_gen(self, l, st, pA, pB):
        P, T = self.P, self.T
        TC = min(T, 1024)
        HALO = 30
        if True:
            sb = lambda n, s, dt=F32: self.sb(st, n, s, dt)
            cw = sb("cw", [128, 2, 31])
            cbias = sb("cbias", [128, 2])
            cg = sb("cg", [128, 2])
            cbeta = sb("cbeta", [128, 2])
            ua = [sb(f"ua{i}", [128, TC + HALO]) for i in range(2)]
            ub = [sb(f"ub{i}", [128, TC + HALO]) for i in range(2)]
            co = [sb(f"co{i}", [128, TC]) for i in range(2)]
            sqt = sb("csq", [128, 512])
            mean = sb("cmean", [128, 512])
            rstd = sb("crstd", [128, 512])
            tmp = [sb(f"ctmp{i}", [128, 512]) for i in range(2)]
            yo = [sb(f"cyo{i}", [128, 512], BF16) for i in range(2)]
            P.dma('sync', cw, self.cmw[l], writes=['cw'])
            P.dma('sync', cbias, self.cmb[l], writes=['cbias'])
            P.dma('sync', cg, self.cmg[l], writes=['cg'])
            P.dma('sync', cbeta, self.cmbeta[l], writes=['cbeta'])
            yield
            n = 0
            for c0 in range(0, T, TC):
                for ct in range(2):
                    A, Bt = ua[ct], ub[ct]
                    ra, rb, rc = f'ua{ct}', f'ub{ct}', f'co{ct}'
                    rows = slice(ct * 128, (ct + 1) * 128)
                    if c0 == 0:
                        P.dma('sync', A[:, HALO:], self.gaT[rows, 0:TC], writes=[ra])
                        P.dma('sync', Bt[:, HALO:], self.gbT[rows, 0:TC], writes=[rb])
                        P.op('gpsimd', 'memset', writes=[ra], ap=A[:, 0:HALO], constant=0.0)
                        P.op('gpsimd', 'memset', writes=[rb], ap=Bt[:, 0:HALO], constant=0.0)
                    else:
                        P.dma('sync', A, self.gaT[rows, c0 - HALO:c0 + TC], writes=[ra])
                        P.dma('sync', Bt, self.gbT[rows, c0 - HALO:c0 + TC], writes=[rb])
                    P.op('scalar', 'activation', reads=[rb], writes=[rb], out=Bt, in_=Bt, func=AF.Sigmoid)
                    P.op('gpsimd', 'tensor_tensor', reads=[ra, rb], writes=[ra], out=A, in0=A, in1=Bt, op=ALU.mult)
                    C = co[ct]
                    P.op('vector', 'tensor_scalar', reads=[ra, 'cw', 'cbias'], writes=[rc], out=C, in0=A[:, 0:TC],
                         scalar1=cw[:, ct, 0:1], scalar2=cbias[:, ct:ct + 1], op0=ALU.mult, op1=ALU.add)
                    for k in range(1, 31):
                        P.op('vector', 'scalar_tensor_tensor', reads=[ra, 'cw', rc], writes=[rc], out=C,
                             in0=A[:, k:k + TC], scalar=cw[:, ct, k:k + 1], in1=C, op0=ALU.mult, op1=ALU.add)
                        if k % 8 == 0:
                            yield
                    yield
                for s0 in range(0, TC, 512):
                    cs_ = slice(s0, s0 + 512)
                    p1, p2 = self.ps[pA], self.ps[pB]
                    for ct in range(2):
                        P.op('tensor', 'matmul', reads=['ones', f'co{ct}'], writes=[f'ps{pA}'], out=p1, lhsT=self.ones,
                             rhs=co[ct][:, cs_], start=(ct == 0), stop=(ct == 1))
                    for ct in range(2):
                        P.op('scalar', 'activation', reads=[f'co{ct}'], writes=['csq'], out=sqt, in_=co[ct][:, cs_],
                             func=AF.Square)
                        P.op('tensor', 'matmul', reads=['ones', 'csq'], writes=[f'ps{pB}'], out=p2, lhsT=self.ones,
                             rhs=sqt, start=(ct == 0), stop=(ct == 1))
                    P.op('vector', 'tensor_scalar', reads=[f'ps{pA}'], writes=['cmean'], out=mean, in0=p1,
                         scalar1=1.0 / 256, scalar2=None, op0=ALU.mult)
                    P.op('vector', 'tensor_tensor', reads=['cmean'], writes=['crstd'], out=rstd, in0=mean, in1=mean,
                         op=ALU.mult)
                    P.op('vector', 'scalar_tensor_tensor', reads=[f'ps{pB}', 'crstd'], writes=['crstd'], out=rstd, in0=p2,
                         scalar=1.0 / 256, in1=rstd, op0=ALU.mult, op1=ALU.subtract)
                    self.rstd_ops(rstd, rstd, 1.0, ['crstd'], ['crstd'])
                    for ct in range(2):
                        n += 1
                        t = tmp[n % 2]
                        rt = f'ctmp{n % 2}'
                        P.op('vector', 'tensor_tensor', reads=[f'co{ct}', 'cmean'], writes=[rt], out=t,
                             in0=co[ct][:, cs_], in1=mean, op=ALU.subtract)
                        P.op('gpsimd', 'tensor_tensor', reads=[rt, 'crstd'], writes=[rt], out=t, in0=t, in1=rstd,
                             op=ALU.mult)
                        y = yo[n % 2]
                        ry = f'cyo{n % 2}'
                        P.op('scalar', 'activation', reads=[rt, 'cg', 'cbeta'], writes=[ry], out=y, in_=t, func=AF.Silu,
                             scale=cg[:, ct:ct + 1], bias=cbeta[:, ct:ct + 1])
                        P.dma('sync', self.yT[768 + ct * 128:768 + (ct + 1) * 128, c0 + s0:c0 + s0 + 512], y,
                              reads=[ry])
                    yield

    def phase_conv(self, l):
        with contextlib.ExitStack() as st:
            self.P.begin()
            for _ in self.conv_gen(l, st, 0, 1):
                pass
            self.P.end()

    def phase_attn(self, l, conv_inside=False):
        P, T, NT, NQ = self.P, self.T, self.NT, self.NQ
        CW = min(T, 2048)
        with contextlib.ExitStack() as st:
            sb = lambda n, s, dt=F32: self.sb(st, n, s, dt)
            fb = sb("fb", [4, 1])
            xx = sb("fx", [4, CW])
            ax = sb("fax", [4, CW])
            mn = sb("fmn", [4, CW])
            cum = [sb(f"fcum{i}", [4, CW]) for i in range(2)]
            r1 = sb("fr1", [4, CW])
            sp = sb("fsp", [4, 6, CW], BF16)
            P.begin()
            P.dma('sync', fb, self.f_bias[l], writes=['fb'])
            for ci, c0 in enumerate(range(0, T, CW)):
                cc = cum[ci % 2]
                rcum = f'fcum{ci % 2}'
                P.dma('sync', xx, self.flT[:, c0:c0 + CW], writes=['fx'])
                P.op('scalar', 'activation', reads=['fx', 'fb'], writes=['fx'], out=xx, in_=xx, func=AF.Identity,
                     bias=fb[:, 0:1], scale=1.0)
                P.op('vector', 'tensor_scalar', reads=['fx'], writes=['fmn'], out=mn, in0=xx, scalar1=-1.0, scalar2=0.0,
                     op0=ALU.mult, op1=ALU.max)
                P.op('vector', 'scalar_tensor_tensor', reads=['fmn', 'fx'], writes=['fax'], out=ax, in0=mn, scalar=-2.0,
                     in1=xx, op0=ALU.mult, op1=ALU.subtract)
                P.op('scalar', 'activation', reads=['fax'], writes=['fax'], out=ax, in_=ax, func=AF.Exp)
                P.op('scalar', 'activation', reads=['fax'], writes=['fax'], out=ax, in_=ax, func=AF.Ln, bias=1.0,
                     scale=1.0)
                P.op('vector', 'scalar_tensor_tensor', reads=['fmn', 'fax'], writes=['fmn'], out=mn, in0=mn, scalar=-1.0,
                     in1=ax, op0=ALU.mult, op1=ALU.subtract)
                init = 0.0 if ci == 0 else cum[(ci - 1) % 2][:, CW - 1:CW]
                P.op('vector', 'tensor_tensor_scan', reads=['fmn', 'ones', f'fcum{(ci - 1) % 2}'], writes=[rcum], out=cc,
                     data0=self.ones[0:4, 0:1].broadcast_to([4, CW]), data1=mn, initial=init, op0=ALU.mult, op1=ALU.add)
                P.op('vector', 'tensor_copy', reads=[rcum], writes=['fsp'], out=sp[:, 0, :], in_=cc)
                P.op('vector', 'tensor_tensor', reads=[rcum, 'fsp'], writes=['fr1'], out=r1, in0=cc, in1=sp[:, 0, :],
                     op=ALU.subtract)
                P.op('vector', 'tensor_copy', reads=['fr1'], writes=['fsp'], out=sp[:, 1, :], in_=r1)
                P.op('vector', 'tensor_tensor', reads=['fr1', 'fsp'], writes=['fr1'], out=r1, in0=r1, in1=sp[:, 1, :],
                     op=ALU.subtract)
                P.op('vector', 'tensor_copy', reads=['fr1'], writes=['fsp'], out=sp[:, 2, :], in_=r1)
                P.op('vector', 'tensor_scalar', reads=['fsp'], writes=['fsp'], out=sp[:, 3:6, :], in0=sp[:, 0:3, :],
                     scalar1=-1.0, scalar2=None, op0=ALU.mult)
                P.dma('sync', self.cs[:, :, c0:c0 + CW], sp, reads=['fsp'])
            P.end()
        with contextlib.ExitStack() as st:
            sb = lambda n, s, dt=F32: self.sb(st, n, s, dt)
            qp = [sb(f"qp{i}", [70, T], BF16) for i in range(2)]
            kp = [sb(f"kp{i}", [70, T], BF16) for i in range(2)]
            vp = [sb(f"vp{i}", [128, NT, 65], BF16) for i in range(2)]
            nm = sb("negmask", [128, 4, 512], BF16)
            pt_ = [sb(f"pT{i}", [128, 512], BF16) for i in range(3)]
            rec = sb("rec", [65, 512])
            bcs = sb("bcs", [64, 512])
            on = [sb(f"on{i}", [64, 512]) for i in range(2)]
            P.begin()
            cgen = self.conv_gen(l, st, 6, 7) if conv_inside else None
            n_units = (T // min(T, 1024)) * (2 * 5 + min(T, 1024) // 512) + 1
            units_done = 0
            work_total = 4 * sum(4 * q_ + 4 for q_ in range(NQ))
            work_done = 0
            P.op('gpsimd', 'memset', writes=['negmask'], ap=nm, constant=0.0)
            for d in range(4):
                P.op('gpsimd', 'affine_select', reads=['negmask'], writes=['negmask'], out=nm[:, d, :], in_=nm[:, d, :],
                     pattern=[[1, 512]], compare_op=ALU.is_ge, fill=-30000.0, base=-128 * d, channel_multiplier=-1)
            step = 0
            for h in range(4):
                hb = h % 2
                Q, Kp, V = qp[hb], kp[hb], vp[hb]
                rq, rk, rv = f'qp{hb}', f'kp{hb}', f'vp{hb}'
                hr = slice(h * 64, (h + 1) * 64)
                P.op('gpsimd', 'memset', writes=[rq], ap=Q[64:70, :], constant=1.0)
                P.op('gpsimd', 'memset', writes=[rk], ap=Kp[64:70, :], constant=1.0)
                P.op('gpsimd', 'memset', writes=[rv], ap=V[:, :, 64:65], constant=1.0)
                P.dma('sync', Q[0:64, :], self.qT[hr, :], writes=[rq])
                P.dma('sync', Kp[0:64, :], self.kT[hr, :], writes=[rk])
                P.dma('sync', Q[67:70, :], self.cs[h, 0:3, :], writes=[rq])
                P.dma('sync', Kp[64:67, :], self.cs[h, 3:6, :], writes=[rk])
                for i0 in range(0, NT, 4):
                    P.dma('sync', V[:, i0:i0 + 4, 0:64],
                          self.vtok[i0 * 128:(i0 + 4) * 128, hr].rearrange("(i p) d -> p i d", p=128), writes=[rv])
                for qc in range(NQ):
                    nk = 4 * qc + 4
                    ob = 3 + qc % 2
                    O = self.ps[ob]
                    qs = slice(qc * 512, (qc + 1) * 512)
                    for s_ in range(nk + 2):
                        if s_ < nk:
                            kt = s_
                            sbk = (step + s_) % 3
                            S = self.ps[sbk]
                            diag = kt >= 4 * qc
                            P.op('tensor', 'matmul', reads=[rq, rk], writes=[f'ps{sbk}'], out=S,
                                 lhsT=Kp[:, kt * 128:(kt + 1) * 128], rhs=Q[:, qs], start=True, stop=not diag)
                            if diag:
                                P.op('tensor', 'matmul', reads=['identb', 'negmask'], writes=[f'ps{sbk}'], out=S,
                                     lhsT=self.identb, rhs=nm[:, kt - 4 * qc, :], start=False, stop=True)
                        if 1 <= s_ <= nk:
                            kt = s_ - 1
                            sbk = (step + kt) % 3
                            P.op('scalar', 'activation', reads=[f'ps{sbk}'], writes=[f'pT{sbk}'], out=pt_[sbk],
                                 in_=self.ps[sbk], func=AF.Exp)
                        if s_ >= 2:
                            kt = s_ - 2
                            sbk = (step + kt) % 3
                            P.op('tensor', 'matmul', reads=[f'pT{sbk}', rv], writes=[f'ps{ob}'], out=O[0:65, :],
                                 lhsT=V[:, kt, :], rhs=pt_[sbk], start=(kt == 0), stop=(kt == nk - 1))
                    step += nk
                    P.op('vector', 'reciprocal', reads=[f'ps{ob}'], writes=['rec'], out=rec[64:65, :], in_=O[64:65, :])
                    P.op('tensor', 'matmul', reads=['ones', 'rec'], writes=['ps5'], out=self.ps[5][0:64, :],
                         lhsT=self.ones[64:65, 0:64], rhs=rec[64:65, :], start=True, stop=True)
                    P.op('scalar', 'activation', reads=['ps5'], writes=['bcs'], out=bcs, in_=self.ps[5][0:64, :],
                         func=AF.Copy)
                    o_ = on[qc % 2]
                    P.op('vector', 'tensor_tensor', reads=[f'ps{ob}', 'bcs'], writes=[f'on{qc % 2}'], out=o_,
                         in0=O[0:64, :], in1=bcs, op=ALU.mult)
                    P.dma('sync', self.attT[hr, qs], o_, reads=[f'on{qc % 2}'])
                    work_done += nk
                    while cgen is not None and units_done * work_total < n_units * work_done:
                        try:
                            next(cgen)
                            units_done += 1
                        except StopIteration:
                            cgen = None
                if h == 3 and cgen is not None:
                    for _ in cgen:
                        pass
                if h < 3:
                    P.end()
                    P.begin()
            P.end()
        with contextlib.ExitStack() as st:
            sb = lambda n, s, dt=F32: self.sb(st, n, s, dt)
            fg = sb("foxg", [128, 2])
            at = [[sb(f"at{i}{c}", [128, 512]) for c in range(2)] for i in range(2)]
            sq = sb("asq", [128, 512])
            rs = sb("ars", [128, 512])
            yo = [sb(f"ayo{i}", [128, 512], BF16) for i in range(2)]
            P.begin()
            P.dma('sync', fg, self.fox_g[l], writes=['foxg'])
            n = 0
            for qc in range(NQ):
                qs = slice(qc * 512, (qc + 1) * 512)
                b = qc % 2
                for ct in range(2):
                    P.dma('sync', at[b][ct], self.attT[ct * 128:(ct + 1) * 128, qs], writes=[f'at{b}{ct}'])
                    P.op('scalar', 'activation', reads=[f'at{b}{ct}'], writes=['asq'], out=sq, in_=at[b][ct],
                         func=AF.Square)
                    P.op('tensor', 'matmul', reads=['ones', 'asq'], writes=['ps0'], out=self.ps[0], lhsT=self.ones,
                         rhs=sq, start=(ct == 0), stop=(ct == 1))
                self.rstd_ops(self.ps[0], rs, 256.0, ['ps0'], ['ars'])
                for ct in range(2):
                    n += 1
                    P.op('vector', 'tensor_tensor', reads=[f'at{b}{ct}', 'ars'], writes=[f'at{b}{ct}'], out=at[b][ct],
                         in0=at[b][ct], in1=rs, op=ALU.mult)
                    y = yo[n % 2]
                    P.op('scalar', 'activation', reads=[f'at{b}{ct}', 'foxg'], writes=[f'ayo{n % 2}'], out=y,
                         in_=at[b][ct], func=AF.Copy, scale=fg[:, ct:ct + 1])
                    P.dma('sync', self.yT[512 + ct * 128:512 + (ct + 1) * 128, qs], y, reads=[f'ayo{n % 2}'])
            P.end()

    def phase_ssd(self, l):
        P, T, NT = self.P, self.T, self.NT
        SC = 512
        assert NT * 8 <= 512
        with contextlib.ExitStack() as st:
            sb = lambda n, s, dt=F32: self.sb(st, n, s, dt)
            cw4 = sb("cw4", [128, 8, 4])
            cb4 = sb("cb4", [128, 8])
            dtb = sb("dtb", [128, 8])
            aneg = sb("aneg", [128, 8])
            dsk = sb("dsk", [128, 8])
            ng = sb("ssdng", [128, 512])
            dt = sb("dt_all", [128, NT, 8])
            dmn = sb("dt_mn", [128, NT, 8])
            dtA = sb("dtA", [128, NT, 8])
            El = sb("El", [128, NT, 8])
            Wl = sb("Wl", [128, NT, 8])
            cd = sb("cd", [128, NT, 8])
            xin = [sb(f"xin{i}", [128, SC + 3]) for i in range(2)]
            cacc = [sb(f"cacc{i}", [128, SC]) for i in range(2)]
            xsT = [sb(f"xsT{i}", [128, SC]) for i in range(4)]
            BT = [sb(f"BT{i}", [128, SC], BF16) for i in range(2)]
            CT = [sb(f"CT{i}", [128, SC], BF16) for i in range(2)]
            x32_2 = [sb(f"x32{i}", [128, 8, 64], F32) for i in range(2)]
            Btok_2 = [sb(f"Btok{i}", [128, 256], BF16) for i in range(2)]
            R_2 = [sb(f"Rall{i}", [128, 8, 128], F32) for i in range(2)]
            E_2 = [sb(f"Eall{i}", [128, 8, 128], F32) for i in range(2)]
            CBm_2 = [sb(f"CBm{i}", [128, 2, 128], F32) for i in range(2)]
            M_2 = [sb(f"Mall{i}", [128, 8, 128], BF16) for i in range(2)]
            xdt_2 = [sb(f"xdt{i}", [128, 8, 64], BF16) for i in range(2)]
            xw_2 = [sb(f"xw{i}", [128, 8, 64], BF16) for i in range(2)]
            H = sb("Hst", [128, 8, 64])
            Hb = sb("Hb", [128, 8, 64], BF16)
            t1_2 = [sb(f"sst1{i}", [128, 8, 64], F32) for i in range(2)]
            t2_2 = [sb(f"sst2{i}", [128, 8, 64], F32) for i in range(2)]
            zt_2 = [sb(f"zt{i}", [128, 512], F32) for i in range(2)]
            ssq_2 = [sb(f"ssq{i}", [128, 512], F32) for i in range(2)]
            gss_2 = [sb(f"gss{i}", [128, 2], F32) for i in range(2)]
            grs_2 = [sb(f"grs{i}", [128, 2], F32) for i in range(2)]
            yTs_2 = [sb(f"yTs{i}", [128, 4, 128], BF16) for i in range(2)]
            ps = self.ps
            psb1 = ps[1].bitcast(BF16)
            P.begin()
            P.dma('sync', cw4, self.convw[l], writes=['cw4'])
            P.dma('sync', cb4, self.convb[l], writes=['cb4'])
            P.dma('sync', dtb, self.dt_bias[l].partition_broadcast(128), writes=['dtb'])
            P.dma('sync', aneg, self.a_log[l].partition_broadcast(128), writes=['aneg'])
            P.dma('sync', dsk, self.ssd_d[l].partition_broadcast(128), writes=['dsk'])
            P.dma('sync', ng, self.ssd_norm_g[l].partition_broadcast(128), writes=['ssdng'])
            for i0 in range(0, NT, 4):
                n_ = min(4, NT - i0)
                P.dma('sync', dt[:, i0:i0 + n_, :],
                      self.dtr[i0 * 128:(i0 + n_) * 128, :].rearrange("(i p) h -> p i h", p=128), writes=['dt_all'])
            P.op('scalar', 'activation', reads=['aneg'], writes=['aneg'], out=aneg, in_=aneg, func=AF.Exp)
            P.op('vector', 'tensor_scalar', reads=['aneg'], writes=['aneg'], out=aneg, in0=aneg, scalar1=-1.0,
                 scalar2=None, op0=ALU.mult)
            bc3 = lambda t: t.unsqueeze(1).broadcast_to([128, NT, 8])
            P.op('vector', 'tensor_tensor', reads=['dt_all', 'dtb'], writes=['dt_all'], out=dt, in0=dt, in1=bc3(dtb),
                 op=ALU.add)
            P.op('vector', 'tensor_scalar', reads=['dt_all'], writes=['dt_mn'], out=dmn, in0=dt, scalar1=0.0,
                 scalar2=None, op0=ALU.max)
            P.op('vector', 'scalar_tensor_tensor', reads=['dt_mn', 'dt_all'], writes=['dt_all'],
                 out=dt.rearrange("p i h -> p (i h)"), in0=dmn.rearrange("p i h -> p (i h)"), scalar=-2.0,
                 in1=dt.rearrange("p i h -> p (i h)"), op0=ALU.mult, op1=ALU.add)
            P.op('scalar', 'activation', reads=['dt_all'], writes=['dt_all'], out=dt, in_=dt, func=AF.Exp)
            P.op('scalar', 'activation', reads=['dt_all'], writes=['dt_all'], out=dt, in_=dt, func=AF.Ln, bias=1.0,
                 scale=1.0)
            P.op('vector', 'tensor_tensor', reads=['dt_all', 'dt_mn'], writes=['dt_all'], out=dt, in0=dt, in1=dmn,
                 op=ALU.add)
            P.op('vector', 'tensor_tensor', reads=['dt_all', 'aneg'], writes=['dtA'], out=dtA, in0=dt, in1=bc3(aneg),
                 op=ALU.mult)
            dtA2 = dtA.rearrange("p i h -> p (i h)")
            P.op('tensor', 'matmul', reads=['tri', 'dtA'], writes=['ps2'], out=ps[2][:, 0:NT * 8], lhsT=self.tri,
                 rhs=dtA2, start=True, stop=True)
            P.op('tensor', 'matmul', reads=['ones', 'dtA'], writes=['ps3'], out=ps[3][:, 0:NT * 8], lhsT=self.ones,
                 rhs=dtA2, start=True, stop=True)
            f2 = lambda t: t.rearrange("p i h -> p (i h)")
            P.op('scalar', 'activation', reads=['ps2'], writes=['El'], out=f2(El), in_=ps[2][:, 0:NT * 8], func=AF.Exp)
            P.op('scalar', 'activation', reads=['ps3'], writes=['cd'], out=f2(cd), in_=ps[3][:, 0:NT * 8], func=AF.Exp)
            P.op('vector', 'tensor_copy', reads=['ps3'], writes=['Wl'], out=f2(Wl), in_=ps[3][:, 0:NT * 8])
            P.op('vector', 'tensor_tensor', reads=['Wl', 'ps2'], writes=['Wl'], out=f2(Wl), in0=f2(Wl),
                 in1=ps[2][:, 0:NT * 8], op=ALU.subtract)
            P.op('scalar', 'activation', reads=['Wl'], writes=['Wl'], out=Wl, in_=Wl, func=AF.Exp)
            P.op('vector', 'tensor_tensor', reads=['Wl', 'dt_all'], writes=['Wl'], out=Wl, in0=Wl, in1=dt, op=ALU.mult)
            P.op('gpsimd', 'memset', writes=['Hst'], ap=H, constant=0.0)
            P.op('gpsimd', 'memset', writes=['Hb'], ap=Hb, constant=0.0)
            for c0 in range(0, T, SC):
                for ct in range(8):
                    X = xin[ct % 2]
                    rx = f'xin{ct % 2}'
                    A = cacc[ct % 2]
                    ra = f'cacc{ct % 2}'
                    rows = slice(ct * 128, (ct + 1) * 128)
                    if c0 == 0:
                        P.op('gpsimd', 'memset', writes=[rx], ap=X[:, 0:3], constant=0.0)
                        P.dma('sync', X[:, 3:], self.xbcT[rows, 0:SC], writes=[rx])
                    else:
                        P.dma('sync', X, self.xbcT[rows, c0 - 3:c0 + SC], writes=[rx])
                    P.op('vector', 'tensor_scalar', reads=[rx, 'cw4', 'cb4'], writes=[ra], out=A, in0=X[:, 0:SC],
                         scalar1=cw4[:, ct, 0:1], scalar2=cb4[:, ct:ct + 1], op0=ALU.mult, op1=ALU.add)
                    for k in range(1, 4):
                        P.op('vector', 'scalar_tensor_tensor', reads=[rx, 'cw4', ra], writes=[ra], out=A,
                             in0=X[:, k:k + SC], scalar=cw4[:, ct, k:k + 1], in1=A, op0=ALU.mult, op1=ALU.add)
                    if ct < 4:
                        dst, rd = xsT[ct], f'xsT{ct}'
                    elif ct < 6:
                        dst, rd = BT[ct - 4], f'BT{ct - 4}'
                    else:
                        dst, rd = CT[ct - 6], f'CT{ct - 6}'
                    P.op('scalar', 'activation', reads=[ra], writes=[rd], out=dst, in_=A, func=AF.Silu)
                def stage_a(c0, cc):
                        c = c0 // 128 + cc
                        cs_ = slice(cc * 128, (cc + 1) * 128)
                        tok = slice(c * 128, (c + 1) * 128)
                        pc = c % 2
                        x32 = x32_2[pc]
                        Btok = Btok_2[pc]
                        R = R_2[pc]
                        E = E_2[pc]
                        CBm = CBm_2[pc]
                        M = M_2[pc]
                        xdt = xdt_2[pc]
                        xw = xw_2[pc]
                        t1 = t1_2[pc]
                        t2 = t2_2[pc]
                        zt = zt_2[pc]
                        ssq = ssq_2[pc]
                        gss = gss_2[pc]
                        grs = grs_2[pc]
                        yTs = yTs_2[pc]
                        n = {k: k + str(pc) for k in ('x32', 'Btok', 'Rall', 'Eall', 'CBm', 'Mall', 'xdt', 'xw', 'sst1', 'sst2', 'zt', 'ssq', 'gss', 'grs', 'yTs')}
                        for ct in range(4):
                            P.op('tensor', 'transpose', reads=[f'xsT{ct}', 'ident'], writes=['ps0'],
                                 out=ps[0][:, ct * 128:(ct + 1) * 128], in_=xsT[ct][:, cs_], identity=self.ident)
                        P.op('scalar', 'activation', reads=['ps0'], writes=[n['x32']], out=x32.rearrange("p h d -> p (h d)"),
                             in_=ps[0], func=AF.Copy)
                        for g in range(2):
                            P.op('tensor', 'transpose', reads=[f'BT{g}', 'identb'], writes=['ps1'],
                                 out=psb1[:, g * 128:(g + 1) * 128], in_=BT[g][:, cs_], identity=self.identb)
                        P.op('vector', 'tensor_copy', reads=['ps1'], writes=[n['Btok']], out=Btok, in_=psb1[:, 0:256])
                        P.dma('sync', zt, self.zs[tok, :], writes=[n['zt']])
                        P.op('vector', 'tensor_tensor', reads=['tri', 'dtA'], writes=[n['Rall']], out=R,
                             in0=self.tri.unsqueeze(1).broadcast_to([128, 8, 128]),
                             in1=dtA[:, c, :].unsqueeze(2).broadcast_to([128, 8, 128]), op=ALU.mult)
                        for hh in range(2):
                            P.op('tensor', 'matmul', reads=['ustr', n['Rall']], writes=[f'ps{2 + hh}'], out=ps[2 + hh],
                                 lhsT=self.ustr, rhs=R[:, hh * 4:(hh + 1) * 4, :].rearrange("p h l -> p (h l)"),
                                 start=True, stop=True)
                            P.op('scalar', 'activation', reads=[f'ps{2 + hh}'], writes=[n['Eall']],
                                 out=E[:, hh * 4:(hh + 1) * 4, :].rearrange("p h l -> p (h l)"), in_=ps[2 + hh], func=AF.Exp)
                        for g in range(2):
                            P.op('tensor', 'matmul', reads=[f'BT{g}', f'CT{g}'], writes=['ps4'],
                                 out=ps[4][:, g * 128:(g + 1) * 128], lhsT=BT[g][:, cs_], rhs=CT[g][:, cs_],
                                 start=True, stop=True)
                        P.op('vector', 'tensor_tensor', reads=['ps4', 'tri'], writes=[n['CBm']], out=CBm,
                             in0=ps[4][:, 0:256].rearrange("p (g l) -> p g l", g=2),
                             in1=self.tri.unsqueeze(1).broadcast_to([128, 2, 128]), op=ALU.mult)
                        for g in range(2):
                            P.op('vector', 'tensor_tensor', reads=[n['Eall'], n['CBm']], writes=[n['Mall']],
                                 out=M[:, g * 4:(g + 1) * 4, :], in0=E[:, g * 4:(g + 1) * 4, :],
                                 in1=CBm[:, g:g + 1, :].broadcast_to([128, 4, 128]), op=ALU.mult)
                        P.op('gpsimd', 'tensor_tensor', reads=[n['x32'], 'dt_all'], writes=[n['xdt']], out=xdt, in0=x32,
                             in1=dt[:, c, :].unsqueeze(2).broadcast_to([128, 8, 64]), op=ALU.mult)
                        P.op('gpsimd', 'tensor_tensor', reads=[n['x32'], 'Wl'], writes=[n['xw']], out=xw, in0=x32,
                             in1=Wl[:, c, :].unsqueeze(2).broadcast_to([128, 8, 64]), op=ALU.mult)
                        for h in range(8):
                            P.op('tensor', 'matmul', reads=[n['Mall'], n['xdt']], writes=['ps5'], out=ps[5][:, h * 64:(h + 1) * 64],
                                 lhsT=M[:, h, :], rhs=xdt[:, h, :], start=True, stop=True)
                        for g in range(2):
                            P.op('tensor', 'matmul', reads=[f'CT{g}', 'Hb'], writes=['ps6'],
                                 out=ps[6][:, g * 256:(g + 1) * 256], lhsT=CT[g][:, cs_],
                                 rhs=Hb[:, g * 4:(g + 1) * 4, :].rearrange("p h d -> p (h d)"), start=True, stop=True)
                        for g in range(2):
                            P.op('tensor', 'matmul', reads=[n['Btok'], n['xw']], writes=['ps7'],
                                 out=ps[7][:, g * 256:(g + 1) * 256], lhsT=Btok[:, g * 128:(g + 1) * 128],
                                 rhs=xw[:, g * 4:(g + 1) * 4, :].rearrange("p h d -> p (h d)"), start=True, stop=True)
                        v3 = lambda t: t.rearrange("p (h d) -> p h d", h=8)
                        b3 = lambda t: t.unsqueeze(2).broadcast_to([128, 8, 64])
                        P.op('vector', 'tensor_tensor', reads=['ps6', 'El'], writes=[n['sst1']], out=t1, in0=v3(ps[6]),
                             in1=b3(El[:, c, :]), op=ALU.mult)
                        P.op('vector', 'tensor_tensor', reads=[n['sst1'], 'ps5'], writes=[n['sst1']], out=t1, in0=t1, in1=v3(ps[5]),
                             op=ALU.add)
                        P.op('gpsimd', 'tensor_tensor', reads=[n['x32'], 'dsk'], writes=[n['sst2']], out=t2, in0=x32, in1=b3(dsk),
                             op=ALU.mult)
                        P.op('gpsimd', 'tensor_tensor', reads=[n['sst1'], n['sst2']], writes=[n['sst1']], out=t1, in0=t1, in1=t2,
                             op=ALU.add)
                        P.op('vector', 'tensor_tensor', reads=['Hst', 'cd'], writes=['Hst'], out=H, in0=H, in1=b3(cd[:, c, :]),
                             op=ALU.mult)
                        P.op('vector', 'tensor_tensor', reads=['Hst', 'ps7'], writes=['Hst'], out=H, in0=H, in1=v3(ps[7]),
                             op=ALU.add)
                        P.op('gpsimd', 'tensor_copy', reads=['Hst'], writes=['Hb'], out=Hb, in_=H)

                def stage_b(c):
                        tok = slice(c * 128, (c + 1) * 128)
                        pc = c % 2
                        x32 = x32_2[pc]
                        Btok = Btok_2[pc]
                        R = R_2[pc]
                        E = E_2[pc]
                        CBm = CBm_2[pc]
                        M = M_2[pc]
                        xdt = xdt_2[pc]
                        xw = xw_2[pc]
                        t1 = t1_2[pc]
                        t2 = t2_2[pc]
                        zt = zt_2[pc]
                        ssq = ssq_2[pc]
                        gss = gss_2[pc]
                        grs = grs_2[pc]
                        yTs = yTs_2[pc]
                        n = {k: k + str(pc) for k in ('x32', 'Btok', 'Rall', 'Eall', 'CBm', 'Mall', 'xdt', 'xw', 'sst1', 'sst2', 'zt', 'ssq', 'gss', 'grs', 'yTs')}
                        y2 = t1.rearrange("p h d -> p (h d)")
                        P.op('gpsimd', 'tensor_tensor', reads=[n['sst1'], n['zt']], writes=[n['sst1']], out=y2, in0=y2, in1=zt,
                             op=ALU.mult)
                        for g in range(2):
                            P.op('scalar', 'activation', reads=[n['sst1']], writes=[n['ssq'], n['gss']],
                                 out=ssq[:, g * 256:(g + 1) * 256], in_=y2[:, g * 256:(g + 1) * 256], func=AF.Square,
                                 accum_out=gss[:, g:g + 1])
                        self.rstd_ops(gss, grs, 256.0, [n['gss']], [n['grs']])
                        for g in range(2):
                            P.op('vector', 'scalar_tensor_tensor', reads=[n['sst1'], n['grs'], 'ssdng'], writes=[n['sst2']],
                                 out=t2.rearrange("p h d -> p (h d)")[:, g * 256:(g + 1) * 256],
                                 in0=y2[:, g * 256:(g + 1) * 256], scalar=grs[:, g:g + 1],
                                 in1=ng[:, g * 256:(g + 1) * 256], op0=ALU.mult, op1=ALU.mult)
                        yn = t2.rearrange("p h d -> p (h d)")
                        for ct in range(4):
                            P.op('tensor', 'transpose', reads=[n['sst2'], 'ident'], writes=['ps0'],
                                 out=ps[0][:, ct * 128:(ct + 1) * 128], in_=yn[:, ct * 128:(ct + 1) * 128],
                                 identity=self.ident)
                        P.op('scalar', 'activation', reads=['ps0'], writes=[n['yTs']], out=yTs.rearrange("p c t -> p (c t)"),
                             in_=ps[0], func=AF.Copy)
                        P.dma('sync', self.yT[0:512, tok].rearrange("(ct p) t -> p ct t", p=128), yTs, reads=[n['yTs']])

                for cc in range(SC // 128):
                    c = c0 // 128 + cc
                    stage_a(c0, cc)
                    if c >= 1:
                        stage_b(c - 1)
                if c0 + SC >= T:
                    stage_b(NT - 1)
            P.end()

    def phase_wout_router(self, l, st0):
        P, T, NT = self.P, self.T, self.NT
        src = self.x_in if l == 0 else self.xres
        ps = self.ps
        self.ohb = self.sb(st0, "ohb", [128, NT, 64], BF16)
        self.rw = self.sb(st0, "rw", [128, NT, 2])
        with contextlib.ExitStack() as st:
            sb = lambda n, s, dt=F32: self.sb(st, n, s, dt)
            wo = sb("wo", [128, 8, D], BF16)
            gsc = sb("gscf", [128, D])
            wr = sb("wr", [128, 8, 36])
            rb = sb("rbias", [128, 36])
            yTc = [sb(f"yTc{i}", [128, 8, 512], BF16) for i in range(2)]
            xl = [sb(f"xl{i}", [128, D]) for i in range(4)]
            tt = sb("wtmp", [128, D])
            hb = [sb(f"hperm{i}", [128, D], BF16) for i in range(2)]
            lg = sb("lg", [128, 36])
            sm = sb("rsm", [128, 64])
            gexp = sb("gexp", [128, 4])
            ohg = sb("ohg", [128, 4])
            em = sb("em", [128, 4, 8])
            es = sb("esel", [128, 8])
            t8 = sb("top8", [128, 8])
            s1 = sb("sel1", [128, 8])
            s2 = sb("sel2", [128, 8])
            W = self.w_out[l].rearrange("(kc p) n -> p kc n", p=128)
            P.begin()
            for kc in range(8):
                self.load_cast(st, wo[:, kc, :], W[:, kc, :], 'wo', D)
            P.dma('sync', wr[:, :, 0:4], self.w_rg[l].rearrange("(kc p) n -> p kc n", p=128), writes=['wr'])
            P.dma('sync', wr[:, :, 4:36], self.w_re[l].rearrange("(kc p) n -> p kc n", p=128), writes=['wr'])
            P.dma('sync', rb[:, 0:4], self.b_rg[l].partition_broadcast(128), writes=['rbias'])
            P.dma('sync', rb[:, 4:36], self.b_re[l].partition_broadcast(128), writes=['rbias'])
            P.dma('sync', gsc, self.norm_ffn_g[l].partition_broadcast(128), writes=['gscf'])
            P.op('vector', 'scalar_tensor_tensor', reads=['gscf', 'modb'], writes=['gscf'], out=gsc, in0=self.mod(4),
                 scalar=1.0, in1=gsc, op0=ALU.add, op1=ALU.mult)

            def pre_load(i):
                q, j = divmod(i, 4)
                if j == 0:
                    P.dma('sync', yTc[q % 2], self.yT[:, q * 512:(q + 1) * 512].rearrange("(kc p) t -> p kc t", p=128),
                          writes=[f'yTc{q % 2}'])
                P.dma('sync', xl[i % 4], src[i * 128:(i + 1) * 128, :], writes=[f'xl{i % 4}'])

            def pre(i, X, rx):
                q, j = divmod(i, 4)
                Y = yTc[q % 2]
                ry = f'yTc{q % 2}'
                XL = xl[i % 4]
                rl = f'xl{i % 4}'
                for half in range(2):
                    pb = 2 + half
                    for kc in range(8):
                        P.op('tensor', 'matmul', reads=[ry, 'wo'], writes=[f'ps{pb}'], out=ps[pb],
                             lhsT=Y[:, kc, j * 128:(j + 1) * 128], rhs=wo[:, kc, half * 512:(half + 1) * 512],
                             start=(kc == 0), stop=(kc == 7))
                    hs = slice(half * 512, (half + 1) * 512)
                    P.op('vector', 'tensor_tensor', reads=[f'ps{pb}', 'modb'], writes=['wtmp'], out=tt[:, hs], in0=ps[pb],
                         in1=self.mod(2)[:, hs], op=ALU.mult)
                P.op('gpsimd', 'tensor_tensor', reads=['wtmp', rl], writes=[rx], out=X, in0=tt, in1=XL, op=ALU.add)
                P.dma('sync', self.xres[i * 128:(i + 1) * 128, :], X, reads=[rx])

            def after_h(i, Hh, rh, X, rx):
                Hp = hb[i % 2]
                rp = f'hperm{i % 2}'
                P.op('gpsimd', 'tensor_copy', reads=[rh], writes=[rp], out=Hp.rearrange("t (kc p) -> t kc p", kc=8),
                     in_=Hh.rearrange("t (p kc) -> t kc p", kc=8))
                P.dma('sync', self.hfp[i * 128:(i + 1) * 128, :], Hp, reads=[rp])

            def consumer(q, hTb, hT32b):
                hT32, r32 = hT32b
                for j in range(4):
                    i = q * 4 + j
                    for kc in range(8):
                        P.op('tensor', 'matmul', reads=[r32, 'wr'], writes=['ps4'], out=ps[4][:, 0:36],
                             lhsT=hT32[:, kc, j * 128:(j + 1) * 128], rhs=wr[:, kc, :], start=(kc == 0), stop=(kc == 7))
                    P.op('vector', 'tensor_tensor', reads=['ps4', 'rbias'], writes=['lg'], out=lg, in0=ps[4][:, 0:36],
                         in1=rb, op=ALU.add)
                    V = lambda name, **kw: P.op('vector', name, **kw)
                    gl = lg[:, 0:4]
                    el = lg[:, 4:36].rearrange("p (g e) -> p g e", g=4)
                    gmax, ngmax, gsum, pg = sm[:, 0:1], sm[:, 1:2], sm[:, 2:3], sm[:, 3:4]
                    ne1, r_, den, w1 = sm[:, 4:5], sm[:, 5:6], sm[:, 6:7], sm[:, 7:8]
                    V('reduce_max', reads=['lg'], writes=['rsm'], out=gmax, in_=gl, axis=AX.X)
                    V('tensor_scalar', reads=['rsm'], writes=['rsm'], out=ngmax, in0=gmax, scalar1=-1.0, scalar2=None,
                      op0=ALU.mult)
                    V('tensor_scalar', reads=['lg', 'rsm'], writes=['ohg'], out=ohg, in0=gl, scalar1=gmax, scalar2=None,
                      op0=ALU.is_ge)
                    P.op('scalar', 'activation', reads=['lg', 'rsm'], writes=['gexp', 'rsm'], out=gexp, in_=gl,
                         func=AF.Exp, bias=ngmax, scale=1.0, accum_out=gsum)
                    V('reciprocal', reads=['rsm'], writes=['rsm'], out=pg, in_=gsum)
                    V('tensor_tensor', reads=['lg', 'ohg'], writes=['em'], out=em, in0=el,
                      in1=ohg.unsqueeze(2).broadcast_to([128, 4, 8]), op=ALU.mult)
                    V('tensor_reduce', reads=['em'], writes=['esel'], out=es, in_=em.rearrange("p g e -> p e g"),
                      axis=AX.X, op=ALU.add)
                    V('max', reads=['esel'], writes=['top8'], out=t8, in_=es)
                    V('tensor_scalar', reads=['esel', 'top8'], writes=['sel1'], out=s1, in0=es, scalar1=t8[:, 0:1],
                      scalar2=None, op0=ALU.is_ge)
                    V('tensor_scalar', reads=['esel', 'top8'], writes=['sel2'], out=s2, in0=es, scalar1=t8[:, 1:2],
                      scalar2=None, op0=ALU.is_ge)
                    V('tensor_tensor', reads=['sel2', 'sel1'], writes=['sel2'], out=s2, in0=s2, in1=s1, op=ALU.subtract)
                    V('tensor_scalar', reads=['top8'], writes=['rsm'], out=ne1, in0=t8[:, 0:1], scalar1=-1.0,
                      scalar2=None, op0=ALU.mult)
                    P.op('scalar', 'activation', reads=['top8', 'rsm'], writes=['rsm'], out=r_, in_=t8[:, 1:2],
                         func=AF.Exp, bias=ne1, scale=1.0)
                    V('tensor_scalar', reads=['rsm'], writes=['rsm'], out=den, in0=r_, scalar1=1.0, scalar2=None,
                      op0=ALU.add)
                    V('reciprocal', reads=['rsm'], writes=['rsm'], out=den, in_=den)
                    V('tensor_tensor', reads=['rsm'], writes=['rw'], out=self.rw[:, i, 0:1], in0=den, in1=pg, op=ALU.mult)
                    V('tensor_tensor', reads=['rw', 'rsm'], writes=['rw'], out=self.rw[:, i, 1:2], in0=self.rw[:, i, 0:1],
                      in1=r_, op=ALU.mult)
                    for k, sel in enumerate((s1, s2)):
                        V('tensor_tensor', reads=['ohg', f'sel{k + 1}'], writes=['ohb'],
                          out=self.ohb[:, i, k * 32:(k + 1) * 32].rearrange("p (g e) -> p g e", g=4),
                          in0=ohg.unsqueeze(2).broadcast_to([128, 4, 8]),
                          in1=sel.unsqueeze(1).broadcast_to([128, 4, 8]), op=ALU.mult)
            self.norm_and_transpose_loop(st, None, gsc, self.mod(3), consumer, pre=pre, after_h=after_h, pre_load=pre_load)
            P.end()

    def phase_moe(self, l, st0):
        P, T, NT, NB, BLK = self.P, self.T, self.NT, self.NB, self.BLK
        ps = self.ps
        last = (l == self.L - 1)
        NR = NB * BLK
        wgv = self.w_gate.rearrange("l e (p two k4) f -> (l e p two) (k4 f)", two=2, k4=4)
        wuv = self.w_up.rearrange("l e (p two k4) f -> (l e p two) (k4 f)", two=2, k4=4)
        wdv = self.w_down.rearrange("l e (p two f2) d -> (l e p two) (f2 d)", two=2, f2=2)
        with contextlib.ExitStack() as st:
            sb = lambda n, s, dt=F32: self.sb(st, n, s, dt)
            dest_i = sb("dest_i", [128, NT, 2], I32)
            widx = sb("widx", [128, NB, 2], I32)
            with contextlib.ExitStack() as s1:
                sb1 = lambda n, s, dt=F32: self.sb(s1, n, s, dt)
                pre = sb1("pre_all", [128, NT, 64])
                tot = sb1("tot_all", [128, NT, 64])
                base = sb1("base_all", [128, NT, 64])
                cnt = sb1("cnt", [128, 64])
                tl = sb1("mtotal", [128, 32])
                md = sb1("mmod", [128, 32])
                pend = sb1("pend", [128, 32])
                off = sb1("moff", [128, 64])
                dest_f = sb1("dest_f", [128, NT, 2])
                blk0 = sb1("blk0", [128, NB])
                cmp_ = sb1("mcmp", [128, NB, 32])
                be = sb1("mbe", [128, NB])
                pidx = sb1("pidx", [128, 2])
                wf = sb1("widx_f", [128, NB, 2])
                dbg_t = sb1("rdbg_t", [128, 68])
                P.begin()
                ohb2 = self.ohb.rearrange("p i c -> p (i c)")
                f2 = lambda t: t.rearrange("p i c -> p (i c)")
                for k, c0 in enumerate(range(0, NT * 64, 512)):
                    n = min(512, NT * 64 - c0)
                    P.op('tensor', 'matmul', reads=['sutb', 'ohb'], writes=['ps0'], out=ps[0][:, 0:n], lhsT=self.sutb,
                         rhs=ohb2[:, c0:c0 + n], start=True, stop=True)
                    P.op('scalar', 'activation', reads=['ps0'], writes=['pre_all'], out=f2(pre)[:, c0:c0 + n],
                         in_=ps[0][:, 0:n], func=AF.Copy)
                    P.op('tensor', 'matmul', reads=['onesb', 'ohb'], writes=['ps1'], out=ps[1][:, 0:n], lhsT=self.onesb,
                         rhs=ohb2[:, c0:c0 + n], start=True, stop=True)
                    P.op('vector', 'tensor_copy', reads=['ps1'], writes=['tot_all'], out=f2(tot)[:, c0:c0 + n],
                         in_=ps[1][:, 0:n])
                V = lambda name, **kw: P.op('vector', name, **kw)
                P.op('gpsimd', 'memset', writes=['base_all'], ap=base[:, 0, :], constant=0.0)
                for i in range(1, NT):
                    V('tensor_tensor', reads=['base_all', 'tot_all'], writes=['base_all'], out=base[:, i, :],
                      in0=base[:, i - 1, :], in1=tot[:, i - 1, :], op=ALU.add)
                V('tensor_tensor', reads=['base_all', 'tot_all'], writes=['cnt'], out=cnt, in0=base[:, NT - 1, :],
                  in1=tot[:, NT - 1, :], op=ALU.add)
                V('tensor_tensor', reads=['cnt'], writes=['mtotal'], out=tl, in0=cnt[:, 0:32], in1=cnt[:, 32:64],
                  op=ALU.add)
                P.op('gpsimd', 'iota', writes=['blk0'], out=blk0, pattern=[[BLK, NB]], base=0, channel_multiplier=0,
                     allow_small_or_imprecise_dtypes=True)
                V('tensor_tensor', reads=['mtotal', 'blk0'], writes=['mcmp'], out=cmp_.rearrange("p b e -> p (b e)").rearrange("p (e b) -> p e b", e=32),
                  in0=blk0.unsqueeze(1).broadcast_to([128, 32, NB]), in1=tl.unsqueeze(2).broadcast_to([128, 32, NB]),
                  op=ALU.is_lt)
                V('tensor_reduce', reads=['mcmp'], writes=['mtotal'], out=tl,
                  in_=cmp_.rearrange("p b e -> p (b e)").rearrange("p (e b) -> p e b", e=32), axis=AX.X, op=ALU.add)
                V('tensor_scalar', reads=['mtotal'], writes=['mtotal'], out=tl, in0=tl, scalar1=float(BLK), scalar2=None,
                  op0=ALU.mult)
                V('tensor_tensor_scan', reads=['mtotal', 'ones'], writes=['pend'], out=pend,
                  data0=self.ones[:, 0:32], data1=tl, initial=0.0, op0=ALU.mult, op1=ALU.add)
                V('tensor_tensor', reads=['pend', 'mtotal'], writes=['moff'], out=off[:, 0:32], in0=pend, in1=tl,
                  op=ALU.subtract)
                V('tensor_tensor', reads=['moff', 'cnt'], writes=['moff'], out=off[:, 32:64], in0=off[:, 0:32],
                  in1=cnt[:, 0:32], op=ALU.add)
                V('tensor_tensor', reads=['pre_all', 'base_all'], writes=['pre_all'], out=pre, in0=pre, in1=base,
                  op=ALU.add)
                V('tensor_tensor', reads=['pre_all', 'moff'], writes=['pre_all'], out=pre, in0=pre,
                  in1=off.unsqueeze(1).broadcast_to([128, NT, 64]), op=ALU.add)
                V('tensor_tensor', reads=['pre_all', 'ohb'], writes=['pre_all'], out=pre, in0=pre, in1=self.ohb,
                  op=ALU.mult)
                V('tensor_reduce', reads=['pre_all'], writes=['dest_f'], out=dest_f,
                  in_=pre.rearrange("p i (k e) -> p i k e", k=2), axis=AX.X, op=ALU.add)
                V('tensor_copy', reads=['dest_f'], writes=['dest_i'], out=dest_i, in_=dest_f)
                V('tensor_tensor', reads=['pend', 'blk0'], writes=['mcmp'], out=cmp_,
                  in0=pend.unsqueeze(1).broadcast_to([128, NB, 32]), in1=blk0.unsqueeze(2).broadcast_to([128, NB, 32]),
                  op=ALU.is_le)
                V('tensor_reduce', reads=['mcmp'], writes=['mbe'], out=be, in_=cmp_, axis=AX.X, op=ALU.add)
                V('tensor_scalar', reads=['mbe'], writes=['mbe'], out=be, in0=be, scalar1=256.0, scalar2=None,
                  op0=ALU.mult)
                P.op('gpsimd', 'iota', writes=['pidx'], out=pidx, pattern=[[1, 2]], base=l * NE * 256, channel_multiplier=2,
                     allow_small_or_imprecise_dtypes=True)
                V('tensor_tensor', reads=['mbe', 'pidx'], writes=['widx_f'], out=wf,
                  in0=be.unsqueeze(2).broadcast_to([128, NB, 2]), in1=pidx.unsqueeze(1).broadcast_to([128, NB, 2]),
                  op=ALU.add)
                V('tensor_copy', reads=['widx_f'], writes=['widx'], out=widx, in_=wf)
                if 'rdbg' in self.dbg:
                    for i in range(NT):
                        V('tensor_copy', reads=['ohb'], writes=['rdbg_t'], out=dbg_t[:, 0:64], in_=self.ohb[:, i, :])
                        V('tensor_copy', reads=['rw'], writes=['rdbg_t'], out=dbg_t[:, 64:66], in_=self.rw[:, i, :])
                        V('tensor_copy', reads=['dest_f'], writes=['rdbg_t'], out=dbg_t[:, 66:68], in_=dest_f[:, i, :])
                        P.dma('sync', self.rdbg[i * 128:(i + 1) * 128, :], dbg_t, reads=['rdbg_t'])
                P.end()
            if '1' in self.opt:
                return
            with contextlib.ExitStack() as s2:
                hrow = [self.sb(s2, f"hrow{i}", [128, D], BF16) for i in range(3)]
                P.begin()
                P.raw('gpsimd', 'reg_mov', self.reg_rows, NR - 1)
                for i in range(NT):
                    Hr = hrow[i % 3]
                    rr = f'hrow{i % 3}'
                    P.dma('sync', Hr, self.hfp[i * 128:(i + 1) * 128, :], writes=[rr])
                    for k in range(2):
                        P.dma('gpsimd', None, None, reads=[rr, 'dest_i'], writes=['xs'],
                              fn=('indirect_dma_start', dict(
                                  out=self.xs, out_offset=bass.IndirectOffsetOnAxis(ap=dest_i[:, i, k:k + 1], axis=0),
                                  in_=Hr, in_offset=None, bounds_check=self.reg_rows, oob_is_err=False)))
                P.end()
            if '2' in self.opt:
                return
            with contextlib.ExitStack() as s3:
                sb3 = lambda n, s, dt=F32: self.sb(s3, n, s, dt)
                Wg = [sb3(f"Wg{i}", [128, 8, 512], BF16) for i in range(2)]
                Wu = [sb3(f"Wu{i}", [128, 8, 512], BF16) for i in range(2)]
                Wd = [sb3(f"Wd{i}", [128, 4, 1024], BF16) for i in range(2)]
                xsT = [sb3(f"xsT{i}", [128, 8, 512], BF16) for i in range(2)]
                hid = [sb3(f"hid{i}", [128, 4, 512], BF16) for i in range(2)]
                sg = [sb3(f"sg{i}", [128, 512]) for i in range(2)]
                yb = [sb3(f"yb{i}", [128, D]) for i in range(2)]
                P.begin()
                P.raw('gpsimd', 'reg_mov', self.reg_w, (l + 1) * NE * 256 - 1)
                n_y = [0]

                def load_blk(b):
                    a = b % 2
                    for (Wt, view, nm) in ((Wg[a], wgv, f'Wg{a}'), (Wu[a], wuv, f'Wu{a}'), (Wd[a], wdv, f'Wd{a}')):
                        flat = Wt.rearrange("p a f -> p (a f)")
                        for hf in range(0 if 'G' in self.opt else 2):
                            P.dma('gpsimd', None, None, reads=['widx'], writes=[nm],
                                  fn=('indirect_dma_start', dict(
                                      out=flat[:, hf * 2048:(hf + 1) * 2048], out_offset=None, in_=view,
                                      in_offset=bass.IndirectOffsetOnAxis(ap=widx[:, b, hf:hf + 1], axis=0),
                                      bounds_check=self.reg_w, oob_is_err=False)))
                    X = xsT[a]
                    for kc in range(8):
                        P.dma('sync', None, None, reads=['xs'], writes=[f'xsT{a}'],
                              fn=('dma_start_transpose', dict(
                                  out=X[:, kc, :], in_=self.xs[b * BLK:(b + 1) * BLK, kc * 128:(kc + 1) * 128])))

                def compute_blk(b):
                    a = b % 2
                    X = xsT[a]
                    rxs = f'xsT{a}'
                    if 'C' in self.opt:
                        return
                    Hd = hid[a]
                    rh = f'hid{a}'
                    wg4 = Wg[a].rearrange("p kc (m four) -> p kc four m", four=4)
                    wu4 = Wu[a].rearrange("p kc (m four) -> p kc four m", four=4)
                    for fc in range(4):
                        pg_, pu_ = 2 + (fc % 2) * 2, 3 + (fc % 2) * 2
                        for kc in range(8):
                            P.op('tensor', 'matmul', reads=[f'Wg{a}', rxs], writes=[f'ps{pg_}'], out=ps[pg_],
                                 lhsT=wg4[:, kc, fc, :], rhs=X[:, kc, :], start=(kc == 0), stop=(kc == 7))
                        for kc in range(8):
                            P.op('tensor', 'matmul', reads=[f'Wu{a}', rxs], writes=[f'ps{pu_}'], out=ps[pu_],
                                 lhsT=wu4[:, kc, fc, :], rhs=X[:, kc, :], start=(kc == 0), stop=(kc == 7))
                        S = sg[fc % 2]
                        P.op('scalar', 'activation', reads=[f'ps{pg_}'], writes=[f'sg{fc % 2}'], out=S, in_=ps[pg_],
                             func=AF.Silu)
                        P.op('vector', 'tensor_tensor', reads=[f'sg{fc % 2}', f'ps{pu_}'], writes=[rh], out=Hd[:, fc, :],
                             in0=S, in1=ps[pu_], op=ALU.mult)
                    for rt in range(4):
                        n_y[0] += 1
                        Y = yb[n_y[0] % 2]
                        ry = f'yb{n_y[0] % 2}'
                        for half in range(2):
                            pb = 6 + half
                            for fc in range(4):
                                P.op('tensor', 'matmul', reads=[rh, f'Wd{a}'], writes=[f'ps{pb}'], out=ps[pb],
                                     lhsT=Hd[:, fc, rt * 128:(rt + 1) * 128], rhs=Wd[a][:, fc, half * 512:(half + 1) * 512],
                                     start=(fc == 0), stop=(fc == 3))
                            self.evac(half, Y[:, half * 512:(half + 1) * 512], ps[pb], [f'ps{pb}'], [ry])
                        r0 = b * BLK + rt * 128
                        P.dma('sync', self.ys[r0:r0 + 128, :], Y, reads=[ry], writes=['ys'])

                load_blk(0)
                for b in range(NB):
                    if b + 1 < NB:
                        load_blk(b + 1)
                    compute_blk(b)
                P.end()
            if '3' in self.opt:
                return
            with contextlib.ExitStack() as s4:
                sb4 = lambda n, s, dt=F32: self.sb(s4, n, s, dt)
                y1 = [sb4(f"y1_{i}", [128, D]) for i in range(2)]
                y2 = [sb4(f"y2_{i}", [128, D]) for i in range(2)]
                xr = [sb4(f"xr{i}", [128, D]) for i in range(2)]
                fgb = sb4("fgb", [128, D])
                fsq = sb4("fsq", [128, D])
                fss = sb4("fss", [128, 1])
                frs = sb4("frs", [128, 1])
                P.begin()
                P.raw('gpsimd', 'reg_mov', self.reg_rows, NR - 1)
                if last:
                    P.dma('sync', fgb, self.final_g.partition_broadcast(128), writes=['fgb'])
                def load_tile(i):
                    a = i % 2
                    rows = slice(i * 128, (i + 1) * 128)
                    P.dma('sync', xr[a], self.xres[rows, :], writes=[f'xr{a}'])
                    for (Yk, rk, k) in ((y1[a], f'y1_{a}', 0), (y2[a], f'y2_{a}', 1)):
                        P.dma('gpsimd', None, None, reads=['dest_i'], writes=[rk],
                              fn=('indirect_dma_start', dict(
                                  out=Yk, out_offset=None, in_=self.ys,
                                  in_offset=bass.IndirectOffsetOnAxis(ap=dest_i[:, i, k:k + 1], axis=0),
                                  bounds_check=self.reg_rows, oob_is_err=False)))
                load_tile(0)
                for i in range(NT):
                    a = i % 2
                    Y1, Y2, X = y1[a], y2[a], xr[a]
                    r1, r2, rx = f'y1_{a}', f'y2_{a}', f'xr{a}'
                    rows = slice(i * 128, (i + 1) * 128)
                    if i + 1 < NT:
                        load_tile(i + 1)
                    P.op('vector', 'tensor_scalar', reads=[r1, 'rw'], writes=[r1], out=Y1, in0=Y1,
                         scalar1=self.rw[:, i, 0:1], scalar2=None, op0=ALU.mult)
                    P.op('vector', 'scalar_tensor_tensor', reads=[r1, r2, 'rw'], writes=[r1], out=Y1, in0=Y2,
                         scalar=self.rw[:, i, 1:2], in1=Y1, op0=ALU.mult, op1=ALU.add)
                    P.op('gpsimd', 'tensor_tensor', reads=[r1, 'modb'], writes=[r1], out=Y1, in0=Y1, in1=self.mod(5),
                         op=ALU.mult)
                    P.op('gpsimd', 'tensor_tensor', reads=[r1, rx], writes=[rx], out=X, in0=X, in1=Y1, op=ALU.add)
                    if not last:
                        P.dma('sync', self.xres[rows, :], X, reads=[rx])
                    else:
                        P.op('scalar', 'activation', reads=[rx], writes=['fsq', 'fss'], out=fsq, in_=X, func=AF.Square,
                             accum_out=fss)
                        self.rstd_ops(fss, frs, D, ['fss'], ['frs'])
                        P.op('vector', 'scalar_tensor_tensor', reads=[rx, 'frs', 'fgb'], writes=[r2], out=Y2, in0=X,
                             scalar=frs, in1=fgb, op0=ALU.mult, op1=ALU.mult)
                        P.dma('sync', self.out[rows, :], Y2, reads=[r2])
                P.end()

    def build_layer(self, l):
        with contextlib.ExitStack() as st:
            ph = self.phases
            self.phase_ada(l, st)
            if 'i' in ph:
                self.phase_inproj(l, st)
            if 'c' in ph and 'f' in ph:
                self.phase_attn(l, conv_inside=True)
            elif 'c' in ph:
                self.phase_conv(l)
            elif 'f' in ph:
                self.phase_attn(l)
            if 's' in ph:
                self.phase_ssd(l)
            if 'o' in ph:
                self.phase_wout_router(l, st)
            if 'm' in ph and self.moe:
                self.phase_moe(l, st)

    def zero_xs(self):
        P = self.P
        NR = self.NB * self.BLK
        with contextlib.ExitStack() as st:
            z = self.sb(st, "zeros", [128, 8192], BF16)
            P.begin()
            P.op('gpsimd', 'memset', writes=['zeros'], ap=z, constant=0.0)
            xv = self.xs.rearrange("(a p r) d -> a p (r d)", p=128, r=8)
            for a in range(NR // 1024):
                P.dma('sync', xv[a], z, reads=['zeros'])
            P.end()

    def build(self):
        if self.moe:
            self.zero_xs()
        for l in range(self.L):
            self.build_layer(l)
        return self.nc


def prep_core_inputs(inp, b, T, L):
    f = lambda a: np.ascontiguousarray(a, dtype=np.float32)
    m = {}
    m["x"] = f(inp["x"][b, :T])
    m["c"] = f(inp["c"][b].reshape(8, 128).T)
    for k in ("ada_w", "ada_b", "norm_mix_g", "w_in", "ssd_dt_bias", "ssd_a_log", "ssd_d", "ssd_norm_g",
              "w_out", "norm_ffn_g", "w_router_group", "b_router_group", "w_router_expert", "b_router_expert",
              "w_gate", "w_up", "w_down"):
        m[k] = f(inp[k][:L])
    m["final_norm_g"] = f(inp["final_norm_g"])
    m["convw_fm"] = f(inp["ssd_conv_w"][:L].reshape(L, 4, 8, 128).transpose(0, 3, 2, 1))
    m["convb_fm"] = f(inp["ssd_conv_b"][:L].reshape(L, 8, 128).transpose(0, 2, 1))
    m["fox_f_bias_fm"] = f(inp["fox_f_bias"][:L].reshape(L, 4, 1))
    m["fox_g_fm"] = f(inp["fox_norm_g"][:L].reshape(L, 2, 128).transpose(0, 2, 1))
    m["cmw_fm"] = f(inp["cm_conv_w"][:L].reshape(L, 31, 2, 128).transpose(0, 3, 2, 1))
    m["cmb_fm"] = f(inp["cm_conv_b"][:L].reshape(L, 2, 128).transpose(0, 2, 1))
    m["cmg_fm"] = f(inp["cm_ln_g"][:L].reshape(L, 2, 128).transpose(0, 2, 1))
    m["cmbeta_fm"] = f(inp["cm_ln_b"][:L].reshape(L, 2, 128).transpose(0, 2, 1))
    return m


_CACHE = {}
T_FULL, L_FULL, B_FULL = 8192, 4, 4


def kernel(**inputs):
    if 'nc' not in _CACHE:
        bd = Builder(T_FULL, L_FULL)
        _CACHE['nc'] = bd.build()
        _CACHE['names'] = set(bd.ins)
    nc = _CACHE['nc']
    inp = {k: np.asarray(v) for k, v in inputs.items()}
    maps = []
    for b in range(B_FULL):
        m = prep_core_inputs(inp, b, T_FULL, L_FULL)
        maps.append({k: v for k, v in m.items() if k in _CACHE['names']})
    res = run_bass_kernel_spmd(nc, maps, core_ids=list(range(B_FULL)))
    out = np.stack([np.asarray(r["out"], dtype=np.float32) for r in res.results], axis=0)
    return out
```

```python
import contextlib
import numpy as np
import concourse.bass as bass
import concourse.mybir as mybir
from concourse.bass_utils import run_bass_kernel_spmd

F32 = mybir.dt.float32
BF16 = mybir.dt.bfloat16
I32 = mybir.dt.int32
AF = mybir.ActivationFunctionType
ALU = mybir.AluOpType
AX = mybir.AxisListType

ENGS = ('tensor', 'vector', 'scalar', 'gpsimd', 'sync')
CENGS = ('tensor', 'vector', 'scalar', 'gpsimd')
NDS = 10

D = 1024
D_IN = 2828
EPS = 1e-6
NE = 32
DE = 512


class Prog:
    def __init__(self, nc, same_engine_sync=True):
        self.nc = nc
        self.same = same_engine_sync
        self.esem = {e: nc.alloc_semaphore(name=f"es_{e}") for e in CENGS}
        self.dsem = {e: [nc.alloc_semaphore(name=f"ds_{e}_{i}") for i in range(NDS)]
                     for e in ('sync', 'scalar', 'gpsimd')}
        self._reset()
        self.q = None

    def _reset(self):
        keep = getattr(self, 'dcnt', {}).get('gpsimd', [0] * NDS)
        self.ecnt = {e: 0 for e in CENGS}
        self.dcnt = {e: [0] * NDS for e in self.dsem}
        self.dcnt['gpsimd'] = list(keep)
        self.dnext = {e: 0 for e in self.dsem}
        self.known = {e: {('d', 'gpsimd', i): keep[i] for i in range(NDS)} for e in ENGS}
        self.res_w = {}
        self.res_r = {}

    def semof(self, key):
        if key[0] == 'e':
            return self.esem[key[1]]
        return self.dsem[key[1]][key[2]]

    def begin(self):
        self.q = {e: [] for e in ENGS}

    def _deps(self, reads, writes):
        deps = []
        for r in reads:
            t = self.res_w.get(r)
            if t is not None:
                deps.append(t)
        for w in writes:
            t = self.res_w.get(w)
            if t is not None:
                deps.append(t)
            deps.extend(self.res_r.get(w, {}).items())
        return deps

    def _record(self, tok, reads, writes):
        for r in reads:
            d = self.res_r.setdefault(r, {})
            if d.get(tok[0], 0) < tok[1]:
                d[tok[0]] = tok[1]
        for w in writes:
            self.res_w[w] = tok
            self.res_r[w] = {}

    def _waits(self, eng, deps):
        waits = []
        best = {}
        for (key, c) in deps:
            if best.get(key, 0) < c:
                best[key] = c
        for key, c in best.items():
            if key == ('e', 'tensor') and eng == 'tensor':
                continue
            if (not self.same) and key == ('e', eng):
                continue
            if self.known[eng].get(key, 0) >= c:
                continue
            self.known[eng][key] = c
            waits.append((self.semof(key), c))
        return waits

    def op(self, eng, name, reads=(), writes=(), **kw):
        writes = list(writes) + [r for r in reads if r.startswith('ps') and r not in writes]
        waits = self._waits(eng, self._deps(reads, writes))
        self.ecnt[eng] += 1
        tok = (('e', eng), self.ecnt[eng])
        sem = self.esem[eng]

        def emit(e, name=name, kw=kw, waits=waits, sem=sem):
            for (s, c) in waits:
                e.wait_ge(s, c)
            getattr(e, name)(**kw).then_inc(sem, 1)
        self.q[eng].append(emit)
        self._record(tok, reads, writes)
        return tok

    def raw(self, eng, name, *args, **kw):
        self.q[eng].append(lambda e: getattr(e, name)(*args, **kw))

    def dma(self, eng, out, in_, reads=(), writes=(), fn=None, **kw):
        deps = self._deps(reads, writes)
        idx = self.dnext[eng]
        self.dnext[eng] = (idx + 1) % NDS
        key = ('d', eng, idx)
        prev = self.dcnt[eng][idx]
        if prev:
            deps.append((key, prev))
        waits = self._waits(eng, deps)
        self.dcnt[eng][idx] += 16
        tok = (key, self.dcnt[eng][idx])
        sem = self.dsem[eng][idx]

        def emit(e, waits=waits, sem=sem):
            for (s, c) in waits:
                e.wait_ge(s, c)
            if fn is not None:
                try:
                    ins = getattr(e, fn[0])(**fn[1])
                except Exception:
                    print("DMA builder failed:", fn[0], {k: (v.shape if hasattr(v, 'shape') else v) for k, v in fn[1].items()})
                    raise
                ins.then_inc(sem, 16)
            else:
                e.dma_start(out=out, in_=in_, **kw).then_inc(sem, 16)
        self.q[eng].append(emit)
        self._record(tok, reads, writes)
        return tok

    def end(self):
        nc = self.nc
        fin = []
        for e in self.dsem:
            for i in range(NDS):
                c = self.dcnt[e][i]
                if c and self.known['sync'].get(('d', e, i), 0) < c:
                    fin.append((self.dsem[e][i], c))

        def drain(e, fin=fin):
            for (s, c) in fin:
                e.wait_ge(s, c)
        self.q['sync'].append(drain)
        with nc.Block() as block:
            for en in ENGS:
                ops = self.q[en]
                if not ops:
                    continue

                def body(e, ops=ops):
                    for o in ops:
                        o(e)
                getattr(block, en)(body)
        allsems = list(self.esem.values()) + [s for e in self.dsem if e != 'gpsimd' for s in self.dsem[e]]
        with nc.Block() as block:
            def clr(e):
                for s in allsems:
                    e.sem_clear(s)
            block.sync(clr)
        self._reset()
        self.q = None


class Builder:
    def __init__(self, T, L, dbg=(), moe=True, phases='aicfsom', opt=''):
        self.opt = opt
        self.T, self.L = T, L
        self.moe = moe
        self.phases = phases
        self.NT = T // 128
        self.NQ = T // 512
        self.dbg = set(dbg)
        nc = self.nc = bass.Bass("TRN2", target_bir_lowering=False)
        self.P = Prog(nc, same_engine_sync=('S' not in self.opt))
        self.ins = {}
        self.outs = {}
        self._uid = 0
        self.declare_io()
        self.consts()

    def din(self, name, shape, dt=F32):
        t = self.nc.dram_tensor(name, list(shape), dt, kind="ExternalInput").ap()
        self.ins[name] = t
        return t

    def dscr(self, name, shape, dt=F32):
        kind = "ExternalOutput" if name in self.dbg else "Internal"
        t = self.nc.dram_tensor(name, list(shape), dt, kind=kind).ap()
        if kind == "ExternalOutput":
            self.outs[name] = t
        return t

    def sb(self, st, name, shape, dt=F32):
        self._uid += 1
        return st.enter_context(self.nc.sbuf_tensor(f"{name}_{self._uid}", list(shape), dt)).ap()

    def declare_io(self):
        T, L = self.T, self.L
        d = self.din
        self.x_in = d("x", [T, D])
        self.c_in = d("c", [128, 8])
        self.ada_w = d("ada_w", [L, D, 6 * D])
        self.ada_b = d("ada_b", [L, 6 * D])
        self.norm_mix_g = d("norm_mix_g", [L, D])
        self.w_in = d("w_in", [L, D, D_IN])
        self.convw = d("convw_fm", [L, 128, 8, 4])
        self.convb = d("convb_fm", [L, 128, 8])
        self.dt_bias = d("ssd_dt_bias", [L, 8])
        self.a_log = d("ssd_a_log", [L, 8])
        self.ssd_d = d("ssd_d", [L, 8])
        self.ssd_norm_g = d("ssd_norm_g", [L, 512])
        self.f_bias = d("fox_f_bias_fm", [L, 4, 1])
        self.fox_g = d("fox_g_fm", [L, 128, 2])
        self.cmw = d("cmw_fm", [L, 128, 2, 31])
        self.cmb = d("cmb_fm", [L, 128, 2])
        self.cmg = d("cmg_fm", [L, 128, 2])
        self.cmbeta = d("cmbeta_fm", [L, 128, 2])
        self.w_out = d("w_out", [L, D, D])
        self.norm_ffn_g = d("norm_ffn_g", [L, D])
        self.w_rg = d("w_router_group", [L, D, 4])
        self.b_rg = d("b_router_group", [L, 4])
        self.w_re = d("w_router_expert", [L, D, NE])
        self.b_re = d("b_router_expert", [L, NE])
        if self.moe:
            self.w_gate = d("w_gate", [L, NE, D, DE])
            self.w_up = d("w_up", [L, NE, D, DE])
            self.w_down = d("w_down", [L, NE, DE, D])
        self.final_g = d("final_norm_g", [D])
        self.out = self.nc.dram_tensor("out", [T, D], F32, kind="ExternalOutput").ap()
        self.outs["out"] = self.out
        s = self.dscr
        self.xres = s("xres", [T, D])
        self.zs = s("zs", [T, 512])
        self.xbcT = s("xbcT", [1024, T])
        self.dtr = s("dtr", [T, 8])
        self.flT = s("flT", [4, T])
        self.qT = s("qT", [256, T], BF16)
        self.kT = s("kT", [256, T], BF16)
        self.vtok = s("vtok", [T, 256], BF16)
        self.gaT = s("gaT", [256, T])
        self.gbT = s("gbT", [256, T])
        self.yT = s("yT", [1024, T], BF16)
        self.attT = s("attT", [256, T])
        self.cs = s("cs", [4, 6, T], BF16)
        self.hfp = s("hfp", [T, D], BF16)
        self.BLK = 512
        self.NB = (2 * T) // self.BLK + NE
        self.xs = s("xs", [self.NB * self.BLK, D], BF16)
        self.ys = s("ys", [self.NB * self.BLK, D])
        self.rdbg = s("rdbg", [T, 68])

    def consts(self):
        nc, P = self.nc, self.P
        a = lambda n, s, dt=F32: nc.alloc_sbuf_tensor(n, list(s), dt).ap()
        self.ident = a("ident", [128, 128])
        self.identb = a("identb", [128, 128], BF16)
        self.tri = a("tri", [128, 128])
        self.ustr = a("ustr", [128, 128])
        self.ones = a("ones", [128, 128])
        self.onesb = a("onesb", [128, 128], BF16)
        self.epsb = a("epsb", [128, 1])
        self.sutb = a("sutb", [128, 128], BF16)
        self.ps = [nc.alloc_psum_tensor(f"ps{i}", [128, 512], F32).ap() for i in range(8)]
        self.reg_rows = nc.gpsimd.alloc_register("bc_rows")
        self.reg_w = nc.gpsimd.alloc_register("bc_w")
        P.begin()
        g = 'gpsimd'
        P.op(g, 'memset', writes=['epsb'], ap=self.epsb, constant=EPS)
        P.op(g, 'memset', writes=['ones'], ap=self.ones, constant=1.0)
        P.op(g, 'memset', writes=['onesb'], ap=self.onesb, constant=1.0)
        P.op(g, 'memset', writes=['ident'], ap=self.ident, constant=1.0)
        P.op(g, 'affine_select', reads=['ident'], writes=['ident'], out=self.ident, in_=self.ident,
             pattern=[[-1, 128]], compare_op=ALU.is_equal, fill=0.0, base=0, channel_multiplier=1)
        P.op(g, 'tensor_copy', reads=['ident'], writes=['identb'], out=self.identb, in_=self.ident)
        P.op(g, 'memset', writes=['tri'], ap=self.tri, constant=1.0)
        P.op(g, 'affine_select', reads=['tri'], writes=['tri'], out=self.tri, in_=self.tri,
             pattern=[[1, 128]], compare_op=ALU.is_ge, fill=0.0, base=0, channel_multiplier=-1)
        P.op(g, 'tensor_tensor', reads=['tri', 'ident'], writes=['sutb'], out=self.sutb, in0=self.tri, in1=self.ident,
             op=ALU.subtract)
        P.op(g, 'memset', writes=['ustr'], ap=self.ustr, constant=1.0)
        P.op(g, 'affine_select', reads=['ustr'], writes=['ustr'], out=self.ustr, in_=self.ustr,
             pattern=[[-1, 128]], compare_op=ALU.is_gt, fill=0.0, base=0, channel_multiplier=1)
        P.end()

    def phase_ada(self, l, st):
        nc, P = self.nc, self.P
        self.modb = self.sb(st, "modb", [128, 6 * D])
        with contextlib.ExitStack() as s2:
            cs = self.sb(s2, "c_s", [128, 8])
            cb = self.sb(s2, "c_b", [128, 8, 128])
            adab = self.sb(s2, "adab", [128, 6 * D])
            wch = [self.sb(s2, f"adaw{i}", [128, 8, 512]) for i in range(2)]
            P.begin()
            P.dma('sync', cs, self.c_in, writes=['c_s'])
            P.dma('sync', adab, self.ada_b[l].partition_broadcast(128), writes=['adab'])
            P.op('scalar', 'activation', reads=['c_s'], writes=['c_s'], out=cs, in_=cs, func=AF.Silu)
            P.op('vector', 'tensor_copy', reads=['c_s'], writes=['c_b'], out=cb,
                 in_=cs.unsqueeze(2).broadcast_to([128, 8, 128]))
            for j in range(12):
                w = wch[j % 2]
                wr = f'adaw{j % 2}'
                P.dma('sync', w,
                      self.ada_w[l][:, j * 512:(j + 1) * 512].rearrange("(kc p) n -> p kc n", p=128),
                      writes=[wr])
                pt = self.ps[j % 2]
                for kc in range(8):
                    P.op('tensor', 'matmul', reads=['c_b', wr], writes=[f'ps{j % 2}'], out=pt,
                         lhsT=cb[:, kc, :], rhs=w[:, kc, :], start=(kc == 0), stop=(kc == 7))
                P.op('vector', 'tensor_tensor', reads=[f'ps{j % 2}', 'adab'], writes=['modb'],
                     out=self.modb[:, j * 512:(j + 1) * 512], in0=pt, in1=adab[:, j * 512:(j + 1) * 512], op=ALU.add)
            P.end()

    def mod(self, i):
        return self.modb[:, i * D:(i + 1) * D]

    def load_cast(self, st, dst, src, res, width):
        P = self.P
        if getattr(self, '_stg_owner', None) is not st:
            self._stg = [self.sb(st, f"stg{i}", [128, 2048]) for i in range(2)]
            self._stg_owner = st
            self._stg_n = 0
        for c0 in range(0, width, 2048):
            n = min(2048, width - c0)
            k = self._stg_n
            self._stg_n += 1
            S = self._stg[k % 2]
            rs = f'stg{k % 2}'
            P.dma('sync', S[:, 0:n], src[:, c0:c0 + n], writes=[rs])
            P.op('gpsimd' if k % 2 else 'vector', 'tensor_copy', reads=[rs], writes=[res], out=dst[:, c0:c0 + n],
                 in_=S[:, 0:n])

    def rstd_ops(self, ss, rstd, n, rd, wr):
        P = self.P
        P.op('scalar', 'activation', reads=rd, writes=wr, out=rstd, in_=ss, func=AF.Ln,
             bias=self.epsb[:ss.shape[0], :], scale=1.0 / n)
        P.op('scalar', 'activation', reads=wr, writes=wr, out=rstd, in_=rstd, func=AF.Exp, scale=-0.5)

    def phase_inproj(self, l, st0):
        nc, P, T = self.nc, self.P, self.T
        src = self.x_in if l == 0 else self.xres
        self._ip_bufs = None
        with contextlib.ExitStack() as st:
            sb = lambda n, s, dt=F32: self.sb(st, n, s, dt)
            wz = sb("wz", [128, 8, 512], BF16)
            wx = sb("wx", [128, 8, 1024], BF16)
            wqk = sb("wqk", [128, 8, 512], BF16)
            wv = sb("wv", [128, 8, 256], BF16)
            wg = sb("wg", [128, 8, 512], BF16)
            wdt = sb("wdt", [128, 8, 8])
            wf = sb("wf", [128, 8, 4])
            gsc = sb("gsc", [128, D])
            W = self.w_in[l].rearrange("(kc p) n -> p kc n", p=128)
            P.begin()
            for kc in range(8):
                self.load_cast(st, wz[:, kc, :], W[:, kc, 0:512], 'wz', 512)
                self.load_cast(st, wx[:, kc, :], W[:, kc, 512:1536], 'wx', 1024)
                self.load_cast(st, wqk[:, kc, :], W[:, kc, 1544:2056], 'wqk', 512)
                self.load_cast(st, wv[:, kc, :], W[:, kc, 2056:2312], 'wv', 256)
                self.load_cast(st, wg[:, kc, :], W[:, kc, 2316:2828], 'wg', 512)
            P.dma('sync', wdt, W[:, :, 1536:1544], writes=['wdt'])
            P.dma('sync', wf, W[:, :, 2312:2316], writes=['wf'])
            P.dma('sync', gsc, self.norm_mix_g[l].partition_broadcast(128), writes=['gsc'])
            P.op('vector', 'scalar_tensor_tensor', reads=['gsc', 'modb'], writes=['gsc'], out=gsc,
                 in0=self.mod(1), scalar=1.0, in1=gsc, op0=ALU.add, op1=ALU.mult)
            self.norm_and_transpose_loop(st, src, gsc, self.mod(0), consumer=lambda q, hT, hT32: self.inproj_chunk(
                q, hT, hT32, wz, wx, wqk, wv, wg, wdt, wf, st))
            P.end()

    def norm_and_transpose_loop(self, st, src, gsc, shift, consumer, pre=None, after_h=None, pre_load=None):
        P = self.P
        sb = lambda n, s, dt=F32: self.sb(st, n, s, dt)
        NXB = 4
        xt = [sb(f"xt{i}", [128, D]) for i in range(NXB)]
        ht = [sb(f"ht{i}", [128, D]) for i in range(2)]
        sq = sb("sq", [128, D])
        ss = [sb(f"ss{i}", [128, 1]) for i in range(2)]
        rs = [sb(f"rs{i}", [128, 1]) for i in range(2)]
        hT32 = [sb(f"hT32_{i}", [128, 8, 512]) for i in range(2)]
        hT = [sb(f"hT_{i}", [128, 8, 512], BF16) for i in range(2)]
        PF = 2

        def load(i):
            if i >= self.NT:
                return
            if pre_load is not None:
                pre_load(i)
            elif pre is None:
                P.dma('sync', xt[i % NXB], src[i * 128:(i + 1) * 128, :], writes=[f'xt{i % NXB}'])
        for i in range(PF):
            load(i)
        for q in range(self.NQ):
            b = q % 2
            for j in range(4):
                i = q * 4 + j
                a = i % 2
                X, H = xt[i % NXB], ht[a]
                rx, rh = f'xt{i % NXB}', f'ht{a}'
                load(i + PF)
                if pre is not None:
                    pre(i, X, rx)
                P.op('scalar', 'activation', reads=[rx], writes=['sq', f'ss{a}'], out=sq, in_=X, func=AF.Square,
                     accum_out=ss[a])
                self.rstd_ops(ss[a], rs[a], D, [f'ss{a}'], [f'rs{a}'])
                P.op('vector', 'scalar_tensor_tensor', reads=[rx, f'rs{a}', 'gsc'], writes=[rh], out=H, in0=X,
                     scalar=rs[a], in1=gsc, op0=ALU.mult, op1=ALU.mult)
                P.op('gpsimd', 'tensor_tensor', reads=[rh, 'modb'], writes=[rh], out=H, in0=H, in1=shift, op=ALU.add)
                if after_h is not None:
                    after_h(i, H, rh, X, rx)
                for half in range(2):
                    pt = self.ps[half]
                    for k4 in range(4):
                        kc = half * 4 + k4
                        P.op('tensor', 'transpose', reads=[rh, 'ident'], writes=[f'ps{half}'],
                             out=pt[:, k4 * 128:(k4 + 1) * 128], in_=H[:, kc * 128:(kc + 1) * 128], identity=self.ident)
                    dst = hT32[b][:, half * 4:(half + 1) * 4, j * 128:(j + 1) * 128]
                    P.op('scalar', 'activation', reads=[f'ps{half}'], writes=[f'hT32_{b}'], out=dst,
                         in_=pt.rearrange("p (k t) -> p k t", k=4), func=AF.Copy)
                P.op('gpsimd', 'tensor_copy', reads=[f'hT32_{b}'], writes=[f'hT_{b}'],
                     out=hT[b][:, :, j * 128:(j + 1) * 128], in_=hT32[b][:, :, j * 128:(j + 1) * 128])
            consumer(q, (hT[b], f'hT_{b}'), (hT32[b], f'hT32_{b}'))

    def evac(self, k, out, in_, reads, writes, scale=None):
        P = self.P
        if k % 2 == 0:
            if scale is None:
                P.op('scalar', 'activation', reads=reads, writes=writes, out=out, in_=in_, func=AF.Copy)
            else:
                P.op('scalar', 'activation', reads=reads, writes=writes, out=out, in_=in_, func=AF.Copy, scale=scale)
        else:
            if scale is None:
                P.op('vector', 'tensor_copy', reads=reads, writes=writes, out=out, in_=in_)
            else:
                P.op('vector', 'tensor_scalar', reads=reads, writes=writes, out=out, in0=in_, scalar1=scale,
                     scalar2=None, op0=ALU.mult)

    def inproj_chunk(self, q, hTb, hT32b, wz, wx, wqk, wv, wg, wdt, wf, st):
        P = self.P
        hT, rhT = hTb
        hT32, rhT32 = hT32b
        if self._ip_bufs is None:
            sb = lambda n, s, dt=F32: self.sb(st, n, s, dt)
            self._ip_bufs = dict(
                o32=[sb(f"o32_{i}", [128, 512]) for i in range(3)],
                o16=[sb(f"o16_{i}", [128, 512], BF16) for i in range(3)],
                osm=[sb(f"osm_{i}", [128, 8]) for i in range(2)],
                ofl=[sb(f"ofl_{i}", [4, 512]) for i in range(2)],
                n=[0],
            )
        B = self._ip_bufs
        tok = slice(q * 512, (q + 1) * 512)

        def nxt():
            B['n'][0] += 1
            return B['n'][0]
        PB = [2, 3, 4, 5]

        def fm(w, wres, c0, dst, dt16=False, scale=None):
            k = nxt()
            pb = PB[k % 4]
            pt = self.ps[pb]
            for kc in range(8):
                P.op('tensor', 'matmul', reads=[wres, rhT], writes=[f'ps{pb}'], out=pt, lhsT=w[:, kc, c0:c0 + 128],
                     rhs=hT[:, kc, :], start=(kc == 0), stop=(kc == 7))
            o = (B['o16'] if dt16 else B['o32'])[k % 3]
            ores = ('o16_' if dt16 else 'o32_') + str(k % 3)
            self.evac(k, o, pt, [f'ps{pb}'], [ores], scale=scale)
            P.dma('sync', dst, o, reads=[ores], writes=[])
        for ct in range(8):
            fm(wx, 'wx', ct * 128, self.xbcT[ct * 128:(ct + 1) * 128, tok])
        for ct in range(2):
            fm(wqk, 'wqk', ct * 128, self.qT[ct * 128:(ct + 1) * 128, tok], dt16=True, scale=0.125)
        for ct in range(2):
            fm(wqk, 'wqk', 256 + ct * 128, self.kT[ct * 128:(ct + 1) * 128, tok], dt16=True)
        for ct in range(2):
            fm(wg, 'wg', ct * 128, self.gaT[ct * 128:(ct + 1) * 128, tok])
        for ct in range(2):
            fm(wg, 'wg', 256 + ct * 128, self.gbT[ct * 128:(ct + 1) * 128, tok])
        k = nxt()
        pb = PB[k % 4]
        pt = self.ps[pb]
        for kc in range(8):
            P.op('tensor', 'matmul', reads=['wf', rhT32], writes=[f'ps{pb}'], out=pt[0:4, :], lhsT=wf[:, kc, :],
                 rhs=hT32[:, kc, :], start=(kc == 0), stop=(kc == 7))
        o = B['ofl'][q % 2]
        P.op('vector', 'tensor_copy', reads=[f'ps{pb}'], writes=[f'ofl_{q % 2}'], out=o, in_=pt[0:4, :])
        P.dma('sync', self.flT[:, tok], o, reads=[f'ofl_{q % 2}'])
        for j in range(4):
            tt = slice(q * 512 + j * 128, q * 512 + (j + 1) * 128)
            k = nxt()
            pb = PB[k % 4]
            pt = self.ps[pb]
            for kc in range(8):
                P.op('tensor', 'matmul', reads=['wz', rhT], writes=[f'ps{pb}'], out=pt,
                     lhsT=hT[:, kc, j * 128:(j + 1) * 128], rhs=wz[:, kc, :], start=(kc == 0), stop=(kc == 7))
            o = B['o32'][k % 3]
            P.op('scalar', 'activation', reads=[f'ps{pb}'], writes=[f'o32_{k % 3}'], out=o, in_=pt, func=AF.Silu)
            P.dma('sync', self.zs[tt, :], o, reads=[f'o32_{k % 3}'])
            k = nxt()
            pb = PB[k % 4]
            pt = self.ps[pb]
            for kc in range(8):
                P.op('tensor', 'matmul', reads=['wv', rhT], writes=[f'ps{pb}'], out=pt[:, 0:256],
                     lhsT=hT[:, kc, j * 128:(j + 1) * 128], rhs=wv[:, kc, :], start=(kc == 0), stop=(kc == 7))
            for kc in range(8):
                P.op('tensor', 'matmul', reads=['wdt', rhT32], writes=[f'ps{pb}'], out=pt[:, 256:264],
                     lhsT=hT32[:, kc, j * 128:(j + 1) * 128], rhs=wdt[:, kc, :], start=(kc == 0), stop=(kc == 7))
            o = B['o16'][k % 3]
            self.evac(k, o[:, 0:256], pt[:, 0:256], [f'ps{pb}'], [f'o16_{k % 3}'])
            P.dma('sync', self.vtok[tt, :], o[:, 0:256], reads=[f'o16_{k % 3}'])
            o2 = B['osm'][j % 2]
            P.op('vector', 'tensor_copy', reads=[f'ps{pb}'], writes=[f'osm_{j % 2}'], out=o2, in_=pt[:, 256:264])
            P.dma('sync', self.dtr[tt, :], o2, reads=[f'osm_{j % 2}'])

    def conv_gen(self, l, st, pA, pB):
        P, T = self.P, self.T
        TC = min(T, 1024)
        HALO = 30
        if True:
            sb = lambda n, s, dt=F32: self.sb(st, n, s, dt)
            cw = sb("cw", [128, 2, 31])
            cbias = sb("cbias", [128, 2])
            cg = sb("cg", [128, 2])
            cbeta = sb("cbeta", [128, 2])
            ua = [sb(f"ua{i}", [128, TC + HALO]) for i in range(2)]
            ub = [sb(f"ub{i}", [128, TC + HALO]) for i in range(2)]
            co = [sb(f"co{i}", [128, TC]) for i in range(2)]
            sqt = sb("csq", [128, 512])
            mean = sb("cmean", [128, 512])
            rstd = sb("crstd", [128, 512])
            tmp = [sb(f"ctmp{i}", [128, 512]) for i in range(2)]
            yo = [sb(f"cyo{i}", [128, 512], BF16) for i in range(2)]
            P.dma('sync', cw, self.cmw[l], writes=['cw'])
            P.dma('sync', cbias, self.cmb[l], writes=['cbias'])
            P.dma('sync', cg, self.cmg[l], writes=['cg'])
            P.dma('sync', cbeta, self.cmbeta[l], writes=['cbeta'])
            yield
            n = 0
            for c0 in range(0, T, TC):
                for ct in range(2):
                    A, Bt = ua[ct], ub[ct]
                    ra, rb, rc = f'ua{ct}', f'ub{ct}', f'co{ct}'
                    rows = slice(ct * 128, (ct + 1) * 128)
                    if c0 == 0:
                        P.dma('sync', A[:, HALO:], self.gaT[rows, 0:TC], writes=[ra])
                        P.dma('sync', Bt[:, HALO:], self.gbT[rows, 0:TC], writes=[rb])
                        P.op('gpsimd', 'memset', writes=[ra], ap=A[:, 0:HALO], constant=0.0)
                        P.op('gpsimd', 'memset', writes=[rb], ap=Bt[:, 0:HALO], constant=0.0)
                    else:
                        P.dma('sync', A, self.gaT[rows, c0 - HALO:c0 + TC], writes=[ra])
                        P.dma('sync', Bt, self.gbT[rows, c0 - HALO:c0 + TC], writes=[rb])
                    P.op('scalar', 'activation', reads=[rb], writes=[rb], out=Bt, in_=Bt, func=AF.Sigmoid)
                    P.op('gpsimd', 'tensor_tensor', reads=[ra, rb], writes=[ra], out=A, in0=A, in1=Bt, op=ALU.mult)
                    C = co[ct]
                    P.op('vector', 'tensor_scalar', reads=[ra, 'cw', 'cbias'], writes=[rc], out=C, in0=A[:, 0:TC],
                         scalar1=cw[:, ct, 0:1], scalar2=cbias[:, ct:ct + 1], op0=ALU.mult, op1=ALU.add)
                    for k in range(1, 31):
                        P.op('vector', 'scalar_tensor_tensor', reads=[ra, 'cw', rc], writes=[rc], out=C,
                             in0=A[:, k:k + TC], scalar=cw[:, ct, k:k + 1], in1=C, op0=ALU.mult, op1=ALU.add)
                        if k % 8 == 0:
                            yield
                    yield
                for s0 in range(0, TC, 512):
                    cs_ = slice(s0, s0 + 512)
                    p1, p2 = self.ps[pA], self.ps[pB]
                    for ct in range(2):
                        P.op('tensor', 'matmul', reads=['ones', f'co{ct}'], writes=[f'ps{pA}'], out=p1, lhsT=self.ones,
                             rhs=co[ct][:, cs_], start=(ct == 0), stop=(ct == 1))
                    P.op('vector', 'tensor_scalar', reads=[f'ps{pA}'], writes=['cmean'], out=mean, in0=p1,
                         scalar1=1.0 / 256, scalar2=None, op0=ALU.mult)
                    for ct in range(2):
                        P.op('scalar', 'activation', reads=[f'co{ct}'], writes=['csq'], out=sqt, in_=co[ct][:, cs_],
                             func=AF.Square)
                        P.op('tensor', 'matmul', reads=['ones', 'csq'], writes=[f'ps{pB}'], out=p2, lhsT=self.ones,
                             rhs=sqt, start=(ct == 0), stop=(ct == 1))
                    P.op('vector', 'tensor_tensor', reads=['cmean'], writes=['crstd'], out=rstd, in0=mean, in1=mean,
                         op=ALU.mult)
                    P.op('vector', 'scalar_tensor_tensor', reads=[f'ps{pB}', 'crstd'], writes=['crstd'], out=rstd, in0=p2,
                         scalar=1.0 / 256, in1=rstd, op0=ALU.mult, op1=ALU.subtract)
                    self.rstd_ops(rstd, rstd, 1.0, ['crstd'], ['crstd'])
                    for ct in range(2):
                        n += 1
                        t = tmp[n % 2]
                        rt = f'ctmp{n % 2}'
                        P.op('vector', 'tensor_tensor', reads=[f'co{ct}', 'cmean'], writes=[rt], out=t,
                             in0=co[ct][:, cs_], in1=mean, op=ALU.subtract)
                        P.op('gpsimd', 'tensor_tensor', reads=[rt, 'crstd'], writes=[rt], out=t, in0=t, in1=rstd,
                             op=ALU.mult)
                        y = yo[n % 2]
                        ry = f'cyo{n % 2}'
                        P.op('scalar', 'activation', reads=[rt, 'cg', 'cbeta'], writes=[ry], out=y, in_=t, func=AF.Silu,
                             scale=cg[:, ct:ct + 1], bias=cbeta[:, ct:ct + 1])
                        P.dma('sync', self.yT[768 + ct * 128:768 + (ct + 1) * 128, c0 + s0:c0 + s0 + 512], y,
                              reads=[ry])
                    yield

    def phase_conv(self, l):
        with contextlib.ExitStack() as st:
            self.P.begin()
            for _ in self.conv_gen(l, st, 0, 1):
                pass
            self.P.end()

    def phase_attn(self, l, conv_inside=False):
        P, T, NT, NQ = self.P, self.T, self.NT, self.NQ
        CW = min(T, 2048)
        with contextlib.ExitStack() as st:
            sb = lambda n, s, dt=F32: self.sb(st, n, s, dt)
            fb = sb("fb", [4, 1])
            xx = sb("fx", [4, CW])
            ax = sb("fax", [4, CW])
            mn = sb("fmn", [4, CW])
            cum = [sb(f"fcum{i}", [4, CW]) for i in range(2)]
            r1 = sb("fr1", [4, CW])
            sp = sb("fsp", [4, 6, CW], BF16)
            P.begin()
            P.dma('sync', fb, self.f_bias[l], writes=['fb'])
            for ci, c0 in enumerate(range(0, T, CW)):
                cc = cum[ci % 2]
                rcum = f'fcum{ci % 2}'
                P.dma('sync', xx, self.flT[:, c0:c0 + CW], writes=['fx'])
                P.op('scalar', 'activation', reads=['fx', 'fb'], writes=['fx'], out=xx, in_=xx, func=AF.Identity,
                     bias=fb[:, 0:1], scale=1.0)
                P.op('vector', 'tensor_scalar', reads=['fx'], writes=['fmn'], out=mn, in0=xx, scalar1=-1.0, scalar2=0.0,
                     op0=ALU.mult, op1=ALU.max)
                P.op('vector', 'scalar_tensor_tensor', reads=['fmn', 'fx'], writes=['fax'], out=ax, in0=mn, scalar=-2.0,
                     in1=xx, op0=ALU.mult, op1=ALU.subtract)
                P.op('scalar', 'activation', reads=['fax'], writes=['fax'], out=ax, in_=ax, func=AF.Exp)
                P.op('scalar', 'activation', reads=['fax'], writes=['fax'], out=ax, in_=ax, func=AF.Ln, bias=1.0,
                     scale=1.0)
                P.op('vector', 'scalar_tensor_tensor', reads=['fmn', 'fax'], writes=['fmn'], out=mn, in0=mn, scalar=-1.0,
                     in1=ax, op0=ALU.mult, op1=ALU.subtract)
                init = 0.0 if ci == 0 else cum[(ci - 1) % 2][:, CW - 1:CW]
                P.op('vector', 'tensor_tensor_scan', reads=['fmn', 'ones', f'fcum{(ci - 1) % 2}'], writes=[rcum], out=cc,
                     data0=self.ones[0:4, 0:1].broadcast_to([4, CW]), data1=mn, initial=init, op0=ALU.mult, op1=ALU.add)
                P.op('vector', 'tensor_copy', reads=[rcum], writes=['fsp'], out=sp[:, 0, :], in_=cc)
                P.op('vector', 'tensor_tensor', reads=[rcum, 'fsp'], writes=['fr1'], out=r1, in0=cc, in1=sp[:, 0, :],
                     op=ALU.subtract)
                P.op('vector', 'tensor_copy', reads=['fr1'], writes=['fsp'], out=sp[:, 1, :], in_=r1)
                P.op('vector', 'tensor_tensor', reads=['fr1', 'fsp'], writes=['fr1'], out=r1, in0=r1, in1=sp[:, 1, :],
                     op=ALU.subtract)
                P.op('vector', 'tensor_copy', reads=['fr1'], writes=['fsp'], out=sp[:, 2, :], in_=r1)
                P.op('vector', 'tensor_scalar', reads=['fsp'], writes=['fsp'], out=sp[:, 3:6, :], in0=sp[:, 0:3, :],
                     scalar1=-1.0, scalar2=None, op0=ALU.mult)
                P.dma('sync', self.cs[:, :, c0:c0 + CW], sp, reads=['fsp'])
            P.end()
        with contextlib.ExitStack() as st:
            sb = lambda n, s, dt=F32: self.sb(st, n, s, dt)
            qp = [sb(f"qp{i}", [70, T], BF16) for i in range(2)]
            kp = [sb(f"kp{i}", [70, T], BF16) for i in range(2)]
            vp = [sb(f"vp{i}", [128, NT, 65], BF16) for i in range(2)]
            nm = sb("negmask", [128, 4, 512], BF16)
            NSB = 5
            LAG = 3
            pt_ = [sb(f"pT{i}", [128, 512], BF16) for i in range(NSB)]
            rec = sb("rec", [65, 512])
            bcs = sb("bcs", [64, 512])
            on = [sb(f"on{i}", [64, 512]) for i in range(2)]
            P.begin()
            cgen = self.conv_gen(l, st, 7, 7) if conv_inside else None
            n_units = (T // min(T, 1024)) * (2 * 5 + min(T, 1024) // 512) + 1
            units_done = 0
            work_total = 4 * sum(4 * q_ + 4 for q_ in range(NQ))
            work_done = 0
            P.op('gpsimd', 'memset', writes=['negmask'], ap=nm, constant=0.0)
            for d in range(4):
                P.op('gpsimd', 'affine_select', reads=['negmask'], writes=['negmask'], out=nm[:, d, :], in_=nm[:, d, :],
                     pattern=[[1, 512]], compare_op=ALU.is_ge, fill=-30000.0, base=-128 * d, channel_multiplier=-1)
            step = 0
            for h in range(4):
                hb = h % 2
                Q, Kp, V = qp[hb], kp[hb], vp[hb]
                rq, rk, rv = f'qp{hb}', f'kp{hb}', f'vp{hb}'
                hr = slice(h * 64, (h + 1) * 64)
                P.op('gpsimd', 'memset', writes=[rq], ap=Q[64:70, :], constant=1.0)
                P.op('gpsimd', 'memset', writes=[rk], ap=Kp[64:70, :], constant=1.0)
                P.op('gpsimd', 'memset', writes=[rv], ap=V[:, :, 64:65], constant=1.0)
                P.dma('sync', Q[0:64, :], self.qT[hr, :], writes=[rq])
                P.dma('sync', Kp[0:64, :], self.kT[hr, :], writes=[rk])
                P.dma('sync', Q[67:70, :], self.cs[h, 0:3, :], writes=[rq])
                P.dma('sync', Kp[64:67, :], self.cs[h, 3:6, :], writes=[rk])
                for i0 in range(0, NT, 4):
                    P.dma('sync', V[:, i0:i0 + 4, 0:64],
                          self.vtok[i0 * 128:(i0 + 4) * 128, hr].rearrange("(i p) d -> p i d", p=128), writes=[rv])
                for qc in range(NQ):
                    nk = 4 * qc + 4
                    ob = 5 + qc % 2
                    O = self.ps[ob]
                    qs = slice(qc * 512, (qc + 1) * 512)
                    for s_ in range(nk + LAG):
                        if s_ < nk:
                            kt = s_
                            sbk = (step + s_) % NSB
                            S = self.ps[sbk]
                            diag = kt >= 4 * qc
                            P.op('tensor', 'matmul', reads=[rq, rk], writes=[f'ps{sbk}'], out=S,
                                 lhsT=Kp[:, kt * 128:(kt + 1) * 128], rhs=Q[:, qs], start=True, stop=not diag)
                            if diag:
                                P.op('tensor', 'matmul', reads=['identb', 'negmask'], writes=[f'ps{sbk}'], out=S,
                                     lhsT=self.identb, rhs=nm[:, kt - 4 * qc, :], start=False, stop=True)
                        if 1 <= s_ <= nk:
                            kt = s_ - 1
                            sbk = (step + kt) % NSB
                            P.op('scalar', 'activation', reads=[f'ps{sbk}'], writes=[f'pT{sbk}'], out=pt_[sbk],
                                 in_=self.ps[sbk], func=AF.Exp)
                        if s_ >= LAG:
                            kt = s_ - LAG
                            sbk = (step + kt) % NSB
                            P.op('tensor', 'matmul', reads=[f'pT{sbk}', rv], writes=[f'ps{ob}'], out=O[0:65, :],
                                 lhsT=V[:, kt, :], rhs=pt_[sbk], start=(kt == 0), stop=(kt == nk - 1))
                    step += nk
                    P.op('vector', 'reciprocal', reads=[f'ps{ob}'], writes=['rec'], out=rec[64:65, :], in_=O[64:65, :])
                    P.op('tensor', 'matmul', reads=['ones', 'rec'], writes=['ps7'], out=self.ps[7][0:64, :],
                         lhsT=self.ones[64:65, 0:64], rhs=rec[64:65, :], start=True, stop=True)
                    P.op('scalar', 'activation', reads=['ps7'], writes=['bcs'], out=bcs, in_=self.ps[7][0:64, :],
                         func=AF.Copy)
                    o_ = on[qc % 2]
                    P.op('vector', 'tensor_tensor', reads=[f'ps{ob}', 'bcs'], writes=[f'on{qc % 2}'], out=o_,
                         in0=O[0:64, :], in1=bcs, op=ALU.mult)
                    P.dma('sync', self.attT[hr, qs], o_, reads=[f'on{qc % 2}'])
                    work_done += nk
                    while cgen is not None and units_done * work_total < n_units * work_done:
                        try:
                            next(cgen)
                            units_done += 1
                        except StopIteration:
                            cgen = None
                if h == 3 and cgen is not None:
                    for _ in cgen:
                        pass
                if h < 3:
                    P.end()
                    P.begin()
            P.end()
        with contextlib.ExitStack() as st:
            sb = lambda n, s, dt=F32: self.sb(st, n, s, dt)
            fg = sb("foxg", [128, 2])
            at = [[sb(f"at{i}{c}", [128, 512]) for c in range(2)] for i in range(2)]
            sq = sb("asq", [128, 512])
            rs = sb("ars", [128, 512])
            yo = [sb(f"ayo{i}", [128, 512], BF16) for i in range(2)]
            P.begin()
            P.dma('sync', fg, self.fox_g[l], writes=['foxg'])
            n = 0
            for qc in range(NQ):
                qs = slice(qc * 512, (qc + 1) * 512)
                b = qc % 2
                for ct in range(2):
                    P.dma('sync', at[b][ct], self.attT[ct * 128:(ct + 1) * 128, qs], writes=[f'at{b}{ct}'])
                    P.op('scalar', 'activation', reads=[f'at{b}{ct}'], writes=['asq'], out=sq, in_=at[b][ct],
                         func=AF.Square)
                    P.op('tensor', 'matmul', reads=['ones', 'asq'], writes=['ps0'], out=self.ps[0], lhsT=self.ones,
                         rhs=sq, start=(ct == 0), stop=(ct == 1))
                self.rstd_ops(self.ps[0], rs, 256.0, ['ps0'], ['ars'])
                for ct in range(2):
                    n += 1
                    P.op('vector', 'tensor_tensor', reads=[f'at{b}{ct}', 'ars'], writes=[f'at{b}{ct}'], out=at[b][ct],
                         in0=at[b][ct], in1=rs, op=ALU.mult)
                    y = yo[n % 2]
                    P.op('scalar', 'activation', reads=[f'at{b}{ct}', 'foxg'], writes=[f'ayo{n % 2}'], out=y,
                         in_=at[b][ct], func=AF.Copy, scale=fg[:, ct:ct + 1])
                    P.dma('sync', self.yT[512 + ct * 128:512 + (ct + 1) * 128, qs], y, reads=[f'ayo{n % 2}'])
            P.end()

    def phase_ssd(self, l):
        P, T, NT = self.P, self.T, self.NT
        SC = 512
        assert NT * 8 <= 512
        with contextlib.ExitStack() as st:
            sb = lambda n, s, dt=F32: self.sb(st, n, s, dt)
            cw4 = sb("cw4", [128, 8, 4])
            cb4 = sb("cb4", [128, 8])
            dtb = sb("dtb", [128, 8])
            aneg = sb("aneg", [128, 8])
            dsk = sb("dsk", [128, 8])
            ng = sb("ssdng", [128, 512])
            dt = sb("dt_all", [128, NT, 8])
            dmn = sb("dt_mn", [128, NT, 8])
            dtA = sb("dtA", [128, NT, 8])
            El = sb("El", [128, NT, 8])
            Wl = sb("Wl", [128, NT, 8])
            cd = sb("cd", [128, NT, 8])
            xin = [sb(f"xin{i}", [128, SC + 3]) for i in range(2)]
            cacc = [sb(f"cacc{i}", [128, SC]) for i in range(2)]
            xsT = [sb(f"xsT{i}", [128, SC]) for i in range(4)]
            BT = [sb(f"BT{i}", [128, SC], BF16) for i in range(2)]
            CT = [sb(f"CT{i}", [128, SC], BF16) for i in range(2)]
            x32_2 = [sb(f"x32{i}", [128, 8, 64], F32) for i in range(2)]
            Btok_2 = [sb(f"Btok{i}", [128, 256], BF16) for i in range(2)]
            R_2 = [sb(f"Rall{i}", [128, 8, 128], F32) for i in range(2)]
            E_2 = [sb(f"Eall{i}", [128, 8, 128], F32) for i in range(2)]
            CBm_2 = [sb(f"CBm{i}", [128, 2, 128], F32) for i in range(2)]
            M_2 = [sb(f"Mall{i}", [128, 8, 128], BF16) for i in range(2)]
            xdt_2 = [sb(f"xdt{i}", [128, 8, 64], BF16) for i in range(2)]
            xw_2 = [sb(f"xw{i}", [128, 8, 64], BF16) for i in range(2)]
            H = sb("Hst", [128, 8, 64])
            Hb = sb("Hb", [128, 8, 64], BF16)
            t1_2 = [sb(f"sst1{i}", [128, 8, 64], F32) for i in range(2)]
            t2_2 = [sb(f"sst2{i}", [128, 8, 64], F32) for i in range(2)]
            zt_2 = [sb(f"zt{i}", [128, 512], F32) for i in range(2)]
            ssq_2 = [sb(f"ssq{i}", [128, 512], F32) for i in range(2)]
            gss_2 = [sb(f"gss{i}", [128, 2], F32) for i in range(2)]
            grs_2 = [sb(f"grs{i}", [128, 2], F32) for i in range(2)]
            yTs_2 = [sb(f"yTs{i}", [128, 4, 128], BF16) for i in range(2)]
            ps = self.ps
            psb1 = ps[1].bitcast(BF16)
            P.begin()
            P.dma('sync', cw4, self.convw[l], writes=['cw4'])
            P.dma('sync', cb4, self.convb[l], writes=['cb4'])
            P.dma('sync', dtb, self.dt_bias[l].partition_broadcast(128), writes=['dtb'])
            P.dma('sync', aneg, self.a_log[l].partition_broadcast(128), writes=['aneg'])
            P.dma('sync', dsk, self.ssd_d[l].partition_broadcast(128), writes=['dsk'])
            P.dma('sync', ng, self.ssd_norm_g[l].partition_broadcast(128), writes=['ssdng'])
            for i0 in range(0, NT, 4):
                n_ = min(4, NT - i0)
                P.dma('sync', dt[:, i0:i0 + n_, :],
                      self.dtr[i0 * 128:(i0 + n_) * 128, :].rearrange("(i p) h -> p i h", p=128), writes=['dt_all'])
            P.op('scalar', 'activation', reads=['aneg'], writes=['aneg'], out=aneg, in_=aneg, func=AF.Exp)
            P.op('vector', 'tensor_scalar', reads=['aneg'], writes=['aneg'], out=aneg, in0=aneg, scalar1=-1.0,
                 scalar2=None, op0=ALU.mult)
            bc3 = lambda t: t.unsqueeze(1).broadcast_to([128, NT, 8])
            P.op('vector', 'tensor_tensor', reads=['dt_all', 'dtb'], writes=['dt_all'], out=dt, in0=dt, in1=bc3(dtb),
                 op=ALU.add)
            P.op('vector', 'tensor_scalar', reads=['dt_all'], writes=['dt_mn'], out=dmn, in0=dt, scalar1=0.0,
                 scalar2=None, op0=ALU.max)
            P.op('vector', 'scalar_tensor_tensor', reads=['dt_mn', 'dt_all'], writes=['dt_all'],
                 out=dt.rearrange("p i h -> p (i h)"), in0=dmn.rearrange("p i h -> p (i h)"), scalar=-2.0,
                 in1=dt.rearrange("p i h -> p (i h)"), op0=ALU.mult, op1=ALU.add)
            P.op('scalar', 'activation', reads=['dt_all'], writes=['dt_all'], out=dt, in_=dt, func=AF.Exp)
            P.op('scalar', 'activation', reads=['dt_all'], writes=['dt_all'], out=dt, in_=dt, func=AF.Ln, bias=1.0,
                 scale=1.0)
            P.op('vector', 'tensor_tensor', reads=['dt_all', 'dt_mn'], writes=['dt_all'], out=dt, in0=dt, in1=dmn,
                 op=ALU.add)
            P.op('vector', 'tensor_tensor', reads=['dt_all', 'aneg'], writes=['dtA'], out=dtA, in0=dt, in1=bc3(aneg),
                 op=ALU.mult)
            dtA2 = dtA.rearrange("p i h -> p (i h)")
            P.op('tensor', 'matmul', reads=['tri', 'dtA'], writes=['ps2'], out=ps[2][:, 0:NT * 8], lhsT=self.tri,
                 rhs=dtA2, start=True, stop=True)
            P.op('tensor', 'matmul', reads=['ones', 'dtA'], writes=['ps3'], out=ps[3][:, 0:NT * 8], lhsT=self.ones,
                 rhs=dtA2, start=True, stop=True)
            f2 = lambda t: t.rearrange("p i h -> p (i h)")
            P.op('scalar', 'activation', reads=['ps2'], writes=['El'], out=f2(El), in_=ps[2][:, 0:NT * 8], func=AF.Exp)
            P.op('scalar', 'activation', reads=['ps3'], writes=['cd'], out=f2(cd), in_=ps[3][:, 0:NT * 8], func=AF.Exp)
            P.op('vector', 'tensor_copy', reads=['ps3'], writes=['Wl'], out=f2(Wl), in_=ps[3][:, 0:NT * 8])
            P.op('vector', 'tensor_tensor', reads=['Wl', 'ps2'], writes=['Wl'], out=f2(Wl), in0=f2(Wl),
                 in1=ps[2][:, 0:NT * 8], op=ALU.subtract)
            P.op('scalar', 'activation', reads=['Wl'], writes=['Wl'], out=Wl, in_=Wl, func=AF.Exp)
            P.op('vector', 'tensor_tensor', reads=['Wl', 'dt_all'], writes=['Wl'], out=Wl, in0=Wl, in1=dt, op=ALU.mult)
            P.op('gpsimd', 'memset', writes=['Hst'], ap=H, constant=0.0)
            P.op('gpsimd', 'memset', writes=['Hb'], ap=Hb, constant=0.0)
            for c0 in range(0, T, SC):
                for ct in range(8):
                    X = xin[ct % 2]
                    rx = f'xin{ct % 2}'
                    A = cacc[ct % 2]
                    ra = f'cacc{ct % 2}'
                    rows = slice(ct * 128, (ct + 1) * 128)
                    if c0 == 0:
                        P.op('gpsimd', 'memset', writes=[rx], ap=X[:, 0:3], constant=0.0)
                        P.dma('sync', X[:, 3:], self.xbcT[rows, 0:SC], writes=[rx])
                    else:
                        P.dma('sync', X, self.xbcT[rows, c0 - 3:c0 + SC], writes=[rx])
                    P.op('vector', 'tensor_scalar', reads=[rx, 'cw4', 'cb4'], writes=[ra], out=A, in0=X[:, 0:SC],
                         scalar1=cw4[:, ct, 0:1], scalar2=cb4[:, ct:ct + 1], op0=ALU.mult, op1=ALU.add)
                    for k in range(1, 4):
                        P.op('vector', 'scalar_tensor_tensor', reads=[rx, 'cw4', ra], writes=[ra], out=A,
                             in0=X[:, k:k + SC], scalar=cw4[:, ct, k:k + 1], in1=A, op0=ALU.mult, op1=ALU.add)
                    if ct < 4:
                        dst, rd = xsT[ct], f'xsT{ct}'
                    elif ct < 6:
                        dst, rd = BT[ct - 4], f'BT{ct - 4}'
                    else:
                        dst, rd = CT[ct - 6], f'CT{ct - 6}'
                    P.op('scalar', 'activation', reads=[ra], writes=[rd], out=dst, in_=A, func=AF.Silu)
                def stage_a(c0, cc):
                        c = c0 // 128 + cc
                        cs_ = slice(cc * 128, (cc + 1) * 128)
                        tok = slice(c * 128, (c + 1) * 128)
                        pc = c % 2
                        x32 = x32_2[pc]
                        Btok = Btok_2[pc]
                        R = R_2[pc]
                        E = E_2[pc]
                        CBm = CBm_2[pc]
                        M = M_2[pc]
                        xdt = xdt_2[pc]
                        xw = xw_2[pc]
                        t1 = t1_2[pc]
                        t2 = t2_2[pc]
                        zt = zt_2[pc]
                        ssq = ssq_2[pc]
                        gss = gss_2[pc]
                        grs = grs_2[pc]
                        yTs = yTs_2[pc]
                        n = {k: k + str(pc) for k in ('x32', 'Btok', 'Rall', 'Eall', 'CBm', 'Mall', 'xdt', 'xw', 'sst1', 'sst2', 'zt', 'ssq', 'gss', 'grs', 'yTs')}
                        for ct in range(4):
                            P.op('tensor', 'transpose', reads=[f'xsT{ct}', 'ident'], writes=['ps0'],
                                 out=ps[0][:, ct * 128:(ct + 1) * 128], in_=xsT[ct][:, cs_], identity=self.ident)
                        P.op('scalar', 'activation', reads=['ps0'], writes=[n['x32']], out=x32.rearrange("p h d -> p (h d)"),
                             in_=ps[0], func=AF.Copy)
                        for g in range(2):
                            P.op('tensor', 'transpose', reads=[f'BT{g}', 'identb'], writes=['ps1'],
                                 out=psb1[:, g * 128:(g + 1) * 128], in_=BT[g][:, cs_], identity=self.identb)
                        P.op('vector', 'tensor_copy', reads=['ps1'], writes=[n['Btok']], out=Btok, in_=psb1[:, 0:256])
                        P.dma('sync', zt, self.zs[tok, :], writes=[n['zt']])
                        P.op('vector', 'tensor_tensor', reads=['tri', 'dtA'], writes=[n['Rall']], out=R,
                             in0=self.tri.unsqueeze(1).broadcast_to([128, 8, 128]),
                             in1=dtA[:, c, :].unsqueeze(2).broadcast_to([128, 8, 128]), op=ALU.mult)
                        for hh in range(2):
                            P.op('tensor', 'matmul', reads=['ustr', n['Rall']], writes=[f'ps{2 + hh}'], out=ps[2 + hh],
                                 lhsT=self.ustr, rhs=R[:, hh * 4:(hh + 1) * 4, :].rearrange("p h l -> p (h l)"),
                                 start=True, stop=True)
                            P.op('scalar', 'activation', reads=[f'ps{2 + hh}'], writes=[n['Eall']],
                                 out=E[:, hh * 4:(hh + 1) * 4, :].rearrange("p h l -> p (h l)"), in_=ps[2 + hh], func=AF.Exp)
                        for g in range(2):
                            P.op('tensor', 'matmul', reads=[f'BT{g}', f'CT{g}'], writes=['ps4'],
                                 out=ps[4][:, g * 128:(g + 1) * 128], lhsT=BT[g][:, cs_], rhs=CT[g][:, cs_],
                                 start=True, stop=True)
                        P.op('vector', 'tensor_tensor', reads=['ps4', 'tri'], writes=[n['CBm']], out=CBm,
                             in0=ps[4][:, 0:256].rearrange("p (g l) -> p g l", g=2),
                             in1=self.tri.unsqueeze(1).broadcast_to([128, 2, 128]), op=ALU.mult)
                        for g in range(2):
                            P.op('vector', 'tensor_tensor', reads=[n['Eall'], n['CBm']], writes=[n['Mall']],
                                 out=M[:, g * 4:(g + 1) * 4, :], in0=E[:, g * 4:(g + 1) * 4, :],
                                 in1=CBm[:, g:g + 1, :].broadcast_to([128, 4, 128]), op=ALU.mult)
                        P.op('gpsimd', 'tensor_tensor', reads=[n['x32'], 'dt_all'], writes=[n['xdt']], out=xdt, in0=x32,
                             in1=dt[:, c, :].unsqueeze(2).broadcast_to([128, 8, 64]), op=ALU.mult)
                        P.op('gpsimd', 'tensor_tensor', reads=[n['x32'], 'Wl'], writes=[n['xw']], out=xw, in0=x32,
                             in1=Wl[:, c, :].unsqueeze(2).broadcast_to([128, 8, 64]), op=ALU.mult)
                        for h in range(8):
                            P.op('tensor', 'matmul', reads=[n['Mall'], n['xdt']], writes=['ps5'], out=ps[5][:, h * 64:(h + 1) * 64],
                                 lhsT=M[:, h, :], rhs=xdt[:, h, :], start=True, stop=True)
                        for g in range(2):
                            P.op('tensor', 'matmul', reads=[f'CT{g}', 'Hb'], writes=['ps6'],
                                 out=ps[6][:, g * 256:(g + 1) * 256], lhsT=CT[g][:, cs_],
                                 rhs=Hb[:, g * 4:(g + 1) * 4, :].rearrange("p h d -> p (h d)"), start=True, stop=True)
                        for g in range(2):
                            P.op('tensor', 'matmul', reads=[n['Btok'], n['xw']], writes=['ps7'],
                                 out=ps[7][:, g * 256:(g + 1) * 256], lhsT=Btok[:, g * 128:(g + 1) * 128],
                                 rhs=xw[:, g * 4:(g + 1) * 4, :].rearrange("p h d -> p (h d)"), start=True, stop=True)
                        v3 = lambda t: t.rearrange("p (h d) -> p h d", h=8)
                        b3 = lambda t: t.unsqueeze(2).broadcast_to([128, 8, 64])
                        P.op('vector', 'tensor_tensor', reads=['ps6', 'El'], writes=[n['sst1']], out=t1, in0=v3(ps[6]),
                             in1=b3(El[:, c, :]), op=ALU.mult)
                        P.op('vector', 'tensor_tensor', reads=[n['sst1'], 'ps5'], writes=[n['sst1']], out=t1, in0=t1, in1=v3(ps[5]),
                             op=ALU.add)
                        P.op('gpsimd', 'tensor_tensor', reads=[n['x32'], 'dsk'], writes=[n['sst2']], out=t2, in0=x32, in1=b3(dsk),
                             op=ALU.mult)
                        P.op('gpsimd', 'tensor_tensor', reads=[n['sst1'], n['sst2']], writes=[n['sst1']], out=t1, in0=t1, in1=t2,
                             op=ALU.add)
                        P.op('vector', 'tensor_tensor', reads=['Hst', 'cd'], writes=['Hst'], out=H, in0=H, in1=b3(cd[:, c, :]),
                             op=ALU.mult)
                        P.op('vector', 'tensor_tensor', reads=['Hst', 'ps7'], writes=['Hst'], out=H, in0=H, in1=v3(ps[7]),
                             op=ALU.add)
                        P.op('gpsimd', 'tensor_copy', reads=['Hst'], writes=['Hb'], out=Hb, in_=H)

                def stage_b(c):
                        tok = slice(c * 128, (c + 1) * 128)
                        pc = c % 2
                        x32 = x32_2[pc]
                        Btok = Btok_2[pc]
                        R = R_2[pc]
                        E = E_2[pc]
                        CBm = CBm_2[pc]
                        M = M_2[pc]
                        xdt = xdt_2[pc]
                        xw = xw_2[pc]
                        t1 = t1_2[pc]
                        t2 = t2_2[pc]
                        zt = zt_2[pc]
                        ssq = ssq_2[pc]
                        gss = gss_2[pc]
                        grs = grs_2[pc]
                        yTs = yTs_2[pc]
                        n = {k: k + str(pc) for k in ('x32', 'Btok', 'Rall', 'Eall', 'CBm', 'Mall', 'xdt', 'xw', 'sst1', 'sst2', 'zt', 'ssq', 'gss', 'grs', 'yTs')}
                        y2 = t1.rearrange("p h d -> p (h d)")
                        P.op('gpsimd', 'tensor_tensor', reads=[n['sst1'], n['zt']], writes=[n['sst1']], out=y2, in0=y2, in1=zt,
                             op=ALU.mult)
                        for g in range(2):
                            P.op('scalar', 'activation', reads=[n['sst1']], writes=[n['ssq'], n['gss']],
                                 out=ssq[:, g * 256:(g + 1) * 256], in_=y2[:, g * 256:(g + 1) * 256], func=AF.Square,
                                 accum_out=gss[:, g:g + 1])
                        self.rstd_ops(gss, grs, 256.0, [n['gss']], [n['grs']])
                        for g in range(2):
                            P.op('vector', 'scalar_tensor_tensor', reads=[n['sst1'], n['grs'], 'ssdng'], writes=[n['sst2']],
                                 out=t2.rearrange("p h d -> p (h d)")[:, g * 256:(g + 1) * 256],
                                 in0=y2[:, g * 256:(g + 1) * 256], scalar=grs[:, g:g + 1],
                                 in1=ng[:, g * 256:(g + 1) * 256], op0=ALU.mult, op1=ALU.mult)
                        yn = t2.rearrange("p h d -> p (h d)")
                        for ct in range(4):
                            P.op('tensor', 'transpose', reads=[n['sst2'], 'ident'], writes=['ps0'],
                                 out=ps[0][:, ct * 128:(ct + 1) * 128], in_=yn[:, ct * 128:(ct + 1) * 128],
                                 identity=self.ident)
                        P.op('scalar', 'activation', reads=['ps0'], writes=[n['yTs']], out=yTs.rearrange("p c t -> p (c t)"),
                             in_=ps[0], func=AF.Copy)
                        P.dma('sync', self.yT[0:512, tok].rearrange("(ct p) t -> p ct t", p=128), yTs, reads=[n['yTs']])

                for cc in range(SC // 128):
                    c = c0 // 128 + cc
                    stage_a(c0, cc)
                    if c >= 1:
                        stage_b(c - 1)
                if c0 + SC >= T:
                    stage_b(NT - 1)
            P.end()

    def phase_wout_router(self, l, st0):
        P, T, NT = self.P, self.T, self.NT
        src = self.x_in if l == 0 else self.xres
        ps = self.ps
        self.ohb = self.sb(st0, "ohb", [128, NT, 64], BF16)
        self.rw = self.sb(st0, "rw", [128, NT, 2])
        with contextlib.ExitStack() as st:
            sb = lambda n, s, dt=F32: self.sb(st, n, s, dt)
            wo = sb("wo", [128, 8, D], BF16)
            gsc = sb("gscf", [128, D])
            wr = sb("wr", [128, 8, 36])
            rb = sb("rbias", [128, 36])
            yTc = [sb(f"yTc{i}", [128, 8, 512], BF16) for i in range(2)]
            xl = [sb(f"xl{i}", [128, D]) for i in range(4)]
            tt = sb("wtmp", [128, D])
            hb = [sb(f"hperm{i}", [128, D], BF16) for i in range(2)]
            lg = sb("lg", [128, 36])
            lg4 = sb("lg4", [128, 4, 36])
            gmax4 = sb("gmax4", [128, 4])
            gsum4 = sb("gsum4", [128, 4])
            r4 = sb("r4", [128, 4])
            den4 = sb("den4", [128, 4])
            ohg4 = sb("ohg4", [128, 4, 4])
            gexp4 = sb("gexp4", [128, 4, 4])
            em4 = sb("em4", [128, 4, 4, 8])
            es4 = sb("es4", [128, 4, 8])
            t84 = sb("t84", [128, 4, 8])
            s14 = sb("s14", [128, 4, 8])
            s24 = sb("s24", [128, 4, 8])
            sm = sb("rsm", [128, 64])
            gexp = sb("gexp", [128, 4])
            ohg = sb("ohg", [128, 4])
            em = sb("em", [128, 4, 8])
            es = sb("esel", [128, 8])
            t8 = sb("top8", [128, 8])
            s1 = sb("sel1", [128, 8])
            s2 = sb("sel2", [128, 8])
            W = self.w_out[l].rearrange("(kc p) n -> p kc n", p=128)
            P.begin()
            for kc in range(8):
                self.load_cast(st, wo[:, kc, :], W[:, kc, :], 'wo', D)
            P.dma('sync', wr[:, :, 0:4], self.w_rg[l].rearrange("(kc p) n -> p kc n", p=128), writes=['wr'])
            P.dma('sync', wr[:, :, 4:36], self.w_re[l].rearrange("(kc p) n -> p kc n", p=128), writes=['wr'])
            P.dma('sync', rb[:, 0:4], self.b_rg[l].partition_broadcast(128), writes=['rbias'])
            P.dma('sync', rb[:, 4:36], self.b_re[l].partition_broadcast(128), writes=['rbias'])
            P.dma('sync', gsc, self.norm_ffn_g[l].partition_broadcast(128), writes=['gscf'])
            P.op('vector', 'scalar_tensor_tensor', reads=['gscf', 'modb'], writes=['gscf'], out=gsc, in0=self.mod(4),
                 scalar=1.0, in1=gsc, op0=ALU.add, op1=ALU.mult)

            def pre_load(i):
                q, j = divmod(i, 4)
                if j == 0:
                    P.dma('sync', yTc[q % 2], self.yT[:, q * 512:(q + 1) * 512].rearrange("(kc p) t -> p kc t", p=128),
                          writes=[f'yTc{q % 2}'])
                P.dma('sync', xl[i % 4], src[i * 128:(i + 1) * 128, :], writes=[f'xl{i % 4}'])

            def pre(i, X, rx):
                q, j = divmod(i, 4)
                Y = yTc[q % 2]
                ry = f'yTc{q % 2}'
                XL = xl[i % 4]
                rl = f'xl{i % 4}'
                for half in range(2):
                    pb = 2 + half
                    for kc in range(8):
                        P.op('tensor', 'matmul', reads=[ry, 'wo'], writes=[f'ps{pb}'], out=ps[pb],
                             lhsT=Y[:, kc, j * 128:(j + 1) * 128], rhs=wo[:, kc, half * 512:(half + 1) * 512],
                             start=(kc == 0), stop=(kc == 7))
                    hs = slice(half * 512, (half + 1) * 512)
                    P.op('vector', 'tensor_tensor', reads=[f'ps{pb}', 'modb'], writes=['wtmp'], out=tt[:, hs], in0=ps[pb],
                         in1=self.mod(2)[:, hs], op=ALU.mult)
                P.op('gpsimd', 'tensor_tensor', reads=['wtmp', rl], writes=[rx], out=X, in0=tt, in1=XL, op=ALU.add)
                P.dma('sync', self.xres[i * 128:(i + 1) * 128, :], X, reads=[rx])

            def after_h(i, Hh, rh, X, rx):
                Hp = hb[i % 2]
                rp = f'hperm{i % 2}'
                P.op('gpsimd', 'tensor_copy', reads=[rh], writes=[rp], out=Hp.rearrange("t (kc p) -> t kc p", kc=8),
                     in_=Hh.rearrange("t (p kc) -> t kc p", kc=8))
                P.dma('sync', self.hfp[i * 128:(i + 1) * 128, :], Hp, reads=[rp])

            def consumer(q, hTb, hT32b):
                hT32, r32 = hT32b
                i0_ = q * 4
                for j in range(4):
                    for kc in range(8):
                        P.op('tensor', 'matmul', reads=[r32, 'wr'], writes=['ps4'], out=ps[4][:, j * 36:(j + 1) * 36],
                             lhsT=hT32[:, kc, j * 128:(j + 1) * 128], rhs=wr[:, kc, :], start=(kc == 0), stop=(kc == 7))
                V = lambda name, **kw: P.op('vector', name, **kw)
                V('tensor_tensor', reads=['ps4', 'rbias'], writes=['lg'], out=lg4,
                  in0=ps[4][:, 0:144].rearrange("p (j c) -> p j c", j=4), in1=rb.unsqueeze(1).broadcast_to([128, 4, 36]),
                  op=ALU.add)
                gl = lg4[:, :, 0:4]
                el = lg4[:, :, 4:36].rearrange("p j (g e) -> p j g e", g=4)
                b3 = lambda t, n_: t.unsqueeze(2).broadcast_to([128, 4, n_])
                V('tensor_reduce', reads=['lg'], writes=['rsm'], out=gmax4, in_=gl, axis=AX.X, op=ALU.max)
                V('tensor_tensor', reads=['lg', 'rsm'], writes=['ohg'], out=ohg4, in0=gl, in1=b3(gmax4, 4), op=ALU.is_ge)
                V('tensor_tensor', reads=['lg', 'rsm'], writes=['gexp'], out=gexp4, in0=gl, in1=b3(gmax4, 4),
                  op=ALU.subtract)
                P.op('scalar', 'activation', reads=['gexp'], writes=['gexp'], out=gexp4, in_=gexp4, func=AF.Exp)
                V('tensor_reduce', reads=['gexp'], writes=['rsm2'], out=gsum4, in_=gexp4, axis=AX.X, op=ALU.add)
                V('reciprocal', reads=['rsm2'], writes=['rsm2'], out=gsum4, in_=gsum4)
                V('tensor_tensor', reads=['lg', 'ohg'], writes=['em'], out=em4, in0=el,
                  in1=ohg4.unsqueeze(3).broadcast_to([128, 4, 4, 8]), op=ALU.mult)
                V('tensor_reduce', reads=['em'], writes=['esel'], out=es4, in_=em4.rearrange("p j g e -> p j e g"),
                  axis=AX.X, op=ALU.add)
                for j in range(4):
                    V('max', reads=['esel'], writes=['top8'], out=t84[:, j, :], in_=es4[:, j, :])
                V('tensor_tensor', reads=['esel', 'top8'], writes=['sel1'], out=s14, in0=es4,
                  in1=t84[:, :, 0:1].broadcast_to([128, 4, 8]), op=ALU.is_ge)
                V('tensor_tensor', reads=['esel', 'top8'], writes=['sel2'], out=s24, in0=es4,
                  in1=t84[:, :, 1:2].broadcast_to([128, 4, 8]), op=ALU.is_ge)
                V('tensor_tensor', reads=['sel2', 'sel1'], writes=['sel2'], out=s24, in0=s24, in1=s14, op=ALU.subtract)
                V('tensor_tensor', reads=['top8'], writes=['rsm3'], out=r4, in0=t84[:, :, 1], in1=t84[:, :, 0],
                  op=ALU.subtract)
                P.op('scalar', 'activation', reads=['rsm3'], writes=['rsm3'], out=r4, in_=r4, func=AF.Exp)
                V('tensor_scalar', reads=['rsm3'], writes=['rsm4'], out=den4, in0=r4, scalar1=1.0, scalar2=None,
                  op0=ALU.add)
                V('reciprocal', reads=['rsm4'], writes=['rsm4'], out=den4, in_=den4)
                V('tensor_tensor', reads=['rsm4', 'rsm2'], writes=['rw'], out=self.rw[:, i0_:i0_ + 4, 0], in0=den4,
                  in1=gsum4, op=ALU.mult)
                V('tensor_tensor', reads=['rw', 'rsm3'], writes=['rw'], out=self.rw[:, i0_:i0_ + 4, 1],
                  in0=self.rw[:, i0_:i0_ + 4, 0], in1=r4, op=ALU.mult)
                for k, sel in enumerate((s14, s24)):
                    V('tensor_tensor', reads=['ohg', f'sel{k + 1}'], writes=['ohb'],
                      out=self.ohb[:, i0_:i0_ + 4, k * 32:(k + 1) * 32].rearrange("p j (g e) -> p j g e", g=4),
                      in0=ohg4.unsqueeze(3).broadcast_to([128, 4, 4, 8]),
                      in1=sel.unsqueeze(2).broadcast_to([128, 4, 4, 8]), op=ALU.mult)
            self.norm_and_transpose_loop(st, None, gsc, self.mod(3), consumer, pre=pre, after_h=after_h, pre_load=pre_load)
            P.end()

    def phase_moe(self, l, st0):
        P, T, NT, NB, BLK = self.P, self.T, self.NT, self.NB, self.BLK
        ps = self.ps
        last = (l == self.L - 1)
        NR = NB * BLK
        wgv = self.w_gate.rearrange("l e (p two k4) f -> (l e p two) (k4 f)", two=2, k4=4)
        wuv = self.w_up.rearrange("l e (p two k4) f -> (l e p two) (k4 f)", two=2, k4=4)
        wdv = self.w_down.rearrange("l e (p two f2) d -> (l e p two) (f2 d)", two=2, f2=2)
        with contextlib.ExitStack() as st:
            sb = lambda n, s, dt=F32: self.sb(st, n, s, dt)
            dest_i = sb("dest_i", [128, NT, 2], I32)
            widx = sb("widx", [128, NB, 2], I32)
            with contextlib.ExitStack() as s1:
                sb1 = lambda n, s, dt=F32: self.sb(s1, n, s, dt)
                pre = sb1("pre_all", [128, NT, 64])
                tot = sb1("tot_all", [128, NT, 64])
                base = sb1("base_all", [128, NT, 64])
                cnt = sb1("cnt", [128, 64])
                tl = sb1("mtotal", [128, 32])
                md = sb1("mmod", [128, 32])
                pend = sb1("pend", [128, 32])
                off = sb1("moff", [128, 64])
                dest_f = sb1("dest_f", [128, NT, 2])
                blk0 = sb1("blk0", [128, NB])
                cmp_ = sb1("mcmp", [128, NB, 32])
                be = sb1("mbe", [128, NB])
                pidx = sb1("pidx", [128, 2])
                wf = sb1("widx_f", [128, NB, 2])
                dbg_t = sb1("rdbg_t", [128, 68])
                P.begin()
                ohb2 = self.ohb.rearrange("p i c -> p (i c)")
                f2 = lambda t: t.rearrange("p i c -> p (i c)")
                for k, c0 in enumerate(range(0, NT * 64, 512)):
                    n = min(512, NT * 64 - c0)
                    P.op('tensor', 'matmul', reads=['sutb', 'ohb'], writes=['ps0'], out=ps[0][:, 0:n], lhsT=self.sutb,
                         rhs=ohb2[:, c0:c0 + n], start=True, stop=True)
                    P.op('scalar', 'activation', reads=['ps0'], writes=['pre_all'], out=f2(pre)[:, c0:c0 + n],
                         in_=ps[0][:, 0:n], func=AF.Copy)
                    P.op('tensor', 'matmul', reads=['onesb', 'ohb'], writes=['ps1'], out=ps[1][:, 0:n], lhsT=self.onesb,
                         rhs=ohb2[:, c0:c0 + n], start=True, stop=True)
                    P.op('vector', 'tensor_copy', reads=['ps1'], writes=['tot_all'], out=f2(tot)[:, c0:c0 + n],
                         in_=ps[1][:, 0:n])
                V = lambda name, **kw: P.op('vector', name, **kw)
                P.op('gpsimd', 'memset', writes=['base_all'], ap=base[:, 0, :], constant=0.0)
                for i in range(1, NT):
                    V('tensor_tensor', reads=['base_all', 'tot_all'], writes=['base_all'], out=base[:, i, :],
                      in0=base[:, i - 1, :], in1=tot[:, i - 1, :], op=ALU.add)
                V('tensor_tensor', reads=['base_all', 'tot_all'], writes=['cnt'], out=cnt, in0=base[:, NT - 1, :],
                  in1=tot[:, NT - 1, :], op=ALU.add)
                V('tensor_tensor', reads=['cnt'], writes=['mtotal'], out=tl, in0=cnt[:, 0:32], in1=cnt[:, 32:64],
                  op=ALU.add)
                P.op('gpsimd', 'iota', writes=['blk0'], out=blk0, pattern=[[BLK, NB]], base=0, channel_multiplier=0,
                     allow_small_or_imprecise_dtypes=True)
                V('tensor_tensor', reads=['mtotal', 'blk0'], writes=['mcmp'], out=cmp_.rearrange("p b e -> p (b e)").rearrange("p (e b) -> p e b", e=32),
                  in0=blk0.unsqueeze(1).broadcast_to([128, 32, NB]), in1=tl.unsqueeze(2).broadcast_to([128, 32, NB]),
                  op=ALU.is_lt)
                V('tensor_reduce', reads=['mcmp'], writes=['mtotal'], out=tl,
                  in_=cmp_.rearrange("p b e -> p (b e)").rearrange("p (e b) -> p e b", e=32), axis=AX.X, op=ALU.add)
                V('tensor_scalar', reads=['mtotal'], writes=['mtotal'], out=tl, in0=tl, scalar1=float(BLK), scalar2=None,
                  op0=ALU.mult)
                V('tensor_tensor_scan', reads=['mtotal', 'ones'], writes=['pend'], out=pend,
                  data0=self.ones[:, 0:32], data1=tl, initial=0.0, op0=ALU.mult, op1=ALU.add)
                V('tensor_tensor', reads=['pend', 'mtotal'], writes=['moff'], out=off[:, 0:32], in0=pend, in1=tl,
                  op=ALU.subtract)
                V('tensor_tensor', reads=['moff', 'cnt'], writes=['moff'], out=off[:, 32:64], in0=off[:, 0:32],
                  in1=cnt[:, 0:32], op=ALU.add)
                V('tensor_tensor', reads=['pre_all', 'base_all'], writes=['pre_all'], out=pre, in0=pre, in1=base,
                  op=ALU.add)
                V('tensor_tensor', reads=['pre_all', 'moff'], writes=['pre_all'], out=pre, in0=pre,
                  in1=off.unsqueeze(1).broadcast_to([128, NT, 64]), op=ALU.add)
                V('tensor_tensor', reads=['pre_all', 'ohb'], writes=['pre_all'], out=pre, in0=pre, in1=self.ohb,
                  op=ALU.mult)
                V('tensor_reduce', reads=['pre_all'], writes=['dest_f'], out=dest_f,
                  in_=pre.rearrange("p i (k e) -> p i k e", k=2), axis=AX.X, op=ALU.add)
                V('tensor_copy', reads=['dest_f'], writes=['dest_i'], out=dest_i, in_=dest_f)
                V('tensor_tensor', reads=['pend', 'blk0'], writes=['mcmp'], out=cmp_,
                  in0=pend.unsqueeze(1).broadcast_to([128, NB, 32]), in1=blk0.unsqueeze(2).broadcast_to([128, NB, 32]),
                  op=ALU.is_le)
                V('tensor_reduce', reads=['mcmp'], writes=['mbe'], out=be, in_=cmp_, axis=AX.X, op=ALU.add)
                V('tensor_scalar', reads=['mbe'], writes=['mbe'], out=be, in0=be, scalar1=256.0, scalar2=None,
                  op0=ALU.mult)
                P.op('gpsimd', 'iota', writes=['pidx'], out=pidx, pattern=[[1, 2]], base=l * NE * 256, channel_multiplier=2,
                     allow_small_or_imprecise_dtypes=True)
                V('tensor_tensor', reads=['mbe', 'pidx'], writes=['widx_f'], out=wf,
                  in0=be.unsqueeze(2).broadcast_to([128, NB, 2]), in1=pidx.unsqueeze(1).broadcast_to([128, NB, 2]),
                  op=ALU.add)
                V('tensor_copy', reads=['widx_f'], writes=['widx'], out=widx, in_=wf)
                if 'rdbg' in self.dbg:
                    for i in range(NT):
                        V('tensor_copy', reads=['ohb'], writes=['rdbg_t'], out=dbg_t[:, 0:64], in_=self.ohb[:, i, :])
                        V('tensor_copy', reads=['rw'], writes=['rdbg_t'], out=dbg_t[:, 64:66], in_=self.rw[:, i, :])
                        V('tensor_copy', reads=['dest_f'], writes=['rdbg_t'], out=dbg_t[:, 66:68], in_=dest_f[:, i, :])
                        P.dma('sync', self.rdbg[i * 128:(i + 1) * 128, :], dbg_t, reads=['rdbg_t'])
                P.end()
            if '1' in self.opt:
                return
            with contextlib.ExitStack() as s2:
                hrow = [self.sb(s2, f"hrow{i}", [128, D], BF16) for i in range(3)]
                P.begin()
                P.raw('gpsimd', 'reg_mov', self.reg_rows, NR - 1)
                for i in range(NT):
                    Hr = hrow[i % 3]
                    rr = f'hrow{i % 3}'
                    P.dma('sync', Hr, self.hfp[i * 128:(i + 1) * 128, :], writes=[rr])
                    for k in range(2):
                        P.dma('gpsimd', None, None, reads=[rr, 'dest_i'], writes=['xs'],
                              fn=('indirect_dma_start', dict(
                                  out=self.xs, out_offset=bass.IndirectOffsetOnAxis(ap=dest_i[:, i, k:k + 1], axis=0),
                                  in_=Hr, in_offset=None, bounds_check=self.reg_rows, oob_is_err=False)))
                P.end()
            if '2' in self.opt:
                return
            with contextlib.ExitStack() as s3:
                sb3 = lambda n, s, dt=F32: self.sb(s3, n, s, dt)
                Wg = [sb3(f"Wg{i}", [128, 8, 512], BF16) for i in range(2)]
                Wu = [sb3(f"Wu{i}", [128, 8, 512], BF16) for i in range(2)]
                Wd = [sb3(f"Wd{i}", [128, 4, 1024], BF16) for i in range(2)]
                xsT = [sb3(f"xsT{i}", [128, 8, 512], BF16) for i in range(2)]
                hid = [sb3(f"hid{i}", [128, 4, 512], BF16) for i in range(2)]
                sg = [sb3(f"sg{i}", [128, 512]) for i in range(2)]
                yb = [sb3(f"yb{i}", [128, D]) for i in range(2)]
                P.begin()
                P.raw('gpsimd', 'reg_mov', self.reg_w, (l + 1) * NE * 256 - 1)
                n_y = [0]

                def load_blk(b):
                    a = b % 2
                    for (Wt, view, nm) in ((Wg[a], wgv, f'Wg{a}'), (Wu[a], wuv, f'Wu{a}'), (Wd[a], wdv, f'Wd{a}')):
                        flat = Wt.rearrange("p a f -> p (a f)")
                        for hf in range(0 if 'G' in self.opt else 2):
                            P.dma('gpsimd', None, None, reads=['widx'], writes=[nm],
                                  fn=('indirect_dma_start', dict(
                                      out=flat[:, hf * 2048:(hf + 1) * 2048], out_offset=None, in_=view,
                                      in_offset=bass.IndirectOffsetOnAxis(ap=widx[:, b, hf:hf + 1], axis=0),
                                      bounds_check=self.reg_w, oob_is_err=False)))
                    X = xsT[a]
                    for kc in range(8):
                        P.dma('sync', None, None, reads=['xs'], writes=[f'xsT{a}'],
                              fn=('dma_start_transpose', dict(
                                  out=X[:, kc, :], in_=self.xs[b * BLK:(b + 1) * BLK, kc * 128:(kc + 1) * 128])))

                def compute_blk(b):
                    a = b % 2
                    X = xsT[a]
                    rxs = f'xsT{a}'
                    if 'C' in self.opt:
                        return
                    Hd = hid[a]
                    rh = f'hid{a}'
                    wg4 = Wg[a].rearrange("p kc (m four) -> p kc four m", four=4)
                    wu4 = Wu[a].rearrange("p kc (m four) -> p kc four m", four=4)
                    for fc in range(4):
                        pg_, pu_ = 2 + (fc % 2) * 2, 3 + (fc % 2) * 2
                        for kc in range(8):
                            P.op('tensor', 'matmul', reads=[f'Wg{a}', rxs], writes=[f'ps{pg_}'], out=ps[pg_],
                                 lhsT=wg4[:, kc, fc, :], rhs=X[:, kc, :], start=(kc == 0), stop=(kc == 7))
                        for kc in range(8):
                            P.op('tensor', 'matmul', reads=[f'Wu{a}', rxs], writes=[f'ps{pu_}'], out=ps[pu_],
                                 lhsT=wu4[:, kc, fc, :], rhs=X[:, kc, :], start=(kc == 0), stop=(kc == 7))
                        S = sg[fc % 2]
                        P.op('scalar', 'activation', reads=[f'ps{pg_}'], writes=[f'sg{fc % 2}'], out=S, in_=ps[pg_],
                             func=AF.Silu)
                        P.op('vector', 'tensor_tensor', reads=[f'sg{fc % 2}', f'ps{pu_}'], writes=[rh], out=Hd[:, fc, :],
                             in0=S, in1=ps[pu_], op=ALU.mult)
                    for rt in range(4):
                        n_y[0] += 1
                        Y = yb[n_y[0] % 2]
                        ry = f'yb{n_y[0] % 2}'
                        for half in range(2):
                            pb = 6 + half
                            for fc in range(4):
                                P.op('tensor', 'matmul', reads=[rh, f'Wd{a}'], writes=[f'ps{pb}'], out=ps[pb],
                                     lhsT=Hd[:, fc, rt * 128:(rt + 1) * 128], rhs=Wd[a][:, fc, half * 512:(half + 1) * 512],
                                     start=(fc == 0), stop=(fc == 3))
                            self.evac(half, Y[:, half * 512:(half + 1) * 512], ps[pb], [f'ps{pb}'], [ry])
                        r0 = b * BLK + rt * 128
                        P.dma('sync', self.ys[r0:r0 + 128, :], Y, reads=[ry], writes=['ys'])

                load_blk(0)
                for b in range(NB):
                    if b + 1 < NB:
                        load_blk(b + 1)
                    compute_blk(b)
                P.end()
            if '3' in self.opt:
                return
            with contextlib.ExitStack() as s4:
                sb4 = lambda n, s, dt=F32: self.sb(s4, n, s, dt)
                y1 = [sb4(f"y1_{i}", [128, D]) for i in range(2)]
                y2 = [sb4(f"y2_{i}", [128, D]) for i in range(2)]
                xr = [sb4(f"xr{i}", [128, D]) for i in range(2)]
                fgb = sb4("fgb", [128, D])
                fsq = sb4("fsq", [128, D])
                fss = sb4("fss", [128, 1])
                frs = sb4("frs", [128, 1])
                P.begin()
                P.raw('gpsimd', 'reg_mov', self.reg_rows, NR - 1)
                if last:
                    P.dma('sync', fgb, self.final_g.partition_broadcast(128), writes=['fgb'])
                def load_tile(i):
                    a = i % 2
                    rows = slice(i * 128, (i + 1) * 128)
                    P.dma('sync', xr[a], self.xres[rows, :], writes=[f'xr{a}'])
                    for (Yk, rk, k) in ((y1[a], f'y1_{a}', 0), (y2[a], f'y2_{a}', 1)):
                        P.dma('gpsimd', None, None, reads=['dest_i'], writes=[rk],
                              fn=('indirect_dma_start', dict(
                                  out=Yk, out_offset=None, in_=self.ys,
                                  in_offset=bass.IndirectOffsetOnAxis(ap=dest_i[:, i, k:k + 1], axis=0),
                                  bounds_check=self.reg_rows, oob_is_err=False)))
                load_tile(0)
                for i in range(NT):
                    a = i % 2
                    Y1, Y2, X = y1[a], y2[a], xr[a]
                    r1, r2, rx = f'y1_{a}', f'y2_{a}', f'xr{a}'
                    rows = slice(i * 128, (i + 1) * 128)
                    if i + 1 < NT:
                        load_tile(i + 1)
                    P.op('vector', 'tensor_scalar', reads=[r1, 'rw'], writes=[r1], out=Y1, in0=Y1,
                         scalar1=self.rw[:, i, 0:1], scalar2=None, op0=ALU.mult)
                    P.op('vector', 'scalar_tensor_tensor', reads=[r1, r2, 'rw'], writes=[r1], out=Y1, in0=Y2,
                         scalar=self.rw[:, i, 1:2], in1=Y1, op0=ALU.mult, op1=ALU.add)
                    P.op('gpsimd', 'tensor_tensor', reads=[r1, 'modb'], writes=[r1], out=Y1, in0=Y1, in1=self.mod(5),
                         op=ALU.mult)
                    P.op('gpsimd', 'tensor_tensor', reads=[r1, rx], writes=[rx], out=X, in0=X, in1=Y1, op=ALU.add)
                    if not last:
                        P.dma('sync', self.xres[rows, :], X, reads=[rx])
                    else:
                        P.op('scalar', 'activation', reads=[rx], writes=['fsq', 'fss'], out=fsq, in_=X, func=AF.Square,
                             accum_out=fss)
                        self.rstd_ops(fss, frs, D, ['fss'], ['frs'])
                        P.op('vector', 'scalar_tensor_tensor', reads=[rx, 'frs', 'fgb'], writes=[r2], out=Y2, in0=X,
                             scalar=frs, in1=fgb, op0=ALU.mult, op1=ALU.mult)
                        P.dma('sync', self.out[rows, :], Y2, reads=[r2])
                P.end()

    def build_layer(self, l):
        with contextlib.ExitStack() as st:
            ph = self.phases
            self.phase_ada(l, st)
            if 'i' in ph:
                self.phase_inproj(l, st)
            if 'c' in ph and 'f' in ph:
                self.phase_attn(l, conv_inside=True)
            elif 'c' in ph:
                self.phase_conv(l)
            elif 'f' in ph:
                self.phase_attn(l)
            if 's' in ph:
                self.phase_ssd(l)
            if 'o' in ph:
                self.phase_wout_router(l, st)
            if 'm' in ph and self.moe:
                self.phase_moe(l, st)

    def zero_xs(self):
        P = self.P
        NR = self.NB * self.BLK
        with contextlib.ExitStack() as st:
            z = self.sb(st, "zeros", [128, 8192], BF16)
            P.begin()
            P.op('gpsimd', 'memset', writes=['zeros'], ap=z, constant=0.0)
            xv = self.xs.rearrange("(a p r) d -> a p (r d)", p=128, r=8)
            for a in range(NR // 1024):
                P.dma('sync', xv[a], z, reads=['zeros'])
            P.end()

    def build(self):
        if self.moe:
            self.zero_xs()
        for l in range(self.L):
            self.build_layer(l)
        return self.nc


def prep_core_inputs(inp, b, T, L):
    f = lambda a: np.ascontiguousarray(a, dtype=np.float32)
    m = {}
    m["x"] = f(inp["x"][b, :T])
    m["c"] = f(inp["c"][b].reshape(8, 128).T)
    for k in ("ada_w", "ada_b", "norm_mix_g", "w_in", "ssd_dt_bias", "ssd_a_log", "ssd_d", "ssd_norm_g",
              "w_out", "norm_ffn_g", "w_router_group", "b_router_group", "w_router_expert", "b_router_expert",
              "w_gate", "w_up", "w_down"):
        m[k] = f(inp[k][:L])
    m["final_norm_g"] = f(inp["final_norm_g"])
    m["convw_fm"] = f(inp["ssd_conv_w"][:L].reshape(L, 4, 8, 128).transpose(0, 3, 2, 1))
    m["convb_fm"] = f(inp["ssd_conv_b"][:L].reshape(L, 8, 128).transpose(0, 2, 1))
    m["fox_f_bias_fm"] = f(inp["fox_f_bias"][:L].reshape(L, 4, 1))
    m["fox_g_fm"] = f(inp["fox_norm_g"][:L].reshape(L, 2, 128).transpose(0, 2, 1))
    m["cmw_fm"] = f(inp["cm_conv_w"][:L].reshape(L, 31, 2, 128).transpose(0, 3, 2, 1))
    m["cmb_fm"] = f(inp["cm_conv_b"][:L].reshape(L, 2, 128).transpose(0, 2, 1))
    m["cmg_fm"] = f(inp["cm_ln_g"][:L].reshape(L, 2, 128).transpose(0, 2, 1))
    m["cmbeta_fm"] = f(inp["cm_ln_b"][:L].reshape(L, 2, 128).transpose(0, 2, 1))
    return m


_CACHE = {}
T_FULL, L_FULL, B_FULL = 8192, 4, 4


def kernel(**inputs):
    if 'nc' not in _CACHE:
        bd = Builder(T_FULL, L_FULL)
        _CACHE['nc'] = bd.build()
        _CACHE['names'] = set(bd.ins)
    nc = _CACHE['nc']
    inp = {k: np.asarray(v) for k, v in inputs.items()}
    maps = []
    for b in range(B_FULL):
        m = prep_core_inputs(inp, b, T_FULL, L_FULL)
        maps.append({k: v for k, v in m.items() if k in _CACHE['names']})
    res = run_bass_kernel_spmd(nc, maps, core_ids=list(range(B_FULL)))
    out = np.stack([np.asarray(r["out"], dtype=np.float32) for r in res.results], axis=0)
    return out
```

```python
import contextlib
import numpy as np
import concourse.bass as bass
import concourse.mybir as mybir
from concourse.bass_utils import run_bass_kernel_spmd

F32 = mybir.dt.float32
BF16 = mybir.dt.bfloat16
I32 = mybir.dt.int32
AF = mybir.ActivationFunctionType
ALU = mybir.AluOpType
AX = mybir.AxisListType

ENGS = ('tensor', 'vector', 'scalar', 'gpsimd', 'sync')
CENGS = ('tensor', 'vector', 'scalar', 'gpsimd')
NDS = 10

D = 1024
D_IN = 2828
EPS = 1e-6
NE = 32
DE = 512


class Prog:
    def __init__(self, nc, same_engine_sync=True):
        self.nc = nc
        self.same = same_engine_sync
        self.esem = {e: nc.alloc_semaphore(name=f"es_{e}") for e in CENGS}
        self.dsem = {e: [nc.alloc_semaphore(name=f"ds_{e}_{i}") for i in range(NDS)]
                     for e in ('sync', 'scalar', 'gpsimd')}
        self._reset()
        self.q = None

    def _reset(self):
        keep = getattr(self, 'dcnt', {}).get('gpsimd', [0] * NDS)
        self.ecnt = {e: 0 for e in CENGS}
        self.dcnt = {e: [0] * NDS for e in self.dsem}
        self.dcnt['gpsimd'] = list(keep)
        self.dnext = {e: 0 for e in self.dsem}
        self.known = {e: {('d', 'gpsimd', i): keep[i] for i in range(NDS)} for e in ENGS}
        self.res_w = {}
        self.res_r = {}

    def semof(self, key):
        if key[0] == 'e':
            return self.esem[key[1]]
        return self.dsem[key[1]][key[2]]

    def begin(self):
        self.q = {e: [] for e in ENGS}

    def _deps(self, reads, writes):
        deps = []
        for r in reads:
            t = self.res_w.get(r)
            if t is not None:
                deps.append(t)
        for w in writes:
            t = self.res_w.get(w)
            if t is not None:
                deps.append(t)
            deps.extend(self.res_r.get(w, {}).items())
        return deps

    def _record(self, tok, reads, writes):
        for r in reads:
            d = self.res_r.setdefault(r, {})
            if d.get(tok[0], 0) < tok[1]:
                d[tok[0]] = tok[1]
        for w in writes:
            self.res_w[w] = tok
            self.res_r[w] = {}

    def _waits(self, eng, deps):
        waits = []
        best = {}
        for (key, c) in deps:
            if best.get(key, 0) < c:
                best[key] = c
        for key, c in best.items():
            if key == ('e', 'tensor') and eng == 'tensor':
                continue
            if (not self.same) and key == ('e', eng):
                continue
            if self.known[eng].get(key, 0) >= c:
                continue
            self.known[eng][key] = c
            waits.append((self.semof(key), c))
        return waits

    def op(self, eng, name, reads=(), writes=(), **kw):
        writes = list(writes) + [r for r in reads if r.startswith('ps') and r not in writes]
        waits = self._waits(eng, self._deps(reads, writes))
        self.ecnt[eng] += 1
        tok = (('e', eng), self.ecnt[eng])
        sem = self.esem[eng]

        def emit(e, name=name, kw=kw, waits=waits, sem=sem):
            for (s, c) in waits:
                e.wait_ge(s, c)
            getattr(e, name)(**kw).then_inc(sem, 1)
        self.q[eng].append(emit)
        self._record(tok, reads, writes)
        return tok

    def raw(self, eng, name, *args, **kw):
        self.q[eng].append(lambda e: getattr(e, name)(*args, **kw))

    def dma(self, eng, out, in_, reads=(), writes=(), fn=None, **kw):
        deps = self._deps(reads, writes)
        idx = self.dnext[eng]
        self.dnext[eng] = (idx + 1) % NDS
        key = ('d', eng, idx)
        prev = self.dcnt[eng][idx]
        if prev:
            deps.append((key, prev))
        waits = self._waits(eng, deps)
        self.dcnt[eng][idx] += 16
        tok = (key, self.dcnt[eng][idx])
        sem = self.dsem[eng][idx]

        def emit(e, waits=waits, sem=sem):
            for (s, c) in waits:
                e.wait_ge(s, c)
            if fn is not None:
                try:
                    ins = getattr(e, fn[0])(**fn[1])
                except Exception:
                    print("DMA builder failed:", fn[0], {k: (v.shape if hasattr(v, 'shape') else v) for k, v in fn[1].items()})
                    raise
                ins.then_inc(sem, 16)
            else:
                e.dma_start(out=out, in_=in_, **kw).then_inc(sem, 16)
        self.q[eng].append(emit)
        self._record(tok, reads, writes)
        return tok

    def end(self):
        nc = self.nc
        fin = []
        for e in self.dsem:
            for i in range(NDS):
                c = self.dcnt[e][i]
                if c and self.known['sync'].get(('d', e, i), 0) < c:
                    fin.append((self.dsem[e][i], c))

        def drain(e, fin=fin):
            for (s, c) in fin:
                e.wait_ge(s, c)
        self.q['sync'].append(drain)
        with nc.Block() as block:
            for en in ENGS:
                ops = self.q[en]
                if not ops:
                    continue

                def body(e, ops=ops):
                    for o in ops:
                        o(e)
                getattr(block, en)(body)
        allsems = list(self.esem.values()) + [s for e in self.dsem if e != 'gpsimd' for s in self.dsem[e]]
        with nc.Block() as block:
            def clr(e):
                for s in allsems:
                    e.sem_clear(s)
            block.sync(clr)
        self._reset()
        self.q = None


class Builder:
    def __init__(self, T, L, dbg=(), moe=True, phases='aicfsom', opt=''):
        self.opt = opt
        self.T, self.L = T, L
        self.moe = moe
        self.phases = phases
        self.NT = T // 128
        self.NQ = T // 512
        self.dbg = set(dbg)
        nc = self.nc = bass.Bass("TRN2", target_bir_lowering=False)
        self.P = Prog(nc, same_engine_sync=('S' not in self.opt))
        self.ins = {}
        self.outs = {}
        self._uid = 0
        self.declare_io()
        self.consts()

    def din(self, name, shape, dt=F32):
        t = self.nc.dram_tensor(name, list(shape), dt, kind="ExternalInput").ap()
        self.ins[name] = t
        return t

    def dscr(self, name, shape, dt=F32):
        kind = "ExternalOutput" if name in self.dbg else "Internal"
        t = self.nc.dram_tensor(name, list(shape), dt, kind=kind).ap()
        if kind == "ExternalOutput":
            self.outs[name] = t
        return t

    def sb(self, st, name, shape, dt=F32):
        self._uid += 1
        return st.enter_context(self.nc.sbuf_tensor(f"{name}_{self._uid}", list(shape), dt)).ap()

    def declare_io(self):
        T, L = self.T, self.L
        d = self.din
        self.x_in = d("x", [T, D])
        self.c_in = d("c", [128, 8])
        self.ada_w = d("ada_w", [L, D, 6 * D])
        self.ada_b = d("ada_b", [L, 6 * D])
        self.norm_mix_g = d("norm_mix_g", [L, D])
        self.w_in = d("w_in", [L, D, D_IN])
        self.convw = d("convw_fm", [L, 128, 8, 4])
        self.convb = d("convb_fm", [L, 128, 8])
        self.dt_bias = d("ssd_dt_bias", [L, 8])
        self.a_log = d("ssd_a_log", [L, 8])
        self.ssd_d = d("ssd_d", [L, 8])
        self.ssd_norm_g = d("ssd_norm_g", [L, 512])
        self.f_bias = d("fox_f_bias_fm", [L, 4, 1])
        self.fox_g = d("fox_g_fm", [L, 128, 2])
        self.cmw = d("cmw_fm", [L, 128, 2, 31])
        self.cmb = d("cmb_fm", [L, 128, 2])
        self.cmg = d("cmg_fm", [L, 128, 2])
        self.cmbeta = d("cmbeta_fm", [L, 128, 2])
        self.w_out = d("w_out", [L, D, D])
        self.norm_ffn_g = d("norm_ffn_g", [L, D])
        self.w_rg = d("w_router_group", [L, D, 4])
        self.b_rg = d("b_router_group", [L, 4])
        self.w_re = d("w_router_expert", [L, D, NE])
        self.b_re = d("b_router_expert", [L, NE])
        if self.moe:
            self.w_gate = d("w_gate", [L, NE, D, DE])
            self.w_up = d("w_up", [L, NE, D, DE])
            self.w_down = d("w_down", [L, NE, DE, D])
        self.final_g = d("final_norm_g", [D])
        self.out = self.nc.dram_tensor("out", [T, D], F32, kind="ExternalOutput").ap()
        self.outs["out"] = self.out
        s = self.dscr
        self.xres = s("xres", [T, D])
        self.zs = s("zs", [T, 512])
        self.xbcT = s("xbcT", [1024, T])
        self.dtr = s("dtr", [T, 8])
        self.flT = s("flT", [4, T])
        self.qT = s("qT", [256, T], BF16)
        self.kT = s("kT", [256, T], BF16)
        self.vtok = s("vtok", [T, 256], BF16)
        self.gaT = s("gaT", [256, T])
        self.gbT = s("gbT", [256, T])
        self.yT = s("yT", [1024, T], BF16)
        self.attT = s("attT", [256, T])
        self.cs = s("cs", [4, 6, T], BF16)
        self.hfp = s("hfp", [T, D], BF16)
        self.BLK = 512
        self.NB = (2 * T) // self.BLK + NE
        self.xs = s("xs", [self.NB * self.BLK, D], BF16)
        self.ys = s("ys", [self.NB * self.BLK, D])
        self.rdbg = s("rdbg", [T, 68])

    def consts(self):
        nc, P = self.nc, self.P
        a = lambda n, s, dt=F32: nc.alloc_sbuf_tensor(n, list(s), dt).ap()
        self.ident = a("ident", [128, 128])
        self.identb = a("identb", [128, 128], BF16)
        self.tri = a("tri", [128, 128])
        self.ustr = a("ustr", [128, 128])
        self.ones = a("ones", [128, 128])
        self.onesb = a("onesb", [128, 128], BF16)
        self.epsb = a("epsb", [128, 1])
        self.sutb = a("sutb", [128, 128], BF16)
        self.ps = [nc.alloc_psum_tensor(f"ps{i}", [128, 512], F32).ap() for i in range(8)]
        self.reg_rows = nc.gpsimd.alloc_register("bc_rows")
        self.reg_w = nc.gpsimd.alloc_register("bc_w")
        P.begin()
        g = 'gpsimd'
        P.op(g, 'memset', writes=['epsb'], ap=self.epsb, constant=EPS)
        P.op(g, 'memset', writes=['ones'], ap=self.ones, constant=1.0)
        P.op(g, 'memset', writes=['onesb'], ap=self.onesb, constant=1.0)
        P.op(g, 'memset', writes=['ident'], ap=self.ident, constant=1.0)
        P.op(g, 'affine_select', reads=['ident'], writes=['ident'], out=self.ident, in_=self.ident,
             pattern=[[-1, 128]], compare_op=ALU.is_equal, fill=0.0, base=0, channel_multiplier=1)
        P.op(g, 'tensor_copy', reads=['ident'], writes=['identb'], out=self.identb, in_=self.ident)
        P.op(g, 'memset', writes=['tri'], ap=self.tri, constant=1.0)
        P.op(g, 'affine_select', reads=['tri'], writes=['tri'], out=self.tri, in_=self.tri,
             pattern=[[1, 128]], compare_op=ALU.is_ge, fill=0.0, base=0, channel_multiplier=-1)
        P.op(g, 'tensor_tensor', reads=['tri', 'ident'], writes=['sutb'], out=self.sutb, in0=self.tri, in1=self.ident,
             op=ALU.subtract)
        P.op(g, 'memset', writes=['ustr'], ap=self.ustr, constant=1.0)
        P.op(g, 'affine_select', reads=['ustr'], writes=['ustr'], out=self.ustr, in_=self.ustr,
             pattern=[[-1, 128]], compare_op=ALU.is_gt, fill=0.0, base=0, channel_multiplier=1)
        P.end()

    def phase_ada(self, l, st):
        nc, P = self.nc, self.P
        self.modb = self.sb(st, "modb", [128, 6 * D])
        with contextlib.ExitStack() as s2:
            cs = self.sb(s2, "c_s", [128, 8])
            cb = self.sb(s2, "c_b", [128, 8, 128])
            adab = self.sb(s2, "adab", [128, 6 * D])
            wch = [self.sb(s2, f"adaw{i}", [128, 8, 512]) for i in range(2)]
            P.begin()
            P.dma('sync', cs, self.c_in, writes=['c_s'])
            P.dma('sync', adab, self.ada_b[l].partition_broadcast(128), writes=['adab'])
            P.op('scalar', 'activation', reads=['c_s'], writes=['c_s'], out=cs, in_=cs, func=AF.Silu)
            P.op('vector', 'tensor_copy', reads=['c_s'], writes=['c_b'], out=cb,
                 in_=cs.unsqueeze(2).broadcast_to([128, 8, 128]))
            for j in range(12):
                w = wch[j % 2]
                wr = f'adaw{j % 2}'
                P.dma('sync', w,
                      self.ada_w[l][:, j * 512:(j + 1) * 512].rearrange("(kc p) n -> p kc n", p=128),
                      writes=[wr])
                pt = self.ps[j % 2]
                for kc in range(8):
                    P.op('tensor', 'matmul', reads=['c_b', wr], writes=[f'ps{j % 2}'], out=pt,
                         lhsT=cb[:, kc, :], rhs=w[:, kc, :], start=(kc == 0), stop=(kc == 7))
                P.op('vector', 'tensor_tensor', reads=[f'ps{j % 2}', 'adab'], writes=['modb'],
                     out=self.modb[:, j * 512:(j + 1) * 512], in0=pt, in1=adab[:, j * 512:(j + 1) * 512], op=ALU.add)
            P.end()

    def mod(self, i):
        return self.modb[:, i * D:(i + 1) * D]

    def load_cast(self, st, dst, src, res, width):
        P = self.P
        if getattr(self, '_stg_owner', None) is not st:
            self._stg = [self.sb(st, f"stg{i}", [128, 2048]) for i in range(2)]
            self._stg_owner = st
            self._stg_n = 0
        for c0 in range(0, width, 2048):
            n = min(2048, width - c0)
            k = self._stg_n
            self._stg_n += 1
            S = self._stg[k % 2]
            rs = f'stg{k % 2}'
            P.dma('sync', S[:, 0:n], src[:, c0:c0 + n], writes=[rs])
            P.op('gpsimd' if k % 2 else 'vector', 'tensor_copy', reads=[rs], writes=[res], out=dst[:, c0:c0 + n],
                 in_=S[:, 0:n])

    def rstd_ops(self, ss, rstd, n, rd, wr):
        P = self.P
        P.op('scalar', 'activation', reads=rd, writes=wr, out=rstd, in_=ss, func=AF.Ln,
             bias=self.epsb[:ss.shape[0], :], scale=1.0 / n)
        P.op('scalar', 'activation', reads=wr, writes=wr, out=rstd, in_=rstd, func=AF.Exp, scale=-0.5)

    def phase_inproj(self, l, st0):
        nc, P, T = self.nc, self.P, self.T
        src = self.x_in if l == 0 else self.xres
        self._ip_bufs = None
        with contextlib.ExitStack() as st:
            sb = lambda n, s, dt=F32: self.sb(st, n, s, dt)
            wz = sb("wz", [128, 8, 512], BF16)
            wx = sb("wx", [128, 8, 1024], BF16)
            wqk = sb("wqk", [128, 8, 512], BF16)
            wv = sb("wv", [128, 8, 256], BF16)
            wg = sb("wg", [128, 8, 512], BF16)
            wdt = sb("wdt", [128, 8, 8])
            wf = sb("wf", [128, 8, 4])
            gsc = sb("gsc", [128, D])
            W = self.w_in[l].rearrange("(kc p) n -> p kc n", p=128)
            P.begin()
            for kc in range(8):
                self.load_cast(st, wz[:, kc, :], W[:, kc, 0:512], 'wz', 512)
                self.load_cast(st, wx[:, kc, :], W[:, kc, 512:1536], 'wx', 1024)
                self.load_cast(st, wqk[:, kc, :], W[:, kc, 1544:2056], 'wqk', 512)
                self.load_cast(st, wv[:, kc, :], W[:, kc, 2056:2312], 'wv', 256)
                self.load_cast(st, wg[:, kc, :], W[:, kc, 2316:2828], 'wg', 512)
            P.dma('sync', wdt, W[:, :, 1536:1544], writes=['wdt'])
            P.dma('sync', wf, W[:, :, 2312:2316], writes=['wf'])
            P.dma('sync', gsc, self.norm_mix_g[l].partition_broadcast(128), writes=['gsc'])
            P.op('vector', 'scalar_tensor_tensor', reads=['gsc', 'modb'], writes=['gsc'], out=gsc,
                 in0=self.mod(1), scalar=1.0, in1=gsc, op0=ALU.add, op1=ALU.mult)
            self.norm_and_transpose_loop(st, src, gsc, self.mod(0), consumer=lambda q, hT, hT32: self.inproj_chunk(
                q, hT, hT32, wz, wx, wqk, wv, wg, wdt, wf, st))
            P.end()

    def norm_and_transpose_loop(self, st, src, gsc, shift, consumer, pre=None, after_h=None, pre_load=None):
        P = self.P
        sb = lambda n, s, dt=F32: self.sb(st, n, s, dt)
        NXB = 4
        xt = [sb(f"xt{i}", [128, D]) for i in range(NXB)]
        ht = [sb(f"ht{i}", [128, D]) for i in range(2)]
        sq = sb("sq", [128, D])
        ss = [sb(f"ss{i}", [128, 1]) for i in range(2)]
        rs = [sb(f"rs{i}", [128, 1]) for i in range(2)]
        hT32 = [sb(f"hT32_{i}", [128, 8, 512]) for i in range(2)]
        hT = [sb(f"hT_{i}", [128, 8, 512], BF16) for i in range(2)]
        PF = 2

        def load(i):
            if i >= self.NT:
                return
            if pre_load is not None:
                pre_load(i)
            elif pre is None:
                P.dma('sync', xt[i % NXB], src[i * 128:(i + 1) * 128, :], writes=[f'xt{i % NXB}'])
        for i in range(PF):
            load(i)
        for q in range(self.NQ):
            b = q % 2
            for j in range(4):
                i = q * 4 + j
                a = i % 2
                X, H = xt[i % NXB], ht[a]
                rx, rh = f'xt{i % NXB}', f'ht{a}'
                load(i + PF)
                if pre is not None:
                    pre(i, X, rx)
                P.op('scalar', 'activation', reads=[rx], writes=['sq', f'ss{a}'], out=sq, in_=X, func=AF.Square,
                     accum_out=ss[a])
                self.rstd_ops(ss[a], rs[a], D, [f'ss{a}'], [f'rs{a}'])
                P.op('vector', 'scalar_tensor_tensor', reads=[rx, f'rs{a}', 'gsc'], writes=[rh], out=H, in0=X,
                     scalar=rs[a], in1=gsc, op0=ALU.mult, op1=ALU.mult)
                P.op('vector', 'tensor_tensor', reads=[rh, 'modb'], writes=[rh], out=H, in0=H, in1=shift, op=ALU.add)
                if after_h is not None:
                    after_h(i, H, rh, X, rx)
                for half in range(2):
                    pt = self.ps[half]
                    for k4 in range(4):
                        kc = half * 4 + k4
                        P.op('tensor', 'transpose', reads=[rh, 'ident'], writes=[f'ps{half}'],
                             out=pt[:, k4 * 128:(k4 + 1) * 128], in_=H[:, kc * 128:(kc + 1) * 128], identity=self.ident)
                    dst = hT32[b][:, half * 4:(half + 1) * 4, j * 128:(j + 1) * 128]
                    P.op('scalar', 'activation', reads=[f'ps{half}'], writes=[f'hT32_{b}'], out=dst,
                         in_=pt.rearrange("p (k t) -> p k t", k=4), func=AF.Copy)
                P.op('vector', 'tensor_copy', reads=[f'hT32_{b}'], writes=[f'hT_{b}'],
                     out=hT[b][:, :, j * 128:(j + 1) * 128], in_=hT32[b][:, :, j * 128:(j + 1) * 128])
            consumer(q, (hT[b], f'hT_{b}'), (hT32[b], f'hT32_{b}'))

    def evac(self, k, out, in_, reads, writes, scale=None):
        P = self.P
        if k % 2 == 0:
            if scale is None:
                P.op('scalar', 'activation', reads=reads, writes=writes, out=out, in_=in_, func=AF.Copy)
            else:
                P.op('scalar', 'activation', reads=reads, writes=writes, out=out, in_=in_, func=AF.Copy, scale=scale)
        else:
            if scale is None:
                P.op('vector', 'tensor_copy', reads=reads, writes=writes, out=out, in_=in_)
            else:
                P.op('vector', 'tensor_scalar', reads=reads, writes=writes, out=out, in0=in_, scalar1=scale,
                     scalar2=None, op0=ALU.mult)

    def inproj_chunk(self, q, hTb, hT32b, wz, wx, wqk, wv, wg, wdt, wf, st):
        P = self.P
        hT, rhT = hTb
        hT32, rhT32 = hT32b
        if self._ip_bufs is None:
            sb = lambda n, s, dt=F32: self.sb(st, n, s, dt)
            self._ip_bufs = dict(
                o32=[sb(f"o32_{i}", [128, 512]) for i in range(3)],
                o16=[sb(f"o16_{i}", [128, 512], BF16) for i in range(3)],
                osm=[sb(f"osm_{i}", [128, 8]) for i in range(2)],
                ofl=[sb(f"ofl_{i}", [4, 512]) for i in range(2)],
                n=[0],
            )
        B = self._ip_bufs
        tok = slice(q * 512, (q + 1) * 512)

        def nxt():
            B['n'][0] += 1
            return B['n'][0]
        PB = [2, 3, 4, 5]

        def fm(w, wres, c0, dst, dt16=False, scale=None):
            k = nxt()
            pb = PB[k % 4]
            pt = self.ps[pb]
            for kc in range(8):
                P.op('tensor', 'matmul', reads=[wres, rhT], writes=[f'ps{pb}'], out=pt, lhsT=w[:, kc, c0:c0 + 128],
                     rhs=hT[:, kc, :], start=(kc == 0), stop=(kc == 7))
            o = (B['o16'] if dt16 else B['o32'])[k % 3]
            ores = ('o16_' if dt16 else 'o32_') + str(k % 3)
            self.evac(k, o, pt, [f'ps{pb}'], [ores], scale=scale)
            P.dma('sync', dst, o, reads=[ores], writes=[])
        for ct in range(8):
            fm(wx, 'wx', ct * 128, self.xbcT[ct * 128:(ct + 1) * 128, tok])
        for ct in range(2):
            fm(wqk, 'wqk', ct * 128, self.qT[ct * 128:(ct + 1) * 128, tok], dt16=True, scale=0.125)
        for ct in range(2):
            fm(wqk, 'wqk', 256 + ct * 128, self.kT[ct * 128:(ct + 1) * 128, tok], dt16=True)
        for ct in range(2):
            fm(wg, 'wg', ct * 128, self.gaT[ct * 128:(ct + 1) * 128, tok])
        for ct in range(2):
            fm(wg, 'wg', 256 + ct * 128, self.gbT[ct * 128:(ct + 1) * 128, tok])
        k = nxt()
        pb = PB[k % 4]
        pt = self.ps[pb]
        for kc in range(8):
            P.op('tensor', 'matmul', reads=['wf', rhT32], writes=[f'ps{pb}'], out=pt[0:4, :], lhsT=wf[:, kc, :],
                 rhs=hT32[:, kc, :], start=(kc == 0), stop=(kc == 7))
        o = B['ofl'][q % 2]
        P.op('vector', 'tensor_copy', reads=[f'ps{pb}'], writes=[f'ofl_{q % 2}'], out=o, in_=pt[0:4, :])
        P.dma('sync', self.flT[:, tok], o, reads=[f'ofl_{q % 2}'])
        for j in range(4):
            tt = slice(q * 512 + j * 128, q * 512 + (j + 1) * 128)
            k = nxt()
            pb = PB[k % 4]
            pt = self.ps[pb]
            for kc in range(8):
                P.op('tensor', 'matmul', reads=['wz', rhT], writes=[f'ps{pb}'], out=pt,
                     lhsT=hT[:, kc, j * 128:(j + 1) * 128], rhs=wz[:, kc, :], start=(kc == 0), stop=(kc == 7))
            o = B['o32'][k % 3]
            P.op('scalar', 'activation', reads=[f'ps{pb}'], writes=[f'o32_{k % 3}'], out=o, in_=pt, func=AF.Silu)
            P.dma('sync', self.zs[tt, :], o, reads=[f'o32_{k % 3}'])
            k = nxt()
            pb = PB[k % 4]
            pt = self.ps[pb]
            for kc in range(8):
                P.op('tensor', 'matmul', reads=['wv', rhT], writes=[f'ps{pb}'], out=pt[:, 0:256],
                     lhsT=hT[:, kc, j * 128:(j + 1) * 128], rhs=wv[:, kc, :], start=(kc == 0), stop=(kc == 7))
            for kc in range(8):
                P.op('tensor', 'matmul', reads=['wdt', rhT32], writes=[f'ps{pb}'], out=pt[:, 256:264],
                     lhsT=hT32[:, kc, j * 128:(j + 1) * 128], rhs=wdt[:, kc, :], start=(kc == 0), stop=(kc == 7))
            o = B['o16'][k % 3]
            self.evac(k, o[:, 0:256], pt[:, 0:256], [f'ps{pb}'], [f'o16_{k % 3}'])
            P.dma('sync', self.vtok[tt, :], o[:, 0:256], reads=[f'o16_{k % 3}'])
            o2 = B['osm'][j % 2]
            P.op('vector', 'tensor_copy', reads=[f'ps{pb}'], writes=[f'osm_{j % 2}'], out=o2, in_=pt[:, 256:264])
            P.dma('sync', self.dtr[tt, :], o2, reads=[f'osm_{j % 2}'])

    def conv_gen(self, l, st, pA, pB):
        P, T = self.P, self.T
        TC = min(T, 1024)
        HALO = 30
        if True:
            sb = lambda n, s, dt=F32: self.sb(st, n, s, dt)
            cw = sb("cw", [128, 2, 31])
            cbias = sb("cbias", [128, 2])
            cg = sb("cg", [128, 2])
            cbeta = sb("cbeta", [128, 2])
            ua = [sb(f"ua{i}", [128, TC + HALO]) for i in range(2)]
            ub = [sb(f"ub{i}", [128, TC + HALO]) for i in range(2)]
            co = [sb(f"co{i}", [128, TC]) for i in range(2)]
            sqt = sb("csq", [128, 512])
            mean = sb("cmean", [128, 512])
            rstd = sb("crstd", [128, 512])
            tmp = [sb(f"ctmp{i}", [128, 512]) for i in range(2)]
            yo = [sb(f"cyo{i}", [128, 512], BF16) for i in range(2)]
            P.dma('sync', cw, self.cmw[l], writes=['cw'])
            P.dma('sync', cbias, self.cmb[l], writes=['cbias'])
            P.dma('sync', cg, self.cmg[l], writes=['cg'])
            P.dma('sync', cbeta, self.cmbeta[l], writes=['cbeta'])
            yield
            n = 0
            for c0 in range(0, T, TC):
                for ct in range(2):
                    A, Bt = ua[ct], ub[ct]
                    ra, rb, rc = f'ua{ct}', f'ub{ct}', f'co{ct}'
                    rows = slice(ct * 128, (ct + 1) * 128)
                    if c0 == 0:
                        P.dma('sync', A[:, HALO:], self.gaT[rows, 0:TC], writes=[ra])
                        P.dma('sync', Bt[:, HALO:], self.gbT[rows, 0:TC], writes=[rb])
                        P.op('gpsimd', 'memset', writes=[ra], ap=A[:, 0:HALO], constant=0.0)
                        P.op('gpsimd', 'memset', writes=[rb], ap=Bt[:, 0:HALO], constant=0.0)
                    else:
                        P.dma('sync', A, self.gaT[rows, c0 - HALO:c0 + TC], writes=[ra])
                        P.dma('sync', Bt, self.gbT[rows, c0 - HALO:c0 + TC], writes=[rb])
                    P.op('scalar', 'activation', reads=[rb], writes=[rb], out=Bt, in_=Bt, func=AF.Sigmoid)
                    P.op('gpsimd', 'tensor_tensor', reads=[ra, rb], writes=[ra], out=A, in0=A, in1=Bt, op=ALU.mult)
                    C = co[ct]
                    P.op('vector', 'tensor_scalar', reads=[ra, 'cw', 'cbias'], writes=[rc], out=C, in0=A[:, 0:TC],
                         scalar1=cw[:, ct, 0:1], scalar2=cbias[:, ct:ct + 1], op0=ALU.mult, op1=ALU.add)
                    for k in range(1, 31):
                        P.op('vector', 'scalar_tensor_tensor', reads=[ra, 'cw', rc], writes=[rc], out=C,
                             in0=A[:, k:k + TC], scalar=cw[:, ct, k:k + 1], in1=C, op0=ALU.mult, op1=ALU.add)
                        if k % 8 == 0:
                            yield
                    yield
                for s0 in range(0, TC, 512):
                    cs_ = slice(s0, s0 + 512)
                    p1, p2 = self.ps[pA], self.ps[pB]
                    for ct in range(2):
                        P.op('tensor', 'matmul', reads=['ones', f'co{ct}'], writes=[f'ps{pA}'], out=p1, lhsT=self.ones,
                             rhs=co[ct][:, cs_], start=(ct == 0), stop=(ct == 1))
                    P.op('vector', 'tensor_scalar', reads=[f'ps{pA}'], writes=['cmean'], out=mean, in0=p1,
                         scalar1=1.0 / 256, scalar2=None, op0=ALU.mult)
                    for ct in range(2):
                        P.op('scalar', 'activation', reads=[f'co{ct}'], writes=['csq'], out=sqt, in_=co[ct][:, cs_],
                             func=AF.Square)
                        P.op('tensor', 'matmul', reads=['ones', 'csq'], writes=[f'ps{pB}'], out=p2, lhsT=self.ones,
                             rhs=sqt, start=(ct == 0), stop=(ct == 1))
                    P.op('vector', 'tensor_tensor', reads=['cmean'], writes=['crstd'], out=rstd, in0=mean, in1=mean,
                         op=ALU.mult)
                    P.op('vector', 'scalar_tensor_tensor', reads=[f'ps{pB}', 'crstd'], writes=['crstd'], out=rstd, in0=p2,
                         scalar=1.0 / 256, in1=rstd, op0=ALU.mult, op1=ALU.subtract)
                    self.rstd_ops(rstd, rstd, 1.0, ['crstd'], ['crstd'])
                    for ct in range(2):
                        n += 1
                        t = tmp[n % 2]
                        rt = f'ctmp{n % 2}'
                        P.op('vector', 'tensor_tensor', reads=[f'co{ct}', 'cmean'], writes=[rt], out=t,
                             in0=co[ct][:, cs_], in1=mean, op=ALU.subtract)
                        P.op('gpsimd', 'tensor_tensor', reads=[rt, 'crstd'], writes=[rt], out=t, in0=t, in1=rstd,
                             op=ALU.mult)
                        y = yo[n % 2]
                        ry = f'cyo{n % 2}'
                        P.op('scalar', 'activation', reads=[rt, 'cg', 'cbeta'], writes=[ry], out=y, in_=t, func=AF.Silu,
                             scale=cg[:, ct:ct + 1], bias=cbeta[:, ct:ct + 1])
                        P.dma('sync', self.yT[768 + ct * 128:768 + (ct + 1) * 128, c0 + s0:c0 + s0 + 512], y,
                              reads=[ry])
                    yield

    def phase_conv(self, l):
        with contextlib.ExitStack() as st:
            self.P.begin()
            for _ in self.conv_gen(l, st, 0, 1):
                pass
            self.P.end()

    def phase_attn(self, l, conv_inside=False):
        P, T, NT, NQ = self.P, self.T, self.NT, self.NQ
        CW = min(T, 2048)
        with contextlib.ExitStack() as st:
            sb = lambda n, s, dt=F32: self.sb(st, n, s, dt)
            fb = sb("fb", [4, 1])
            xx = sb("fx", [4, CW])
            ax = sb("fax", [4, CW])
            mn = sb("fmn", [4, CW])
            cum = [sb(f"fcum{i}", [4, CW]) for i in range(2)]
            r1 = sb("fr1", [4, CW])
            sp = sb("fsp", [4, 6, CW], BF16)
            P.begin()
            P.dma('sync', fb, self.f_bias[l], writes=['fb'])
            for ci, c0 in enumerate(range(0, T, CW)):
                cc = cum[ci % 2]
                rcum = f'fcum{ci % 2}'
                P.dma('sync', xx, self.flT[:, c0:c0 + CW], writes=['fx'])
                P.op('scalar', 'activation', reads=['fx', 'fb'], writes=['fx'], out=xx, in_=xx, func=AF.Identity,
                     bias=fb[:, 0:1], scale=1.0)
                P.op('vector', 'tensor_scalar', reads=['fx'], writes=['fmn'], out=mn, in0=xx, scalar1=-1.0, scalar2=0.0,
                     op0=ALU.mult, op1=ALU.max)
                P.op('vector', 'scalar_tensor_tensor', reads=['fmn', 'fx'], writes=['fax'], out=ax, in0=mn, scalar=-2.0,
                     in1=xx, op0=ALU.mult, op1=ALU.subtract)
                P.op('scalar', 'activation', reads=['fax'], writes=['fax'], out=ax, in_=ax, func=AF.Exp)
                P.op('scalar', 'activation', reads=['fax'], writes=['fax'], out=ax, in_=ax, func=AF.Ln, bias=1.0,
                     scale=1.0)
                P.op('vector', 'scalar_tensor_tensor', reads=['fmn', 'fax'], writes=['fmn'], out=mn, in0=mn, scalar=-1.0,
                     in1=ax, op0=ALU.mult, op1=ALU.subtract)
                init = 0.0 if ci == 0 else cum[(ci - 1) % 2][:, CW - 1:CW]
                P.op('vector', 'tensor_tensor_scan', reads=['fmn', 'ones', f'fcum{(ci - 1) % 2}'], writes=[rcum], out=cc,
                     data0=self.ones[0:4, 0:1].broadcast_to([4, CW]), data1=mn, initial=init, op0=ALU.mult, op1=ALU.add)
                P.op('vector', 'tensor_copy', reads=[rcum], writes=['fsp'], out=sp[:, 0, :], in_=cc)
                P.op('vector', 'tensor_tensor', reads=[rcum, 'fsp'], writes=['fr1'], out=r1, in0=cc, in1=sp[:, 0, :],
                     op=ALU.subtract)
                P.op('vector', 'tensor_copy', reads=['fr1'], writes=['fsp'], out=sp[:, 1, :], in_=r1)
                P.op('vector', 'tensor_tensor', reads=['fr1', 'fsp'], writes=['fr1'], out=r1, in0=r1, in1=sp[:, 1, :],
                     op=ALU.subtract)
                P.op('vector', 'tensor_copy', reads=['fr1'], writes=['fsp'], out=sp[:, 2, :], in_=r1)
                P.op('vector', 'tensor_scalar', reads=['fsp'], writes=['fsp'], out=sp[:, 3:6, :], in0=sp[:, 0:3, :],
                     scalar1=-1.0, scalar2=None, op0=ALU.mult)
                P.dma('sync', self.cs[:, :, c0:c0 + CW], sp, reads=['fsp'])
            P.end()
        with contextlib.ExitStack() as st:
            sb = lambda n, s, dt=F32: self.sb(st, n, s, dt)
            qp = [sb(f"qp{i}", [70, T], BF16) for i in range(2)]
            kp = [sb(f"kp{i}", [70, T], BF16) for i in range(2)]
            vp = [sb(f"vp{i}", [128, NT, 65], BF16) for i in range(2)]
            nm = sb("negmask", [128, 4, 512], BF16)
            NSB = 5
            LAG = 3
            pt_ = [sb(f"pT{i}", [128, 512], BF16) for i in range(NSB)]
            rec = sb("rec", [65, 512])
            bcs = sb("bcs", [64, 512])
            on = [sb(f"on{i}", [64, 512]) for i in range(2)]
            P.begin()
            cgen = self.conv_gen(l, st, 7, 7) if conv_inside else None
            n_units = (T // min(T, 1024)) * (2 * 5 + min(T, 1024) // 512) + 1
            units_done = 0
            work_total = 4 * sum(4 * q_ + 4 for q_ in range(NQ))
            work_done = 0
            P.op('gpsimd', 'memset', writes=['negmask'], ap=nm, constant=0.0)
            for d in range(4):
                P.op('gpsimd', 'affine_select', reads=['negmask'], writes=['negmask'], out=nm[:, d, :], in_=nm[:, d, :],
                     pattern=[[1, 512]], compare_op=ALU.is_ge, fill=-30000.0, base=-128 * d, channel_multiplier=-1)
            step = 0
            for h in range(4):
                hb = h % 2
                Q, Kp, V = qp[hb], kp[hb], vp[hb]
                rq, rk, rv = f'qp{hb}', f'kp{hb}', f'vp{hb}'
                hr = slice(h * 64, (h + 1) * 64)
                P.op('gpsimd', 'memset', writes=[rq], ap=Q[64:70, :], constant=1.0)
                P.op('gpsimd', 'memset', writes=[rk], ap=Kp[64:70, :], constant=1.0)
                P.op('gpsimd', 'memset', writes=[rv], ap=V[:, :, 64:65], constant=1.0)
                P.dma('sync', Q[0:64, :], self.qT[hr, :], writes=[rq])
                P.dma('sync', Kp[0:64, :], self.kT[hr, :], writes=[rk])
                P.dma('sync', Q[67:70, :], self.cs[h, 0:3, :], writes=[rq])
                P.dma('sync', Kp[64:67, :], self.cs[h, 3:6, :], writes=[rk])
                for i0 in range(0, NT, 4):
                    P.dma('sync', V[:, i0:i0 + 4, 0:64],
                          self.vtok[i0 * 128:(i0 + 4) * 128, hr].rearrange("(i p) d -> p i d", p=128), writes=[rv])
                for qc in range(NQ):
                    nk = 4 * qc + 4
                    ob = 5 + qc % 2
                    O = self.ps[ob]
                    qs = slice(qc * 512, (qc + 1) * 512)
                    for s_ in range(nk + LAG):
                        if s_ < nk:
                            kt = s_
                            sbk = (step + s_) % NSB
                            S = self.ps[sbk]
                            diag = kt >= 4 * qc
                            P.op('tensor', 'matmul', reads=[rq, rk], writes=[f'ps{sbk}'], out=S,
                                 lhsT=Kp[:, kt * 128:(kt + 1) * 128], rhs=Q[:, qs], start=True, stop=not diag)
                            if diag:
                                P.op('tensor', 'matmul', reads=['identb', 'negmask'], writes=[f'ps{sbk}'], out=S,
                                     lhsT=self.identb, rhs=nm[:, kt - 4 * qc, :], start=False, stop=True)
                        if 1 <= s_ <= nk:
                            kt = s_ - 1
                            sbk = (step + kt) % NSB
                            P.op('scalar', 'activation', reads=[f'ps{sbk}'], writes=[f'pT{sbk}'], out=pt_[sbk],
                                 in_=self.ps[sbk], func=AF.Exp)
                        if s_ >= LAG:
                            kt = s_ - LAG
                            sbk = (step + kt) % NSB
                            P.op('tensor', 'matmul', reads=[f'pT{sbk}', rv], writes=[f'ps{ob}'], out=O[0:65, :],
                                 lhsT=V[:, kt, :], rhs=pt_[sbk], start=(kt == 0), stop=(kt == nk - 1))
                    step += nk
                    P.op('vector', 'reciprocal', reads=[f'ps{ob}'], writes=['rec'], out=rec[64:65, :], in_=O[64:65, :])
                    P.op('tensor', 'matmul', reads=['ones', 'rec'], writes=['ps7'], out=self.ps[7][0:64, :],
                         lhsT=self.ones[64:65, 0:64], rhs=rec[64:65, :], start=True, stop=True)
                    P.op('scalar', 'activation', reads=['ps7'], writes=['bcs'], out=bcs, in_=self.ps[7][0:64, :],
                         func=AF.Copy)
                    o_ = on[qc % 2]
                    P.op('vector', 'tensor_tensor', reads=[f'ps{ob}', 'bcs'], writes=[f'on{qc % 2}'], out=o_,
                         in0=O[0:64, :], in1=bcs, op=ALU.mult)
                    P.dma('sync', self.attT[hr, qs], o_, reads=[f'on{qc % 2}'])
                    work_done += nk
                    while cgen is not None and units_done * work_total < n_units * work_done:
                        try:
                            next(cgen)
                            units_done += 1
                        except StopIteration:
                            cgen = None
                if h == 3 and cgen is not None:
                    for _ in cgen:
                        pass
                if h < 3:
                    P.end()
                    P.begin()
            P.end()
        with contextlib.ExitStack() as st:
            sb = lambda n, s, dt=F32: self.sb(st, n, s, dt)
            fg = sb("foxg", [128, 2])
            at = [[sb(f"at{i}{c}", [128, 512]) for c in range(2)] for i in range(2)]
            sq = sb("asq", [128, 512])
            rs = sb("ars", [128, 512])
            yo = [sb(f"ayo{i}", [128, 512], BF16) for i in range(2)]
            P.begin()
            P.dma('sync', fg, self.fox_g[l], writes=['foxg'])
            n = 0
            for qc in range(NQ):
                qs = slice(qc * 512, (qc + 1) * 512)
                b = qc % 2
                for ct in range(2):
                    P.dma('sync', at[b][ct], self.attT[ct * 128:(ct + 1) * 128, qs], writes=[f'at{b}{ct}'])
                    P.op('scalar', 'activation', reads=[f'at{b}{ct}'], writes=['asq'], out=sq, in_=at[b][ct],
                         func=AF.Square)
                    P.op('tensor', 'matmul', reads=['ones', 'asq'], writes=['ps0'], out=self.ps[0], lhsT=self.ones,
                         rhs=sq, start=(ct == 0), stop=(ct == 1))
                self.rstd_ops(self.ps[0], rs, 256.0, ['ps0'], ['ars'])
                for ct in range(2):
                    n += 1
                    P.op('vector', 'tensor_tensor', reads=[f'at{b}{ct}', 'ars'], writes=[f'at{b}{ct}'], out=at[b][ct],
                         in0=at[b][ct], in1=rs, op=ALU.mult)
                    y = yo[n % 2]
                    P.op('scalar', 'activation', reads=[f'at{b}{ct}', 'foxg'], writes=[f'ayo{n % 2}'], out=y,
                         in_=at[b][ct], func=AF.Copy, scale=fg[:, ct:ct + 1])
                    P.dma('sync', self.yT[512 + ct * 128:512 + (ct + 1) * 128, qs], y, reads=[f'ayo{n % 2}'])
            P.end()

    def phase_ssd(self, l):
        P, T, NT = self.P, self.T, self.NT
        SC = 512
        assert NT * 8 <= 512
        with contextlib.ExitStack() as st:
            sb = lambda n, s, dt=F32: self.sb(st, n, s, dt)
            cw4 = sb("cw4", [128, 8, 4])
            cb4 = sb("cb4", [128, 8])
            dtb = sb("dtb", [128, 8])
            aneg = sb("aneg", [128, 8])
            dsk = sb("dsk", [128, 8])
            ng = sb("ssdng", [128, 512])
            dt = sb("dt_all", [128, NT, 8])
            dmn = sb("dt_mn", [128, NT, 8])
            dtA = sb("dtA", [128, NT, 8])
            El = sb("El", [128, NT, 8])
            Wl = sb("Wl", [128, NT, 8])
            cd = sb("cd", [128, NT, 8])
            xin = [sb(f"xin{i}", [128, SC + 3]) for i in range(2)]
            cacc = [sb(f"cacc{i}", [128, SC]) for i in range(2)]
            xsT = [sb(f"xsT{i}", [128, SC]) for i in range(4)]
            BT = [sb(f"BT{i}", [128, SC], BF16) for i in range(2)]
            CT = [sb(f"CT{i}", [128, SC], BF16) for i in range(2)]
            x32_2 = [sb(f"x32{i}", [128, 8, 64], F32) for i in range(2)]
            Btok_2 = [sb(f"Btok{i}", [128, 256], BF16) for i in range(2)]
            R_2 = [sb(f"Rall{i}", [128, 8, 128], F32) for i in range(2)]
            E_2 = [sb(f"Eall{i}", [128, 8, 128], F32) for i in range(2)]
            CBm_2 = [sb(f"CBm{i}", [128, 2, 128], F32) for i in range(2)]
            M_2 = [sb(f"Mall{i}", [128, 8, 128], BF16) for i in range(2)]
            xdt_2 = [sb(f"xdt{i}", [128, 8, 64], BF16) for i in range(2)]
            xw_2 = [sb(f"xw{i}", [128, 8, 64], BF16) for i in range(2)]
            H = sb("Hst", [128, 8, 64])
            Hb = sb("Hb", [128, 8, 64], BF16)
            t1_2 = [sb(f"sst1{i}", [128, 8, 64], F32) for i in range(2)]
            t2_2 = [sb(f"sst2{i}", [128, 8, 64], F32) for i in range(2)]
            zt_2 = [sb(f"zt{i}", [128, 512], F32) for i in range(2)]
            ssq_2 = [sb(f"ssq{i}", [128, 512], F32) for i in range(2)]
            gss_2 = [sb(f"gss{i}", [128, 2], F32) for i in range(2)]
            grs_2 = [sb(f"grs{i}", [128, 2], F32) for i in range(2)]
            yTs_2 = [sb(f"yTs{i}", [128, 4, 128], BF16) for i in range(2)]
            ps = self.ps
            psb1 = ps[1].bitcast(BF16)
            P.begin()
            P.dma('sync', cw4, self.convw[l], writes=['cw4'])
            P.dma('sync', cb4, self.convb[l], writes=['cb4'])
            P.dma('sync', dtb, self.dt_bias[l].partition_broadcast(128), writes=['dtb'])
            P.dma('sync', aneg, self.a_log[l].partition_broadcast(128), writes=['aneg'])
            P.dma('sync', dsk, self.ssd_d[l].partition_broadcast(128), writes=['dsk'])
            P.dma('sync', ng, self.ssd_norm_g[l].partition_broadcast(128), writes=['ssdng'])
            for i0 in range(0, NT, 4):
                n_ = min(4, NT - i0)
                P.dma('sync', dt[:, i0:i0 + n_, :],
                      self.dtr[i0 * 128:(i0 + n_) * 128, :].rearrange("(i p) h -> p i h", p=128), writes=['dt_all'])
            P.op('scalar', 'activation', reads=['aneg'], writes=['aneg'], out=aneg, in_=aneg, func=AF.Exp)
            P.op('vector', 'tensor_scalar', reads=['aneg'], writes=['aneg'], out=aneg, in0=aneg, scalar1=-1.0,
                 scalar2=None, op0=ALU.mult)
            bc3 = lambda t: t.unsqueeze(1).broadcast_to([128, NT, 8])
            P.op('vector', 'tensor_tensor', reads=['dt_all', 'dtb'], writes=['dt_all'], out=dt, in0=dt, in1=bc3(dtb),
                 op=ALU.add)
            P.op('vector', 'tensor_scalar', reads=['dt_all'], writes=['dt_mn'], out=dmn, in0=dt, scalar1=0.0,
                 scalar2=None, op0=ALU.max)
            P.op('vector', 'scalar_tensor_tensor', reads=['dt_mn', 'dt_all'], writes=['dt_all'],
                 out=dt.rearrange("p i h -> p (i h)"), in0=dmn.rearrange("p i h -> p (i h)"), scalar=-2.0,
                 in1=dt.rearrange("p i h -> p (i h)"), op0=ALU.mult, op1=ALU.add)
            P.op('scalar', 'activation', reads=['dt_all'], writes=['dt_all'], out=dt, in_=dt, func=AF.Exp)
            P.op('scalar', 'activation', reads=['dt_all'], writes=['dt_all'], out=dt, in_=dt, func=AF.Ln, bias=1.0,
                 scale=1.0)
            P.op('vector', 'tensor_tensor', reads=['dt_all', 'dt_mn'], writes=['dt_all'], out=dt, in0=dt, in1=dmn,
                 op=ALU.add)
            P.op('vector', 'tensor_tensor', reads=['dt_all', 'aneg'], writes=['dtA'], out=dtA, in0=dt, in1=bc3(aneg),
                 op=ALU.mult)
            dtA2 = dtA.rearrange("p i h -> p (i h)")
            P.op('tensor', 'matmul', reads=['tri', 'dtA'], writes=['ps2'], out=ps[2][:, 0:NT * 8], lhsT=self.tri,
                 rhs=dtA2, start=True, stop=True)
            P.op('tensor', 'matmul', reads=['ones', 'dtA'], writes=['ps3'], out=ps[3][:, 0:NT * 8], lhsT=self.ones,
                 rhs=dtA2, start=True, stop=True)
            f2 = lambda t: t.rearrange("p i h -> p (i h)")
            P.op('scalar', 'activation', reads=['ps2'], writes=['El'], out=f2(El), in_=ps[2][:, 0:NT * 8], func=AF.Exp)
            P.op('scalar', 'activation', reads=['ps3'], writes=['cd'], out=f2(cd), in_=ps[3][:, 0:NT * 8], func=AF.Exp)
            P.op('vector', 'tensor_copy', reads=['ps3'], writes=['Wl'], out=f2(Wl), in_=ps[3][:, 0:NT * 8])
            P.op('vector', 'tensor_tensor', reads=['Wl', 'ps2'], writes=['Wl'], out=f2(Wl), in0=f2(Wl),
                 in1=ps[2][:, 0:NT * 8], op=ALU.subtract)
            P.op('scalar', 'activation', reads=['Wl'], writes=['Wl'], out=Wl, in_=Wl, func=AF.Exp)
            P.op('vector', 'tensor_tensor', reads=['Wl', 'dt_all'], writes=['Wl'], out=Wl, in0=Wl, in1=dt, op=ALU.mult)
            P.op('gpsimd', 'memset', writes=['Hst'], ap=H, constant=0.0)
            P.op('gpsimd', 'memset', writes=['Hb'], ap=Hb, constant=0.0)
            for c0 in range(0, T, SC):
                for ct in range(8):
                    X = xin[ct % 2]
                    rx = f'xin{ct % 2}'
                    A = cacc[ct % 2]
                    ra = f'cacc{ct % 2}'
                    rows = slice(ct * 128, (ct + 1) * 128)
                    if c0 == 0:
                        P.op('gpsimd', 'memset', writes=[rx], ap=X[:, 0:3], constant=0.0)
                        P.dma('sync', X[:, 3:], self.xbcT[rows, 0:SC], writes=[rx])
                    else:
                        P.dma('sync', X, self.xbcT[rows, c0 - 3:c0 + SC], writes=[rx])
                    P.op('vector', 'tensor_scalar', reads=[rx, 'cw4', 'cb4'], writes=[ra], out=A, in0=X[:, 0:SC],
                         scalar1=cw4[:, ct, 0:1], scalar2=cb4[:, ct:ct + 1], op0=ALU.mult, op1=ALU.add)
                    for k in range(1, 4):
                        P.op('vector', 'scalar_tensor_tensor', reads=[rx, 'cw4', ra], writes=[ra], out=A,
                             in0=X[:, k:k + SC], scalar=cw4[:, ct, k:k + 1], in1=A, op0=ALU.mult, op1=ALU.add)
                    if ct < 4:
                        dst, rd = xsT[ct], f'xsT{ct}'
                    elif ct < 6:
                        dst, rd = BT[ct - 4], f'BT{ct - 4}'
                    else:
                        dst, rd = CT[ct - 6], f'CT{ct - 6}'
                    P.op('scalar', 'activation', reads=[ra], writes=[rd], out=dst, in_=A, func=AF.Silu)
                def stage_a(c0, cc):
                        c = c0 // 128 + cc
                        cs_ = slice(cc * 128, (cc + 1) * 128)
                        tok = slice(c * 128, (c + 1) * 128)
                        pc = c % 2
                        x32 = x32_2[pc]
                        Btok = Btok_2[pc]
                        R = R_2[pc]
                        E = E_2[pc]
                        CBm = CBm_2[pc]
                        M = M_2[pc]
                        xdt = xdt_2[pc]
                        xw = xw_2[pc]
                        t1 = t1_2[pc]
                        t2 = t2_2[pc]
                        zt = zt_2[pc]
                        ssq = ssq_2[pc]
                        gss = gss_2[pc]
                        grs = grs_2[pc]
                        yTs = yTs_2[pc]
                        n = {k: k + str(pc) for k in ('x32', 'Btok', 'Rall', 'Eall', 'CBm', 'Mall', 'xdt', 'xw', 'sst1', 'sst2', 'zt', 'ssq', 'gss', 'grs', 'yTs')}
                        for ct in range(4):
                            P.op('tensor', 'transpose', reads=[f'xsT{ct}', 'ident'], writes=['ps0'],
                                 out=ps[0][:, ct * 128:(ct + 1) * 128], in_=xsT[ct][:, cs_], identity=self.ident)
                        P.op('scalar', 'activation', reads=['ps0'], writes=[n['x32']], out=x32.rearrange("p h d -> p (h d)"),
                             in_=ps[0], func=AF.Copy)
                        for g in range(2):
                            P.op('tensor', 'transpose', reads=[f'BT{g}', 'identb'], writes=['ps1'],
                                 out=psb1[:, g * 128:(g + 1) * 128], in_=BT[g][:, cs_], identity=self.identb)
                        P.op('vector', 'tensor_copy', reads=['ps1'], writes=[n['Btok']], out=Btok, in_=psb1[:, 0:256])
                        P.dma('sync', zt, self.zs[tok, :], writes=[n['zt']])
                        P.op('vector', 'tensor_tensor', reads=['tri', 'dtA'], writes=[n['Rall']], out=R,
                             in0=self.tri.unsqueeze(1).broadcast_to([128, 8, 128]),
                             in1=dtA[:, c, :].unsqueeze(2).broadcast_to([128, 8, 128]), op=ALU.mult)
                        for hh in range(2):
                            P.op('tensor', 'matmul', reads=['ustr', n['Rall']], writes=[f'ps{2 + hh}'], out=ps[2 + hh],
                                 lhsT=self.ustr, rhs=R[:, hh * 4:(hh + 1) * 4, :].rearrange("p h l -> p (h l)"),
                                 start=True, stop=True)
                            P.op('scalar', 'activation', reads=[f'ps{2 + hh}'], writes=[n['Eall']],
                                 out=E[:, hh * 4:(hh + 1) * 4, :].rearrange("p h l -> p (h l)"), in_=ps[2 + hh], func=AF.Exp)
                        for g in range(2):
                            P.op('tensor', 'matmul', reads=[f'BT{g}', f'CT{g}'], writes=['ps4'],
                                 out=ps[4][:, g * 128:(g + 1) * 128], lhsT=BT[g][:, cs_], rhs=CT[g][:, cs_],
                                 start=True, stop=True)
                        P.op('vector', 'tensor_tensor', reads=['ps4', 'tri'], writes=[n['CBm']], out=CBm,
                             in0=ps[4][:, 0:256].rearrange("p (g l) -> p g l", g=2),
                             in1=self.tri.unsqueeze(1).broadcast_to([128, 2, 128]), op=ALU.mult)
                        for g in range(2):
                            P.op('vector', 'tensor_tensor', reads=[n['Eall'], n['CBm']], writes=[n['Mall']],
                                 out=M[:, g * 4:(g + 1) * 4, :], in0=E[:, g * 4:(g + 1) * 4, :],
                                 in1=CBm[:, g:g + 1, :].broadcast_to([128, 4, 128]), op=ALU.mult)
                        P.op('gpsimd', 'tensor_tensor', reads=[n['x32'], 'dt_all'], writes=[n['xdt']], out=xdt, in0=x32,
                             in1=dt[:, c, :].unsqueeze(2).broadcast_to([128, 8, 64]), op=ALU.mult)
                        P.op('gpsimd', 'tensor_tensor', reads=[n['x32'], 'Wl'], writes=[n['xw']], out=xw, in0=x32,
                             in1=Wl[:, c, :].unsqueeze(2).broadcast_to([128, 8, 64]), op=ALU.mult)
                        for h in range(8):
                            P.op('tensor', 'matmul', reads=[n['Mall'], n['xdt']], writes=['ps5'], out=ps[5][:, h * 64:(h + 1) * 64],
                                 lhsT=M[:, h, :], rhs=xdt[:, h, :], start=True, stop=True)
                        for g in range(2):
                            P.op('tensor', 'matmul', reads=[f'CT{g}', 'Hb'], writes=['ps6'],
                                 out=ps[6][:, g * 256:(g + 1) * 256], lhsT=CT[g][:, cs_],
                                 rhs=Hb[:, g * 4:(g + 1) * 4, :].rearrange("p h d -> p (h d)"), start=True, stop=True)
                        for g in range(2):
                            P.op('tensor', 'matmul', reads=[n['Btok'], n['xw']], writes=['ps7'],
                                 out=ps[7][:, g * 256:(g + 1) * 256], lhsT=Btok[:, g * 128:(g + 1) * 128],
                                 rhs=xw[:, g * 4:(g + 1) * 4, :].rearrange("p h d -> p (h d)"), start=True, stop=True)
                        v3 = lambda t: t.rearrange("p (h d) -> p h d", h=8)
                        b3 = lambda t: t.unsqueeze(2).broadcast_to([128, 8, 64])
                        P.op('vector', 'tensor_tensor', reads=['ps6', 'El'], writes=[n['sst1']], out=t1, in0=v3(ps[6]),
                             in1=b3(El[:, c, :]), op=ALU.mult)
                        P.op('vector', 'tensor_tensor', reads=[n['sst1'], 'ps5'], writes=[n['sst1']], out=t1, in0=t1, in1=v3(ps[5]),
                             op=ALU.add)
                        P.op('gpsimd', 'tensor_tensor', reads=[n['x32'], 'dsk'], writes=[n['sst2']], out=t2, in0=x32, in1=b3(dsk),
                             op=ALU.mult)
                        P.op('gpsimd', 'tensor_tensor', reads=[n['sst1'], n['sst2']], writes=[n['sst1']], out=t1, in0=t1, in1=t2,
                             op=ALU.add)
                        P.op('vector', 'tensor_tensor', reads=['Hst', 'cd'], writes=['Hst'], out=H, in0=H, in1=b3(cd[:, c, :]),
                             op=ALU.mult)
                        P.op('vector', 'tensor_tensor', reads=['Hst', 'ps7'], writes=['Hst'], out=H, in0=H, in1=v3(ps[7]),
                             op=ALU.add)
                        P.op('gpsimd', 'tensor_copy', reads=['Hst'], writes=['Hb'], out=Hb, in_=H)

                def stage_b(c):
                        tok = slice(c * 128, (c + 1) * 128)
                        pc = c % 2
                        x32 = x32_2[pc]
                        Btok = Btok_2[pc]
                        R = R_2[pc]
                        E = E_2[pc]
                        CBm = CBm_2[pc]
                        M = M_2[pc]
                        xdt = xdt_2[pc]
                        xw = xw_2[pc]
                        t1 = t1_2[pc]
                        t2 = t2_2[pc]
                        zt = zt_2[pc]
                        ssq = ssq_2[pc]
                        gss = gss_2[pc]
                        grs = grs_2[pc]
                        yTs = yTs_2[pc]
                        n = {k: k + str(pc) for k in ('x32', 'Btok', 'Rall', 'Eall', 'CBm', 'Mall', 'xdt', 'xw', 'sst1', 'sst2', 'zt', 'ssq', 'gss', 'grs', 'yTs')}
                        y2 = t1.rearrange("p h d -> p (h d)")
                        P.op('gpsimd', 'tensor_tensor', reads=[n['sst1'], n['zt']], writes=[n['sst1']], out=y2, in0=y2, in1=zt,
                             op=ALU.mult)
                        for g in range(2):
                            P.op('scalar', 'activation', reads=[n['sst1']], writes=[n['ssq'], n['gss']],
                                 out=ssq[:, g * 256:(g + 1) * 256], in_=y2[:, g * 256:(g + 1) * 256], func=AF.Square,
                                 accum_out=gss[:, g:g + 1])
                        self.rstd_ops(gss, grs, 256.0, [n['gss']], [n['grs']])
                        for g in range(2):
                            P.op('vector', 'scalar_tensor_tensor', reads=[n['sst1'], n['grs'], 'ssdng'], writes=[n['sst2']],
                                 out=t2.rearrange("p h d -> p (h d)")[:, g * 256:(g + 1) * 256],
                                 in0=y2[:, g * 256:(g + 1) * 256], scalar=grs[:, g:g + 1],
                                 in1=ng[:, g * 256:(g + 1) * 256], op0=ALU.mult, op1=ALU.mult)
                        yn = t2.rearrange("p h d -> p (h d)")
                        for ct in range(4):
                            P.op('tensor', 'transpose', reads=[n['sst2'], 'ident'], writes=['ps0'],
                                 out=ps[0][:, ct * 128:(ct + 1) * 128], in_=yn[:, ct * 128:(ct + 1) * 128],
                                 identity=self.ident)
                        P.op('scalar', 'activation', reads=['ps0'], writes=[n['yTs']], out=yTs.rearrange("p c t -> p (c t)"),
                             in_=ps[0], func=AF.Copy)
                        P.dma('sync', self.yT[0:512, tok].rearrange("(ct p) t -> p ct t", p=128), yTs, reads=[n['yTs']])

                for cc in range(SC // 128):
                    c = c0 // 128 + cc
                    stage_a(c0, cc)
                    if c >= 1:
                        stage_b(c - 1)
                if c0 + SC >= T:
                    stage_b(NT - 1)
            P.end()

    def phase_wout_router(self, l, st0):
        P, T, NT = self.P, self.T, self.NT
        src = self.x_in if l == 0 else self.xres
        ps = self.ps
        self.ohb = self.sb(st0, "ohb", [128, NT, 64], BF16)
        self.rw = self.sb(st0, "rw", [128, NT, 2])
        with contextlib.ExitStack() as st:
            sb = lambda n, s, dt=F32: self.sb(st, n, s, dt)
            wo = sb("wo", [128, 8, D], BF16)
            gsc = sb("gscf", [128, D])
            wr = sb("wr", [128, 8, 36])
            rb = sb("rbias", [128, 36])
            yTc = [sb(f"yTc{i}", [128, 8, 512], BF16) for i in range(2)]
            xl = [sb(f"xl{i}", [128, D]) for i in range(4)]
            tt = sb("wtmp", [128, D])
            hb = [sb(f"hperm{i}", [128, D], BF16) for i in range(2)]
            lg = sb("lg", [128, 36])
            lg4 = sb("lg4", [128, 4, 36])
            gmax4 = sb("gmax4", [128, 4])
            gsum4 = sb("gsum4", [128, 4])
            r4 = sb("r4", [128, 4])
            den4 = sb("den4", [128, 4])
            ohg4 = sb("ohg4", [128, 4, 4])
            gexp4 = sb("gexp4", [128, 4, 4])
            em4 = sb("em4", [128, 4, 4, 8])
            es4 = sb("es4", [128, 4, 8])
            t84 = sb("t84", [128, 4, 8])
            s14 = sb("s14", [128, 4, 8])
            s24 = sb("s24", [128, 4, 8])
            sm = sb("rsm", [128, 64])
            gexp = sb("gexp", [128, 4])
            ohg = sb("ohg", [128, 4])
            em = sb("em", [128, 4, 8])
            es = sb("esel", [128, 8])
            t8 = sb("top8", [128, 8])
            s1 = sb("sel1", [128, 8])
            s2 = sb("sel2", [128, 8])
            W = self.w_out[l].rearrange("(kc p) n -> p kc n", p=128)
            P.begin()
            for kc in range(8):
                self.load_cast(st, wo[:, kc, :], W[:, kc, :], 'wo', D)
            P.dma('sync', wr[:, :, 0:4], self.w_rg[l].rearrange("(kc p) n -> p kc n", p=128), writes=['wr'])
            P.dma('sync', wr[:, :, 4:36], self.w_re[l].rearrange("(kc p) n -> p kc n", p=128), writes=['wr'])
            P.dma('sync', rb[:, 0:4], self.b_rg[l].partition_broadcast(128), writes=['rbias'])
            P.dma('sync', rb[:, 4:36], self.b_re[l].partition_broadcast(128), writes=['rbias'])
            P.dma('sync', gsc, self.norm_ffn_g[l].partition_broadcast(128), writes=['gscf'])
            P.op('vector', 'scalar_tensor_tensor', reads=['gscf', 'modb'], writes=['gscf'], out=gsc, in0=self.mod(4),
                 scalar=1.0, in1=gsc, op0=ALU.add, op1=ALU.mult)

            def pre_load(i):
                q, j = divmod(i, 4)
                if j == 0:
                    P.dma('sync', yTc[q % 2], self.yT[:, q * 512:(q + 1) * 512].rearrange("(kc p) t -> p kc t", p=128),
                          writes=[f'yTc{q % 2}'])
                P.dma('sync', xl[i % 4], src[i * 128:(i + 1) * 128, :], writes=[f'xl{i % 4}'])

            def pre(i, X, rx):
                q, j = divmod(i, 4)
                Y = yTc[q % 2]
                ry = f'yTc{q % 2}'
                XL = xl[i % 4]
                rl = f'xl{i % 4}'
                for half in range(2):
                    pb = 2 + half
                    for kc in range(8):
                        P.op('tensor', 'matmul', reads=[ry, 'wo'], writes=[f'ps{pb}'], out=ps[pb],
                             lhsT=Y[:, kc, j * 128:(j + 1) * 128], rhs=wo[:, kc, half * 512:(half + 1) * 512],
                             start=(kc == 0), stop=(kc == 7))
                    hs = slice(half * 512, (half + 1) * 512)
                    P.op('vector', 'tensor_tensor', reads=[f'ps{pb}', 'modb'], writes=['wtmp'], out=tt[:, hs], in0=ps[pb],
                         in1=self.mod(2)[:, hs], op=ALU.mult)
                P.op('vector', 'tensor_tensor', reads=['wtmp', rl], writes=[rx], out=X, in0=tt, in1=XL, op=ALU.add)
                P.dma('sync', self.xres[i * 128:(i + 1) * 128, :], X, reads=[rx])

            def after_h(i, Hh, rh, X, rx):
                Hp = hb[i % 2]
                rp = f'hperm{i % 2}'
                P.op('gpsimd', 'tensor_copy', reads=[rh], writes=[rp], out=Hp.rearrange("t (kc p) -> t kc p", kc=8),
                     in_=Hh.rearrange("t (p kc) -> t kc p", kc=8))
                P.dma('sync', self.hfp[i * 128:(i + 1) * 128, :], Hp, reads=[rp])

            def consumer(q, hTb, hT32b):
                hT32, r32 = hT32b
                i0_ = q * 4
                for j in range(4):
                    for kc in range(8):
                        P.op('tensor', 'matmul', reads=[r32, 'wr'], writes=['ps4'], out=ps[4][:, j * 36:(j + 1) * 36],
                             lhsT=hT32[:, kc, j * 128:(j + 1) * 128], rhs=wr[:, kc, :], start=(kc == 0), stop=(kc == 7))
                V = lambda name, **kw: P.op('vector', name, **kw)
                V('tensor_tensor', reads=['ps4', 'rbias'], writes=['lg'], out=lg4,
                  in0=ps[4][:, 0:144].rearrange("p (j c) -> p j c", j=4), in1=rb.unsqueeze(1).broadcast_to([128, 4, 36]),
                  op=ALU.add)
                gl = lg4[:, :, 0:4]
                el = lg4[:, :, 4:36].rearrange("p j (g e) -> p j g e", g=4)
                b3 = lambda t, n_: t.unsqueeze(2).broadcast_to([128, 4, n_])
                V('tensor_reduce', reads=['lg'], writes=['rsm'], out=gmax4, in_=gl, axis=AX.X, op=ALU.max)
                V('tensor_tensor', reads=['lg', 'rsm'], writes=['ohg'], out=ohg4, in0=gl, in1=b3(gmax4, 4), op=ALU.is_ge)
                V('tensor_tensor', reads=['lg', 'rsm'], writes=['gexp'], out=gexp4, in0=gl, in1=b3(gmax4, 4),
                  op=ALU.subtract)
                P.op('scalar', 'activation', reads=['gexp'], writes=['gexp'], out=gexp4, in_=gexp4, func=AF.Exp)
                V('tensor_reduce', reads=['gexp'], writes=['rsm2'], out=gsum4, in_=gexp4, axis=AX.X, op=ALU.add)
                V('reciprocal', reads=['rsm2'], writes=['rsm2'], out=gsum4, in_=gsum4)
                V('tensor_tensor', reads=['lg', 'ohg'], writes=['em'], out=em4, in0=el,
                  in1=ohg4.unsqueeze(3).broadcast_to([128, 4, 4, 8]), op=ALU.mult)
                V('tensor_reduce', reads=['em'], writes=['esel'], out=es4, in_=em4.rearrange("p j g e -> p j e g"),
                  axis=AX.X, op=ALU.add)
                for j in range(4):
                    V('max', reads=['esel'], writes=['top8'], out=t84[:, j, :], in_=es4[:, j, :])
                V('tensor_tensor', reads=['esel', 'top8'], writes=['sel1'], out=s14, in0=es4,
                  in1=t84[:, :, 0:1].broadcast_to([128, 4, 8]), op=ALU.is_ge)
                V('tensor_tensor', reads=['esel', 'top8'], writes=['sel2'], out=s24, in0=es4,
                  in1=t84[:, :, 1:2].broadcast_to([128, 4, 8]), op=ALU.is_ge)
                V('tensor_tensor', reads=['sel2', 'sel1'], writes=['sel2'], out=s24, in0=s24, in1=s14, op=ALU.subtract)
                V('tensor_tensor', reads=['top8'], writes=['rsm3'], out=r4, in0=t84[:, :, 1], in1=t84[:, :, 0],
                  op=ALU.subtract)
                P.op('scalar', 'activation', reads=['rsm3'], writes=['rsm3'], out=r4, in_=r4, func=AF.Exp)
                V('tensor_scalar', reads=['rsm3'], writes=['rsm4'], out=den4, in0=r4, scalar1=1.0, scalar2=None,
                  op0=ALU.add)
                V('reciprocal', reads=['rsm4'], writes=['rsm4'], out=den4, in_=den4)
                V('tensor_tensor', reads=['rsm4', 'rsm2'], writes=['rw'], out=self.rw[:, i0_:i0_ + 4, 0], in0=den4,
                  in1=gsum4, op=ALU.mult)
                V('tensor_tensor', reads=['rw', 'rsm3'], writes=['rw'], out=self.rw[:, i0_:i0_ + 4, 1],
                  in0=self.rw[:, i0_:i0_ + 4, 0], in1=r4, op=ALU.mult)
                for k, sel in enumerate((s14, s24)):
                    V('tensor_tensor', reads=['ohg', f'sel{k + 1}'], writes=['ohb'],
                      out=self.ohb[:, i0_:i0_ + 4, k * 32:(k + 1) * 32].rearrange("p j (g e) -> p j g e", g=4),
                      in0=ohg4.unsqueeze(3).broadcast_to([128, 4, 4, 8]),
                      in1=sel.unsqueeze(2).broadcast_to([128, 4, 4, 8]), op=ALU.mult)
            self.norm_and_transpose_loop(st, None, gsc, self.mod(3), consumer, pre=pre, after_h=after_h, pre_load=pre_load)
            P.end()

    def phase_moe(self, l, st0):
        P, T, NT, NB, BLK = self.P, self.T, self.NT, self.NB, self.BLK
        ps = self.ps
        last = (l == self.L - 1)
        NR = NB * BLK
        wgv = self.w_gate.rearrange("l e (p two k4) f -> (l e p two) (k4 f)", two=2, k4=4)
        wuv = self.w_up.rearrange("l e (p two k4) f -> (l e p two) (k4 f)", two=2, k4=4)
        wdv = self.w_down.rearrange("l e (p two f2) d -> (l e p two) (f2 d)", two=2, f2=2)
        with contextlib.ExitStack() as st:
            sb = lambda n, s, dt=F32: self.sb(st, n, s, dt)
            dest_i = sb("dest_i", [128, NT, 2], I32)
            widx = sb("widx", [128, NB, 2], I32)
            with contextlib.ExitStack() as s1:
                sb1 = lambda n, s, dt=F32: self.sb(s1, n, s, dt)
                pre = sb1("pre_all", [128, NT, 64])
                tot = sb1("tot_all", [128, NT, 64])
                base = sb1("base_all", [128, NT, 64])
                cnt = sb1("cnt", [128, 64])
                tl = sb1("mtotal", [128, 32])
                md = sb1("mmod", [128, 32])
                pend = sb1("pend", [128, 32])
                off = sb1("moff", [128, 64])
                dest_f = sb1("dest_f", [128, NT, 2])
                blk0 = sb1("blk0", [128, NB])
                cmp_ = sb1("mcmp", [128, NB, 32])
                be = sb1("mbe", [128, NB])
                pidx = sb1("pidx", [128, 2])
                wf = sb1("widx_f", [128, NB, 2])
                dbg_t = sb1("rdbg_t", [128, 68])
                P.begin()
                ohb2 = self.ohb.rearrange("p i c -> p (i c)")
                f2 = lambda t: t.rearrange("p i c -> p (i c)")
                for k, c0 in enumerate(range(0, NT * 64, 512)):
                    n = min(512, NT * 64 - c0)
                    P.op('tensor', 'matmul', reads=['sutb', 'ohb'], writes=['ps0'], out=ps[0][:, 0:n], lhsT=self.sutb,
                         rhs=ohb2[:, c0:c0 + n], start=True, stop=True)
                    P.op('scalar', 'activation', reads=['ps0'], writes=['pre_all'], out=f2(pre)[:, c0:c0 + n],
                         in_=ps[0][:, 0:n], func=AF.Copy)
                    P.op('tensor', 'matmul', reads=['onesb', 'ohb'], writes=['ps1'], out=ps[1][:, 0:n], lhsT=self.onesb,
                         rhs=ohb2[:, c0:c0 + n], start=True, stop=True)
                    P.op('vector', 'tensor_copy', reads=['ps1'], writes=['tot_all'], out=f2(tot)[:, c0:c0 + n],
                         in_=ps[1][:, 0:n])
                V = lambda name, **kw: P.op('vector', name, **kw)
                P.op('gpsimd', 'memset', writes=['base_all'], ap=base[:, 0, :], constant=0.0)
                for i in range(1, NT):
                    V('tensor_tensor', reads=['base_all', 'tot_all'], writes=['base_all'], out=base[:, i, :],
                      in0=base[:, i - 1, :], in1=tot[:, i - 1, :], op=ALU.add)
                V('tensor_tensor', reads=['base_all', 'tot_all'], writes=['cnt'], out=cnt, in0=base[:, NT - 1, :],
                  in1=tot[:, NT - 1, :], op=ALU.add)
                V('tensor_tensor', reads=['cnt'], writes=['mtotal'], out=tl, in0=cnt[:, 0:32], in1=cnt[:, 32:64],
                  op=ALU.add)
                P.op('gpsimd', 'iota', writes=['blk0'], out=blk0, pattern=[[BLK, NB]], base=0, channel_multiplier=0,
                     allow_small_or_imprecise_dtypes=True)
                V('tensor_tensor', reads=['mtotal', 'blk0'], writes=['mcmp'], out=cmp_.rearrange("p b e -> p (b e)").rearrange("p (e b) -> p e b", e=32),
                  in0=blk0.unsqueeze(1).broadcast_to([128, 32, NB]), in1=tl.unsqueeze(2).broadcast_to([128, 32, NB]),
                  op=ALU.is_lt)
                V('tensor_reduce', reads=['mcmp'], writes=['mtotal'], out=tl,
                  in_=cmp_.rearrange("p b e -> p (b e)").rearrange("p (e b) -> p e b", e=32), axis=AX.X, op=ALU.add)
                V('tensor_scalar', reads=['mtotal'], writes=['mtotal'], out=tl, in0=tl, scalar1=float(BLK), scalar2=None,
                  op0=ALU.mult)
                V('tensor_tensor_scan', reads=['mtotal', 'ones'], writes=['pend'], out=pend,
                  data0=self.ones[:, 0:32], data1=tl, initial=0.0, op0=ALU.mult, op1=ALU.add)
                V('tensor_tensor', reads=['pend', 'mtotal'], writes=['moff'], out=off[:, 0:32], in0=pend, in1=tl,
                  op=ALU.subtract)
                V('tensor_tensor', reads=['moff', 'cnt'], writes=['moff'], out=off[:, 32:64], in0=off[:, 0:32],
                  in1=cnt[:, 0:32], op=ALU.add)
                V('tensor_tensor', reads=['pre_all', 'base_all'], writes=['pre_all'], out=pre, in0=pre, in1=base,
                  op=ALU.add)
                V('tensor_tensor', reads=['pre_all', 'moff'], writes=['pre_all'], out=pre, in0=pre,
                  in1=off.unsqueeze(1).broadcast_to([128, NT, 64]), op=ALU.add)
                V('tensor_tensor', reads=['pre_all', 'ohb'], writes=['pre_all'], out=pre, in0=pre, in1=self.ohb,
                  op=ALU.mult)
                V('tensor_reduce', reads=['pre_all'], writes=['dest_f'], out=dest_f,
                  in_=pre.rearrange("p i (k e) -> p i k e", k=2), axis=AX.X, op=ALU.add)
                V('tensor_copy', reads=['dest_f'], writes=['dest_i'], out=dest_i, in_=dest_f)
                V('tensor_tensor', reads=['pend', 'blk0'], writes=['mcmp'], out=cmp_,
                  in0=pend.unsqueeze(1).broadcast_to([128, NB, 32]), in1=blk0.unsqueeze(2).broadcast_to([128, NB, 32]),
                  op=ALU.is_le)
                V('tensor_reduce', reads=['mcmp'], writes=['mbe'], out=be, in_=cmp_, axis=AX.X, op=ALU.add)
                V('tensor_scalar', reads=['mbe'], writes=['mbe'], out=be, in0=be, scalar1=256.0, scalar2=None,
                  op0=ALU.mult)
                P.op('gpsimd', 'iota', writes=['pidx'], out=pidx, pattern=[[1, 2]], base=l * NE * 256, channel_multiplier=2,
                     allow_small_or_imprecise_dtypes=True)
                V('tensor_tensor', reads=['mbe', 'pidx'], writes=['widx_f'], out=wf,
                  in0=be.unsqueeze(2).broadcast_to([128, NB, 2]), in1=pidx.unsqueeze(1).broadcast_to([128, NB, 2]),
                  op=ALU.add)
                V('tensor_copy', reads=['widx_f'], writes=['widx'], out=widx, in_=wf)
                if 'rdbg' in self.dbg:
                    for i in range(NT):
                        V('tensor_copy', reads=['ohb'], writes=['rdbg_t'], out=dbg_t[:, 0:64], in_=self.ohb[:, i, :])
                        V('tensor_copy', reads=['rw'], writes=['rdbg_t'], out=dbg_t[:, 64:66], in_=self.rw[:, i, :])
                        V('tensor_copy', reads=['dest_f'], writes=['rdbg_t'], out=dbg_t[:, 66:68], in_=dest_f[:, i, :])
                        P.dma('sync', self.rdbg[i * 128:(i + 1) * 128, :], dbg_t, reads=['rdbg_t'])
                P.end()
            if '1' in self.opt:
                return
            with contextlib.ExitStack() as s2:
                hrow = [self.sb(s2, f"hrow{i}", [128, D], BF16) for i in range(3)]
                P.begin()
                P.raw('gpsimd', 'reg_mov', self.reg_rows, NR - 1)
                for i in range(NT):
                    Hr = hrow[i % 3]
                    rr = f'hrow{i % 3}'
                    P.dma('sync', Hr, self.hfp[i * 128:(i + 1) * 128, :], writes=[rr])
                    for k in range(2):
                        P.dma('gpsimd', None, None, reads=[rr, 'dest_i'], writes=['xs'],
                              fn=('indirect_dma_start', dict(
                                  out=self.xs, out_offset=bass.IndirectOffsetOnAxis(ap=dest_i[:, i, k:k + 1], axis=0),
                                  in_=Hr, in_offset=None, bounds_check=self.reg_rows, oob_is_err=False)))
                P.end()
            if '2' in self.opt:
                return
            with contextlib.ExitStack() as s3:
                sb3 = lambda n, s, dt=F32: self.sb(s3, n, s, dt)
                Wg = [sb3(f"Wg{i}", [128, 8, 512], BF16) for i in range(2)]
                Wu = [sb3(f"Wu{i}", [128, 8, 512], BF16) for i in range(2)]
                Wd = [sb3(f"Wd{i}", [128, 4, 1024], BF16) for i in range(2)]
                xsT = [sb3(f"xsT{i}", [128, 8, 512], BF16) for i in range(2)]
                hid = [sb3(f"hid{i}", [128, 4, 512], BF16) for i in range(2)]
                sg = [sb3(f"sg{i}", [128, 512]) for i in range(2)]
                yb = [sb3(f"yb{i}", [128, D]) for i in range(2)]
                P.begin()
                P.raw('gpsimd', 'reg_mov', self.reg_w, (l + 1) * NE * 256 - 1)
                n_y = [0]

                def load_blk(b):
                    a = b % 2
                    for (Wt, view, nm) in ((Wg[a], wgv, f'Wg{a}'), (Wu[a], wuv, f'Wu{a}'), (Wd[a], wdv, f'Wd{a}')):
                        flat = Wt.rearrange("p a f -> p (a f)")
                        for hf in range(0 if 'G' in self.opt else 2):
                            P.dma('gpsimd', None, None, reads=['widx'], writes=[nm],
                                  fn=('indirect_dma_start', dict(
                                      out=flat[:, hf * 2048:(hf + 1) * 2048], out_offset=None, in_=view,
                                      in_offset=bass.IndirectOffsetOnAxis(ap=widx[:, b, hf:hf + 1], axis=0),
                                      bounds_check=self.reg_w, oob_is_err=False)))
                    X = xsT[a]
                    for kc in range(8):
                        P.dma('sync', None, None, reads=['xs'], writes=[f'xsT{a}'],
                              fn=('dma_start_transpose', dict(
                                  out=X[:, kc, :], in_=self.xs[b * BLK:(b + 1) * BLK, kc * 128:(kc + 1) * 128])))

                def compute_blk(b):
                    a = b % 2
                    X = xsT[a]
                    rxs = f'xsT{a}'
                    if 'C' in self.opt:
                        return
                    Hd = hid[a]
                    rh = f'hid{a}'
                    wg4 = Wg[a].rearrange("p kc (m four) -> p kc four m", four=4)
                    wu4 = Wu[a].rearrange("p kc (m four) -> p kc four m", four=4)
                    for fc in range(4):
                        pg_, pu_ = 2 + (fc % 2) * 2, 3 + (fc % 2) * 2
                        for kc in range(8):
                            P.op('tensor', 'matmul', reads=[f'Wg{a}', rxs], writes=[f'ps{pg_}'], out=ps[pg_],
                                 lhsT=wg4[:, kc, fc, :], rhs=X[:, kc, :], start=(kc == 0), stop=(kc == 7))
                        for kc in range(8):
                            P.op('tensor', 'matmul', reads=[f'Wu{a}', rxs], writes=[f'ps{pu_}'], out=ps[pu_],
                                 lhsT=wu4[:, kc, fc, :], rhs=X[:, kc, :], start=(kc == 0), stop=(kc == 7))
                        S = sg[fc % 2]
                        P.op('scalar', 'activation', reads=[f'ps{pg_}'], writes=[f'sg{fc % 2}'], out=S, in_=ps[pg_],
                             func=AF.Silu)
                        P.op('vector', 'tensor_tensor', reads=[f'sg{fc % 2}', f'ps{pu_}'], writes=[rh], out=Hd[:, fc, :],
                             in0=S, in1=ps[pu_], op=ALU.mult)
                    for rt in range(4):
                        n_y[0] += 1
                        Y = yb[n_y[0] % 2]
                        ry = f'yb{n_y[0] % 2}'
                        for half in range(2):
                            pb = 6 + half
                            for fc in range(4):
                                P.op('tensor', 'matmul', reads=[rh, f'Wd{a}'], writes=[f'ps{pb}'], out=ps[pb],
                                     lhsT=Hd[:, fc, rt * 128:(rt + 1) * 128], rhs=Wd[a][:, fc, half * 512:(half + 1) * 512],
                                     start=(fc == 0), stop=(fc == 3))
                            self.evac(half, Y[:, half * 512:(half + 1) * 512], ps[pb], [f'ps{pb}'], [ry])
                        r0 = b * BLK + rt * 128
                        P.dma('sync', self.ys[r0:r0 + 128, :], Y, reads=[ry], writes=['ys'])

                load_blk(0)
                for b in range(NB):
                    if b + 1 < NB:
                        load_blk(b + 1)
                    compute_blk(b)
                P.end()
            if '3' in self.opt:
                return
            with contextlib.ExitStack() as s4:
                sb4 = lambda n, s, dt=F32: self.sb(s4, n, s, dt)
                y1 = [sb4(f"y1_{i}", [128, D]) for i in range(2)]
                y2 = [sb4(f"y2_{i}", [128, D]) for i in range(2)]
                xr = [sb4(f"xr{i}", [128, D]) for i in range(2)]
                fgb = sb4("fgb", [128, D])
                fsq = sb4("fsq", [128, D])
                fss = sb4("fss", [128, 1])
                frs = sb4("frs", [128, 1])
                P.begin()
                P.raw('gpsimd', 'reg_mov', self.reg_rows, NR - 1)
                if last:
                    P.dma('sync', fgb, self.final_g.partition_broadcast(128), writes=['fgb'])
                def load_tile(i):
                    a = i % 2
                    rows = slice(i * 128, (i + 1) * 128)
                    P.dma('sync', xr[a], self.xres[rows, :], writes=[f'xr{a}'])
                    for (Yk, rk, k) in ((y1[a], f'y1_{a}', 0), (y2[a], f'y2_{a}', 1)):
                        P.dma('gpsimd', None, None, reads=['dest_i'], writes=[rk],
                              fn=('indirect_dma_start', dict(
                                  out=Yk, out_offset=None, in_=self.ys,
                                  in_offset=bass.IndirectOffsetOnAxis(ap=dest_i[:, i, k:k + 1], axis=0),
                                  bounds_check=self.reg_rows, oob_is_err=False)))
                load_tile(0)
                for i in range(NT):
                    a = i % 2
                    Y1, Y2, X = y1[a], y2[a], xr[a]
                    r1, r2, rx = f'y1_{a}', f'y2_{a}', f'xr{a}'
                    rows = slice(i * 128, (i + 1) * 128)
                    if i + 1 < NT:
                        load_tile(i + 1)
                    P.op('vector', 'tensor_scalar', reads=[r1, 'rw'], writes=[r1], out=Y1, in0=Y1,
                         scalar1=self.rw[:, i, 0:1], scalar2=None, op0=ALU.mult)
                    P.op('vector', 'scalar_tensor_tensor', reads=[r1, r2, 'rw'], writes=[r1], out=Y1, in0=Y2,
                         scalar=self.rw[:, i, 1:2], in1=Y1, op0=ALU.mult, op1=ALU.add)
                    P.op('gpsimd', 'tensor_tensor', reads=[r1, 'modb'], writes=[r1], out=Y1, in0=Y1, in1=self.mod(5),
                         op=ALU.mult)
                    P.op('vector', 'tensor_tensor', reads=[r1, rx], writes=[rx], out=X, in0=X, in1=Y1, op=ALU.add)
                    if not last:
                        P.dma('sync', self.xres[rows, :], X, reads=[rx])
                    else:
                        P.op('scalar', 'activation', reads=[rx], writes=['fsq', 'fss'], out=fsq, in_=X, func=AF.Square,
                             accum_out=fss)
                        self.rstd_ops(fss, frs, D, ['fss'], ['frs'])
                        P.op('vector', 'scalar_tensor_tensor', reads=[rx, 'frs', 'fgb'], writes=[r2], out=Y2, in0=X,
                             scalar=frs, in1=fgb, op0=ALU.mult, op1=ALU.mult)
                        P.dma('sync', self.out[rows, :], Y2, reads=[r2])
                P.end()

    def build_layer(self, l):
        with contextlib.ExitStack() as st:
            ph = self.phases
            self.phase_ada(l, st)
            if 'i' in ph:
                self.phase_inproj(l, st)
            if 'c' in ph and 'f' in ph:
                self.phase_attn(l, conv_inside=True)
            elif 'c' in ph:
                self.phase_conv(l)
            elif 'f' in ph:
                self.phase_attn(l)
            if 's' in ph:
                self.phase_ssd(l)
            if 'o' in ph:
                self.phase_wout_router(l, st)
            if 'm' in ph and self.moe:
                self.phase_moe(l, st)

    def zero_xs(self):
        P = self.P
        NR = self.NB * self.BLK
        with contextlib.ExitStack() as st:
            z = self.sb(st, "zeros", [128, 8192], BF16)
            P.begin()
            P.op('gpsimd', 'memset', writes=['zeros'], ap=z, constant=0.0)
            xv = self.xs.rearrange("(a p r) d -> a p (r d)", p=128, r=8)
            for a in range(NR // 1024):
                P.dma('sync', xv[a], z, reads=['zeros'])
            P.end()

    def build(self):
        if self.moe:
            self.zero_xs()
        for l in range(self.L):
            self.build_layer(l)
        return self.nc


def prep_core_inputs(inp, b, T, L):
    f = lambda a: np.ascontiguousarray(a, dtype=np.float32)
    m = {}
    m["x"] = f(inp["x"][b, :T])
    m["c"] = f(inp["c"][b].reshape(8, 128).T)
    for k in ("ada_w", "ada_b", "norm_mix_g", "w_in", "ssd_dt_bias", "ssd_a_log", "ssd_d", "ssd_norm_g",
              "w_out", "norm_ffn_g", "w_router_group", "b_router_group", "w_router_expert", "b_router_expert",
              "w_gate", "w_up", "w_down"):
        m[k] = f(inp[k][:L])
    m["final_norm_g"] = f(inp["final_norm_g"])
    m["convw_fm"] = f(inp["ssd_conv_w"][:L].reshape(L, 4, 8, 128).transpose(0, 3, 2, 1))
    m["convb_fm"] = f(inp["ssd_conv_b"][:L].reshape(L, 8, 128).transpose(0, 2, 1))
    m["fox_f_bias_fm"] = f(inp["fox_f_bias"][:L].reshape(L, 4, 1))
    m["fox_g_fm"] = f(inp["fox_norm_g"][:L].reshape(L, 2, 128).transpose(0, 2, 1))
    m["cmw_fm"] = f(inp["cm_conv_w"][:L].reshape(L, 31, 2, 128).transpose(0, 3, 2, 1))
    m["cmb_fm"] = f(inp["cm_conv_b"][:L].reshape(L, 2, 128).transpose(0, 2, 1))
    m["cmg_fm"] = f(inp["cm_ln_g"][:L].reshape(L, 2, 128).transpose(0, 2, 1))
    m["cmbeta_fm"] = f(inp["cm_ln_b"][:L].reshape(L, 2, 128).transpose(0, 2, 1))
    return m


_CACHE = {}
T_FULL, L_FULL, B_FULL = 8192, 4, 4


def kernel(**inputs):
    if 'nc' not in _CACHE:
        bd = Builder(T_FULL, L_FULL)
        _CACHE['nc'] = bd.build()
        _CACHE['names'] = set(bd.ins)
    nc = _CACHE['nc']
    inp = {k: np.asarray(v) for k, v in inputs.items()}
    maps = []
    for b in range(B_FULL):
        m = prep_core_inputs(inp, b, T_FULL, L_FULL)
        maps.append({k: v for k, v in m.items() if k in _CACHE['names']})
    res = run_bass_kernel_spmd(nc, maps, core_ids=list(range(B_FULL)))
    out = np.stack([np.asarray(r["out"], dtype=np.float32) for r in res.results], axis=0)
    return out
```

```python
import contextlib
import numpy as np
import concourse.bass as bass
import concourse.mybir as mybir
from concourse.bass_utils import run_bass_kernel_spmd

F32 = mybir.dt.float32
BF16 = mybir.dt.bfloat16
I32 = mybir.dt.int32
AF = mybir.ActivationFunctionType
ALU = mybir.AluOpType
AX = mybir.AxisListType

ENGS = ('tensor', 'vector', 'scalar', 'gpsimd', 'sync')
CENGS = ('tensor', 'vector', 'scalar', 'gpsimd')
NDS = 10

D = 1024
D_IN = 2828
EPS = 1e-6
NE = 32
DE = 512


class Prog:
    def __init__(self, nc, same_engine_sync=True):
        self.nc = nc
        self.same = same_engine_sync
        self.esem = {e: nc.alloc_semaphore(name=f"es_{e}") for e in CENGS}
        self.dsem = {e: [nc.alloc_semaphore(name=f"ds_{e}_{i}") for i in range(NDS)]
                     for e in ('sync', 'scalar', 'gpsimd')}
        self._reset()
        self.q = None

    def _reset(self):
        keep = getattr(self, 'dcnt', {}).get('gpsimd', [0] * NDS)
        self.ecnt = {e: 0 for e in CENGS}
        self.dcnt = {e: [0] * NDS for e in self.dsem}
        self.dcnt['gpsimd'] = list(keep)
        self.dnext = {e: 0 for e in self.dsem}
        self.known = {e: {('d', 'gpsimd', i): keep[i] for i in range(NDS)} for e in ENGS}
        self.res_w = {}
        self.res_r = {}

    def semof(self, key):
        if key[0] == 'e':
            return self.esem[key[1]]
        return self.dsem[key[1]][key[2]]

    def begin(self):
        self.q = {e: [] for e in ENGS}

    def _deps(self, reads, writes):
        deps = []
        for r in reads:
            t = self.res_w.get(r)
            if t is not None:
                deps.append(t)
        for w in writes:
            t = self.res_w.get(w)
            if t is not None:
                deps.append(t)
            deps.extend(self.res_r.get(w, {}).items())
        return deps

    def _record(self, tok, reads, writes):
        for r in reads:
            d = self.res_r.setdefault(r, {})
            if d.get(tok[0], 0) < tok[1]:
                d[tok[0]] = tok[1]
        for w in writes:
            self.res_w[w] = tok
            self.res_r[w] = {}

    def _waits(self, eng, deps):
        waits = []
        best = {}
        for (key, c) in deps:
            if best.get(key, 0) < c:
                best[key] = c
        for key, c in best.items():
            if key == ('e', 'tensor') and eng == 'tensor':
                continue
            if (not self.same) and key == ('e', eng):
                continue
            if self.known[eng].get(key, 0) >= c:
                continue
            self.known[eng][key] = c
            waits.append((self.semof(key), c))
        return waits

    def op(self, eng, name, reads=(), writes=(), **kw):
        writes = list(writes) + [r for r in reads if r.startswith('ps') and r not in writes]
        waits = self._waits(eng, self._deps(reads, writes))
        self.ecnt[eng] += 1
        tok = (('e', eng), self.ecnt[eng])
        sem = self.esem[eng]

        def emit(e, name=name, kw=kw, waits=waits, sem=sem):
            for (s, c) in waits:
                e.wait_ge(s, c)
            getattr(e, name)(**kw).then_inc(sem, 1)
        self.q[eng].append(emit)
        self._record(tok, reads, writes)
        return tok

    def raw(self, eng, name, *args, **kw):
        self.q[eng].append(lambda e: getattr(e, name)(*args, **kw))

    def dma(self, eng, out, in_, reads=(), writes=(), fn=None, **kw):
        deps = self._deps(reads, writes)
        idx = self.dnext[eng]
        self.dnext[eng] = (idx + 1) % NDS
        key = ('d', eng, idx)
        prev = self.dcnt[eng][idx]
        if prev:
            deps.append((key, prev))
        waits = self._waits(eng, deps)
        self.dcnt[eng][idx] += 16
        tok = (key, self.dcnt[eng][idx])
        sem = self.dsem[eng][idx]

        def emit(e, waits=waits, sem=sem):
            for (s, c) in waits:
                e.wait_ge(s, c)
            if fn is not None:
                try:
                    ins = getattr(e, fn[0])(**fn[1])
                except Exception:
                    print("DMA builder failed:", fn[0], {k: (v.shape if hasattr(v, 'shape') else v) for k, v in fn[1].items()})
                    raise
                ins.then_inc(sem, 16)
            else:
                e.dma_start(out=out, in_=in_, **kw).then_inc(sem, 16)
        self.q[eng].append(emit)
        self._record(tok, reads, writes)
        return tok

    def end(self):
        nc = self.nc
        fin = []
        for e in self.dsem:
            for i in range(NDS):
                c = self.dcnt[e][i]
                if c and self.known['sync'].get(('d', e, i), 0) < c:
                    fin.append((self.dsem[e][i], c))

        def drain(e, fin=fin):
            for (s, c) in fin:
                e.wait_ge(s, c)
        self.q['sync'].append(drain)
        with nc.Block() as block:
            for en in ENGS:
                ops = self.q[en]
                if not ops:
                    continue

                def body(e, ops=ops):
                    for o in ops:
                        o(e)
                getattr(block, en)(body)
        allsems = list(self.esem.values()) + [s for e in self.dsem if e != 'gpsimd' for s in self.dsem[e]]
        with nc.Block() as block:
            def clr(e):
                for s in allsems:
                    e.sem_clear(s)
            block.sync(clr)
        self._reset()
        self.q = None


class Builder:
    def __init__(self, T, L, dbg=(), moe=True, phases='aicfsom', opt=''):
        self.opt = opt
        self.T, self.L = T, L
        self.moe = moe
        self.phases = phases
        self.NT = T // 128
        self.NQ = T // 512
        self.dbg = set(dbg)
        nc = self.nc = bass.Bass("TRN2", target_bir_lowering=False)
        self.P = Prog(nc, same_engine_sync=('S' not in self.opt))
        self.ins = {}
        self.outs = {}
        self._uid = 0
        self.declare_io()
        self.consts()

    def din(self, name, shape, dt=F32):
        t = self.nc.dram_tensor(name, list(shape), dt, kind="ExternalInput").ap()
        self.ins[name] = t
        return t

    def dscr(self, name, shape, dt=F32):
        kind = "ExternalOutput" if name in self.dbg else "Internal"
        t = self.nc.dram_tensor(name, list(shape), dt, kind=kind).ap()
        if kind == "ExternalOutput":
            self.outs[name] = t
        return t

    def sb(self, st, name, shape, dt=F32):
        self._uid += 1
        return st.enter_context(self.nc.sbuf_tensor(f"{name}_{self._uid}", list(shape), dt)).ap()

    def declare_io(self):
        T, L = self.T, self.L
        d = self.din
        self.x_in = d("x", [T, D])
        self.c_in = d("c", [128, 8])
        self.ada_w = d("ada_w", [L, D, 6 * D])
        self.ada_b = d("ada_b", [L, 6 * D])
        self.norm_mix_g = d("norm_mix_g", [L, D])
        self.w_in = d("w_in", [L, D, D_IN])
        self.convw = d("convw_fm", [L, 128, 8, 4])
        self.convb = d("convb_fm", [L, 128, 8])
        self.dt_bias = d("ssd_dt_bias", [L, 8])
        self.a_log = d("ssd_a_log", [L, 8])
        self.ssd_d = d("ssd_d", [L, 8])
        self.ssd_norm_g = d("ssd_norm_g", [L, 512])
        self.f_bias = d("fox_f_bias_fm", [L, 4, 1])
        self.fox_g = d("fox_g_fm", [L, 128, 2])
        self.cmw = d("cmw_fm", [L, 128, 2, 31])
        self.cmb = d("cmb_fm", [L, 128, 2])
        self.cmg = d("cmg_fm", [L, 128, 2])
        self.cmbeta = d("cmbeta_fm", [L, 128, 2])
        self.w_out = d("w_out", [L, D, D])
        self.norm_ffn_g = d("norm_ffn_g", [L, D])
        self.w_rg = d("w_router_group", [L, D, 4])
        self.b_rg = d("b_router_group", [L, 4])
        self.w_re = d("w_router_expert", [L, D, NE])
        self.b_re = d("b_router_expert", [L, NE])
        if self.moe:
            self.w_gate = d("w_gate", [L, NE, D, DE])
            self.w_up = d("w_up", [L, NE, D, DE])
            self.w_down = d("w_down", [L, NE, DE, D])
        self.final_g = d("final_norm_g", [D])
        self.out = self.nc.dram_tensor("out", [T, D], F32, kind="ExternalOutput").ap()
        self.outs["out"] = self.out
        s = self.dscr
        self.xres = s("xres", [T, D])
        self.zs = s("zs", [T, 512])
        self.xbcT = s("xbcT", [1024, T])
        self.dtr = s("dtr", [T, 8])
        self.flT = s("flT", [4, T])
        self.qT = s("qT", [256, T], BF16)
        self.kT = s("kT", [256, T], BF16)
        self.vtok = s("vtok", [T, 256], BF16)
        self.gaT = s("gaT", [256, T])
        self.gbT = s("gbT", [256, T])
        self.yT = s("yT", [1024, T], BF16)
        self.attT = s("attT", [256, T])
        self.cs = s("cs", [4, 6, T], BF16)
        self.hfp = s("hfp", [T, D], BF16)
        self.BLK = 512
        self.NB = (2 * T) // self.BLK + NE
        self.xs = s("xs", [self.NB * self.BLK, D], BF16)
        self.ys = s("ys", [self.NB * self.BLK, D])
        self.rdbg = s("rdbg", [T, 68])

    def consts(self):
        nc, P = self.nc, self.P
        a = lambda n, s, dt=F32: nc.alloc_sbuf_tensor(n, list(s), dt).ap()
        self.ident = a("ident", [128, 128])
        self.identb = a("identb", [128, 128], BF16)
        self.tri = a("tri", [128, 128])
        self.ustr = a("ustr", [128, 128])
        self.ones = a("ones", [128, 128])
        self.onesb = a("onesb", [128, 128], BF16)
        self.epsb = a("epsb", [128, 1])
        self.sutb = a("sutb", [128, 128], BF16)
        self.ps = [nc.alloc_psum_tensor(f"ps{i}", [128, 512], F32).ap() for i in range(8)]
        self.reg_rows = nc.gpsimd.alloc_register("bc_rows")
        self.reg_w = nc.gpsimd.alloc_register("bc_w")
        P.begin()
        g = 'gpsimd'
        P.op(g, 'memset', writes=['epsb'], ap=self.epsb, constant=EPS)
        P.op(g, 'memset', writes=['ones'], ap=self.ones, constant=1.0)
        P.op(g, 'memset', writes=['onesb'], ap=self.onesb, constant=1.0)
        P.op(g, 'memset', writes=['ident'], ap=self.ident, constant=1.0)
        P.op(g, 'affine_select', reads=['ident'], writes=['ident'], out=self.ident, in_=self.ident,
             pattern=[[-1, 128]], compare_op=ALU.is_equal, fill=0.0, base=0, channel_multiplier=1)
        P.op(g, 'tensor_copy', reads=['ident'], writes=['identb'], out=self.identb, in_=self.ident)
        P.op(g, 'memset', writes=['tri'], ap=self.tri, constant=1.0)
        P.op(g, 'affine_select', reads=['tri'], writes=['tri'], out=self.tri, in_=self.tri,
             pattern=[[1, 128]], compare_op=ALU.is_ge, fill=0.0, base=0, channel_multiplier=-1)
        P.op(g, 'tensor_tensor', reads=['tri', 'ident'], writes=['sutb'], out=self.sutb, in0=self.tri, in1=self.ident,
             op=ALU.subtract)
        P.op(g, 'memset', writes=['ustr'], ap=self.ustr, constant=1.0)
        P.op(g, 'affine_select', reads=['ustr'], writes=['ustr'], out=self.ustr, in_=self.ustr,
             pattern=[[-1, 128]], compare_op=ALU.is_gt, fill=0.0, base=0, channel_multiplier=1)
        P.end()

    def phase_ada(self, l, st):
        nc, P = self.nc, self.P
        self.modb = self.sb(st, "modb", [128, 6 * D])
        with contextlib.ExitStack() as s2:
            cs = self.sb(s2, "c_s", [128, 8])
            cb = self.sb(s2, "c_b", [128, 8, 128])
            adab = self.sb(s2, "adab", [128, 6 * D])
            wch = [self.sb(s2, f"adaw{i}", [128, 8, 512]) for i in range(2)]
            P.begin()
            P.dma('sync', cs, self.c_in, writes=['c_s'])
            P.dma('sync', adab, self.ada_b[l].partition_broadcast(128), writes=['adab'])
            P.op('scalar', 'activation', reads=['c_s'], writes=['c_s'], out=cs, in_=cs, func=AF.Silu)
            P.op('vector', 'tensor_copy', reads=['c_s'], writes=['c_b'], out=cb,
                 in_=cs.unsqueeze(2).broadcast_to([128, 8, 128]))
            for j in range(12):
                w = wch[j % 2]
                wr = f'adaw{j % 2}'
                P.dma('sync', w,
                      self.ada_w[l][:, j * 512:(j + 1) * 512].rearrange("(kc p) n -> p kc n", p=128),
                      writes=[wr])
                pt = self.ps[j % 2]
                for kc in range(8):
                    P.op('tensor', 'matmul', reads=['c_b', wr], writes=[f'ps{j % 2}'], out=pt,
                         lhsT=cb[:, kc, :], rhs=w[:, kc, :], start=(kc == 0), stop=(kc == 7))
                P.op('vector', 'tensor_tensor', reads=[f'ps{j % 2}', 'adab'], writes=['modb'],
                     out=self.modb[:, j * 512:(j + 1) * 512], in0=pt, in1=adab[:, j * 512:(j + 1) * 512], op=ALU.add)
            P.end()

    def mod(self, i):
        return self.modb[:, i * D:(i + 1) * D]

    def load_cast(self, st, dst, src, res, width):
        P = self.P
        if getattr(self, '_stg_owner', None) is not st:
            self._stg = [self.sb(st, f"stg{i}", [128, 2048]) for i in range(2)]
            self._stg_owner = st
            self._stg_n = 0
        for c0 in range(0, width, 2048):
            n = min(2048, width - c0)
            k = self._stg_n
            self._stg_n += 1
            S = self._stg[k % 2]
            rs = f'stg{k % 2}'
            P.dma('sync', S[:, 0:n], src[:, c0:c0 + n], writes=[rs])
            P.op('gpsimd' if k % 2 else 'vector', 'tensor_copy', reads=[rs], writes=[res], out=dst[:, c0:c0 + n],
                 in_=S[:, 0:n])

    def rstd_ops(self, ss, rstd, n, rd, wr):
        P = self.P
        P.op('scalar', 'activation', reads=rd, writes=wr, out=rstd, in_=ss, func=AF.Ln,
             bias=self.epsb[:ss.shape[0], :], scale=1.0 / n)
        P.op('scalar', 'activation', reads=wr, writes=wr, out=rstd, in_=rstd, func=AF.Exp, scale=-0.5)

    def phase_inproj(self, l, st0):
        nc, P, T = self.nc, self.P, self.T
        src = self.x_in if l == 0 else self.xres
        self._ip_bufs = None
        with contextlib.ExitStack() as st:
            sb = lambda n, s, dt=F32: self.sb(st, n, s, dt)
            wz = sb("wz", [128, 8, 512], BF16)
            wx = sb("wx", [128, 8, 1024], BF16)
            wqk = sb("wqk", [128, 8, 512], BF16)
            wv = sb("wv", [128, 8, 256], BF16)
            wg = sb("wg", [128, 8, 512], BF16)
            wdt = sb("wdt", [128, 8, 8])
            wf = sb("wf", [128, 8, 4])
            gsc = sb("gsc", [128, D])
            W = self.w_in[l].rearrange("(kc p) n -> p kc n", p=128)
            P.begin()
            for kc in range(8):
                self.load_cast(st, wz[:, kc, :], W[:, kc, 0:512], 'wz', 512)
                self.load_cast(st, wx[:, kc, :], W[:, kc, 512:1536], 'wx', 1024)
                self.load_cast(st, wqk[:, kc, :], W[:, kc, 1544:2056], 'wqk', 512)
                self.load_cast(st, wv[:, kc, :], W[:, kc, 2056:2312], 'wv', 256)
                self.load_cast(st, wg[:, kc, :], W[:, kc, 2316:2828], 'wg', 512)
            P.dma('sync', wdt, W[:, :, 1536:1544], writes=['wdt'])
            P.dma('sync', wf, W[:, :, 2312:2316], writes=['wf'])
            P.dma('sync', gsc, self.norm_mix_g[l].partition_broadcast(128), writes=['gsc'])
            P.op('vector', 'scalar_tensor_tensor', reads=['gsc', 'modb'], writes=['gsc'], out=gsc,
                 in0=self.mod(1), scalar=1.0, in1=gsc, op0=ALU.add, op1=ALU.mult)
            self.norm_and_transpose_loop(st, src, gsc, self.mod(0), consumer=lambda q, hT, hT32: self.inproj_chunk(
                q, hT, hT32, wz, wx, wqk, wv, wg, wdt, wf, st))
            P.end()

    def norm_and_transpose_loop(self, st, src, gsc, shift, consumer, pre=None, after_h=None, pre_load=None):
        P = self.P
        sb = lambda n, s, dt=F32: self.sb(st, n, s, dt)
        NXB = 4
        xt = [sb(f"xt{i}", [128, D]) for i in range(NXB)]
        ht = [sb(f"ht{i}", [128, D]) for i in range(2)]
        sq = sb("sq", [128, D])
        ss = [sb(f"ss{i}", [128, 1]) for i in range(2)]
        rs = [sb(f"rs{i}", [128, 1]) for i in range(2)]
        hT32 = [sb(f"hT32_{i}", [128, 8, 512]) for i in range(2)]
        hT = [sb(f"hT_{i}", [128, 8, 512], BF16) for i in range(2)]
        PF = 2

        def load(i):
            if i >= self.NT:
                return
            if pre_load is not None:
                pre_load(i)
            elif pre is None:
                P.dma('sync', xt[i % NXB], src[i * 128:(i + 1) * 128, :], writes=[f'xt{i % NXB}'])
        for i in range(PF):
            load(i)
        for q in range(self.NQ):
            b = q % 2
            for j in range(4):
                i = q * 4 + j
                a = i % 2
                X, H = xt[i % NXB], ht[a]
                rx, rh = f'xt{i % NXB}', f'ht{a}'
                load(i + PF)
                if pre is not None:
                    pre(i, X, rx)
                P.op('scalar', 'activation', reads=[rx], writes=['sq', f'ss{a}'], out=sq, in_=X, func=AF.Square,
                     accum_out=ss[a])
                self.rstd_ops(ss[a], rs[a], D, [f'ss{a}'], [f'rs{a}'])
                P.op('vector', 'scalar_tensor_tensor', reads=[rx, f'rs{a}', 'gsc'], writes=[rh], out=H, in0=X,
                     scalar=rs[a], in1=gsc, op0=ALU.mult, op1=ALU.mult)
                P.op('vector', 'tensor_tensor', reads=[rh, 'modb'], writes=[rh], out=H, in0=H, in1=shift, op=ALU.add)
                if after_h is not None:
                    after_h(i, H, rh, X, rx)
                for half in range(2):
                    pt = self.ps[half]
                    for k4 in range(4):
                        kc = half * 4 + k4
                        P.op('tensor', 'transpose', reads=[rh, 'ident'], writes=[f'ps{half}'],
                             out=pt[:, k4 * 128:(k4 + 1) * 128], in_=H[:, kc * 128:(kc + 1) * 128], identity=self.ident)
                    dst = hT32[b][:, half * 4:(half + 1) * 4, j * 128:(j + 1) * 128]
                    P.op('scalar', 'activation', reads=[f'ps{half}'], writes=[f'hT32_{b}'], out=dst,
                         in_=pt.rearrange("p (k t) -> p k t", k=4), func=AF.Copy)
                P.op('vector', 'tensor_copy', reads=[f'hT32_{b}'], writes=[f'hT_{b}'],
                     out=hT[b][:, :, j * 128:(j + 1) * 128], in_=hT32[b][:, :, j * 128:(j + 1) * 128])
            consumer(q, (hT[b], f'hT_{b}'), (hT32[b], f'hT32_{b}'))

    def evac(self, k, out, in_, reads, writes, scale=None):
        P = self.P
        if k % 2 == 0:
            if scale is None:
                P.op('scalar', 'activation', reads=reads, writes=writes, out=out, in_=in_, func=AF.Copy)
            else:
                P.op('scalar', 'activation', reads=reads, writes=writes, out=out, in_=in_, func=AF.Copy, scale=scale)
        else:
            if scale is None:
                P.op('vector', 'tensor_copy', reads=reads, writes=writes, out=out, in_=in_)
            else:
                P.op('vector', 'tensor_scalar', reads=reads, writes=writes, out=out, in0=in_, scalar1=scale,
                     scalar2=None, op0=ALU.mult)

    def inproj_chunk(self, q, hTb, hT32b, wz, wx, wqk, wv, wg, wdt, wf, st):
        P = self.P
        hT, rhT = hTb
        hT32, rhT32 = hT32b
        if self._ip_bufs is None:
            sb = lambda n, s, dt=F32: self.sb(st, n, s, dt)
            self._ip_bufs = dict(
                o32=[sb(f"o32_{i}", [128, 512]) for i in range(3)],
                o16=[sb(f"o16_{i}", [128, 512], BF16) for i in range(3)],
                osm=[sb(f"osm_{i}", [128, 8]) for i in range(2)],
                ofl=[sb(f"ofl_{i}", [4, 512]) for i in range(2)],
                n=[0],
            )
        B = self._ip_bufs
        tok = slice(q * 512, (q + 1) * 512)

        def nxt():
            B['n'][0] += 1
            return B['n'][0]
        PB = [2, 3, 4, 5]

        def fm(w, wres, c0, dst, dt16=False, scale=None):
            k = nxt()
            pb = PB[k % 4]
            pt = self.ps[pb]
            for kc in range(8):
                P.op('tensor', 'matmul', reads=[wres, rhT], writes=[f'ps{pb}'], out=pt, lhsT=w[:, kc, c0:c0 + 128],
                     rhs=hT[:, kc, :], start=(kc == 0), stop=(kc == 7))
            o = (B['o16'] if dt16 else B['o32'])[k % 3]
            ores = ('o16_' if dt16 else 'o32_') + str(k % 3)
            self.evac(k, o, pt, [f'ps{pb}'], [ores], scale=scale)
            P.dma('sync', dst, o, reads=[ores], writes=[])
        for ct in range(8):
            fm(wx, 'wx', ct * 128, self.xbcT[ct * 128:(ct + 1) * 128, tok])
        for ct in range(2):
            fm(wqk, 'wqk', ct * 128, self.qT[ct * 128:(ct + 1) * 128, tok], dt16=True, scale=0.125)
        for ct in range(2):
            fm(wqk, 'wqk', 256 + ct * 128, self.kT[ct * 128:(ct + 1) * 128, tok], dt16=True)
        for ct in range(2):
            fm(wg, 'wg', ct * 128, self.gaT[ct * 128:(ct + 1) * 128, tok])
        for ct in range(2):
            fm(wg, 'wg', 256 + ct * 128, self.gbT[ct * 128:(ct + 1) * 128, tok])
        k = nxt()
        pb = PB[k % 4]
        pt = self.ps[pb]
        for kc in range(8):
            P.op('tensor', 'matmul', reads=['wf', rhT32], writes=[f'ps{pb}'], out=pt[0:4, :], lhsT=wf[:, kc, :],
                 rhs=hT32[:, kc, :], start=(kc == 0), stop=(kc == 7))
        o = B['ofl'][q % 2]
        P.op('vector', 'tensor_copy', reads=[f'ps{pb}'], writes=[f'ofl_{q % 2}'], out=o, in_=pt[0:4, :])
        P.dma('sync', self.flT[:, tok], o, reads=[f'ofl_{q % 2}'])
        for j in range(4):
            tt = slice(q * 512 + j * 128, q * 512 + (j + 1) * 128)
            k = nxt()
            pb = PB[k % 4]
            pt = self.ps[pb]
            for kc in range(8):
                P.op('tensor', 'matmul', reads=['wz', rhT], writes=[f'ps{pb}'], out=pt,
                     lhsT=hT[:, kc, j * 128:(j + 1) * 128], rhs=wz[:, kc, :], start=(kc == 0), stop=(kc == 7))
            o = B['o32'][k % 3]
            P.op('scalar', 'activation', reads=[f'ps{pb}'], writes=[f'o32_{k % 3}'], out=o, in_=pt, func=AF.Silu)
            P.dma('sync', self.zs[tt, :], o, reads=[f'o32_{k % 3}'])
            k = nxt()
            pb = PB[k % 4]
            pt = self.ps[pb]
            for kc in range(8):
                P.op('tensor', 'matmul', reads=['wv', rhT], writes=[f'ps{pb}'], out=pt[:, 0:256],
                     lhsT=hT[:, kc, j * 128:(j + 1) * 128], rhs=wv[:, kc, :], start=(kc == 0), stop=(kc == 7))
            for kc in range(8):
                P.op('tensor', 'matmul', reads=['wdt', rhT32], writes=[f'ps{pb}'], out=pt[:, 256:264],
                     lhsT=hT32[:, kc, j * 128:(j + 1) * 128], rhs=wdt[:, kc, :], start=(kc == 0), stop=(kc == 7))
            o = B['o16'][k % 3]
            self.evac(k, o[:, 0:256], pt[:, 0:256], [f'ps{pb}'], [f'o16_{k % 3}'])
            P.dma('sync', self.vtok[tt, :], o[:, 0:256], reads=[f'o16_{k % 3}'])
            o2 = B['osm'][j % 2]
            P.op('vector', 'tensor_copy', reads=[f'ps{pb}'], writes=[f'osm_{j % 2}'], out=o2, in_=pt[:, 256:264])
            P.dma('sync', self.dtr[tt, :], o2, reads=[f'osm_{j % 2}'])

    def conv_gen(self, l, st, pA, pB):
        P, T = self.P, self.T
        TC = min(T, 1024)
        HALO = 30
        if True:
            sb = lambda n, s, dt=F32: self.sb(st, n, s, dt)
            cw = sb("cw", [128, 2, 31])
            cbias = sb("cbias", [128, 2])
            cg = sb("cg", [128, 2])
            cbeta = sb("cbeta", [128, 2])
            ua = [sb(f"ua{i}", [128, TC + HALO]) for i in range(2)]
            ub = [sb(f"ub{i}", [128, TC + HALO]) for i in range(2)]
            co = [sb(f"co{i}", [128, TC]) for i in range(2)]
            sqt = sb("csq", [128, 512])
            mean = sb("cmean", [128, 512])
            rstd = sb("crstd", [128, 512])
            tmp = [sb(f"ctmp{i}", [128, 512]) for i in range(2)]
            yo = [sb(f"cyo{i}", [128, 512], BF16) for i in range(2)]
            P.dma('sync', cw, self.cmw[l], writes=['cw'])
            P.dma('sync', cbias, self.cmb[l], writes=['cbias'])
            P.dma('sync', cg, self.cmg[l], writes=['cg'])
            P.dma('sync', cbeta, self.cmbeta[l], writes=['cbeta'])
            yield
            n = 0
            for c0 in range(0, T, TC):
                for ct in range(2):
                    A, Bt = ua[ct], ub[ct]
                    ra, rb, rc = f'ua{ct}', f'ub{ct}', f'co{ct}'
                    rows = slice(ct * 128, (ct + 1) * 128)
                    if c0 == 0:
                        P.dma('sync', A[:, HALO:], self.gaT[rows, 0:TC], writes=[ra])
                        P.dma('sync', Bt[:, HALO:], self.gbT[rows, 0:TC], writes=[rb])
                        P.op('gpsimd', 'memset', writes=[ra], ap=A[:, 0:HALO], constant=0.0)
                        P.op('gpsimd', 'memset', writes=[rb], ap=Bt[:, 0:HALO], constant=0.0)
                    else:
                        P.dma('sync', A, self.gaT[rows, c0 - HALO:c0 + TC], writes=[ra])
                        P.dma('sync', Bt, self.gbT[rows, c0 - HALO:c0 + TC], writes=[rb])
                    P.op('scalar', 'activation', reads=[rb], writes=[rb], out=Bt, in_=Bt, func=AF.Sigmoid)
                    P.op('gpsimd', 'tensor_tensor', reads=[ra, rb], writes=[ra], out=A, in0=A, in1=Bt, op=ALU.mult)
                    C = co[ct]
                    P.op('vector', 'tensor_scalar', reads=[ra, 'cw', 'cbias'], writes=[rc], out=C, in0=A[:, 0:TC],
                         scalar1=cw[:, ct, 0:1], scalar2=cbias[:, ct:ct + 1], op0=ALU.mult, op1=ALU.add)
                    for k in range(1, 31):
                        P.op('vector', 'scalar_tensor_tensor', reads=[ra, 'cw', rc], writes=[rc], out=C,
                             in0=A[:, k:k + TC], scalar=cw[:, ct, k:k + 1], in1=C, op0=ALU.mult, op1=ALU.add)
                        if k % 8 == 0:
                            yield
                    yield
                for s0 in range(0, TC, 512):
                    cs_ = slice(s0, s0 + 512)
                    p1, p2 = self.ps[pA], self.ps[pB]
                    for ct in range(2):
                        P.op('tensor', 'matmul', reads=['ones', f'co{ct}'], writes=[f'ps{pA}'], out=p1, lhsT=self.ones,
                             rhs=co[ct][:, cs_], start=(ct == 0), stop=(ct == 1))
                    P.op('vector', 'tensor_scalar', reads=[f'ps{pA}'], writes=['cmean'], out=mean, in0=p1,
                         scalar1=1.0 / 256, scalar2=None, op0=ALU.mult)
                    for ct in range(2):
                        P.op('scalar', 'activation', reads=[f'co{ct}'], writes=['csq'], out=sqt, in_=co[ct][:, cs_],
                             func=AF.Square)
                        P.op('tensor', 'matmul', reads=['ones', 'csq'], writes=[f'ps{pB}'], out=p2, lhsT=self.ones,
                             rhs=sqt, start=(ct == 0), stop=(ct == 1))
                    P.op('vector', 'tensor_tensor', reads=['cmean'], writes=['crstd'], out=rstd, in0=mean, in1=mean,
                         op=ALU.mult)
                    P.op('vector', 'scalar_tensor_tensor', reads=[f'ps{pB}', 'crstd'], writes=['crstd'], out=rstd, in0=p2,
                         scalar=1.0 / 256, in1=rstd, op0=ALU.mult, op1=ALU.subtract)
                    self.rstd_ops(rstd, rstd, 1.0, ['crstd'], ['crstd'])
                    for ct in range(2):
                        n += 1
                        t = tmp[n % 2]
                        rt = f'ctmp{n % 2}'
                        P.op('vector', 'tensor_tensor', reads=[f'co{ct}', 'cmean'], writes=[rt], out=t,
                             in0=co[ct][:, cs_], in1=mean, op=ALU.subtract)
                        P.op('gpsimd', 'tensor_tensor', reads=[rt, 'crstd'], writes=[rt], out=t, in0=t, in1=rstd,
                             op=ALU.mult)
                        y = yo[n % 2]
                        ry = f'cyo{n % 2}'
                        P.op('scalar', 'activation', reads=[rt, 'cg', 'cbeta'], writes=[ry], out=y, in_=t, func=AF.Silu,
                             scale=cg[:, ct:ct + 1], bias=cbeta[:, ct:ct + 1])
                        P.dma('sync', self.yT[768 + ct * 128:768 + (ct + 1) * 128, c0 + s0:c0 + s0 + 512], y,
                              reads=[ry])
                    yield

    def phase_conv(self, l):
        with contextlib.ExitStack() as st:
            self.P.begin()
            for _ in self.conv_gen(l, st, 0, 1):
                pass
            self.P.end()

    def phase_attn(self, l, conv_inside=False):
        P, T, NT, NQ = self.P, self.T, self.NT, self.NQ
        CW = min(T, 2048)
        with contextlib.ExitStack() as st:
            sb = lambda n, s, dt=F32: self.sb(st, n, s, dt)
            fb = sb("fb", [4, 1])
            xx = sb("fx", [4, CW])
            ax = sb("fax", [4, CW])
            mn = sb("fmn", [4, CW])
            cum = [sb(f"fcum{i}", [4, CW]) for i in range(2)]
            r1 = sb("fr1", [4, CW])
            sp = sb("fsp", [4, 6, CW], BF16)
            P.begin()
            P.dma('sync', fb, self.f_bias[l], writes=['fb'])
            for ci, c0 in enumerate(range(0, T, CW)):
                cc = cum[ci % 2]
                rcum = f'fcum{ci % 2}'
                P.dma('sync', xx, self.flT[:, c0:c0 + CW], writes=['fx'])
                P.op('scalar', 'activation', reads=['fx', 'fb'], writes=['fx'], out=xx, in_=xx, func=AF.Identity,
                     bias=fb[:, 0:1], scale=1.0)
                P.op('vector', 'tensor_scalar', reads=['fx'], writes=['fmn'], out=mn, in0=xx, scalar1=-1.0, scalar2=0.0,
                     op0=ALU.mult, op1=ALU.max)
                P.op('vector', 'scalar_tensor_tensor', reads=['fmn', 'fx'], writes=['fax'], out=ax, in0=mn, scalar=-2.0,
                     in1=xx, op0=ALU.mult, op1=ALU.subtract)
                P.op('scalar', 'activation', reads=['fax'], writes=['fax'], out=ax, in_=ax, func=AF.Exp)
                P.op('scalar', 'activation', reads=['fax'], writes=['fax'], out=ax, in_=ax, func=AF.Ln, bias=1.0,
                     scale=1.0)
                P.op('vector', 'scalar_tensor_tensor', reads=['fmn', 'fax'], writes=['fmn'], out=mn, in0=mn, scalar=-1.0,
                     in1=ax, op0=ALU.mult, op1=ALU.subtract)
                init = 0.0 if ci == 0 else cum[(ci - 1) % 2][:, CW - 1:CW]
                P.op('vector', 'tensor_tensor_scan', reads=['fmn', 'ones', f'fcum{(ci - 1) % 2}'], writes=[rcum], out=cc,
                     data0=self.ones[0:4, 0:1].broadcast_to([4, CW]), data1=mn, initial=init, op0=ALU.mult, op1=ALU.add)
                P.op('vector', 'tensor_copy', reads=[rcum], writes=['fsp'], out=sp[:, 0, :], in_=cc)
                P.op('vector', 'tensor_tensor', reads=[rcum, 'fsp'], writes=['fr1'], out=r1, in0=cc, in1=sp[:, 0, :],
                     op=ALU.subtract)
                P.op('vector', 'tensor_copy', reads=['fr1'], writes=['fsp'], out=sp[:, 1, :], in_=r1)
                P.op('vector', 'tensor_tensor', reads=['fr1', 'fsp'], writes=['fr1'], out=r1, in0=r1, in1=sp[:, 1, :],
                     op=ALU.subtract)
                P.op('vector', 'tensor_copy', reads=['fr1'], writes=['fsp'], out=sp[:, 2, :], in_=r1)
                P.op('vector', 'tensor_scalar', reads=['fsp'], writes=['fsp'], out=sp[:, 3:6, :], in0=sp[:, 0:3, :],
                     scalar1=-1.0, scalar2=None, op0=ALU.mult)
                P.dma('sync', self.cs[:, :, c0:c0 + CW], sp, reads=['fsp'])
            P.end()
        with contextlib.ExitStack() as st:
            sb = lambda n, s, dt=F32: self.sb(st, n, s, dt)
            qp = [sb(f"qp{i}", [70, T], BF16) for i in range(2)]
            kp = [sb(f"kp{i}", [70, T], BF16) for i in range(2)]
            vp = [sb(f"vp{i}", [128, NT, 65], BF16) for i in range(2)]
            nm = sb("negmask", [128, 4, 512], BF16)
            NSB = 5
            LAG = 3
            pt_ = [sb(f"pT{i}", [128, 512], BF16) for i in range(NSB)]
            rec = sb("rec", [65, 512])
            bcs = sb("bcs", [64, 512])
            on = [sb(f"on{i}", [64, 512]) for i in range(2)]
            P.begin()
            cgen = self.conv_gen(l, st, 7, 7) if conv_inside else None
            n_units = (T // min(T, 1024)) * (2 * 5 + min(T, 1024) // 512) + 1
            units_done = 0
            work_total = 4 * sum(4 * q_ + 4 for q_ in range(NQ))
            work_done = 0
            P.op('gpsimd', 'memset', writes=['negmask'], ap=nm, constant=0.0)
            for d in range(4):
                P.op('gpsimd', 'affine_select', reads=['negmask'], writes=['negmask'], out=nm[:, d, :], in_=nm[:, d, :],
                     pattern=[[1, 512]], compare_op=ALU.is_ge, fill=-30000.0, base=-128 * d, channel_multiplier=-1)
            step = 0
            for h in range(4):
                hb = h % 2
                Q, Kp, V = qp[hb], kp[hb], vp[hb]
                rq, rk, rv = f'qp{hb}', f'kp{hb}', f'vp{hb}'
                hr = slice(h * 64, (h + 1) * 64)
                P.op('gpsimd', 'memset', writes=[rq], ap=Q[64:70, :], constant=1.0)
                P.op('gpsimd', 'memset', writes=[rk], ap=Kp[64:70, :], constant=1.0)
                P.op('gpsimd', 'memset', writes=[rv], ap=V[:, :, 64:65], constant=1.0)
                P.dma('sync', Q[0:64, :], self.qT[hr, :], writes=[rq])
                P.dma('sync', Kp[0:64, :], self.kT[hr, :], writes=[rk])
                P.dma('sync', Q[67:70, :], self.cs[h, 0:3, :], writes=[rq])
                P.dma('sync', Kp[64:67, :], self.cs[h, 3:6, :], writes=[rk])
                for i0 in range(0, NT, 4):
                    P.dma('sync', V[:, i0:i0 + 4, 0:64],
                          self.vtok[i0 * 128:(i0 + 4) * 128, hr].rearrange("(i p) d -> p i d", p=128), writes=[rv])
                for qc in range(NQ):
                    nk = 4 * qc + 4
                    ob = 5 + qc % 2
                    O = self.ps[ob]
                    qs = slice(qc * 512, (qc + 1) * 512)
                    for s_ in range(nk + LAG):
                        if s_ < nk:
                            kt = s_
                            sbk = (step + s_) % NSB
                            S = self.ps[sbk]
                            diag = kt >= 4 * qc
                            P.op('tensor', 'matmul', reads=[rq, rk], writes=[f'ps{sbk}'], out=S,
                                 lhsT=Kp[:, kt * 128:(kt + 1) * 128], rhs=Q[:, qs], start=True, stop=not diag)
                            if diag:
                                P.op('tensor', 'matmul', reads=['identb', 'negmask'], writes=[f'ps{sbk}'], out=S,
                                     lhsT=self.identb, rhs=nm[:, kt - 4 * qc, :], start=False, stop=True)
                        if 1 <= s_ <= nk:
                            kt = s_ - 1
                            sbk = (step + kt) % NSB
                            P.op('scalar', 'activation', reads=[f'ps{sbk}'], writes=[f'pT{sbk}'], out=pt_[sbk],
                                 in_=self.ps[sbk], func=AF.Exp)
                        if s_ >= LAG:
                            kt = s_ - LAG
                            sbk = (step + kt) % NSB
                            P.op('tensor', 'matmul', reads=[f'pT{sbk}', rv], writes=[f'ps{ob}'], out=O[0:65, :],
                                 lhsT=V[:, kt, :], rhs=pt_[sbk], start=(kt == 0), stop=(kt == nk - 1))
                    step += nk
                    P.op('vector', 'reciprocal', reads=[f'ps{ob}'], writes=['rec'], out=rec[64:65, :], in_=O[64:65, :])
                    P.op('tensor', 'matmul', reads=['ones', 'rec'], writes=['ps7'], out=self.ps[7][0:64, :],
                         lhsT=self.ones[64:65, 0:64], rhs=rec[64:65, :], start=True, stop=True)
                    P.op('scalar', 'activation', reads=['ps7'], writes=['bcs'], out=bcs, in_=self.ps[7][0:64, :],
                         func=AF.Copy)
                    o_ = on[qc % 2]
                    P.op('vector', 'tensor_tensor', reads=[f'ps{ob}', 'bcs'], writes=[f'on{qc % 2}'], out=o_,
                         in0=O[0:64, :], in1=bcs, op=ALU.mult)
                    P.dma('sync', self.attT[hr, qs], o_, reads=[f'on{qc % 2}'])
                    work_done += nk
                    while cgen is not None and units_done * work_total < n_units * work_done:
                        try:
                            next(cgen)
                            units_done += 1
                        except StopIteration:
                            cgen = None
                if h == 3 and cgen is not None:
                    for _ in cgen:
                        pass
                if h < 3:
                    P.end()
                    P.begin()
            P.end()
        with contextlib.ExitStack() as st:
            sb = lambda n, s, dt=F32: self.sb(st, n, s, dt)
            fg = sb("foxg", [128, 2])
            at = [[sb(f"at{i}{c}", [128, 512]) for c in range(2)] for i in range(2)]
            sq = sb("asq", [128, 512])
            rs = sb("ars", [128, 512])
            yo = [sb(f"ayo{i}", [128, 512], BF16) for i in range(2)]
            P.begin()
            P.dma('sync', fg, self.fox_g[l], writes=['foxg'])
            n = 0
            for qc in range(NQ):
                qs = slice(qc * 512, (qc + 1) * 512)
                b = qc % 2
                for ct in range(2):
                    P.dma('sync', at[b][ct], self.attT[ct * 128:(ct + 1) * 128, qs], writes=[f'at{b}{ct}'])
                    P.op('scalar', 'activation', reads=[f'at{b}{ct}'], writes=['asq'], out=sq, in_=at[b][ct],
                         func=AF.Square)
                    P.op('tensor', 'matmul', reads=['ones', 'asq'], writes=['ps0'], out=self.ps[0], lhsT=self.ones,
                         rhs=sq, start=(ct == 0), stop=(ct == 1))
                self.rstd_ops(self.ps[0], rs, 256.0, ['ps0'], ['ars'])
                for ct in range(2):
                    n += 1
                    P.op('vector', 'tensor_tensor', reads=[f'at{b}{ct}', 'ars'], writes=[f'at{b}{ct}'], out=at[b][ct],
                         in0=at[b][ct], in1=rs, op=ALU.mult)
                    y = yo[n % 2]
                    P.op('scalar', 'activation', reads=[f'at{b}{ct}', 'foxg'], writes=[f'ayo{n % 2}'], out=y,
                         in_=at[b][ct], func=AF.Copy, scale=fg[:, ct:ct + 1])
                    P.dma('sync', self.yT[512 + ct * 128:512 + (ct + 1) * 128, qs], y, reads=[f'ayo{n % 2}'])
            P.end()

    def phase_ssd(self, l):
        P, T, NT = self.P, self.T, self.NT
        SC = 512
        assert NT * 8 <= 512
        with contextlib.ExitStack() as st:
            sb = lambda n, s, dt=F32: self.sb(st, n, s, dt)
            cw4 = sb("cw4", [128, 8, 4])
            cb4 = sb("cb4", [128, 8])
            dtb = sb("dtb", [128, 8])
            aneg = sb("aneg", [128, 8])
            dsk = sb("dsk", [128, 8])
            ng = sb("ssdng", [128, 512])
            dt = sb("dt_all", [128, NT, 8])
            dmn = sb("dt_mn", [128, NT, 8])
            dtA = sb("dtA", [128, NT, 8])
            El = sb("El", [128, NT, 8])
            Wl = sb("Wl", [128, NT, 8])
            cd = sb("cd", [128, NT, 8])
            xin = [sb(f"xin{i}", [128, SC + 3]) for i in range(2)]
            cacc = [sb(f"cacc{i}", [128, SC]) for i in range(2)]
            xsT = [sb(f"xsT{i}", [128, SC]) for i in range(4)]
            BT = [sb(f"BT{i}", [128, SC], BF16) for i in range(2)]
            CT = [sb(f"CT{i}", [128, SC], BF16) for i in range(2)]
            x32_2 = [sb(f"x32{i}", [128, 8, 64], F32) for i in range(2)]
            Btok_2 = [sb(f"Btok{i}", [128, 256], BF16) for i in range(2)]
            R_2 = [sb(f"Rall{i}", [128, 8, 128], F32) for i in range(2)]
            E_2 = [sb(f"Eall{i}", [128, 8, 128], F32) for i in range(2)]
            CBm_2 = [sb(f"CBm{i}", [128, 2, 128], F32) for i in range(2)]
            M_2 = [sb(f"Mall{i}", [128, 8, 128], BF16) for i in range(2)]
            xdt_2 = [sb(f"xdt{i}", [128, 8, 64], BF16) for i in range(2)]
            xw_2 = [sb(f"xw{i}", [128, 8, 64], BF16) for i in range(2)]
            H = sb("Hst", [128, 8, 64])
            Hb = sb("Hb", [128, 8, 64], BF16)
            t1_2 = [sb(f"sst1{i}", [128, 8, 64], F32) for i in range(2)]
            t2_2 = [sb(f"sst2{i}", [128, 8, 64], F32) for i in range(2)]
            zt_2 = [sb(f"zt{i}", [128, 512], F32) for i in range(2)]
            ssq_2 = [sb(f"ssq{i}", [128, 512], F32) for i in range(2)]
            gss_2 = [sb(f"gss{i}", [128, 2], F32) for i in range(2)]
            grs_2 = [sb(f"grs{i}", [128, 2], F32) for i in range(2)]
            yTs_2 = [sb(f"yTs{i}", [128, 4, 128], BF16) for i in range(2)]
            ps = self.ps
            psb1 = ps[1].bitcast(BF16)
            P.begin()
            P.dma('sync', cw4, self.convw[l], writes=['cw4'])
            P.dma('sync', cb4, self.convb[l], writes=['cb4'])
            P.dma('sync', dtb, self.dt_bias[l].partition_broadcast(128), writes=['dtb'])
            P.dma('sync', aneg, self.a_log[l].partition_broadcast(128), writes=['aneg'])
            P.dma('sync', dsk, self.ssd_d[l].partition_broadcast(128), writes=['dsk'])
            P.dma('sync', ng, self.ssd_norm_g[l].partition_broadcast(128), writes=['ssdng'])
            for i0 in range(0, NT, 4):
                n_ = min(4, NT - i0)
                P.dma('sync', dt[:, i0:i0 + n_, :],
                      self.dtr[i0 * 128:(i0 + n_) * 128, :].rearrange("(i p) h -> p i h", p=128), writes=['dt_all'])
            P.op('scalar', 'activation', reads=['aneg'], writes=['aneg'], out=aneg, in_=aneg, func=AF.Exp)
            P.op('vector', 'tensor_scalar', reads=['aneg'], writes=['aneg'], out=aneg, in0=aneg, scalar1=-1.0,
                 scalar2=None, op0=ALU.mult)
            bc3 = lambda t: t.unsqueeze(1).broadcast_to([128, NT, 8])
            P.op('vector', 'tensor_tensor', reads=['dt_all', 'dtb'], writes=['dt_all'], out=dt, in0=dt, in1=bc3(dtb),
                 op=ALU.add)
            P.op('vector', 'tensor_scalar', reads=['dt_all'], writes=['dt_mn'], out=dmn, in0=dt, scalar1=0.0,
                 scalar2=None, op0=ALU.max)
            P.op('vector', 'scalar_tensor_tensor', reads=['dt_mn', 'dt_all'], writes=['dt_all'],
                 out=dt.rearrange("p i h -> p (i h)"), in0=dmn.rearrange("p i h -> p (i h)"), scalar=-2.0,
                 in1=dt.rearrange("p i h -> p (i h)"), op0=ALU.mult, op1=ALU.add)
            P.op('scalar', 'activation', reads=['dt_all'], writes=['dt_all'], out=dt, in_=dt, func=AF.Exp)
            P.op('scalar', 'activation', reads=['dt_all'], writes=['dt_all'], out=dt, in_=dt, func=AF.Ln, bias=1.0,
                 scale=1.0)
            P.op('vector', 'tensor_tensor', reads=['dt_all', 'dt_mn'], writes=['dt_all'], out=dt, in0=dt, in1=dmn,
                 op=ALU.add)
            P.op('vector', 'tensor_tensor', reads=['dt_all', 'aneg'], writes=['dtA'], out=dtA, in0=dt, in1=bc3(aneg),
                 op=ALU.mult)
            dtA2 = dtA.rearrange("p i h -> p (i h)")
            P.op('tensor', 'matmul', reads=['tri', 'dtA'], writes=['ps2'], out=ps[2][:, 0:NT * 8], lhsT=self.tri,
                 rhs=dtA2, start=True, stop=True)
            P.op('tensor', 'matmul', reads=['ones', 'dtA'], writes=['ps3'], out=ps[3][:, 0:NT * 8], lhsT=self.ones,
                 rhs=dtA2, start=True, stop=True)
            f2 = lambda t: t.rearrange("p i h -> p (i h)")
            P.op('scalar', 'activation', reads=['ps2'], writes=['El'], out=f2(El), in_=ps[2][:, 0:NT * 8], func=AF.Exp)
            P.op('scalar', 'activation', reads=['ps3'], writes=['cd'], out=f2(cd), in_=ps[3][:, 0:NT * 8], func=AF.Exp)
            P.op('vector', 'tensor_copy', reads=['ps3'], writes=['Wl'], out=f2(Wl), in_=ps[3][:, 0:NT * 8])
            P.op('vector', 'tensor_tensor', reads=['Wl', 'ps2'], writes=['Wl'], out=f2(Wl), in0=f2(Wl),
                 in1=ps[2][:, 0:NT * 8], op=ALU.subtract)
            P.op('scalar', 'activation', reads=['Wl'], writes=['Wl'], out=Wl, in_=Wl, func=AF.Exp)
            P.op('vector', 'tensor_tensor', reads=['Wl', 'dt_all'], writes=['Wl'], out=Wl, in0=Wl, in1=dt, op=ALU.mult)
            P.op('gpsimd', 'memset', writes=['Hst'], ap=H, constant=0.0)
            P.op('gpsimd', 'memset', writes=['Hb'], ap=Hb, constant=0.0)
            for c0 in range(0, T, SC):
                for ct in range(8):
                    X = xin[ct % 2]
                    rx = f'xin{ct % 2}'
                    A = cacc[ct % 2]
                    ra = f'cacc{ct % 2}'
                    rows = slice(ct * 128, (ct + 1) * 128)
                    if c0 == 0:
                        P.op('gpsimd', 'memset', writes=[rx], ap=X[:, 0:3], constant=0.0)
                        P.dma('sync', X[:, 3:], self.xbcT[rows, 0:SC], writes=[rx])
                    else:
                        P.dma('sync', X, self.xbcT[rows, c0 - 3:c0 + SC], writes=[rx])
                    P.op('vector', 'tensor_scalar', reads=[rx, 'cw4', 'cb4'], writes=[ra], out=A, in0=X[:, 0:SC],
                         scalar1=cw4[:, ct, 0:1], scalar2=cb4[:, ct:ct + 1], op0=ALU.mult, op1=ALU.add)
                    for k in range(1, 4):
                        P.op('vector', 'scalar_tensor_tensor', reads=[rx, 'cw4', ra], writes=[ra], out=A,
                             in0=X[:, k:k + SC], scalar=cw4[:, ct, k:k + 1], in1=A, op0=ALU.mult, op1=ALU.add)
                    if ct < 4:
                        dst, rd = xsT[ct], f'xsT{ct}'
                    elif ct < 6:
                        dst, rd = BT[ct - 4], f'BT{ct - 4}'
                    else:
                        dst, rd = CT[ct - 6], f'CT{ct - 6}'
                    P.op('scalar', 'activation', reads=[ra], writes=[rd], out=dst, in_=A, func=AF.Silu)
                def stage_a(c0, cc):
                        c = c0 // 128 + cc
                        cs_ = slice(cc * 128, (cc + 1) * 128)
                        tok = slice(c * 128, (c + 1) * 128)
                        pc = c % 2
                        x32 = x32_2[pc]
                        Btok = Btok_2[pc]
                        R = R_2[pc]
                        E = E_2[pc]
                        CBm = CBm_2[pc]
                        M = M_2[pc]
                        xdt = xdt_2[pc]
                        xw = xw_2[pc]
                        t1 = t1_2[pc]
                        t2 = t2_2[pc]
                        zt = zt_2[pc]
                        ssq = ssq_2[pc]
                        gss = gss_2[pc]
                        grs = grs_2[pc]
                        yTs = yTs_2[pc]
                        n = {k: k + str(pc) for k in ('x32', 'Btok', 'Rall', 'Eall', 'CBm', 'Mall', 'xdt', 'xw', 'sst1', 'sst2', 'zt', 'ssq', 'gss', 'grs', 'yTs')}
                        for ct in range(4):
                            P.op('tensor', 'transpose', reads=[f'xsT{ct}', 'ident'], writes=['ps0'],
                                 out=ps[0][:, ct * 128:(ct + 1) * 128], in_=xsT[ct][:, cs_], identity=self.ident)
                        P.op('scalar', 'activation', reads=['ps0'], writes=[n['x32']], out=x32.rearrange("p h d -> p (h d)"),
                             in_=ps[0], func=AF.Copy)
                        for g in range(2):
                            P.op('tensor', 'transpose', reads=[f'BT{g}', 'identb'], writes=['ps1'],
                                 out=psb1[:, g * 128:(g + 1) * 128], in_=BT[g][:, cs_], identity=self.identb)
                        P.op('vector', 'tensor_copy', reads=['ps1'], writes=[n['Btok']], out=Btok, in_=psb1[:, 0:256])
                        P.dma('sync', zt, self.zs[tok, :], writes=[n['zt']])
                        P.op('vector', 'tensor_tensor', reads=['tri', 'dtA'], writes=[n['Rall']], out=R,
                             in0=self.tri.unsqueeze(1).broadcast_to([128, 8, 128]),
                             in1=dtA[:, c, :].unsqueeze(2).broadcast_to([128, 8, 128]), op=ALU.mult)
                        for hh in range(2):
                            P.op('tensor', 'matmul', reads=['ustr', n['Rall']], writes=[f'ps{2 + hh}'], out=ps[2 + hh],
                                 lhsT=self.ustr, rhs=R[:, hh * 4:(hh + 1) * 4, :].rearrange("p h l -> p (h l)"),
                                 start=True, stop=True)
                            P.op('scalar', 'activation', reads=[f'ps{2 + hh}'], writes=[n['Eall']],
                                 out=E[:, hh * 4:(hh + 1) * 4, :].rearrange("p h l -> p (h l)"), in_=ps[2 + hh], func=AF.Exp)
                        for g in range(2):
                            P.op('tensor', 'matmul', reads=[f'BT{g}', f'CT{g}'], writes=['ps4'],
                                 out=ps[4][:, g * 128:(g + 1) * 128], lhsT=BT[g][:, cs_], rhs=CT[g][:, cs_],
                                 start=True, stop=True)
                        P.op('vector', 'tensor_tensor', reads=['ps4', 'tri'], writes=[n['CBm']], out=CBm,
                             in0=ps[4][:, 0:256].rearrange("p (g l) -> p g l", g=2),
                             in1=self.tri.unsqueeze(1).broadcast_to([128, 2, 128]), op=ALU.mult)
                        for g in range(2):
                            P.op('vector', 'tensor_tensor', reads=[n['Eall'], n['CBm']], writes=[n['Mall']],
                                 out=M[:, g * 4:(g + 1) * 4, :], in0=E[:, g * 4:(g + 1) * 4, :],
                                 in1=CBm[:, g:g + 1, :].broadcast_to([128, 4, 128]), op=ALU.mult)
                        P.op('gpsimd', 'tensor_tensor', reads=[n['x32'], 'dt_all'], writes=[n['xdt']], out=xdt, in0=x32,
                             in1=dt[:, c, :].unsqueeze(2).broadcast_to([128, 8, 64]), op=ALU.mult)
                        P.op('gpsimd', 'tensor_tensor', reads=[n['x32'], 'Wl'], writes=[n['xw']], out=xw, in0=x32,
                             in1=Wl[:, c, :].unsqueeze(2).broadcast_to([128, 8, 64]), op=ALU.mult)
                        for h in range(8):
                            P.op('tensor', 'matmul', reads=[n['Mall'], n['xdt']], writes=['ps5'], out=ps[5][:, h * 64:(h + 1) * 64],
                                 lhsT=M[:, h, :], rhs=xdt[:, h, :], start=True, stop=True)
                        for g in range(2):
                            P.op('tensor', 'matmul', reads=[f'CT{g}', 'Hb'], writes=['ps6'],
                                 out=ps[6][:, g * 256:(g + 1) * 256], lhsT=CT[g][:, cs_],
                                 rhs=Hb[:, g * 4:(g + 1) * 4, :].rearrange("p h d -> p (h d)"), start=True, stop=True)
                        for g in range(2):
                            P.op('tensor', 'matmul', reads=[n['Btok'], n['xw']], writes=['ps7'],
                                 out=ps[7][:, g * 256:(g + 1) * 256], lhsT=Btok[:, g * 128:(g + 1) * 128],
                                 rhs=xw[:, g * 4:(g + 1) * 4, :].rearrange("p h d -> p (h d)"), start=True, stop=True)
                        v3 = lambda t: t.rearrange("p (h d) -> p h d", h=8)
                        b3 = lambda t: t.unsqueeze(2).broadcast_to([128, 8, 64])
                        P.op('vector', 'tensor_tensor', reads=['ps6', 'El'], writes=[n['sst1']], out=t1, in0=v3(ps[6]),
                             in1=b3(El[:, c, :]), op=ALU.mult)
                        P.op('vector', 'tensor_tensor', reads=[n['sst1'], 'ps5'], writes=[n['sst1']], out=t1, in0=t1, in1=v3(ps[5]),
                             op=ALU.add)
                        P.op('gpsimd', 'tensor_tensor', reads=[n['x32'], 'dsk'], writes=[n['sst2']], out=t2, in0=x32, in1=b3(dsk),
                             op=ALU.mult)
                        P.op('gpsimd', 'tensor_tensor', reads=[n['sst1'], n['sst2']], writes=[n['sst1']], out=t1, in0=t1, in1=t2,
                             op=ALU.add)
                        P.op('vector', 'tensor_tensor', reads=['Hst', 'cd'], writes=['Hst'], out=H, in0=H, in1=b3(cd[:, c, :]),
                             op=ALU.mult)
                        P.op('vector', 'tensor_tensor', reads=['Hst', 'ps7'], writes=['Hst'], out=H, in0=H, in1=v3(ps[7]),
                             op=ALU.add)
                        P.op('gpsimd', 'tensor_copy', reads=['Hst'], writes=['Hb'], out=Hb, in_=H)

                def stage_b(c):
                        tok = slice(c * 128, (c + 1) * 128)
                        pc = c % 2
                        x32 = x32_2[pc]
                        Btok = Btok_2[pc]
                        R = R_2[pc]
                        E = E_2[pc]
                        CBm = CBm_2[pc]
                        M = M_2[pc]
                        xdt = xdt_2[pc]
                        xw = xw_2[pc]
                        t1 = t1_2[pc]
                        t2 = t2_2[pc]
                        zt = zt_2[pc]
                        ssq = ssq_2[pc]
                        gss = gss_2[pc]
                        grs = grs_2[pc]
                        yTs = yTs_2[pc]
                        n = {k: k + str(pc) for k in ('x32', 'Btok', 'Rall', 'Eall', 'CBm', 'Mall', 'xdt', 'xw', 'sst1', 'sst2', 'zt', 'ssq', 'gss', 'grs', 'yTs')}
                        y2 = t1.rearrange("p h d -> p (h d)")
                        P.op('gpsimd', 'tensor_tensor', reads=[n['sst1'], n['zt']], writes=[n['sst1']], out=y2, in0=y2, in1=zt,
                             op=ALU.mult)
                        for g in range(2):
                            P.op('scalar', 'activation', reads=[n['sst1']], writes=[n['ssq'], n['gss']],
                                 out=ssq[:, g * 256:(g + 1) * 256], in_=y2[:, g * 256:(g + 1) * 256], func=AF.Square,
                                 accum_out=gss[:, g:g + 1])
                        self.rstd_ops(gss, grs, 256.0, [n['gss']], [n['grs']])
                        for g in range(2):
                            P.op('vector', 'scalar_tensor_tensor', reads=[n['sst1'], n['grs'], 'ssdng'], writes=[n['sst2']],
                                 out=t2.rearrange("p h d -> p (h d)")[:, g * 256:(g + 1) * 256],
                                 in0=y2[:, g * 256:(g + 1) * 256], scalar=grs[:, g:g + 1],
                                 in1=ng[:, g * 256:(g + 1) * 256], op0=ALU.mult, op1=ALU.mult)
                        yn = t2.rearrange("p h d -> p (h d)")
                        for ct in range(4):
                            P.op('tensor', 'transpose', reads=[n['sst2'], 'ident'], writes=['ps0'],
                                 out=ps[0][:, ct * 128:(ct + 1) * 128], in_=yn[:, ct * 128:(ct + 1) * 128],
                                 identity=self.ident)
                        P.op('scalar', 'activation', reads=['ps0'], writes=[n['yTs']], out=yTs.rearrange("p c t -> p (c t)"),
                             in_=ps[0], func=AF.Copy)
                        P.dma('sync', self.yT[0:512, tok].rearrange("(ct p) t -> p ct t", p=128), yTs, reads=[n['yTs']])

                for cc in range(SC // 128):
                    c = c0 // 128 + cc
                    stage_a(c0, cc)
                    if c >= 1:
                        stage_b(c - 1)
                if c0 + SC >= T:
                    stage_b(NT - 1)
            P.end()

    def phase_wout_router(self, l, st0):
        P, T, NT = self.P, self.T, self.NT
        src = self.x_in if l == 0 else self.xres
        ps = self.ps
        self.ohb = self.sb(st0, "ohb", [128, NT, 64], BF16)
        self.rw = self.sb(st0, "rw", [128, NT, 2])
        with contextlib.ExitStack() as st:
            sb = lambda n, s, dt=F32: self.sb(st, n, s, dt)
            wo = sb("wo", [128, 8, D], BF16)
            gsc = sb("gscf", [128, D])
            wr = sb("wr", [128, 8, 36])
            rb = sb("rbias", [128, 36])
            yTc = [sb(f"yTc{i}", [128, 8, 512], BF16) for i in range(2)]
            xl = [sb(f"xl{i}", [128, D]) for i in range(4)]
            tt = sb("wtmp", [128, D])
            hb = [sb(f"hperm{i}", [128, D], BF16) for i in range(2)]
            lg = sb("lg", [128, 36])
            lg4 = sb("lg4", [128, 4, 36])
            gmax4 = sb("gmax4", [128, 4])
            gsum4 = sb("gsum4", [128, 4])
            r4 = sb("r4", [128, 4])
            den4 = sb("den4", [128, 4])
            ohg4 = sb("ohg4", [128, 4, 4])
            gexp4 = sb("gexp4", [128, 4, 4])
            em4 = sb("em4", [128, 4, 4, 8])
            es4 = sb("es4", [128, 4, 8])
            t84 = sb("t84", [128, 4, 8])
            s14 = sb("s14", [128, 4, 8])
            s24 = sb("s24", [128, 4, 8])
            sm = sb("rsm", [128, 64])
            gexp = sb("gexp", [128, 4])
            ohg = sb("ohg", [128, 4])
            em = sb("em", [128, 4, 8])
            es = sb("esel", [128, 8])
            t8 = sb("top8", [128, 8])
            s1 = sb("sel1", [128, 8])
            s2 = sb("sel2", [128, 8])
            W = self.w_out[l].rearrange("(kc p) n -> p kc n", p=128)
            P.begin()
            for kc in range(8):
                self.load_cast(st, wo[:, kc, :], W[:, kc, :], 'wo', D)
            P.dma('sync', wr[:, :, 0:4], self.w_rg[l].rearrange("(kc p) n -> p kc n", p=128), writes=['wr'])
            P.dma('sync', wr[:, :, 4:36], self.w_re[l].rearrange("(kc p) n -> p kc n", p=128), writes=['wr'])
            P.dma('sync', rb[:, 0:4], self.b_rg[l].partition_broadcast(128), writes=['rbias'])
            P.dma('sync', rb[:, 4:36], self.b_re[l].partition_broadcast(128), writes=['rbias'])
            P.dma('sync', gsc, self.norm_ffn_g[l].partition_broadcast(128), writes=['gscf'])
            P.op('vector', 'scalar_tensor_tensor', reads=['gscf', 'modb'], writes=['gscf'], out=gsc, in0=self.mod(4),
                 scalar=1.0, in1=gsc, op0=ALU.add, op1=ALU.mult)

            def pre_load(i):
                q, j = divmod(i, 4)
                if j == 0:
                    P.dma('sync', yTc[q % 2], self.yT[:, q * 512:(q + 1) * 512].rearrange("(kc p) t -> p kc t", p=128),
                          writes=[f'yTc{q % 2}'])
                P.dma('sync', xl[i % 4], src[i * 128:(i + 1) * 128, :], writes=[f'xl{i % 4}'])

            def pre(i, X, rx):
                q, j = divmod(i, 4)
                Y = yTc[q % 2]
                ry = f'yTc{q % 2}'
                XL = xl[i % 4]
                rl = f'xl{i % 4}'
                for half in range(2):
                    pb = 2 + half
                    for kc in range(8):
                        P.op('tensor', 'matmul', reads=[ry, 'wo'], writes=[f'ps{pb}'], out=ps[pb],
                             lhsT=Y[:, kc, j * 128:(j + 1) * 128], rhs=wo[:, kc, half * 512:(half + 1) * 512],
                             start=(kc == 0), stop=(kc == 7))
                    hs = slice(half * 512, (half + 1) * 512)
                    P.op('vector', 'tensor_tensor', reads=[f'ps{pb}', 'modb'], writes=['wtmp'], out=tt[:, hs], in0=ps[pb],
                         in1=self.mod(2)[:, hs], op=ALU.mult)
                P.op('vector', 'tensor_tensor', reads=['wtmp', rl], writes=[rx], out=X, in0=tt, in1=XL, op=ALU.add)
                P.dma('sync', self.xres[i * 128:(i + 1) * 128, :], X, reads=[rx])

            def after_h(i, Hh, rh, X, rx):
                Hp = hb[i % 2]
                rp = f'hperm{i % 2}'
                P.op('gpsimd', 'tensor_copy', reads=[rh], writes=[rp], out=Hp.rearrange("t (kc p) -> t kc p", kc=8),
                     in_=Hh.rearrange("t (p kc) -> t kc p", kc=8))
                P.dma('sync', self.hfp[i * 128:(i + 1) * 128, :], Hp, reads=[rp])

            def consumer(q, hTb, hT32b):
                hT32, r32 = hT32b
                i0_ = q * 4
                for j in range(4):
                    for kc in range(8):
                        P.op('tensor', 'matmul', reads=[r32, 'wr'], writes=['ps4'], out=ps[4][:, j * 36:(j + 1) * 36],
                             lhsT=hT32[:, kc, j * 128:(j + 1) * 128], rhs=wr[:, kc, :], start=(kc == 0), stop=(kc == 7))
                V = lambda name, **kw: P.op('vector', name, **kw)
                V('tensor_tensor', reads=['ps4', 'rbias'], writes=['lg'], out=lg4,
                  in0=ps[4][:, 0:144].rearrange("p (j c) -> p j c", j=4), in1=rb.unsqueeze(1).broadcast_to([128, 4, 36]),
                  op=ALU.add)
                gl = lg4[:, :, 0:4]
                el = lg4[:, :, 4:36].rearrange("p j (g e) -> p j g e", g=4)
                b3 = lambda t, n_: t.unsqueeze(2).broadcast_to([128, 4, n_])
                V('tensor_reduce', reads=['lg'], writes=['rsm'], out=gmax4, in_=gl, axis=AX.X, op=ALU.max)
                V('tensor_tensor', reads=['lg', 'rsm'], writes=['ohg'], out=ohg4, in0=gl, in1=b3(gmax4, 4), op=ALU.is_ge)
                V('tensor_tensor', reads=['lg', 'rsm'], writes=['gexp'], out=gexp4, in0=gl, in1=b3(gmax4, 4),
                  op=ALU.subtract)
                P.op('scalar', 'activation', reads=['gexp'], writes=['gexp'], out=gexp4, in_=gexp4, func=AF.Exp)
                V('tensor_reduce', reads=['gexp'], writes=['rsm2'], out=gsum4, in_=gexp4, axis=AX.X, op=ALU.add)
                V('reciprocal', reads=['rsm2'], writes=['rsm2'], out=gsum4, in_=gsum4)
                V('tensor_tensor', reads=['lg', 'ohg'], writes=['em'], out=em4, in0=el,
                  in1=ohg4.unsqueeze(3).broadcast_to([128, 4, 4, 8]), op=ALU.mult)
                V('tensor_reduce', reads=['em'], writes=['esel'], out=es4, in_=em4.rearrange("p j g e -> p j e g"),
                  axis=AX.X, op=ALU.add)
                for j in range(4):
                    V('max', reads=['esel'], writes=['top8'], out=t84[:, j, :], in_=es4[:, j, :])
                V('tensor_tensor', reads=['esel', 'top8'], writes=['sel1'], out=s14, in0=es4,
                  in1=t84[:, :, 0:1].broadcast_to([128, 4, 8]), op=ALU.is_ge)
                V('tensor_tensor', reads=['esel', 'top8'], writes=['sel2'], out=s24, in0=es4,
                  in1=t84[:, :, 1:2].broadcast_to([128, 4, 8]), op=ALU.is_ge)
                V('tensor_tensor', reads=['sel2', 'sel1'], writes=['sel2'], out=s24, in0=s24, in1=s14, op=ALU.subtract)
                V('tensor_tensor', reads=['top8'], writes=['rsm3'], out=r4, in0=t84[:, :, 1], in1=t84[:, :, 0],
                  op=ALU.subtract)
                P.op('scalar', 'activation', reads=['rsm3'], writes=['rsm3'], out=r4, in_=r4, func=AF.Exp)
                V('tensor_scalar', reads=['rsm3'], writes=['rsm4'], out=den4, in0=r4, scalar1=1.0, scalar2=None,
                  op0=ALU.add)
                V('reciprocal', reads=['rsm4'], writes=['rsm4'], out=den4, in_=den4)
                V('tensor_tensor', reads=['rsm4', 'rsm2'], writes=['rw'], out=self.rw[:, i0_:i0_ + 4, 0], in0=den4,
                  in1=gsum4, op=ALU.mult)
                V('tensor_tensor', reads=['rw', 'rsm3'], writes=['rw'], out=self.rw[:, i0_:i0_ + 4, 1],
                  in0=self.rw[:, i0_:i0_ + 4, 0], in1=r4, op=ALU.mult)
                for k, sel in enumerate((s14, s24)):
                    V('tensor_tensor', reads=['ohg', f'sel{k + 1}'], writes=['ohb'],
                      out=self.ohb[:, i0_:i0_ + 4, k * 32:(k + 1) * 32].rearrange("p j (g e) -> p j g e", g=4),
                      in0=ohg4.unsqueeze(3).broadcast_to([128, 4, 4, 8]),
                      in1=sel.unsqueeze(2).broadcast_to([128, 4, 4, 8]), op=ALU.mult)
            self.norm_and_transpose_loop(st, None, gsc, self.mod(3), consumer, pre=pre, after_h=after_h, pre_load=pre_load)
            P.end()

    def phase_moe(self, l, st0):
        P, T, NT, NB, BLK = self.P, self.T, self.NT, self.NB, self.BLK
        ps = self.ps
        last = (l == self.L - 1)
        NR = NB * BLK
        wgv = self.w_gate.rearrange("l e (p two k4) f -> (l e p two) (k4 f)", two=2, k4=4)
        wuv = self.w_up.rearrange("l e (p two k4) f -> (l e p two) (k4 f)", two=2, k4=4)
        wdv = self.w_down.rearrange("l e (p two f2) d -> (l e p two) (f2 d)", two=2, f2=2)
        with contextlib.ExitStack() as st:
            sb = lambda n, s, dt=F32: self.sb(st, n, s, dt)
            dest_i = sb("dest_i", [128, NT, 2], I32)
            widx = sb("widx", [128, NB, 2], I32)
            with contextlib.ExitStack() as s1:
                sb1 = lambda n, s, dt=F32: self.sb(s1, n, s, dt)
                pre = sb1("pre_all", [128, NT, 64])
                tot = sb1("tot_all", [128, NT, 64])
                base = sb1("base_all", [128, NT, 64])
                cnt = sb1("cnt", [128, 64])
                tl = sb1("mtotal", [128, 32])
                md = sb1("mmod", [128, 32])
                pend = sb1("pend", [128, 32])
                off = sb1("moff", [128, 64])
                dest_f = sb1("dest_f", [128, NT, 2])
                blk0 = sb1("blk0", [128, NB])
                cmp_ = sb1("mcmp", [128, NB, 32])
                be = sb1("mbe", [128, NB])
                pidx = sb1("pidx", [128, 2])
                wf = sb1("widx_f", [128, NB, 2])
                dbg_t = sb1("rdbg_t", [128, 68])
                P.begin()
                ohb2 = self.ohb.rearrange("p i c -> p (i c)")
                f2 = lambda t: t.rearrange("p i c -> p (i c)")
                for k, c0 in enumerate(range(0, NT * 64, 512)):
                    n = min(512, NT * 64 - c0)
                    P.op('tensor', 'matmul', reads=['sutb', 'ohb'], writes=['ps0'], out=ps[0][:, 0:n], lhsT=self.sutb,
                         rhs=ohb2[:, c0:c0 + n], start=True, stop=True)
                    P.op('scalar', 'activation', reads=['ps0'], writes=['pre_all'], out=f2(pre)[:, c0:c0 + n],
                         in_=ps[0][:, 0:n], func=AF.Copy)
                    P.op('tensor', 'matmul', reads=['onesb', 'ohb'], writes=['ps1'], out=ps[1][:, 0:n], lhsT=self.onesb,
                         rhs=ohb2[:, c0:c0 + n], start=True, stop=True)
                    P.op('vector', 'tensor_copy', reads=['ps1'], writes=['tot_all'], out=f2(tot)[:, c0:c0 + n],
                         in_=ps[1][:, 0:n])
                V = lambda name, **kw: P.op('vector', name, **kw)
                P.op('gpsimd', 'memset', writes=['base_all'], ap=base[:, 0, :], constant=0.0)
                for i in range(1, NT):
                    V('tensor_tensor', reads=['base_all', 'tot_all'], writes=['base_all'], out=base[:, i, :],
                      in0=base[:, i - 1, :], in1=tot[:, i - 1, :], op=ALU.add)
                V('tensor_tensor', reads=['base_all', 'tot_all'], writes=['cnt'], out=cnt, in0=base[:, NT - 1, :],
                  in1=tot[:, NT - 1, :], op=ALU.add)
                V('tensor_tensor', reads=['cnt'], writes=['mtotal'], out=tl, in0=cnt[:, 0:32], in1=cnt[:, 32:64],
                  op=ALU.add)
                P.op('gpsimd', 'iota', writes=['blk0'], out=blk0, pattern=[[BLK, NB]], base=0, channel_multiplier=0,
                     allow_small_or_imprecise_dtypes=True)
                V('tensor_tensor', reads=['mtotal', 'blk0'], writes=['mcmp'], out=cmp_.rearrange("p b e -> p (b e)").rearrange("p (e b) -> p e b", e=32),
                  in0=blk0.unsqueeze(1).broadcast_to([128, 32, NB]), in1=tl.unsqueeze(2).broadcast_to([128, 32, NB]),
                  op=ALU.is_lt)
                V('tensor_reduce', reads=['mcmp'], writes=['mtotal'], out=tl,
                  in_=cmp_.rearrange("p b e -> p (b e)").rearrange("p (e b) -> p e b", e=32), axis=AX.X, op=ALU.add)
                V('tensor_scalar', reads=['mtotal'], writes=['mtotal'], out=tl, in0=tl, scalar1=float(BLK), scalar2=None,
                  op0=ALU.mult)
                V('tensor_tensor_scan', reads=['mtotal', 'ones'], writes=['pend'], out=pend,
                  data0=self.ones[:, 0:32], data1=tl, initial=0.0, op0=ALU.mult, op1=ALU.add)
                V('tensor_tensor', reads=['pend', 'mtotal'], writes=['moff'], out=off[:, 0:32], in0=pend, in1=tl,
                  op=ALU.subtract)
                V('tensor_tensor', reads=['moff', 'cnt'], writes=['moff'], out=off[:, 32:64], in0=off[:, 0:32],
                  in1=cnt[:, 0:32], op=ALU.add)
                V('tensor_tensor', reads=['pre_all', 'base_all'], writes=['pre_all'], out=pre, in0=pre, in1=base,
                  op=ALU.add)
                V('tensor_tensor', reads=['pre_all', 'moff'], writes=['pre_all'], out=pre, in0=pre,
                  in1=off.unsqueeze(1).broadcast_to([128, NT, 64]), op=ALU.add)
                V('tensor_tensor', reads=['pre_all', 'ohb'], writes=['pre_all'], out=pre, in0=pre, in1=self.ohb,
                  op=ALU.mult)
                V('tensor_reduce', reads=['pre_all'], writes=['dest_f'], out=dest_f,
                  in_=pre.rearrange("p i (k e) -> p i k e", k=2), axis=AX.X, op=ALU.add)
                V('tensor_copy', reads=['dest_f'], writes=['dest_i'], out=dest_i, in_=dest_f)
                V('tensor_tensor', reads=['pend', 'blk0'], writes=['mcmp'], out=cmp_,
                  in0=pend.unsqueeze(1).broadcast_to([128, NB, 32]), in1=blk0.unsqueeze(2).broadcast_to([128, NB, 32]),
                  op=ALU.is_le)
                V('tensor_reduce', reads=['mcmp'], writes=['mbe'], out=be, in_=cmp_, axis=AX.X, op=ALU.add)
                V('tensor_scalar', reads=['mbe'], writes=['mbe'], out=be, in0=be, scalar1=256.0, scalar2=None,
                  op0=ALU.mult)
                P.op('gpsimd', 'iota', writes=['pidx'], out=pidx, pattern=[[1, 2]], base=l * NE * 256, channel_multiplier=2,
                     allow_small_or_imprecise_dtypes=True)
                V('tensor_tensor', reads=['mbe', 'pidx'], writes=['widx_f'], out=wf,
                  in0=be.unsqueeze(2).broadcast_to([128, NB, 2]), in1=pidx.unsqueeze(1).broadcast_to([128, NB, 2]),
                  op=ALU.add)
                V('tensor_copy', reads=['widx_f'], writes=['widx'], out=widx, in_=wf)
                if 'rdbg' in self.dbg:
                    for i in range(NT):
                        V('tensor_copy', reads=['ohb'], writes=['rdbg_t'], out=dbg_t[:, 0:64], in_=self.ohb[:, i, :])
                        V('tensor_copy', reads=['rw'], writes=['rdbg_t'], out=dbg_t[:, 64:66], in_=self.rw[:, i, :])
                        V('tensor_copy', reads=['dest_f'], writes=['rdbg_t'], out=dbg_t[:, 66:68], in_=dest_f[:, i, :])
                        P.dma('sync', self.rdbg[i * 128:(i + 1) * 128, :], dbg_t, reads=['rdbg_t'])
                P.end()
            if '1' in self.opt:
                return
            with contextlib.ExitStack() as s2:
                hrow = [self.sb(s2, f"hrow{i}", [128, D], BF16) for i in range(3)]
                P.begin()
                P.raw('gpsimd', 'reg_mov', self.reg_rows, NR - 1)
                for i in range(NT):
                    Hr = hrow[i % 3]
                    rr = f'hrow{i % 3}'
                    P.dma('sync', Hr, self.hfp[i * 128:(i + 1) * 128, :], writes=[rr])
                    for k in range(2):
                        P.dma('gpsimd', None, None, reads=[rr, 'dest_i'], writes=['xs'],
                              fn=('indirect_dma_start', dict(
                                  out=self.xs, out_offset=bass.IndirectOffsetOnAxis(ap=dest_i[:, i, k:k + 1], axis=0),
                                  in_=Hr, in_offset=None, bounds_check=self.reg_rows, oob_is_err=False)))
                P.end()
            if '2' in self.opt:
                return
            with contextlib.ExitStack() as s3:
                sb3 = lambda n, s, dt=F32: self.sb(s3, n, s, dt)
                Wg = [sb3(f"Wg{i}", [128, 8, 512], BF16) for i in range(2)]
                Wu = [sb3(f"Wu{i}", [128, 8, 512], BF16) for i in range(2)]
                Wd = [sb3(f"Wd{i}", [128, 4, 1024], BF16) for i in range(2)]
                xsT = [sb3(f"xsT{i}", [128, 8, 512], BF16) for i in range(2)]
                hid = [sb3(f"hid{i}", [128, 4, 512], BF16) for i in range(2)]
                sg = [sb3(f"sg{i}", [128, 512]) for i in range(2)]
                yb = [sb3(f"yb{i}", [128, D]) for i in range(2)]
                P.begin()
                P.raw('gpsimd', 'reg_mov', self.reg_w, (l + 1) * NE * 256 - 1)
                n_y = [0]

                def load_blk(b):
                    a = b % 2
                    for (Wt, view, nm) in ((Wg[a], wgv, f'Wg{a}'), (Wu[a], wuv, f'Wu{a}'), (Wd[a], wdv, f'Wd{a}')):
                        flat = Wt.rearrange("p a f -> p (a f)")
                        for hf in range(0 if 'G' in self.opt else 2):
                            P.dma('gpsimd', None, None, reads=['widx'], writes=[nm],
                                  fn=('indirect_dma_start', dict(
                                      out=flat[:, hf * 2048:(hf + 1) * 2048], out_offset=None, in_=view,
                                      in_offset=bass.IndirectOffsetOnAxis(ap=widx[:, b, hf:hf + 1], axis=0),
                                      bounds_check=self.reg_w, oob_is_err=False)))
                    X = xsT[a]
                    for kc in range(8):
                        P.dma('sync', None, None, reads=['xs'], writes=[f'xsT{a}'],
                              fn=('dma_start_transpose', dict(
                                  out=X[:, kc, :], in_=self.xs[b * BLK:(b + 1) * BLK, kc * 128:(kc + 1) * 128])))

                def compute_blk(b):
                    a = b % 2
                    X = xsT[a]
                    rxs = f'xsT{a}'
                    if 'C' in self.opt:
                        return
                    Hd = hid[a]
                    rh = f'hid{a}'
                    wg4 = Wg[a].rearrange("p kc (m four) -> p kc four m", four=4)
                    wu4 = Wu[a].rearrange("p kc (m four) -> p kc four m", four=4)
                    for fc in range(4):
                        pg_, pu_ = 2 + (fc % 2) * 2, 3 + (fc % 2) * 2
                        for kc in range(8):
                            P.op('tensor', 'matmul', reads=[f'Wg{a}', rxs], writes=[f'ps{pg_}'], out=ps[pg_],
                                 lhsT=wg4[:, kc, fc, :], rhs=X[:, kc, :], start=(kc == 0), stop=(kc == 7))
                        for kc in range(8):
                            P.op('tensor', 'matmul', reads=[f'Wu{a}', rxs], writes=[f'ps{pu_}'], out=ps[pu_],
                                 lhsT=wu4[:, kc, fc, :], rhs=X[:, kc, :], start=(kc == 0), stop=(kc == 7))
                        S = sg[fc % 2]
                        P.op('scalar', 'activation', reads=[f'ps{pg_}'], writes=[f'sg{fc % 2}'], out=S, in_=ps[pg_],
                             func=AF.Silu)
                        P.op('vector', 'tensor_tensor', reads=[f'sg{fc % 2}', f'ps{pu_}'], writes=[rh], out=Hd[:, fc, :],
                             in0=S, in1=ps[pu_], op=ALU.mult)
                    for rt in range(4):
                        n_y[0] += 1
                        Y = yb[n_y[0] % 2]
                        ry = f'yb{n_y[0] % 2}'
                        for half in range(2):
                            pb = 6 + half
                            for fc in range(4):
                                P.op('tensor', 'matmul', reads=[rh, f'Wd{a}'], writes=[f'ps{pb}'], out=ps[pb],
                                     lhsT=Hd[:, fc, rt * 128:(rt + 1) * 128], rhs=Wd[a][:, fc, half * 512:(half + 1) * 512],
                                     start=(fc == 0), stop=(fc == 3))
                            self.evac(half, Y[:, half * 512:(half + 1) * 512], ps[pb], [f'ps{pb}'], [ry])
                        r0 = b * BLK + rt * 128
                        P.dma('sync', self.ys[r0:r0 + 128, :], Y, reads=[ry], writes=['ys'])

                load_blk(0)
                for b in range(NB):
                    if b + 1 < NB:
                        load_blk(b + 1)
                    compute_blk(b)
                P.end()
            if '3' in self.opt:
                return
            with contextlib.ExitStack() as s4:
                sb4 = lambda n, s, dt=F32: self.sb(s4, n, s, dt)
                y1 = [sb4(f"y1_{i}", [128, D]) for i in range(2)]
                y2 = [sb4(f"y2_{i}", [128, D]) for i in range(2)]
                xr = [sb4(f"xr{i}", [128, D]) for i in range(2)]
                fgb = sb4("fgb", [128, D])
                fsq = sb4("fsq", [128, D])
                fss = sb4("fss", [128, 1])
                frs = sb4("frs", [128, 1])
                P.begin()
                P.raw('gpsimd', 'reg_mov', self.reg_rows, NR - 1)
                if last:
                    P.dma('sync', fgb, self.final_g.partition_broadcast(128), writes=['fgb'])
                def load_tile(i):
                    a = i % 2
                    rows = slice(i * 128, (i + 1) * 128)
                    P.dma('sync', xr[a], self.xres[rows, :], writes=[f'xr{a}'])
                    for (Yk, rk, k) in ((y1[a], f'y1_{a}', 0), (y2[a], f'y2_{a}', 1)):
                        P.dma('gpsimd', None, None, reads=['dest_i'], writes=[rk],
                              fn=('indirect_dma_start', dict(
                                  out=Yk, out_offset=None, in_=self.ys,
                                  in_offset=bass.IndirectOffsetOnAxis(ap=dest_i[:, i, k:k + 1], axis=0),
                                  bounds_check=self.reg_rows, oob_is_err=False)))
                load_tile(0)
                for i in range(NT):
                    a = i % 2
                    Y1, Y2, X = y1[a], y2[a], xr[a]
                    r1, r2, rx = f'y1_{a}', f'y2_{a}', f'xr{a}'
                    rows = slice(i * 128, (i + 1) * 128)
                    if i + 1 < NT:
                        load_tile(i + 1)
                    P.op('vector', 'tensor_scalar', reads=[r1, 'rw'], writes=[r1], out=Y1, in0=Y1,
                         scalar1=self.rw[:, i, 0:1], scalar2=None, op0=ALU.mult)
                    P.op('vector', 'scalar_tensor_tensor', reads=[r1, r2, 'rw'], writes=[r1], out=Y1, in0=Y2,
                         scalar=self.rw[:, i, 1:2], in1=Y1, op0=ALU.mult, op1=ALU.add)
                    P.op('vector', 'tensor_tensor', reads=[r1, 'modb'], writes=[r1], out=Y1, in0=Y1, in1=self.mod(5),
                         op=ALU.mult)
                    P.op('vector', 'tensor_tensor', reads=[r1, rx], writes=[rx], out=X, in0=X, in1=Y1, op=ALU.add)
                    if not last:
                        P.dma('sync', self.xres[rows, :], X, reads=[rx])
                    else:
                        P.op('scalar', 'activation', reads=[rx], writes=['fsq', 'fss'], out=fsq, in_=X, func=AF.Square,
                             accum_out=fss)
                        self.rstd_ops(fss, frs, D, ['fss'], ['frs'])
                        P.op('vector', 'scalar_tensor_tensor', reads=[rx, 'frs', 'fgb'], writes=[r2], out=Y2, in0=X,
                             scalar=frs, in1=fgb, op0=ALU.mult, op1=ALU.mult)
                        P.dma('sync', self.out[rows, :], Y2, reads=[r2])
                P.end()

    def build_layer(self, l):
        with contextlib.ExitStack() as st:
            ph = self.phases
            self.phase_ada(l, st)
            if 'i' in ph:
                self.phase_inproj(l, st)
            if 'c' in ph and 'f' in ph:
                self.phase_attn(l, conv_inside=True)
            elif 'c' in ph:
                self.phase_conv(l)
            elif 'f' in ph:
                self.phase_attn(l)
            if 's' in ph:
                self.phase_ssd(l)
            if 'o' in ph:
                self.phase_wout_router(l, st)
            if 'm' in ph and self.moe:
                self.phase_moe(l, st)

    def zero_xs(self):
        P = self.P
        NR = self.NB * self.BLK
        with contextlib.ExitStack() as st:
            z = self.sb(st, "zeros", [128, 8192], BF16)
            P.begin()
            P.op('gpsimd', 'memset', writes=['zeros'], ap=z, constant=0.0)
            xv = self.xs.rearrange("(a p r) d -> a p (r d)", p=128, r=8)
            for a in range(NR // 1024):
                P.dma('sync', xv[a], z, reads=['zeros'])
            P.end()

    def build(self):
        if self.moe:
            self.zero_xs()
        for l in range(self.L):
            self.build_layer(l)
        return self.nc


def prep_core_inputs(inp, b, T, L):
    f = lambda a: np.ascontiguousarray(a, dtype=np.float32)
    m = {}
    m["x"] = f(inp["x"][b, :T])
    m["c"] = f(inp["c"][b].reshape(8, 128).T)
    for k in ("ada_w", "ada_b", "norm_mix_g", "w_in", "ssd_dt_bias", "ssd_a_log", "ssd_d", "ssd_norm_g",
              "w_out", "norm_ffn_g", "w_router_group", "b_router_group", "w_router_expert", "b_router_expert",
              "w_gate", "w_up", "w_down"):
        m[k] = f(inp[k][:L])
    m["final_norm_g"] = f(inp["final_norm_g"])
    m["convw_fm"] = f(inp["ssd_conv_w"][:L].reshape(L, 4, 8, 128).transpose(0, 3, 2, 1))
    m["convb_fm"] = f(inp["ssd_conv_b"][:L].reshape(L, 8, 128).transpose(0, 2, 1))
    m["fox_f_bias_fm"] = f(inp["fox_f_bias"][:L].reshape(L, 4, 1))
    m["fox_g_fm"] = f(inp["fox_norm_g"][:L].reshape(L, 2, 128).transpose(0, 2, 1))
    m["cmw_fm"] = f(inp["cm_conv_w"][:L].reshape(L, 31, 2, 128).transpose(0, 3, 2, 1))
    m["cmb_fm"] = f(inp["cm_conv_b"][:L].reshape(L, 2, 128).transpose(0, 2, 1))
    m["cmg_fm"] = f(inp["cm_ln_g"][:L].reshape(L, 2, 128).transpose(0, 2, 1))
    m["cmbeta_fm"] = f(inp["cm_ln_b"][:L].reshape(L, 2, 128).transpose(0, 2, 1))
    return m


_CACHE = {}
T_FULL, L_FULL, B_FULL = 8192, 4, 4


def kernel(**inputs):
    if 'nc' not in _CACHE:
        bd = Builder(T_FULL, L_FULL)
        _CACHE['nc'] = bd.build()
        _CACHE['names'] = set(bd.ins)
    nc = _CACHE['nc']
    inp = {k: np.asarray(v) for k, v in inputs.items()}
    maps = []
    for b in range(B_FULL):
        m = prep_core_inputs(inp, b, T_FULL, L_FULL)
        maps.append({k: v for k, v in m.items() if k in _CACHE['names']})
    res = run_bass_kernel_spmd(nc, maps, core_ids=list(range(B_FULL)))
    out = np.stack([np.asarray(r["out"], dtype=np.float32) for r in res.results], axis=0)
    return out
```

```python
import contextlib
import numpy as np
import concourse.bass as bass
import concourse.mybir as mybir
from concourse.bass_utils import run_bass_kernel_spmd

F32 = mybir.dt.float32
BF16 = mybir.dt.bfloat16
I32 = mybir.dt.int32
AF = mybir.ActivationFunctionType
ALU = mybir.AluOpType
AX = mybir.AxisListType

ENGS = ('tensor', 'vector', 'scalar', 'gpsimd', 'sync')
CENGS = ('tensor', 'vector', 'scalar', 'gpsimd')
NDS = 10

D = 1024
D_IN = 2828
EPS = 1e-6
NE = 32
DE = 512


class Prog:
    def __init__(self, nc, same_engine_sync=True):
        self.nc = nc
        self.same = same_engine_sync
        self.esem = {e: nc.alloc_semaphore(name=f"es_{e}") for e in CENGS}
        self.dsem = {e: [nc.alloc_semaphore(name=f"ds_{e}_{i}") for i in range(NDS)]
                     for e in ('sync', 'scalar', 'gpsimd')}
        self._reset()
        self.q = None

    def _reset(self):
        keep = getattr(self, 'dcnt', {}).get('gpsimd', [0] * NDS)
        self.ecnt = {e: 0 for e in CENGS}
        self.dcnt = {e: [0] * NDS for e in self.dsem}
        self.dcnt['gpsimd'] = list(keep)
        self.dnext = {e: 0 for e in self.dsem}
        self.known = {e: {('d', 'gpsimd', i): keep[i] for i in range(NDS)} for e in ENGS}
        self.res_w = {}
        self.res_r = {}

    def semof(self, key):
        if key[0] == 'e':
            return self.esem[key[1]]
        return self.dsem[key[1]][key[2]]

    def begin(self):
        self.q = {e: [] for e in ENGS}

    def _deps(self, reads, writes):
        deps = []
        for r in reads:
            t = self.res_w.get(r)
            if t is not None:
                deps.append(t)
        for w in writes:
            t = self.res_w.get(w)
            if t is not None:
                deps.append(t)
            deps.extend(self.res_r.get(w, {}).items())
        return deps

    def _record(self, tok, reads, writes):
        for r in reads:
            d = self.res_r.setdefault(r, {})
            if d.get(tok[0], 0) < tok[1]:
                d[tok[0]] = tok[1]
        for w in writes:
            self.res_w[w] = tok
            self.res_r[w] = {}

    def _waits(self, eng, deps):
        waits = []
        best = {}
        for (key, c) in deps:
            if best.get(key, 0) < c:
                best[key] = c
        for key, c in best.items():
            if key == ('e', 'tensor') and eng == 'tensor':
                continue
            if (not self.same) and key == ('e', eng):
                continue
            if self.known[eng].get(key, 0) >= c:
                continue
            self.known[eng][key] = c
            waits.append((self.semof(key), c))
        return waits

    def op(self, eng, name, reads=(), writes=(), **kw):
        writes = list(writes) + [r for r in reads if r.startswith('ps') and r not in writes]
        waits = self._waits(eng, self._deps(reads, writes))
        self.ecnt[eng] += 1
        tok = (('e', eng), self.ecnt[eng])
        sem = self.esem[eng]

        def emit(e, name=name, kw=kw, waits=waits, sem=sem):
            for (s, c) in waits:
                e.wait_ge(s, c)
            getattr(e, name)(**kw).then_inc(sem, 1)
        self.q[eng].append(emit)
        self._record(tok, reads, writes)
        return tok

    def raw(self, eng, name, *args, **kw):
        self.q[eng].append(lambda e: getattr(e, name)(*args, **kw))

    def dma(self, eng, out, in_, reads=(), writes=(), fn=None, **kw):
        deps = self._deps(reads, writes)
        idx = self.dnext[eng]
        self.dnext[eng] = (idx + 1) % NDS
        key = ('d', eng, idx)
        prev = self.dcnt[eng][idx]
        if prev:
            deps.append((key, prev))
        waits = self._waits(eng, deps)
        self.dcnt[eng][idx] += 16
        tok = (key, self.dcnt[eng][idx])
        sem = self.dsem[eng][idx]

        def emit(e, waits=waits, sem=sem):
            for (s, c) in waits:
                e.wait_ge(s, c)
            if fn is not None:
                try:
                    ins = getattr(e, fn[0])(**fn[1])
                except Exception:
                    print("DMA builder failed:", fn[0], {k: (v.shape if hasattr(v, 'shape') else v) for k, v in fn[1].items()})
                    raise
                ins.then_inc(sem, 16)
            else:
                e.dma_start(out=out, in_=in_, **kw).then_inc(sem, 16)
        self.q[eng].append(emit)
        self._record(tok, reads, writes)
        return tok

    def end(self):
        nc = self.nc
        fin = []
        for e in self.dsem:
            for i in range(NDS):
                c = self.dcnt[e][i]
                if c and self.known['sync'].get(('d', e, i), 0) < c:
                    fin.append((self.dsem[e][i], c))

        def drain(e, fin=fin):
            for (s, c) in fin:
                e.wait_ge(s, c)
        self.q['sync'].append(drain)
        with nc.Block() as block:
            for en in ENGS:
                ops = self.q[en]
                if not ops:
                    continue

                def body(e, ops=ops):
                    for o in ops:
                        o(e)
                getattr(block, en)(body)
        allsems = list(self.esem.values()) + [s for e in self.dsem if e != 'gpsimd' for s in self.dsem[e]]
        with nc.Block() as block:
            def clr(e):
                for s in allsems:
                    e.sem_clear(s)
            block.sync(clr)
        self._reset()
        self.q = None


class Builder:
    def __init__(self, T, L, dbg=(), moe=True, phases='aicfsom', opt=''):
        self.opt = opt
        self.T, self.L = T, L
        self.moe = moe
        self.phases = phases
        self.NT = T // 128
        self.NQ = T // 512
        self.dbg = set(dbg)
        nc = self.nc = bass.Bass("TRN2", target_bir_lowering=False)
        self.P = Prog(nc, same_engine_sync=('S' not in self.opt))
        self.ins = {}
        self.outs = {}
        self._uid = 0
        self.declare_io()
        self.consts()

    def din(self, name, shape, dt=F32):
        t = self.nc.dram_tensor(name, list(shape), dt, kind="ExternalInput").ap()
        self.ins[name] = t
        return t

    def dscr(self, name, shape, dt=F32):
        kind = "ExternalOutput" if name in self.dbg else "Internal"
        t = self.nc.dram_tensor(name, list(shape), dt, kind=kind).ap()
        if kind == "ExternalOutput":
            self.outs[name] = t
        return t

    def sb(self, st, name, shape, dt=F32):
        self._uid += 1
        return st.enter_context(self.nc.sbuf_tensor(f"{name}_{self._uid}", list(shape), dt)).ap()

    def declare_io(self):
        T, L = self.T, self.L
        d = self.din
        self.x_in = d("x", [T, D])
        self.c_in = d("c", [128, 8])
        self.ada_w = d("ada_w", [L, D, 6 * D])
        self.ada_b = d("ada_b", [L, 6 * D])
        self.norm_mix_g = d("norm_mix_g", [L, D])
        self.w_in = d("w_in", [L, D, D_IN])
        self.convw = d("convw_fm", [L, 128, 8, 4])
        self.convb = d("convb_fm", [L, 128, 8])
        self.dt_bias = d("ssd_dt_bias", [L, 8])
        self.a_log = d("ssd_a_log", [L, 8])
        self.ssd_d = d("ssd_d", [L, 8])
        self.ssd_norm_g = d("ssd_norm_g", [L, 512])
        self.f_bias = d("fox_f_bias_fm", [L, 4, 1])
        self.fox_g = d("fox_g_fm", [L, 128, 2])
        self.cmw = d("cmw_fm", [L, 128, 2, 31])
        self.cmb = d("cmb_fm", [L, 128, 2])
        self.cmg = d("cmg_fm", [L, 128, 2])
        self.cmbeta = d("cmbeta_fm", [L, 128, 2])
        self.w_out = d("w_out", [L, D, D])
        self.norm_ffn_g = d("norm_ffn_g", [L, D])
        self.w_rg = d("w_router_group", [L, D, 4])
        self.b_rg = d("b_router_group", [L, 4])
        self.w_re = d("w_router_expert", [L, D, NE])
        self.b_re = d("b_router_expert", [L, NE])
        if self.moe:
            self.w_gate = d("w_gate", [L, NE, D, DE])
            self.w_up = d("w_up", [L, NE, D, DE])
            self.w_down = d("w_down", [L, NE, DE, D])
        self.final_g = d("final_norm_g", [D])
        self.out = self.nc.dram_tensor("out", [T, D], F32, kind="ExternalOutput").ap()
        self.outs["out"] = self.out
        s = self.dscr
        self.xres = s("xres", [T, D])
        self.zs = s("zs", [T, 512])
        self.xbcT = s("xbcT", [1024, T])
        self.dtr = s("dtr", [T, 8])
        self.flT = s("flT", [4, T])
        self.qT = s("qT", [256, T], BF16)
        self.kT = s("kT", [256, T], BF16)
        self.vtok = s("vtok", [T, 256], BF16)
        self.gaT = s("gaT", [256, T])
        self.gbT = s("gbT", [256, T])
        self.yT = s("yT", [1024, T], BF16)
        self.attT = s("attT", [256, T])
        self.cs = s("cs", [4, 6, T], BF16)
        self.hfp = s("hfp", [T, D], BF16)
        self.BLK = 512
        self.NB = (2 * T) // self.BLK + NE
        self.xs = s("xs", [self.NB * self.BLK, D], BF16)
        self.ys = s("ys", [self.NB * self.BLK, D])
        self.rdbg = s("rdbg", [T, 68])

    def consts(self):
        nc, P = self.nc, self.P
        a = lambda n, s, dt=F32: nc.alloc_sbuf_tensor(n, list(s), dt).ap()
        self.ident = a("ident", [128, 128])
        self.identb = a("identb", [128, 128], BF16)
        self.tri = a("tri", [128, 128])
        self.ustr = a("ustr", [128, 128])
        self.ones = a("ones", [128, 128])
        self.onesb = a("onesb", [128, 128], BF16)
        self.epsb = a("epsb", [128, 1])
        self.sutb = a("sutb", [128, 128], BF16)
        self.ps = [nc.alloc_psum_tensor(f"ps{i}", [128, 512], F32).ap() for i in range(8)]
        self.reg_rows = nc.gpsimd.alloc_register("bc_rows")
        self.reg_w = nc.gpsimd.alloc_register("bc_w")
        P.begin()
        g = 'gpsimd'
        P.op(g, 'memset', writes=['epsb'], ap=self.epsb, constant=EPS)
        P.op(g, 'memset', writes=['ones'], ap=self.ones, constant=1.0)
        P.op(g, 'memset', writes=['onesb'], ap=self.onesb, constant=1.0)
        P.op(g, 'memset', writes=['ident'], ap=self.ident, constant=1.0)
        P.op(g, 'affine_select', reads=['ident'], writes=['ident'], out=self.ident, in_=self.ident,
             pattern=[[-1, 128]], compare_op=ALU.is_equal, fill=0.0, base=0, channel_multiplier=1)
        P.op(g, 'tensor_copy', reads=['ident'], writes=['identb'], out=self.identb, in_=self.ident)
        P.op(g, 'memset', writes=['tri'], ap=self.tri, constant=1.0)
        P.op(g, 'affine_select', reads=['tri'], writes=['tri'], out=self.tri, in_=self.tri,
             pattern=[[1, 128]], compare_op=ALU.is_ge, fill=0.0, base=0, channel_multiplier=-1)
        P.op(g, 'tensor_tensor', reads=['tri', 'ident'], writes=['sutb'], out=self.sutb, in0=self.tri, in1=self.ident,
             op=ALU.subtract)
        P.op(g, 'memset', writes=['ustr'], ap=self.ustr, constant=1.0)
        P.op(g, 'affine_select', reads=['ustr'], writes=['ustr'], out=self.ustr, in_=self.ustr,
             pattern=[[-1, 128]], compare_op=ALU.is_gt, fill=0.0, base=0, channel_multiplier=1)
        P.end()

    def phase_ada(self, l, st):
        nc, P = self.nc, self.P
        self.modb = self.sb(st, "modb", [128, 6 * D])
        with contextlib.ExitStack() as s2:
            cs = self.sb(s2, "c_s", [128, 8])
            cb = self.sb(s2, "c_b", [128, 8, 128])
            adab = self.sb(s2, "adab", [128, 6 * D])
            wch = [self.sb(s2, f"adaw{i}", [128, 8, 512]) for i in range(2)]
            P.begin()
            P.dma('sync', cs, self.c_in, writes=['c_s'])
            P.dma('sync', adab, self.ada_b[l].partition_broadcast(128), writes=['adab'])
            P.op('scalar', 'activation', reads=['c_s'], writes=['c_s'], out=cs, in_=cs, func=AF.Silu)
            P.op('vector', 'tensor_copy', reads=['c_s'], writes=['c_b'], out=cb,
                 in_=cs.unsqueeze(2).broadcast_to([128, 8, 128]))
            for j in range(12):
                w = wch[j % 2]
                wr = f'adaw{j % 2}'
                P.dma('sync', w,
                      self.ada_w[l][:, j * 512:(j + 1) * 512].rearrange("(kc p) n -> p kc n", p=128),
                      writes=[wr])
                pt = self.ps[j % 2]
                for kc in range(8):
                    P.op('tensor', 'matmul', reads=['c_b', wr], writes=[f'ps{j % 2}'], out=pt,
                         lhsT=cb[:, kc, :], rhs=w[:, kc, :], start=(kc == 0), stop=(kc == 7))
                P.op('vector', 'tensor_tensor', reads=[f'ps{j % 2}', 'adab'], writes=['modb'],
                     out=self.modb[:, j * 512:(j + 1) * 512], in0=pt, in1=adab[:, j * 512:(j + 1) * 512], op=ALU.add)
            P.end()

    def mod(self, i):
        return self.modb[:, i * D:(i + 1) * D]

    def load_cast(self, st, dst, src, res, width):
        P = self.P
        if getattr(self, '_stg_owner', None) is not st:
            self._stg = [self.sb(st, f"stg{i}", [128, 2048]) for i in range(2)]
            self._stg_owner = st
            self._stg_n = 0
        for c0 in range(0, width, 2048):
            n = min(2048, width - c0)
            k = self._stg_n
            self._stg_n += 1
            S = self._stg[k % 2]
            rs = f'stg{k % 2}'
            P.dma('sync', S[:, 0:n], src[:, c0:c0 + n], writes=[rs])
            P.op('gpsimd' if k % 2 else 'vector', 'tensor_copy', reads=[rs], writes=[res], out=dst[:, c0:c0 + n],
                 in_=S[:, 0:n])

    def rstd_ops(self, ss, rstd, n, rd, wr):
        P = self.P
        P.op('scalar', 'activation', reads=rd, writes=wr, out=rstd, in_=ss, func=AF.Ln,
             bias=self.epsb[:ss.shape[0], :], scale=1.0 / n)
        P.op('scalar', 'activation', reads=wr, writes=wr, out=rstd, in_=rstd, func=AF.Exp, scale=-0.5)

    def phase_inproj(self, l, st0):
        nc, P, T = self.nc, self.P, self.T
        src = self.x_in if l == 0 else self.xres
        self._ip_bufs = None
        with contextlib.ExitStack() as st:
            sb = lambda n, s, dt=F32: self.sb(st, n, s, dt)
            wz = sb("wz", [128, 8, 512], BF16)
            wx = sb("wx", [128, 8, 1024], BF16)
            wqk = sb("wqk", [128, 8, 512], BF16)
            wv = sb("wv", [128, 8, 256], BF16)
            wg = sb("wg", [128, 8, 512], BF16)
            wdt = sb("wdt", [128, 8, 8])
            wf = sb("wf", [128, 8, 4])
            gsc = sb("gsc", [128, D])
            W = self.w_in[l].rearrange("(kc p) n -> p kc n", p=128)
            P.begin()
            for kc in range(8):
                self.load_cast(st, wz[:, kc, :], W[:, kc, 0:512], 'wz', 512)
                self.load_cast(st, wx[:, kc, :], W[:, kc, 512:1536], 'wx', 1024)
                self.load_cast(st, wqk[:, kc, :], W[:, kc, 1544:2056], 'wqk', 512)
                self.load_cast(st, wv[:, kc, :], W[:, kc, 2056:2312], 'wv', 256)
                self.load_cast(st, wg[:, kc, :], W[:, kc, 2316:2828], 'wg', 512)
            P.dma('sync', wdt, W[:, :, 1536:1544], writes=['wdt'])
            P.dma('sync', wf, W[:, :, 2312:2316], writes=['wf'])
            P.dma('sync', gsc, self.norm_mix_g[l].partition_broadcast(128), writes=['gsc'])
            P.op('vector', 'scalar_tensor_tensor', reads=['gsc', 'modb'], writes=['gsc'], out=gsc,
                 in0=self.mod(1), scalar=1.0, in1=gsc, op0=ALU.add, op1=ALU.mult)
            self.norm_and_transpose_loop(st, src, gsc, self.mod(0), consumer=lambda q, hT, hT32: self.inproj_chunk(
                q, hT, hT32, wz, wx, wqk, wv, wg, wdt, wf, st))
            P.end()

    def norm_and_transpose_loop(self, st, src, gsc, shift, consumer, pre=None, after_h=None, pre_load=None):
        P = self.P
        sb = lambda n, s, dt=F32: self.sb(st, n, s, dt)
        NXB = 4
        xt = [sb(f"xt{i}", [128, D]) for i in range(NXB)]
        ht = [sb(f"ht{i}", [128, D]) for i in range(2)]
        sq = sb("sq", [128, D])
        ss = [sb(f"ss{i}", [128, 1]) for i in range(2)]
        rs = [sb(f"rs{i}", [128, 1]) for i in range(2)]
        hT32 = [sb(f"hT32_{i}", [128, 8, 512]) for i in range(2)]
        hT = [sb(f"hT_{i}", [128, 8, 512], BF16) for i in range(2)]
        PF = 2

        def load(i):
            if i >= self.NT:
                return
            if pre_load is not None:
                pre_load(i)
            elif pre is None:
                P.dma('sync', xt[i % NXB], src[i * 128:(i + 1) * 128, :], writes=[f'xt{i % NXB}'])
        for i in range(PF):
            load(i)
        for q in range(self.NQ):
            b = q % 2
            for j in range(4):
                i = q * 4 + j
                a = i % 2
                X, H = xt[i % NXB], ht[a]
                rx, rh = f'xt{i % NXB}', f'ht{a}'
                load(i + PF)
                if pre is not None:
                    pre(i, X, rx)
                P.op('scalar', 'activation', reads=[rx], writes=['sq', f'ss{a}'], out=sq, in_=X, func=AF.Square,
                     accum_out=ss[a])
                self.rstd_ops(ss[a], rs[a], D, [f'ss{a}'], [f'rs{a}'])
                P.op('vector', 'scalar_tensor_tensor', reads=[rx, f'rs{a}', 'gsc'], writes=[rh], out=H, in0=X,
                     scalar=rs[a], in1=gsc, op0=ALU.mult, op1=ALU.mult)
                P.op('vector', 'tensor_tensor', reads=[rh, 'modb'], writes=[rh], out=H, in0=H, in1=shift, op=ALU.add)
                if after_h is not None:
                    after_h(i, H, rh, X, rx)
                for half in range(2):
                    pt = self.ps[half]
                    for k4 in range(4):
                        kc = half * 4 + k4
                        P.op('tensor', 'transpose', reads=[rh, 'ident'], writes=[f'ps{half}'],
                             out=pt[:, k4 * 128:(k4 + 1) * 128], in_=H[:, kc * 128:(kc + 1) * 128], identity=self.ident)
                    dst = hT32[b][:, half * 4:(half + 1) * 4, j * 128:(j + 1) * 128]
                    P.op('scalar', 'activation', reads=[f'ps{half}'], writes=[f'hT32_{b}'], out=dst,
                         in_=pt.rearrange("p (k t) -> p k t", k=4), func=AF.Copy)
                P.op('vector', 'tensor_copy', reads=[f'hT32_{b}'], writes=[f'hT_{b}'],
                     out=hT[b][:, :, j * 128:(j + 1) * 128], in_=hT32[b][:, :, j * 128:(j + 1) * 128])
            consumer(q, (hT[b], f'hT_{b}'), (hT32[b], f'hT32_{b}'))

    def evac(self, k, out, in_, reads, writes, scale=None):
        P = self.P
        if k % 2 == 0:
            if scale is None:
                P.op('scalar', 'activation', reads=reads, writes=writes, out=out, in_=in_, func=AF.Copy)
            else:
                P.op('scalar', 'activation', reads=reads, writes=writes, out=out, in_=in_, func=AF.Copy, scale=scale)
        else:
            if scale is None:
                P.op('vector', 'tensor_copy', reads=reads, writes=writes, out=out, in_=in_)
            else:
                P.op('vector', 'tensor_scalar', reads=reads, writes=writes, out=out, in0=in_, scalar1=scale,
                     scalar2=None, op0=ALU.mult)

    def inproj_chunk(self, q, hTb, hT32b, wz, wx, wqk, wv, wg, wdt, wf, st):
        P = self.P
        hT, rhT = hTb
        hT32, rhT32 = hT32b
        if self._ip_bufs is None:
            sb = lambda n, s, dt=F32: self.sb(st, n, s, dt)
            self._ip_bufs = dict(
                o32=[sb(f"o32_{i}", [128, 512]) for i in range(3)],
                o16=[sb(f"o16_{i}", [128, 512], BF16) for i in range(3)],
                osm=[sb(f"osm_{i}", [128, 8]) for i in range(2)],
                ofl=[sb(f"ofl_{i}", [4, 512]) for i in range(2)],
                n=[0],
            )
        B = self._ip_bufs
        tok = slice(q * 512, (q + 1) * 512)

        def nxt():
            B['n'][0] += 1
            return B['n'][0]
        PB = [2, 3, 4, 5]

        def fm(w, wres, c0, dst, dt16=False, scale=None):
            k = nxt()
            pb = PB[k % 4]
            pt = self.ps[pb]
            for kc in range(8):
                P.op('tensor', 'matmul', reads=[wres, rhT], writes=[f'ps{pb}'], out=pt, lhsT=w[:, kc, c0:c0 + 128],
                     rhs=hT[:, kc, :], start=(kc == 0), stop=(kc == 7))
            o = (B['o16'] if dt16 else B['o32'])[k % 3]
            ores = ('o16_' if dt16 else 'o32_') + str(k % 3)
            self.evac(k, o, pt, [f'ps{pb}'], [ores], scale=scale)
            P.dma('sync', dst, o, reads=[ores], writes=[])
        for ct in range(8):
            fm(wx, 'wx', ct * 128, self.xbcT[ct * 128:(ct + 1) * 128, tok])
        for ct in range(2):
            fm(wqk, 'wqk', ct * 128, self.qT[ct * 128:(ct + 1) * 128, tok], dt16=True, scale=0.125)
        for ct in range(2):
            fm(wqk, 'wqk', 256 + ct * 128, self.kT[ct * 128:(ct + 1) * 128, tok], dt16=True)
        for ct in range(2):
            fm(wg, 'wg', ct * 128, self.gaT[ct * 128:(ct + 1) * 128, tok])
        for ct in range(2):
            fm(wg, 'wg', 256 + ct * 128, self.gbT[ct * 128:(ct + 1) * 128, tok])
        k = nxt()
        pb = PB[k % 4]
        pt = self.ps[pb]
        for kc in range(8):
            P.op('tensor', 'matmul', reads=['wf', rhT32], writes=[f'ps{pb}'], out=pt[0:4, :], lhsT=wf[:, kc, :],
                 rhs=hT32[:, kc, :], start=(kc == 0), stop=(kc == 7))
        o = B['ofl'][q % 2]
        P.op('vector', 'tensor_copy', reads=[f'ps{pb}'], writes=[f'ofl_{q % 2}'], out=o, in_=pt[0:4, :])
        P.dma('sync', self.flT[:, tok], o, reads=[f'ofl_{q % 2}'])
        for j in range(4):
            tt = slice(q * 512 + j * 128, q * 512 + (j + 1) * 128)
            k = nxt()
            pb = PB[k % 4]
            pt = self.ps[pb]
            for kc in range(8):
                P.op('tensor', 'matmul', reads=['wz', rhT], writes=[f'ps{pb}'], out=pt,
                     lhsT=hT[:, kc, j * 128:(j + 1) * 128], rhs=wz[:, kc, :], start=(kc == 0), stop=(kc == 7))
            o = B['o32'][k % 3]
            P.op('scalar', 'activation', reads=[f'ps{pb}'], writes=[f'o32_{k % 3}'], out=o, in_=pt, func=AF.Silu)
            P.dma('sync', self.zs[tt, :], o, reads=[f'o32_{k % 3}'])
            k = nxt()
            pb = PB[k % 4]
            pt = self.ps[pb]
            for kc in range(8):
                P.op('tensor', 'matmul', reads=['wv', rhT], writes=[f'ps{pb}'], out=pt[:, 0:256],
                     lhsT=hT[:, kc, j * 128:(j + 1) * 128], rhs=wv[:, kc, :], start=(kc == 0), stop=(kc == 7))
            for kc in range(8):
                P.op('tensor', 'matmul', reads=['wdt', rhT32], writes=[f'ps{pb}'], out=pt[:, 256:264],
                     lhsT=hT32[:, kc, j * 128:(j + 1) * 128], rhs=wdt[:, kc, :], start=(kc == 0), stop=(kc == 7))
            o = B['o16'][k % 3]
            self.evac(k, o[:, 0:256], pt[:, 0:256], [f'ps{pb}'], [f'o16_{k % 3}'])
            P.dma('sync', self.vtok[tt, :], o[:, 0:256], reads=[f'o16_{k % 3}'])
            o2 = B['osm'][j % 2]
            P.op('vector', 'tensor_copy', reads=[f'ps{pb}'], writes=[f'osm_{j % 2}'], out=o2, in_=pt[:, 256:264])
            P.dma('sync', self.dtr[tt, :], o2, reads=[f'osm_{j % 2}'])

    def conv_gen(self, l, st, pA, pB):
        P, T = self.P, self.T
        TC = min(T, 1024)
        HALO = 30
        if True:
            sb = lambda n, s, dt=F32: self.sb(st, n, s, dt)
            cw = sb("cw", [128, 2, 31])
            cbias = sb("cbias", [128, 2])
            cg = sb("cg", [128, 2])
            cbeta = sb("cbeta", [128, 2])
            ua = [sb(f"ua{i}", [128, TC + HALO]) for i in range(2)]
            ub = [sb(f"ub{i}", [128, TC + HALO]) for i in range(2)]
            co = [sb(f"co{i}", [128, TC]) for i in range(2)]
            sqt = sb("csq", [128, 512])
            mean = sb("cmean", [128, 512])
            rstd = sb("crstd", [128, 512])
            tmp = [sb(f"ctmp{i}", [128, 512]) for i in range(2)]
            yo = [sb(f"cyo{i}", [128, 512], BF16) for i in range(2)]
            P.dma('sync', cw, self.cmw[l], writes=['cw'])
            P.dma('sync', cbias, self.cmb[l], writes=['cbias'])
            P.dma('sync', cg, self.cmg[l], writes=['cg'])
            P.dma('sync', cbeta, self.cmbeta[l], writes=['cbeta'])
            yield
            n = 0
            for c0 in range(0, T, TC):
                for ct in range(2):
                    A, Bt = ua[ct], ub[ct]
                    ra, rb, rc = f'ua{ct}', f'ub{ct}', f'co{ct}'
                    rows = slice(ct * 128, (ct + 1) * 128)
                    if c0 == 0:
                        P.dma('sync', A[:, HALO:], self.gaT[rows, 0:TC], writes=[ra])
                        P.dma('sync', Bt[:, HALO:], self.gbT[rows, 0:TC], writes=[rb])
                        P.op('gpsimd', 'memset', writes=[ra], ap=A[:, 0:HALO], constant=0.0)
                        P.op('gpsimd', 'memset', writes=[rb], ap=Bt[:, 0:HALO], constant=0.0)
                    else:
                        P.dma('sync', A, self.gaT[rows, c0 - HALO:c0 + TC], writes=[ra])
                        P.dma('sync', Bt, self.gbT[rows, c0 - HALO:c0 + TC], writes=[rb])
                    P.op('scalar', 'activation', reads=[rb], writes=[rb], out=Bt, in_=Bt, func=AF.Sigmoid)
                    P.op('gpsimd', 'tensor_tensor', reads=[ra, rb], writes=[ra], out=A, in0=A, in1=Bt, op=ALU.mult)
                    C = co[ct]
                    P.op('vector', 'tensor_scalar', reads=[ra, 'cw', 'cbias'], writes=[rc], out=C, in0=A[:, 0:TC],
                         scalar1=cw[:, ct, 0:1], scalar2=cbias[:, ct:ct + 1], op0=ALU.mult, op1=ALU.add)
                    for k in range(1, 31):
                        P.op('vector', 'scalar_tensor_tensor', reads=[ra, 'cw', rc], writes=[rc], out=C,
                             in0=A[:, k:k + TC], scalar=cw[:, ct, k:k + 1], in1=C, op0=ALU.mult, op1=ALU.add)
                        if k % 8 == 0:
                            yield
                    yield
                for s0 in range(0, TC, 512):
                    cs_ = slice(s0, s0 + 512)
                    p1, p2 = self.ps[pA], self.ps[pB]
                    for ct in range(2):
                        P.op('tensor', 'matmul', reads=['ones', f'co{ct}'], writes=[f'ps{pA}'], out=p1, lhsT=self.ones,
                             rhs=co[ct][:, cs_], start=(ct == 0), stop=(ct == 1))
                    P.op('vector', 'tensor_scalar', reads=[f'ps{pA}'], writes=['cmean'], out=mean, in0=p1,
                         scalar1=1.0 / 256, scalar2=None, op0=ALU.mult)
                    for ct in range(2):
                        P.op('scalar', 'activation', reads=[f'co{ct}'], writes=['csq'], out=sqt, in_=co[ct][:, cs_],
                             func=AF.Square)
                        P.op('tensor', 'matmul', reads=['ones', 'csq'], writes=[f'ps{pB}'], out=p2, lhsT=self.ones,
                             rhs=sqt, start=(ct == 0), stop=(ct == 1))
                    P.op('vector', 'tensor_tensor', reads=['cmean'], writes=['crstd'], out=rstd, in0=mean, in1=mean,
                         op=ALU.mult)
                    P.op('vector', 'scalar_tensor_tensor', reads=[f'ps{pB}', 'crstd'], writes=['crstd'], out=rstd, in0=p2,
                         scalar=1.0 / 256, in1=rstd, op0=ALU.mult, op1=ALU.subtract)
                    self.rstd_ops(rstd, rstd, 1.0, ['crstd'], ['crstd'])
                    for ct in range(2):
                        n += 1
                        t = tmp[n % 2]
                        rt = f'ctmp{n % 2}'
                        P.op('vector', 'tensor_tensor', reads=[f'co{ct}', 'cmean'], writes=[rt], out=t,
                             in0=co[ct][:, cs_], in1=mean, op=ALU.subtract)
                        P.op('gpsimd', 'tensor_tensor', reads=[rt, 'crstd'], writes=[rt], out=t, in0=t, in1=rstd,
                             op=ALU.mult)
                        y = yo[n % 2]
                        ry = f'cyo{n % 2}'
                        P.op('scalar', 'activation', reads=[rt, 'cg', 'cbeta'], writes=[ry], out=y, in_=t, func=AF.Silu,
                             scale=cg[:, ct:ct + 1], bias=cbeta[:, ct:ct + 1])
                        P.dma('sync', self.yT[768 + ct * 128:768 + (ct + 1) * 128, c0 + s0:c0 + s0 + 512], y,
                              reads=[ry])
                    yield

    def phase_conv(self, l):
        with contextlib.ExitStack() as st:
            self.P.begin()
            for _ in self.conv_gen(l, st, 0, 1):
                pass
            self.P.end()

    def phase_attn(self, l, conv_inside=False):
        P, T, NT, NQ = self.P, self.T, self.NT, self.NQ
        CW = min(T, 2048)
        with contextlib.ExitStack() as st:
            sb = lambda n, s, dt=F32: self.sb(st, n, s, dt)
            fb = sb("fb", [4, 1])
            xx = sb("fx", [4, CW])
            ax = sb("fax", [4, CW])
            mn = sb("fmn", [4, CW])
            cum = [sb(f"fcum{i}", [4, CW]) for i in range(2)]
            r1 = sb("fr1", [4, CW])
            sp = sb("fsp", [4, 6, CW], BF16)
            P.begin()
            P.dma('sync', fb, self.f_bias[l], writes=['fb'])
            for ci, c0 in enumerate(range(0, T, CW)):
                cc = cum[ci % 2]
                rcum = f'fcum{ci % 2}'
                P.dma('sync', xx, self.flT[:, c0:c0 + CW], writes=['fx'])
                P.op('scalar', 'activation', reads=['fx', 'fb'], writes=['fx'], out=xx, in_=xx, func=AF.Identity,
                     bias=fb[:, 0:1], scale=1.0)
                P.op('vector', 'tensor_scalar', reads=['fx'], writes=['fmn'], out=mn, in0=xx, scalar1=-1.0, scalar2=0.0,
                     op0=ALU.mult, op1=ALU.max)
                P.op('vector', 'scalar_tensor_tensor', reads=['fmn', 'fx'], writes=['fax'], out=ax, in0=mn, scalar=-2.0,
                     in1=xx, op0=ALU.mult, op1=ALU.subtract)
                P.op('scalar', 'activation', reads=['fax'], writes=['fax'], out=ax, in_=ax, func=AF.Exp)
                P.op('scalar', 'activation', reads=['fax'], writes=['fax'], out=ax, in_=ax, func=AF.Ln, bias=1.0,
                     scale=1.0)
                P.op('vector', 'scalar_tensor_tensor', reads=['fmn', 'fax'], writes=['fmn'], out=mn, in0=mn, scalar=-1.0,
                     in1=ax, op0=ALU.mult, op1=ALU.subtract)
                init = 0.0 if ci == 0 else cum[(ci - 1) % 2][:, CW - 1:CW]
                P.op('vector', 'tensor_tensor_scan', reads=['fmn', 'ones', f'fcum{(ci - 1) % 2}'], writes=[rcum], out=cc,
                     data0=self.ones[0:4, 0:1].broadcast_to([4, CW]), data1=mn, initial=init, op0=ALU.mult, op1=ALU.add)
                P.op('vector', 'tensor_copy', reads=[rcum], writes=['fsp'], out=sp[:, 0, :], in_=cc)
                P.op('vector', 'tensor_tensor', reads=[rcum, 'fsp'], writes=['fr1'], out=r1, in0=cc, in1=sp[:, 0, :],
                     op=ALU.subtract)
                P.op('vector', 'tensor_copy', reads=['fr1'], writes=['fsp'], out=sp[:, 1, :], in_=r1)
                P.op('vector', 'tensor_tensor', reads=['fr1', 'fsp'], writes=['fr1'], out=r1, in0=r1, in1=sp[:, 1, :],
                     op=ALU.subtract)
                P.op('vector', 'tensor_copy', reads=['fr1'], writes=['fsp'], out=sp[:, 2, :], in_=r1)
                P.op('vector', 'tensor_scalar', reads=['fsp'], writes=['fsp'], out=sp[:, 3:6, :], in0=sp[:, 0:3, :],
                     scalar1=-1.0, scalar2=None, op0=ALU.mult)
                P.dma('sync', self.cs[:, :, c0:c0 + CW], sp, reads=['fsp'])
            P.end()
        with contextlib.ExitStack() as st:
            sb = lambda n, s, dt=F32: self.sb(st, n, s, dt)
            qp = [sb(f"qp{i}", [70, T], BF16) for i in range(2)]
            kp = [sb(f"kp{i}", [70, T], BF16) for i in range(2)]
            vp = [sb(f"vp{i}", [128, NT, 65], BF16) for i in range(2)]
            nm = sb("negmask", [128, 4, 512], BF16)
            NSB = 5
            LAG = 3
            pt_ = [sb(f"pT{i}", [128, 512], BF16) for i in range(NSB)]
            rec = sb("rec", [65, 512])
            bcs = sb("bcs", [64, 512])
            on = [sb(f"on{i}", [64, 512]) for i in range(2)]
            P.begin()
            cgen = self.conv_gen(l, st, 7, 7) if conv_inside else None
            n_units = (T // min(T, 1024)) * (2 * 5 + min(T, 1024) // 512) + 1
            units_done = 0
            work_total = 4 * sum(4 * q_ + 4 for q_ in range(NQ))
            work_done = 0
            P.op('gpsimd', 'memset', writes=['negmask'], ap=nm, constant=0.0)
            for d in range(4):
                P.op('gpsimd', 'affine_select', reads=['negmask'], writes=['negmask'], out=nm[:, d, :], in_=nm[:, d, :],
                     pattern=[[1, 512]], compare_op=ALU.is_ge, fill=-30000.0, base=-128 * d, channel_multiplier=-1)
            step = 0
            for h in range(4):
                hb = h % 2
                Q, Kp, V = qp[hb], kp[hb], vp[hb]
                rq, rk, rv = f'qp{hb}', f'kp{hb}', f'vp{hb}'
                hr = slice(h * 64, (h + 1) * 64)
                P.op('gpsimd', 'memset', writes=[rq], ap=Q[64:70, :], constant=1.0)
                P.op('gpsimd', 'memset', writes=[rk], ap=Kp[64:70, :], constant=1.0)
                P.op('gpsimd', 'memset', writes=[rv], ap=V[:, :, 64:65], constant=1.0)
                P.dma('sync', Q[0:64, :], self.qT[hr, :], writes=[rq])
                P.dma('sync', Kp[0:64, :], self.kT[hr, :], writes=[rk])
                P.dma('sync', Q[67:70, :], self.cs[h, 0:3, :], writes=[rq])
                P.dma('sync', Kp[64:67, :], self.cs[h, 3:6, :], writes=[rk])
                for i0 in range(0, NT, 4):
                    P.dma('sync', V[:, i0:i0 + 4, 0:64],
                          self.vtok[i0 * 128:(i0 + 4) * 128, hr].rearrange("(i p) d -> p i d", p=128), writes=[rv])
                for qc in range(NQ):
                    nk = 4 * qc + 4
                    ob = 5 + qc % 2
                    O = self.ps[ob]
                    qs = slice(qc * 512, (qc + 1) * 512)
                    for s_ in range(nk + LAG):
                        if s_ < nk:
                            kt = s_
                            sbk = (step + s_) % NSB
                            S = self.ps[sbk]
                            diag = kt >= 4 * qc
                            P.op('tensor', 'matmul', reads=[rq, rk], writes=[f'ps{sbk}'], out=S,
                                 lhsT=Kp[:, kt * 128:(kt + 1) * 128], rhs=Q[:, qs], start=True, stop=not diag)
                            if diag:
                                P.op('tensor', 'matmul', reads=['identb', 'negmask'], writes=[f'ps{sbk}'], out=S,
                                     lhsT=self.identb, rhs=nm[:, kt - 4 * qc, :], start=False, stop=True)
                        if 1 <= s_ <= nk:
                            kt = s_ - 1
                            sbk = (step + kt) % NSB
                            P.op('scalar', 'activation', reads=[f'ps{sbk}'], writes=[f'pT{sbk}'], out=pt_[sbk],
                                 in_=self.ps[sbk], func=AF.Exp)
                        if s_ >= LAG:
                            kt = s_ - LAG
                            sbk = (step + kt) % NSB
                            P.op('tensor', 'matmul', reads=[f'pT{sbk}', rv], writes=[f'ps{ob}'], out=O[0:65, :],
                                 lhsT=V[:, kt, :], rhs=pt_[sbk], start=(kt == 0), stop=(kt == nk - 1))
                    step += nk
                    P.op('vector', 'reciprocal', reads=[f'ps{ob}'], writes=['rec'], out=rec[64:65, :], in_=O[64:65, :])
                    P.op('tensor', 'matmul', reads=['ones', 'rec'], writes=['ps7'], out=self.ps[7][0:64, :],
                         lhsT=self.ones[64:65, 0:64], rhs=rec[64:65, :], start=True, stop=True)
                    P.op('scalar', 'activation', reads=['ps7'], writes=['bcs'], out=bcs, in_=self.ps[7][0:64, :],
                         func=AF.Copy)
                    o_ = on[qc % 2]
                    P.op('vector', 'tensor_tensor', reads=[f'ps{ob}', 'bcs'], writes=[f'on{qc % 2}'], out=o_,
                         in0=O[0:64, :], in1=bcs, op=ALU.mult)
                    P.dma('sync', self.attT[hr, qs], o_, reads=[f'on{qc % 2}'])
                    work_done += nk
                    while cgen is not None and units_done * work_total < n_units * work_done:
                        try:
                            next(cgen)
                            units_done += 1
                        except StopIteration:
                            cgen = None
                if h == 3 and cgen is not None:
                    for _ in cgen:
                        pass
                if h < 3:
                    P.end()
                    P.begin()
            P.end()
        with contextlib.ExitStack() as st:
            sb = lambda n, s, dt=F32: self.sb(st, n, s, dt)
            fg = sb("foxg", [128, 2])
            at = [[sb(f"at{i}{c}", [128, 512]) for c in range(2)] for i in range(2)]
            sq = sb("asq", [128, 512])
            rs = sb("ars", [128, 512])
            yo = [sb(f"ayo{i}", [128, 512], BF16) for i in range(2)]
            P.begin()
            P.dma('sync', fg, self.fox_g[l], writes=['foxg'])
            n = 0
            for qc in range(NQ):
                qs = slice(qc * 512, (qc + 1) * 512)
                b = qc % 2
                for ct in range(2):
                    P.dma('sync', at[b][ct], self.attT[ct * 128:(ct + 1) * 128, qs], writes=[f'at{b}{ct}'])
                    P.op('scalar', 'activation', reads=[f'at{b}{ct}'], writes=['asq'], out=sq, in_=at[b][ct],
                         func=AF.Square)
                    P.op('tensor', 'matmul', reads=['ones', 'asq'], writes=['ps0'], out=self.ps[0], lhsT=self.ones,
                         rhs=sq, start=(ct == 0), stop=(ct == 1))
                self.rstd_ops(self.ps[0], rs, 256.0, ['ps0'], ['ars'])
                for ct in range(2):
                    n += 1
                    P.op('vector', 'tensor_tensor', reads=[f'at{b}{ct}', 'ars'], writes=[f'at{b}{ct}'], out=at[b][ct],
                         in0=at[b][ct], in1=rs, op=ALU.mult)
                    y = yo[n % 2]
                    P.op('scalar', 'activation', reads=[f'at{b}{ct}', 'foxg'], writes=[f'ayo{n % 2}'], out=y,
                         in_=at[b][ct], func=AF.Copy, scale=fg[:, ct:ct + 1])
                    P.dma('sync', self.yT[512 + ct * 128:512 + (ct + 1) * 128, qs], y, reads=[f'ayo{n % 2}'])
            P.end()

    def phase_ssd(self, l):
        P, T, NT = self.P, self.T, self.NT
        SC = 512
        assert NT * 8 <= 512
        with contextlib.ExitStack() as st:
            sb = lambda n, s, dt=F32: self.sb(st, n, s, dt)
            cw4 = sb("cw4", [128, 8, 4])
            cb4 = sb("cb4", [128, 8])
            dtb = sb("dtb", [128, 8])
            aneg = sb("aneg", [128, 8])
            dsk = sb("dsk", [128, 8])
            ng = sb("ssdng", [128, 512])
            dt = sb("dt_all", [128, NT, 8])
            dmn = sb("dt_mn", [128, NT, 8])
            dtA = sb("dtA", [128, NT, 8])
            El = sb("El", [128, NT, 8])
            Wl = sb("Wl", [128, NT, 8])
            cd = sb("cd", [128, NT, 8])
            xin = [sb(f"xin{i}", [128, SC + 3]) for i in range(2)]
            cacc = [sb(f"cacc{i}", [128, SC]) for i in range(2)]
            xsT = [sb(f"xsT{i}", [128, SC]) for i in range(4)]
            BT = [sb(f"BT{i}", [128, SC], BF16) for i in range(2)]
            CT = [sb(f"CT{i}", [128, SC], BF16) for i in range(2)]
            x32_2 = [sb(f"x32{i}", [128, 8, 64], F32) for i in range(2)]
            Btok_2 = [sb(f"Btok{i}", [128, 256], BF16) for i in range(2)]
            R_2 = [sb(f"Rall{i}", [128, 8, 128], F32) for i in range(2)]
            E_2 = [sb(f"Eall{i}", [128, 8, 128], F32) for i in range(2)]
            CBm_2 = [sb(f"CBm{i}", [128, 2, 128], F32) for i in range(2)]
            M_2 = [sb(f"Mall{i}", [128, 8, 128], BF16) for i in range(2)]
            xdt_2 = [sb(f"xdt{i}", [128, 8, 64], BF16) for i in range(2)]
            xw_2 = [sb(f"xw{i}", [128, 8, 64], BF16) for i in range(2)]
            H = sb("Hst", [128, 8, 64])
            Hb = sb("Hb", [128, 8, 64], BF16)
            t1_2 = [sb(f"sst1{i}", [128, 8, 64], F32) for i in range(2)]
            t2_2 = [sb(f"sst2{i}", [128, 8, 64], F32) for i in range(2)]
            zt_2 = [sb(f"zt{i}", [128, 512], F32) for i in range(2)]
            ssq_2 = [sb(f"ssq{i}", [128, 512], F32) for i in range(2)]
            gss_2 = [sb(f"gss{i}", [128, 2], F32) for i in range(2)]
            grs_2 = [sb(f"grs{i}", [128, 2], F32) for i in range(2)]
            yTs_2 = [sb(f"yTs{i}", [128, 4, 128], BF16) for i in range(2)]
            ps = self.ps
            psb1 = ps[1].bitcast(BF16)
            P.begin()
            P.dma('sync', cw4, self.convw[l], writes=['cw4'])
            P.dma('sync', cb4, self.convb[l], writes=['cb4'])
            P.dma('sync', dtb, self.dt_bias[l].partition_broadcast(128), writes=['dtb'])
            P.dma('sync', aneg, self.a_log[l].partition_broadcast(128), writes=['aneg'])
            P.dma('sync', dsk, self.ssd_d[l].partition_broadcast(128), writes=['dsk'])
            P.dma('sync', ng, self.ssd_norm_g[l].partition_broadcast(128), writes=['ssdng'])
            for i0 in range(0, NT, 4):
                n_ = min(4, NT - i0)
                P.dma('sync', dt[:, i0:i0 + n_, :],
                      self.dtr[i0 * 128:(i0 + n_) * 128, :].rearrange("(i p) h -> p i h", p=128), writes=['dt_all'])
            P.op('scalar', 'activation', reads=['aneg'], writes=['aneg'], out=aneg, in_=aneg, func=AF.Exp)
            P.op('vector', 'tensor_scalar', reads=['aneg'], writes=['aneg'], out=aneg, in0=aneg, scalar1=-1.0,
                 scalar2=None, op0=ALU.mult)
            bc3 = lambda t: t.unsqueeze(1).broadcast_to([128, NT, 8])
            P.op('vector', 'tensor_tensor', reads=['dt_all', 'dtb'], writes=['dt_all'], out=dt, in0=dt, in1=bc3(dtb),
                 op=ALU.add)
            P.op('vector', 'tensor_scalar', reads=['dt_all'], writes=['dt_mn'], out=dmn, in0=dt, scalar1=0.0,
                 scalar2=None, op0=ALU.max)
            P.op('vector', 'scalar_tensor_tensor', reads=['dt_mn', 'dt_all'], writes=['dt_all'],
                 out=dt.rearrange("p i h -> p (i h)"), in0=dmn.rearrange("p i h -> p (i h)"), scalar=-2.0,
                 in1=dt.rearrange("p i h -> p (i h)"), op0=ALU.mult, op1=ALU.add)
            P.op('scalar', 'activation', reads=['dt_all'], writes=['dt_all'], out=dt, in_=dt, func=AF.Exp)
            P.op('scalar', 'activation', reads=['dt_all'], writes=['dt_all'], out=dt, in_=dt, func=AF.Ln, bias=1.0,
                 scale=1.0)
            P.op('vector', 'tensor_tensor', reads=['dt_all', 'dt_mn'], writes=['dt_all'], out=dt, in0=dt, in1=dmn,
                 op=ALU.add)
            P.op('vector', 'tensor_tensor', reads=['dt_all', 'aneg'], writes=['dtA'], out=dtA, in0=dt, in1=bc3(aneg),
                 op=ALU.mult)
            dtA2 = dtA.rearrange("p i h -> p (i h)")
            P.op('tensor', 'matmul', reads=['tri', 'dtA'], writes=['ps2'], out=ps[2][:, 0:NT * 8], lhsT=self.tri,
                 rhs=dtA2, start=True, stop=True)
            P.op('tensor', 'matmul', reads=['ones', 'dtA'], writes=['ps3'], out=ps[3][:, 0:NT * 8], lhsT=self.ones,
                 rhs=dtA2, start=True, stop=True)
            f2 = lambda t: t.rearrange("p i h -> p (i h)")
            P.op('scalar', 'activation', reads=['ps2'], writes=['El'], out=f2(El), in_=ps[2][:, 0:NT * 8], func=AF.Exp)
            P.op('scalar', 'activation', reads=['ps3'], writes=['cd'], out=f2(cd), in_=ps[3][:, 0:NT * 8], func=AF.Exp)
            P.op('vector', 'tensor_copy', reads=['ps3'], writes=['Wl'], out=f2(Wl), in_=ps[3][:, 0:NT * 8])
            P.op('vector', 'tensor_tensor', reads=['Wl', 'ps2'], writes=['Wl'], out=f2(Wl), in0=f2(Wl),
                 in1=ps[2][:, 0:NT * 8], op=ALU.subtract)
            P.op('scalar', 'activation', reads=['Wl'], writes=['Wl'], out=Wl, in_=Wl, func=AF.Exp)
            P.op('vector', 'tensor_tensor', reads=['Wl', 'dt_all'], writes=['Wl'], out=Wl, in0=Wl, in1=dt, op=ALU.mult)
            P.op('gpsimd', 'memset', writes=['Hst'], ap=H, constant=0.0)
            P.op('gpsimd', 'memset', writes=['Hb'], ap=Hb, constant=0.0)
            for c0 in range(0, T, SC):
                for ct in range(8):
                    X = xin[ct % 2]
                    rx = f'xin{ct % 2}'
                    A = cacc[ct % 2]
                    ra = f'cacc{ct % 2}'
                    rows = slice(ct * 128, (ct + 1) * 128)
                    if c0 == 0:
                        P.op('gpsimd', 'memset', writes=[rx], ap=X[:, 0:3], constant=0.0)
                        P.dma('sync', X[:, 3:], self.xbcT[rows, 0:SC], writes=[rx])
                    else:
                        P.dma('sync', X, self.xbcT[rows, c0 - 3:c0 + SC], writes=[rx])
                    P.op('vector', 'tensor_scalar', reads=[rx, 'cw4', 'cb4'], writes=[ra], out=A, in0=X[:, 0:SC],
                         scalar1=cw4[:, ct, 0:1], scalar2=cb4[:, ct:ct + 1], op0=ALU.mult, op1=ALU.add)
                    for k in range(1, 4):
                        P.op('vector', 'scalar_tensor_tensor', reads=[rx, 'cw4', ra], writes=[ra], out=A,
                             in0=X[:, k:k + SC], scalar=cw4[:, ct, k:k + 1], in1=A, op0=ALU.mult, op1=ALU.add)
                    if ct < 4:
                        dst, rd = xsT[ct], f'xsT{ct}'
                    elif ct < 6:
                        dst, rd = BT[ct - 4], f'BT{ct - 4}'
                    else:
                        dst, rd = CT[ct - 6], f'CT{ct - 6}'
                    P.op('scalar', 'activation', reads=[ra], writes=[rd], out=dst, in_=A, func=AF.Silu)
                def stage_a(c0, cc):
                        c = c0 // 128 + cc
                        cs_ = slice(cc * 128, (cc + 1) * 128)
                        tok = slice(c * 128, (c + 1) * 128)
                        pc = c % 2
                        x32 = x32_2[pc]
                        Btok = Btok_2[pc]
                        R = R_2[pc]
                        E = E_2[pc]
                        CBm = CBm_2[pc]
                        M = M_2[pc]
                        xdt = xdt_2[pc]
                        xw = xw_2[pc]
                        t1 = t1_2[pc]
                        t2 = t2_2[pc]
                        zt = zt_2[pc]
                        ssq = ssq_2[pc]
                        gss = gss_2[pc]
                        grs = grs_2[pc]
                        yTs = yTs_2[pc]
                        n = {k: k + str(pc) for k in ('x32', 'Btok', 'Rall', 'Eall', 'CBm', 'Mall', 'xdt', 'xw', 'sst1', 'sst2', 'zt', 'ssq', 'gss', 'grs', 'yTs')}
                        for ct in range(4):
                            P.op('tensor', 'transpose', reads=[f'xsT{ct}', 'ident'], writes=['ps0'],
                                 out=ps[0][:, ct * 128:(ct + 1) * 128], in_=xsT[ct][:, cs_], identity=self.ident)
                        P.op('scalar', 'activation', reads=['ps0'], writes=[n['x32']], out=x32.rearrange("p h d -> p (h d)"),
                             in_=ps[0], func=AF.Copy)
                        for g in range(2):
                            P.op('tensor', 'transpose', reads=[f'BT{g}', 'identb'], writes=['ps1'],
                                 out=psb1[:, g * 128:(g + 1) * 128], in_=BT[g][:, cs_], identity=self.identb)
                        P.op('vector', 'tensor_copy', reads=['ps1'], writes=[n['Btok']], out=Btok, in_=psb1[:, 0:256])
                        P.dma('sync', zt, self.zs[tok, :], writes=[n['zt']])
                        P.op('vector', 'tensor_tensor', reads=['tri', 'dtA'], writes=[n['Rall']], out=R,
                             in0=self.tri.unsqueeze(1).broadcast_to([128, 8, 128]),
                             in1=dtA[:, c, :].unsqueeze(2).broadcast_to([128, 8, 128]), op=ALU.mult)
                        for hh in range(2):
                            P.op('tensor', 'matmul', reads=['ustr', n['Rall']], writes=[f'ps{2 + hh}'], out=ps[2 + hh],
                                 lhsT=self.ustr, rhs=R[:, hh * 4:(hh + 1) * 4, :].rearrange("p h l -> p (h l)"),
                                 start=True, stop=True)
                            P.op('scalar', 'activation', reads=[f'ps{2 + hh}'], writes=[n['Eall']],
                                 out=E[:, hh * 4:(hh + 1) * 4, :].rearrange("p h l -> p (h l)"), in_=ps[2 + hh], func=AF.Exp)
                        for g in range(2):
                            P.op('tensor', 'matmul', reads=[f'BT{g}', f'CT{g}'], writes=['ps4'],
                                 out=ps[4][:, g * 128:(g + 1) * 128], lhsT=BT[g][:, cs_], rhs=CT[g][:, cs_],
                                 start=True, stop=True)
                        P.op('vector', 'tensor_tensor', reads=['ps4', 'tri'], writes=[n['CBm']], out=CBm,
                             in0=ps[4][:, 0:256].rearrange("p (g l) -> p g l", g=2),
                             in1=self.tri.unsqueeze(1).broadcast_to([128, 2, 128]), op=ALU.mult)
                        for g in range(2):
                            P.op('vector', 'tensor_tensor', reads=[n['Eall'], n['CBm']], writes=[n['Mall']],
                                 out=M[:, g * 4:(g + 1) * 4, :], in0=E[:, g * 4:(g + 1) * 4, :],
                                 in1=CBm[:, g:g + 1, :].broadcast_to([128, 4, 128]), op=ALU.mult)
                        P.op('gpsimd', 'tensor_tensor', reads=[n['x32'], 'dt_all'], writes=[n['xdt']], out=xdt, in0=x32,
                             in1=dt[:, c, :].unsqueeze(2).broadcast_to([128, 8, 64]), op=ALU.mult)
                        P.op('gpsimd', 'tensor_tensor', reads=[n['x32'], 'Wl'], writes=[n['xw']], out=xw, in0=x32,
                             in1=Wl[:, c, :].unsqueeze(2).broadcast_to([128, 8, 64]), op=ALU.mult)
                        for h in range(8):
                            P.op('tensor', 'matmul', reads=[n['Mall'], n['xdt']], writes=['ps5'], out=ps[5][:, h * 64:(h + 1) * 64],
                                 lhsT=M[:, h, :], rhs=xdt[:, h, :], start=True, stop=True)
                        for g in range(2):
                            P.op('tensor', 'matmul', reads=[f'CT{g}', 'Hb'], writes=['ps6'],
                                 out=ps[6][:, g * 256:(g + 1) * 256], lhsT=CT[g][:, cs_],
                                 rhs=Hb[:, g * 4:(g + 1) * 4, :].rearrange("p h d -> p (h d)"), start=True, stop=True)
                        for g in range(2):
                            P.op('tensor', 'matmul', reads=[n['Btok'], n['xw']], writes=['ps7'],
                                 out=ps[7][:, g * 256:(g + 1) * 256], lhsT=Btok[:, g * 128:(g + 1) * 128],
                                 rhs=xw[:, g * 4:(g + 1) * 4, :].rearrange("p h d -> p (h d)"), start=True, stop=True)
                        v3 = lambda t: t.rearrange("p (h d) -> p h d", h=8)
                        b3 = lambda t: t.unsqueeze(2).broadcast_to([128, 8, 64])
                        P.op('vector', 'tensor_tensor', reads=['ps6', 'El'], writes=[n['sst1']], out=t1, in0=v3(ps[6]),
                             in1=b3(El[:, c, :]), op=ALU.mult)
                        P.op('vector', 'tensor_tensor', reads=[n['sst1'], 'ps5'], writes=[n['sst1']], out=t1, in0=t1, in1=v3(ps[5]),
                             op=ALU.add)
                        P.op('gpsimd', 'tensor_tensor', reads=[n['x32'], 'dsk'], writes=[n['sst2']], out=t2, in0=x32, in1=b3(dsk),
                             op=ALU.mult)
                        P.op('gpsimd', 'tensor_tensor', reads=[n['sst1'], n['sst2']], writes=[n['sst1']], out=t1, in0=t1, in1=t2,
                             op=ALU.add)
                        P.op('vector', 'tensor_tensor', reads=['Hst', 'cd'], writes=['Hst'], out=H, in0=H, in1=b3(cd[:, c, :]),
                             op=ALU.mult)
                        P.op('vector', 'tensor_tensor', reads=['Hst', 'ps7'], writes=['Hst'], out=H, in0=H, in1=v3(ps[7]),
                             op=ALU.add)
                        P.op('scalar', 'activation', reads=['Hst'], writes=['Hb'], out=Hb, in_=H, func=AF.Copy)

                def stage_b(c):
                        tok = slice(c * 128, (c + 1) * 128)
                        pc = c % 2
                        x32 = x32_2[pc]
                        Btok = Btok_2[pc]
                        R = R_2[pc]
                        E = E_2[pc]
                        CBm = CBm_2[pc]
                        M = M_2[pc]
                        xdt = xdt_2[pc]
                        xw = xw_2[pc]
                        t1 = t1_2[pc]
                        t2 = t2_2[pc]
                        zt = zt_2[pc]
                        ssq = ssq_2[pc]
                        gss = gss_2[pc]
                        grs = grs_2[pc]
                        yTs = yTs_2[pc]
                        n = {k: k + str(pc) for k in ('x32', 'Btok', 'Rall', 'Eall', 'CBm', 'Mall', 'xdt', 'xw', 'sst1', 'sst2', 'zt', 'ssq', 'gss', 'grs', 'yTs')}
                        y2 = t1.rearrange("p h d -> p (h d)")
                        P.op('gpsimd', 'tensor_tensor', reads=[n['sst1'], n['zt']], writes=[n['sst1']], out=y2, in0=y2, in1=zt,
                             op=ALU.mult)
                        for g in range(2):
                            P.op('scalar', 'activation', reads=[n['sst1']], writes=[n['ssq'], n['gss']],
                                 out=ssq[:, g * 256:(g + 1) * 256], in_=y2[:, g * 256:(g + 1) * 256], func=AF.Square,
                                 accum_out=gss[:, g:g + 1])
                        self.rstd_ops(gss, grs, 256.0, [n['gss']], [n['grs']])
                        for g in range(2):
                            P.op('vector', 'scalar_tensor_tensor', reads=[n['sst1'], n['grs'], 'ssdng'], writes=[n['sst2']],
                                 out=t2.rearrange("p h d -> p (h d)")[:, g * 256:(g + 1) * 256],
                                 in0=y2[:, g * 256:(g + 1) * 256], scalar=grs[:, g:g + 1],
                                 in1=ng[:, g * 256:(g + 1) * 256], op0=ALU.mult, op1=ALU.mult)
                        yn = t2.rearrange("p h d -> p (h d)")
                        for ct in range(4):
                            P.op('tensor', 'transpose', reads=[n['sst2'], 'ident'], writes=['ps0'],
                                 out=ps[0][:, ct * 128:(ct + 1) * 128], in_=yn[:, ct * 128:(ct + 1) * 128],
                                 identity=self.ident)
                        P.op('scalar', 'activation', reads=['ps0'], writes=[n['yTs']], out=yTs.rearrange("p c t -> p (c t)"),
                             in_=ps[0], func=AF.Copy)
                        P.dma('sync', self.yT[0:512, tok].rearrange("(ct p) t -> p ct t", p=128), yTs, reads=[n['yTs']])

                for cc in range(SC // 128):
                    c = c0 // 128 + cc
                    stage_a(c0, cc)
                    if c >= 1:
                        stage_b(c - 1)
                if c0 + SC >= T:
                    stage_b(NT - 1)
            P.end()

    def phase_wout_router(self, l, st0):
        P, T, NT = self.P, self.T, self.NT
        src = self.x_in if l == 0 else self.xres
        ps = self.ps
        self.ohb = self.sb(st0, "ohb", [128, NT, 64], BF16)
        self.rw = self.sb(st0, "rw", [128, NT, 2])
        with contextlib.ExitStack() as st:
            sb = lambda n, s, dt=F32: self.sb(st, n, s, dt)
            wo = sb("wo", [128, 8, D], BF16)
            gsc = sb("gscf", [128, D])
            wr = sb("wr", [128, 8, 36])
            rb = sb("rbias", [128, 36])
            yTc = [sb(f"yTc{i}", [128, 8, 512], BF16) for i in range(2)]
            xl = [sb(f"xl{i}", [128, D]) for i in range(4)]
            tt = sb("wtmp", [128, D])
            hb = [sb(f"hperm{i}", [128, D], BF16) for i in range(2)]
            lg = sb("lg", [128, 36])
            lg4 = sb("lg4", [128, 4, 36])
            gmax4 = sb("gmax4", [128, 4])
            gsum4 = sb("gsum4", [128, 4])
            r4 = sb("r4", [128, 4])
            den4 = sb("den4", [128, 4])
            ohg4 = sb("ohg4", [128, 4, 4])
            gexp4 = sb("gexp4", [128, 4, 4])
            em4 = sb("em4", [128, 4, 4, 8])
            es4 = sb("es4", [128, 4, 8])
            t84 = sb("t84", [128, 4, 8])
            s14 = sb("s14", [128, 4, 8])
            s24 = sb("s24", [128, 4, 8])
            sm = sb("rsm", [128, 64])
            gexp = sb("gexp", [128, 4])
            ohg = sb("ohg", [128, 4])
            em = sb("em", [128, 4, 8])
            es = sb("esel", [128, 8])
            t8 = sb("top8", [128, 8])
            s1 = sb("sel1", [128, 8])
            s2 = sb("sel2", [128, 8])
            W = self.w_out[l].rearrange("(kc p) n -> p kc n", p=128)
            P.begin()
            for kc in range(8):
                self.load_cast(st, wo[:, kc, :], W[:, kc, :], 'wo', D)
            P.dma('sync', wr[:, :, 0:4], self.w_rg[l].rearrange("(kc p) n -> p kc n", p=128), writes=['wr'])
            P.dma('sync', wr[:, :, 4:36], self.w_re[l].rearrange("(kc p) n -> p kc n", p=128), writes=['wr'])
            P.dma('sync', rb[:, 0:4], self.b_rg[l].partition_broadcast(128), writes=['rbias'])
            P.dma('sync', rb[:, 4:36], self.b_re[l].partition_broadcast(128), writes=['rbias'])
            P.dma('sync', gsc, self.norm_ffn_g[l].partition_broadcast(128), writes=['gscf'])
            P.op('vector', 'scalar_tensor_tensor', reads=['gscf', 'modb'], writes=['gscf'], out=gsc, in0=self.mod(4),
                 scalar=1.0, in1=gsc, op0=ALU.add, op1=ALU.mult)

            def pre_load(i):
                q, j = divmod(i, 4)
                if j == 0:
                    P.dma('sync', yTc[q % 2], self.yT[:, q * 512:(q + 1) * 512].rearrange("(kc p) t -> p kc t", p=128),
                          writes=[f'yTc{q % 2}'])
                P.dma('sync', xl[i % 4], src[i * 128:(i + 1) * 128, :], writes=[f'xl{i % 4}'])

            def pre(i, X, rx):
                q, j = divmod(i, 4)
                Y = yTc[q % 2]
                ry = f'yTc{q % 2}'
                XL = xl[i % 4]
                rl = f'xl{i % 4}'
                for half in range(2):
                    pb = 2 + half
                    for kc in range(8):
                        P.op('tensor', 'matmul', reads=[ry, 'wo'], writes=[f'ps{pb}'], out=ps[pb],
                             lhsT=Y[:, kc, j * 128:(j + 1) * 128], rhs=wo[:, kc, half * 512:(half + 1) * 512],
                             start=(kc == 0), stop=(kc == 7))
                    hs = slice(half * 512, (half + 1) * 512)
                    P.op('vector', 'tensor_tensor', reads=[f'ps{pb}', 'modb'], writes=['wtmp'], out=tt[:, hs], in0=ps[pb],
                         in1=self.mod(2)[:, hs], op=ALU.mult)
                P.op('vector', 'tensor_tensor', reads=['wtmp', rl], writes=[rx], out=X, in0=tt, in1=XL, op=ALU.add)
                P.dma('sync', self.xres[i * 128:(i + 1) * 128, :], X, reads=[rx])

            def after_h(i, Hh, rh, X, rx):
                Hp = hb[i % 2]
                rp = f'hperm{i % 2}'
                P.op('gpsimd', 'tensor_copy', reads=[rh], writes=[rp], out=Hp.rearrange("t (kc p) -> t kc p", kc=8),
                     in_=Hh.rearrange("t (p kc) -> t kc p", kc=8))
                P.dma('sync', self.hfp[i * 128:(i + 1) * 128, :], Hp, reads=[rp])

            def consumer(q, hTb, hT32b):
                hT32, r32 = hT32b
                i0_ = q * 4
                for j in range(4):
                    for kc in range(8):
                        P.op('tensor', 'matmul', reads=[r32, 'wr'], writes=['ps4'], out=ps[4][:, j * 36:(j + 1) * 36],
                             lhsT=hT32[:, kc, j * 128:(j + 1) * 128], rhs=wr[:, kc, :], start=(kc == 0), stop=(kc == 7))
                V = lambda name, **kw: P.op('vector', name, **kw)
                V('tensor_tensor', reads=['ps4', 'rbias'], writes=['lg'], out=lg4,
                  in0=ps[4][:, 0:144].rearrange("p (j c) -> p j c", j=4), in1=rb.unsqueeze(1).broadcast_to([128, 4, 36]),
                  op=ALU.add)
                gl = lg4[:, :, 0:4]
                el = lg4[:, :, 4:36].rearrange("p j (g e) -> p j g e", g=4)
                b3 = lambda t, n_: t.unsqueeze(2).broadcast_to([128, 4, n_])
                V('tensor_reduce', reads=['lg'], writes=['rsm'], out=gmax4, in_=gl, axis=AX.X, op=ALU.max)
                V('tensor_tensor', reads=['lg', 'rsm'], writes=['ohg'], out=ohg4, in0=gl, in1=b3(gmax4, 4), op=ALU.is_ge)
                V('tensor_tensor', reads=['lg', 'rsm'], writes=['gexp'], out=gexp4, in0=gl, in1=b3(gmax4, 4),
                  op=ALU.subtract)
                P.op('scalar', 'activation', reads=['gexp'], writes=['gexp'], out=gexp4, in_=gexp4, func=AF.Exp)
                V('tensor_reduce', reads=['gexp'], writes=['rsm2'], out=gsum4, in_=gexp4, axis=AX.X, op=ALU.add)
                V('reciprocal', reads=['rsm2'], writes=['rsm2'], out=gsum4, in_=gsum4)
                V('tensor_tensor', reads=['lg', 'ohg'], writes=['em'], out=em4, in0=el,
                  in1=ohg4.unsqueeze(3).broadcast_to([128, 4, 4, 8]), op=ALU.mult)
                V('tensor_reduce', reads=['em'], writes=['esel'], out=es4, in_=em4.rearrange("p j g e -> p j e g"),
                  axis=AX.X, op=ALU.add)
                for j in range(4):
                    V('max', reads=['esel'], writes=['top8'], out=t84[:, j, :], in_=es4[:, j, :])
                V('tensor_tensor', reads=['esel', 'top8'], writes=['sel1'], out=s14, in0=es4,
                  in1=t84[:, :, 0:1].broadcast_to([128, 4, 8]), op=ALU.is_ge)
                V('tensor_tensor', reads=['esel', 'top8'], writes=['sel2'], out=s24, in0=es4,
                  in1=t84[:, :, 1:2].broadcast_to([128, 4, 8]), op=ALU.is_ge)
                V('tensor_tensor', reads=['sel2', 'sel1'], writes=['sel2'], out=s24, in0=s24, in1=s14, op=ALU.subtract)
                V('tensor_tensor', reads=['top8'], writes=['rsm3'], out=r4, in0=t84[:, :, 1], in1=t84[:, :, 0],
                  op=ALU.subtract)
                P.op('scalar', 'activation', reads=['rsm3'], writes=['rsm3'], out=r4, in_=r4, func=AF.Exp)
                V('tensor_scalar', reads=['rsm3'], writes=['rsm4'], out=den4, in0=r4, scalar1=1.0, scalar2=None,
                  op0=ALU.add)
                V('reciprocal', reads=['rsm4'], writes=['rsm4'], out=den4, in_=den4)
                V('tensor_tensor', reads=['rsm4', 'rsm2'], writes=['rw'], out=self.rw[:, i0_:i0_ + 4, 0], in0=den4,
                  in1=gsum4, op=ALU.mult)
                V('tensor_tensor', reads=['rw', 'rsm3'], writes=['rw'], out=self.rw[:, i0_:i0_ + 4, 1],
                  in0=self.rw[:, i0_:i0_ + 4, 0], in1=r4, op=ALU.mult)
                for k, sel in enumerate((s14, s24)):
                    V('tensor_tensor', reads=['ohg', f'sel{k + 1}'], writes=['ohb'],
                      out=self.ohb[:, i0_:i0_ + 4, k * 32:(k + 1) * 32].rearrange("p j (g e) -> p j g e", g=4),
                      in0=ohg4.unsqueeze(3).broadcast_to([128, 4, 4, 8]),
                      in1=sel.unsqueeze(2).broadcast_to([128, 4, 4, 8]), op=ALU.mult)
            self.norm_and_transpose_loop(st, None, gsc, self.mod(3), consumer, pre=pre, after_h=after_h, pre_load=pre_load)
            P.end()

    def phase_moe(self, l, st0):
        P, T, NT, NB, BLK = self.P, self.T, self.NT, self.NB, self.BLK
        ps = self.ps
        last = (l == self.L - 1)
        NR = NB * BLK
        wgv = self.w_gate.rearrange("l e (p two k4) f -> (l e p two) (k4 f)", two=2, k4=4)
        wuv = self.w_up.rearrange("l e (p two k4) f -> (l e p two) (k4 f)", two=2, k4=4)
        wdv = self.w_down.rearrange("l e (p two f2) d -> (l e p two) (f2 d)", two=2, f2=2)
        with contextlib.ExitStack() as st:
            sb = lambda n, s, dt=F32: self.sb(st, n, s, dt)
            dest_i = sb("dest_i", [128, NT, 2], I32)
            widx = sb("widx", [128, NB, 2], I32)
            with contextlib.ExitStack() as s1:
                sb1 = lambda n, s, dt=F32: self.sb(s1, n, s, dt)
                pre = sb1("pre_all", [128, NT, 64])
                tot = sb1("tot_all", [128, NT, 64])
                base = sb1("base_all", [128, NT, 64])
                cnt = sb1("cnt", [128, 64])
                tl = sb1("mtotal", [128, 32])
                md = sb1("mmod", [128, 32])
                pend = sb1("pend", [128, 32])
                off = sb1("moff", [128, 64])
                dest_f = sb1("dest_f", [128, NT, 2])
                blk0 = sb1("blk0", [128, NB])
                cmp_ = sb1("mcmp", [128, NB, 32])
                be = sb1("mbe", [128, NB])
                pidx = sb1("pidx", [128, 2])
                wf = sb1("widx_f", [128, NB, 2])
                dbg_t = sb1("rdbg_t", [128, 68])
                P.begin()
                ohb2 = self.ohb.rearrange("p i c -> p (i c)")
                f2 = lambda t: t.rearrange("p i c -> p (i c)")
                for k, c0 in enumerate(range(0, NT * 64, 512)):
                    n = min(512, NT * 64 - c0)
                    P.op('tensor', 'matmul', reads=['sutb', 'ohb'], writes=['ps0'], out=ps[0][:, 0:n], lhsT=self.sutb,
                         rhs=ohb2[:, c0:c0 + n], start=True, stop=True)
                    P.op('scalar', 'activation', reads=['ps0'], writes=['pre_all'], out=f2(pre)[:, c0:c0 + n],
                         in_=ps[0][:, 0:n], func=AF.Copy)
                    P.op('tensor', 'matmul', reads=['onesb', 'ohb'], writes=['ps1'], out=ps[1][:, 0:n], lhsT=self.onesb,
                         rhs=ohb2[:, c0:c0 + n], start=True, stop=True)
                    P.op('vector', 'tensor_copy', reads=['ps1'], writes=['tot_all'], out=f2(tot)[:, c0:c0 + n],
                         in_=ps[1][:, 0:n])
                V = lambda name, **kw: P.op('vector', name, **kw)
                P.op('gpsimd', 'memset', writes=['base_all'], ap=base[:, 0, :], constant=0.0)
                for i in range(1, NT):
                    V('tensor_tensor', reads=['base_all', 'tot_all'], writes=['base_all'], out=base[:, i, :],
                      in0=base[:, i - 1, :], in1=tot[:, i - 1, :], op=ALU.add)
                V('tensor_tensor', reads=['base_all', 'tot_all'], writes=['cnt'], out=cnt, in0=base[:, NT - 1, :],
                  in1=tot[:, NT - 1, :], op=ALU.add)
                V('tensor_tensor', reads=['cnt'], writes=['mtotal'], out=tl, in0=cnt[:, 0:32], in1=cnt[:, 32:64],
                  op=ALU.add)
                P.op('gpsimd', 'iota', writes=['blk0'], out=blk0, pattern=[[BLK, NB]], base=0, channel_multiplier=0,
                     allow_small_or_imprecise_dtypes=True)
                V('tensor_tensor', reads=['mtotal', 'blk0'], writes=['mcmp'], out=cmp_.rearrange("p b e -> p (b e)").rearrange("p (e b) -> p e b", e=32),
                  in0=blk0.unsqueeze(1).broadcast_to([128, 32, NB]), in1=tl.unsqueeze(2).broadcast_to([128, 32, NB]),
                  op=ALU.is_lt)
                V('tensor_reduce', reads=['mcmp'], writes=['mtotal'], out=tl,
                  in_=cmp_.rearrange("p b e -> p (b e)").rearrange("p (e b) -> p e b", e=32), axis=AX.X, op=ALU.add)
                V('tensor_scalar', reads=['mtotal'], writes=['mtotal'], out=tl, in0=tl, scalar1=float(BLK), scalar2=None,
                  op0=ALU.mult)
                V('tensor_tensor_scan', reads=['mtotal', 'ones'], writes=['pend'], out=pend,
                  data0=self.ones[:, 0:32], data1=tl, initial=0.0, op0=ALU.mult, op1=ALU.add)
                V('tensor_tensor', reads=['pend', 'mtotal'], writes=['moff'], out=off[:, 0:32], in0=pend, in1=tl,
                  op=ALU.subtract)
                V('tensor_tensor', reads=['moff', 'cnt'], writes=['moff'], out=off[:, 32:64], in0=off[:, 0:32],
                  in1=cnt[:, 0:32], op=ALU.add)
                V('tensor_tensor', reads=['pre_all', 'base_all'], writes=['pre_all'], out=pre, in0=pre, in1=base,
                  op=ALU.add)
                V('tensor_tensor', reads=['pre_all', 'moff'], writes=['pre_all'], out=pre, in0=pre,
                  in1=off.unsqueeze(1).broadcast_to([128, NT, 64]), op=ALU.add)
                V('tensor_tensor', reads=['pre_all', 'ohb'], writes=['pre_all'], out=pre, in0=pre, in1=self.ohb,
                  op=ALU.mult)
                V('tensor_reduce', reads=['pre_all'], writes=['dest_f'], out=dest_f,
                  in_=pre.rearrange("p i (k e) -> p i k e", k=2), axis=AX.X, op=ALU.add)
                V('tensor_copy', reads=['dest_f'], writes=['dest_i'], out=dest_i, in_=dest_f)
                V('tensor_tensor', reads=['pend', 'blk0'], writes=['mcmp'], out=cmp_,
                  in0=pend.unsqueeze(1).broadcast_to([128, NB, 32]), in1=blk0.unsqueeze(2).broadcast_to([128, NB, 32]),
                  op=ALU.is_le)
                V('tensor_reduce', reads=['mcmp'], writes=['mbe'], out=be, in_=cmp_, axis=AX.X, op=ALU.add)
                V('tensor_scalar', reads=['mbe'], writes=['mbe'], out=be, in0=be, scalar1=256.0, scalar2=None,
                  op0=ALU.mult)
                P.op('gpsimd', 'iota', writes=['pidx'], out=pidx, pattern=[[1, 2]], base=l * NE * 256, channel_multiplier=2,
                     allow_small_or_imprecise_dtypes=True)
                V('tensor_tensor', reads=['mbe', 'pidx'], writes=['widx_f'], out=wf,
                  in0=be.unsqueeze(2).broadcast_to([128, NB, 2]), in1=pidx.unsqueeze(1).broadcast_to([128, NB, 2]),
                  op=ALU.add)
                V('tensor_copy', reads=['widx_f'], writes=['widx'], out=widx, in_=wf)
                if 'rdbg' in self.dbg:
                    for i in range(NT):
                        V('tensor_copy', reads=['ohb'], writes=['rdbg_t'], out=dbg_t[:, 0:64], in_=self.ohb[:, i, :])
                        V('tensor_copy', reads=['rw'], writes=['rdbg_t'], out=dbg_t[:, 64:66], in_=self.rw[:, i, :])
                        V('tensor_copy', reads=['dest_f'], writes=['rdbg_t'], out=dbg_t[:, 66:68], in_=dest_f[:, i, :])
                        P.dma('sync', self.rdbg[i * 128:(i + 1) * 128, :], dbg_t, reads=['rdbg_t'])
                P.end()
            if '1' in self.opt:
                return
            with contextlib.ExitStack() as s2:
                hrow = [self.sb(s2, f"hrow{i}", [128, D], BF16) for i in range(3)]
                P.begin()
                P.raw('gpsimd', 'reg_mov', self.reg_rows, NR - 1)
                for i in range(NT):
                    Hr = hrow[i % 3]
                    rr = f'hrow{i % 3}'
                    P.dma('sync', Hr, self.hfp[i * 128:(i + 1) * 128, :], writes=[rr])
                    for k in range(2):
                        P.dma('gpsimd', None, None, reads=[rr, 'dest_i'], writes=['xs'],
                              fn=('indirect_dma_start', dict(
                                  out=self.xs, out_offset=bass.IndirectOffsetOnAxis(ap=dest_i[:, i, k:k + 1], axis=0),
                                  in_=Hr, in_offset=None, bounds_check=self.reg_rows, oob_is_err=False)))
                P.end()
            if '2' in self.opt:
                return
            with contextlib.ExitStack() as s3:
                sb3 = lambda n, s, dt=F32: self.sb(s3, n, s, dt)
                Wg = [sb3(f"Wg{i}", [128, 8, 512], BF16) for i in range(2)]
                Wu = [sb3(f"Wu{i}", [128, 8, 512], BF16) for i in range(2)]
                Wd = [sb3(f"Wd{i}", [128, 4, 1024], BF16) for i in range(2)]
                xsT = [sb3(f"xsT{i}", [128, 8, 512], BF16) for i in range(2)]
                hid = [sb3(f"hid{i}", [128, 4, 512], BF16) for i in range(2)]
                sg = [sb3(f"sg{i}", [128, 512]) for i in range(2)]
                yb = [sb3(f"yb{i}", [128, D]) for i in range(2)]
                P.begin()
                P.raw('gpsimd', 'reg_mov', self.reg_w, (l + 1) * NE * 256 - 1)
                n_y = [0]

                def load_blk(b):
                    a = b % 2
                    for (Wt, view, nm) in ((Wg[a], wgv, f'Wg{a}'), (Wu[a], wuv, f'Wu{a}'), (Wd[a], wdv, f'Wd{a}')):
                        flat = Wt.rearrange("p a f -> p (a f)")
                        for hf in range(0 if 'G' in self.opt else 2):
                            P.dma('gpsimd', None, None, reads=['widx'], writes=[nm],
                                  fn=('indirect_dma_start', dict(
                                      out=flat[:, hf * 2048:(hf + 1) * 2048], out_offset=None, in_=view,
                                      in_offset=bass.IndirectOffsetOnAxis(ap=widx[:, b, hf:hf + 1], axis=0),
                                      bounds_check=self.reg_w, oob_is_err=False)))
                    X = xsT[a]
                    for kc in range(8):
                        P.dma('sync', None, None, reads=['xs'], writes=[f'xsT{a}'],
                              fn=('dma_start_transpose', dict(
                                  out=X[:, kc, :], in_=self.xs[b * BLK:(b + 1) * BLK, kc * 128:(kc + 1) * 128])))

                def compute_blk(b):
                    a = b % 2
                    X = xsT[a]
                    rxs = f'xsT{a}'
                    if 'C' in self.opt:
                        return
                    Hd = hid[a]
                    rh = f'hid{a}'
                    wg4 = Wg[a].rearrange("p kc (m four) -> p kc four m", four=4)
                    wu4 = Wu[a].rearrange("p kc (m four) -> p kc four m", four=4)
                    for fc in range(4):
                        pg_, pu_ = 2 + (fc % 2) * 2, 3 + (fc % 2) * 2
                        for kc in range(8):
                            P.op('tensor', 'matmul', reads=[f'Wg{a}', rxs], writes=[f'ps{pg_}'], out=ps[pg_],
                                 lhsT=wg4[:, kc, fc, :], rhs=X[:, kc, :], start=(kc == 0), stop=(kc == 7))
                        for kc in range(8):
                            P.op('tensor', 'matmul', reads=[f'Wu{a}', rxs], writes=[f'ps{pu_}'], out=ps[pu_],
                                 lhsT=wu4[:, kc, fc, :], rhs=X[:, kc, :], start=(kc == 0), stop=(kc == 7))
                        S = sg[fc % 2]
                        P.op('scalar', 'activation', reads=[f'ps{pg_}'], writes=[f'sg{fc % 2}'], out=S, in_=ps[pg_],
                             func=AF.Silu)
                        P.op('vector', 'tensor_tensor', reads=[f'sg{fc % 2}', f'ps{pu_}'], writes=[rh], out=Hd[:, fc, :],
                             in0=S, in1=ps[pu_], op=ALU.mult)
                    for rt in range(4):
                        n_y[0] += 1
                        Y = yb[n_y[0] % 2]
                        ry = f'yb{n_y[0] % 2}'
                        for half in range(2):
                            pb = 6 + half
                            for fc in range(4):
                                P.op('tensor', 'matmul', reads=[rh, f'Wd{a}'], writes=[f'ps{pb}'], out=ps[pb],
                                     lhsT=Hd[:, fc, rt * 128:(rt + 1) * 128], rhs=Wd[a][:, fc, half * 512:(half + 1) * 512],
                                     start=(fc == 0), stop=(fc == 3))
                            self.evac(half, Y[:, half * 512:(half + 1) * 512], ps[pb], [f'ps{pb}'], [ry])
                        r0 = b * BLK + rt * 128
                        P.dma('sync', self.ys[r0:r0 + 128, :], Y, reads=[ry], writes=['ys'])

                load_blk(0)
                for b in range(NB):
                    if b + 1 < NB:
                        load_blk(b + 1)
                    compute_blk(b)
                P.end()
            if '3' in self.opt:
                return
            with contextlib.ExitStack() as s4:
                sb4 = lambda n, s, dt=F32: self.sb(s4, n, s, dt)
                y1 = [sb4(f"y1_{i}", [128, D]) for i in range(2)]
                y2 = [sb4(f"y2_{i}", [128, D]) for i in range(2)]
                xr = [sb4(f"xr{i}", [128, D]) for i in range(2)]
                fgb = sb4("fgb", [128, D])
                fsq = sb4("fsq", [128, D])
                fss = sb4("fss", [128, 1])
                frs = sb4("frs", [128, 1])
                P.begin()
                P.raw('gpsimd', 'reg_mov', self.reg_rows, NR - 1)
                if last:
                    P.dma('sync', fgb, self.final_g.partition_broadcast(128), writes=['fgb'])
                def load_tile(i):
                    a = i % 2
                    rows = slice(i * 128, (i + 1) * 128)
                    P.dma('sync', xr[a], self.xres[rows, :], writes=[f'xr{a}'])
                    for (Yk, rk, k) in ((y1[a], f'y1_{a}', 0), (y2[a], f'y2_{a}', 1)):
                        P.dma('gpsimd', None, None, reads=['dest_i'], writes=[rk],
                              fn=('indirect_dma_start', dict(
                                  out=Yk, out_offset=None, in_=self.ys,
                                  in_offset=bass.IndirectOffsetOnAxis(ap=dest_i[:, i, k:k + 1], axis=0),
                                  bounds_check=self.reg_rows, oob_is_err=False)))
                load_tile(0)
                for i in range(NT):
                    a = i % 2
                    Y1, Y2, X = y1[a], y2[a], xr[a]
                    r1, r2, rx = f'y1_{a}', f'y2_{a}', f'xr{a}'
                    rows = slice(i * 128, (i + 1) * 128)
                    if i + 1 < NT:
                        load_tile(i + 1)
                    P.op('vector', 'tensor_scalar', reads=[r1, 'rw'], writes=[r1], out=Y1, in0=Y1,
                         scalar1=self.rw[:, i, 0:1], scalar2=None, op0=ALU.mult)
                    P.op('vector', 'scalar_tensor_tensor', reads=[r1, r2, 'rw'], writes=[r1], out=Y1, in0=Y2,
                         scalar=self.rw[:, i, 1:2], in1=Y1, op0=ALU.mult, op1=ALU.add)
                    P.op('vector', 'tensor_tensor', reads=[r1, 'modb'], writes=[r1], out=Y1, in0=Y1, in1=self.mod(5),
                         op=ALU.mult)
                    P.op('vector', 'tensor_tensor', reads=[r1, rx], writes=[rx], out=X, in0=X, in1=Y1, op=ALU.add)
                    if not last:
                        P.dma('sync', self.xres[rows, :], X, reads=[rx])
                    else:
                        P.op('scalar', 'activation', reads=[rx], writes=['fsq', 'fss'], out=fsq, in_=X, func=AF.Square,
                             accum_out=fss)
                        self.rstd_ops(fss, frs, D, ['fss'], ['frs'])
                        P.op('vector', 'scalar_tensor_tensor', reads=[rx, 'frs', 'fgb'], writes=[r2], out=Y2, in0=X,
                             scalar=frs, in1=fgb, op0=ALU.mult, op1=ALU.mult)
                        P.dma('sync', self.out[rows, :], Y2, reads=[r2])
                P.end()

    def build_layer(self, l):
        with contextlib.ExitStack() as st:
            ph = self.phases
            self.phase_ada(l, st)
            if 'i' in ph:
                self.phase_inproj(l, st)
            if 'c' in ph and 'f' in ph:
                self.phase_attn(l, conv_inside=True)
            elif 'c' in ph:
                self.phase_conv(l)
            elif 'f' in ph:
                self.phase_attn(l)
            if 's' in ph:
                self.phase_ssd(l)
            if 'o' in ph:
                self.phase_wout_router(l, st)
            if 'm' in ph and self.moe:
                self.phase_moe(l, st)

    def zero_xs(self):
        P = self.P
        NR = self.NB * self.BLK
        with contextlib.ExitStack() as st:
            z = self.sb(st, "zeros", [128, 8192], BF16)
            P.begin()
            P.op('gpsimd', 'memset', writes=['zeros'], ap=z, constant=0.0)
            xv = self.xs.rearrange("(a p r) d -> a p (r d)", p=128, r=8)
            for a in range(NR // 1024):
                P.dma('sync', xv[a], z, reads=['zeros'])
            P.end()

    def build(self):
        if self.moe:
            self.zero_xs()
        for l in range(self.L):
            self.build_layer(l)
        return self.nc


def prep_core_inputs(inp, b, T, L):
    f = lambda a: np.ascontiguousarray(a, dtype=np.float32)
    m = {}
    m["x"] = f(inp["x"][b, :T])
    m["c"] = f(inp["c"][b].reshape(8, 128).T)
    for k in ("ada_w", "ada_b", "norm_mix_g", "w_in", "ssd_dt_bias", "ssd_a_log", "ssd_d", "ssd_norm_g",
              "w_out", "norm_ffn_g", "w_router_group", "b_router_group", "w_router_expert", "b_router_expert",
              "w_gate", "w_up", "w_down"):
        m[k] = f(inp[k][:L])
    m["final_norm_g"] = f(inp["final_norm_g"])
    m["convw_fm"] = f(inp["ssd_conv_w"][:L].reshape(L, 4, 8, 128).transpose(0, 3, 2, 1))
    m["convb_fm"] = f(inp["ssd_conv_b"][:L].reshape(L, 8, 128).transpose(0, 2, 1))
    m["fox_f_bias_fm"] = f(inp["fox_f_bias"][:L].reshape(L, 4, 1))
    m["fox_g_fm"] = f(inp["fox_norm_g"][:L].reshape(L, 2, 128).transpose(0, 2, 1))
    m["cmw_fm"] = f(inp["cm_conv_w"][:L].reshape(L, 31, 2, 128).transpose(0, 3, 2, 1))
    m["cmb_fm"] = f(inp["cm_conv_b"][:L].reshape(L, 2, 128).transpose(0, 2, 1))
    m["cmg_fm"] = f(inp["cm_ln_g"][:L].reshape(L, 2, 128).transpose(0, 2, 1))
    m["cmbeta_fm"] = f(inp["cm_ln_b"][:L].reshape(L, 2, 128).transpose(0, 2, 1))
    return m


_CACHE = {}
T_FULL, L_FULL, B_FULL = 8192, 4, 4


def kernel(**inputs):
    if 'nc' not in _CACHE:
        bd = Builder(T_FULL, L_FULL)
        _CACHE['nc'] = bd.build()
        _CACHE['names'] = set(bd.ins)
    nc = _CACHE['nc']
    inp = {k: np.asarray(v) for k, v in inputs.items()}
    maps = []
    for b in range(B_FULL):
        m = prep_core_inputs(inp, b, T_FULL, L_FULL)
        maps.append({k: v for k, v in m.items() if k in _CACHE['names']})
    res = run_bass_kernel_spmd(nc, maps, core_ids=list(range(B_FULL)))
    out = np.stack([np.asarray(r["out"], dtype=np.float32) for r in res.results], axis=0)
    return out
```
